# Optimizing a Trainium2 kernel written in Bass

```python
import math
import jax, jax.numpy as jnp
from jax import lax
import numpy as np

D_MODEL = 1024
BATCH = 16
SEQ = 2048
DEPTH = 4

GRID_W = 64
CTX_LEN = 256
HEAD_DIM = 64
NA_HEADS = D_MODEL // (2 * HEAD_DIM)
NA_WIN_H = 8
NA_WIN_W = 16
NA_COL_BLOCK = 16
NA_BAND_W = NA_COL_BLOCK + NA_WIN_W
DIFF_HEADS = D_MODEL // (4 * HEAD_DIM)
DIFF_DV = 2 * HEAD_DIM
Q_BLOCK = 128
A_W = NA_HEADS * HEAD_DIM
B_QK_W = DIFF_HEADS * 2 * HEAD_DIM
B_V_W = DIFF_HEADS * DIFF_DV
ATT_IN_W = 3 * A_W + 2 * B_QK_W + B_V_W
ATT_OUT_W = A_W + B_V_W
REC_HEADS = 8
REC_DK = D_MODEL // REC_HEADS
REC_DV = D_MODEL // REC_HEADS
REC_CHUNK = 32
REC_IN_W = 5 * D_MODEL
N_EXPERTS = 16
EXPERT_FF = 2 * D_MODEL
EC_CAPACITY_FACTOR = 2
ROPE_THETA = 10000.0
NORM_EPS = 1e-6
NEG_INF = -1e30
N_EVEN = (DEPTH + 1) // 2
N_ODD = DEPTH // 2

kernel_name = 'hybrid_na_diffattn_hgrn2_ec_dit'


def rmsnorm(x, g):
    xf = x.astype(jnp.float32)
    y = xf * lax.rsqrt(jnp.mean(xf * xf, axis=-1, keepdims=True) + NORM_EPS)
    return (y * g.astype(jnp.float32)).astype(x.dtype)


def modulation(cvec, w_mod, b_mod):
    m = jnp.einsum('...d,de->...e', jax.nn.silu(cvec), w_mod) + b_mod
    return jnp.split(m, 6, axis=-1)


def _split_heads(a, n_heads):
    b, t, _ = a.shape
    return jnp.transpose(a.reshape(b, t, n_heads, -1), (0, 2, 1, 3))


def _split_pair_heads(a):
    b, t, _ = a.shape
    return jnp.transpose(a.reshape(b, t, DIFF_HEADS, 2, HEAD_DIM), (0, 2, 3, 1, 4))


def _merge_heads(a):
    b, h, t, d = a.shape
    return jnp.transpose(a, (0, 2, 1, 3)).reshape(b, t, h * d)


def axial_rope(n, dim):
    t = jnp.arange(n, dtype=jnp.int32)
    rows = (t // GRID_W).astype(jnp.float32)
    cols = (t % GRID_W).astype(jnp.float32)
    n_freq = dim // 4
    inv = ROPE_THETA ** (-jnp.arange(n_freq, dtype=jnp.float32) / n_freq)
    ang = jnp.concatenate([rows[:, None] * inv, cols[:, None] * inv], axis=-1)
    return jnp.cos(ang), jnp.sin(ang)


def apply_rope(a, cos, sin):
    a1, a2 = jnp.split(a, 2, axis=-1)
    cos = cos.astype(a.dtype)
    sin = sin.astype(a.dtype)
    return jnp.concatenate([a1 * cos - a2 * sin, a2 * cos + a1 * sin], axis=-1)


def _softmax_attend(q, k, v, scale):
    s = jnp.einsum('bhqd,bhkd->bhqk', q, k).astype(jnp.float32) * scale
    p = jax.nn.softmax(s, axis=-1).astype(v.dtype)
    return jnp.einsum('bhqk,bhkd->bhqd', p, v)


def _diff_attend(q12, k12, v, lam, scale):
    s = jnp.einsum('bhmqd,bhmkd->bhmqk', q12, k12).astype(jnp.float32) * scale
    p = jax.nn.softmax(s, axis=-1)
    a = (p[:, :, 0] - lam * p[:, :, 1]).astype(v.dtype)
    return jnp.einsum('bhqk,bhkd->bhqd', a, v)


def diff_attention_latent(q12, k12_all, v_all, lam, scale):
    b, h, _, n, d = q12.shape
    nb = n // Q_BLOCK
    qb = jnp.moveaxis(q12.reshape(b, h, 2, nb, Q_BLOCK, d), 3, 0)
    o = lax.map(lambda qi: _diff_attend(qi, k12_all, v_all, lam, scale), qb)
    return jnp.moveaxis(o, 0, 2).reshape(b, h, n, -1)


def neighbourhood_attention(q, k, v, k_ctx, v_ctx, rpb):
    b, h, n, d = q.shape
    rows = n // GRID_W
    kh = min(NA_WIN_H, rows)
    n_cb = GRID_W // NA_COL_BLOCK
    scale = d ** -0.5
    qcol = np.arange(GRID_W).reshape(n_cb, NA_COL_BLOCK)
    cstart = np.clip(qcol - NA_WIN_W // 2, 0, GRID_W - NA_WIN_W)
    bstart = np.clip(np.arange(n_cb) * NA_COL_BLOCK - NA_WIN_W // 2, 0, GRID_W - NA_BAND_W)
    kcol = bstart[:, None] + np.arange(NA_BAND_W)[None, :]
    col_ok = (kcol[:, None, :] >= cstart[:, :, None]) & (kcol[:, None, :] < cstart[:, :, None] + NA_WIN_W)
    mask = jnp.asarray(np.broadcast_to(col_ok[:, :, None, :], (n_cb, NA_COL_BLOCK, kh, NA_BAND_W)).reshape(n_cb, NA_COL_BLOCK, kh * NA_BAND_W))
    dcol = np.clip(kcol[:, None, :] - qcol[:, :, None] + NA_WIN_W - 1, 0, 2 * NA_WIN_W - 2)
    rpb_c = rpb[:, :, dcol]
    kg = k.reshape(b, h, rows, GRID_W, d)
    vg = v.reshape(b, h, rows, GRID_W, d)
    qg = jnp.moveaxis(q.reshape(b, h, rows, n_cb, NA_COL_BLOCK, d), 2, 0)
    n_loc = kh * NA_BAND_W

    def one_row(args):
        q_r, r = args
        rs = jnp.clip(r - kh // 2, 0, rows - kh)
        k_band = lax.dynamic_slice_in_dim(kg, rs, kh, axis=2)[:, :, :, kcol]
        v_band = lax.dynamic_slice_in_dim(vg, rs, kh, axis=2)[:, :, :, kcol]
        k_band = jnp.moveaxis(k_band, 3, 2).reshape(b, h, n_cb, n_loc, d)
        v_band = jnp.moveaxis(v_band, 3, 2).reshape(b, h, n_cb, n_loc, d)
        bias = jnp.take(rpb_c, rs - r + jnp.arange(kh, dtype=jnp.int32) + NA_WIN_H - 1, axis=1)
        bias = jnp.transpose(bias, (0, 2, 3, 1, 4)).reshape(h, n_cb, NA_COL_BLOCK, n_loc)
        s_loc = jnp.einsum('bhjqd,bhjkd->bhjqk', q_r, k_band).astype(jnp.float32) * scale + bias.astype(jnp.float32)
        s_loc = jnp.where(mask, s_loc, NEG_INF)
        s_ctx = jnp.einsum('bhjqd,bhkd->bhjqk', q_r, k_ctx).astype(jnp.float32) * scale
        p = jax.nn.softmax(jnp.concatenate([s_loc, s_ctx], axis=-1), axis=-1).astype(v.dtype)
        return (jnp.einsum('bhjqk,bhjkd->bhjqd', p[..., :n_loc], v_band)
                + jnp.einsum('bhjqk,bhkd->bhjqd', p[..., n_loc:], v_ctx))

    o = lax.map(one_row, (qg, jnp.arange(rows, dtype=jnp.int32)))
    return jnp.moveaxis(o, 0, 2).reshape(b, h, n, d)


def even_layer_mixer(h_lat, h_ctx, w_in, w_out, rpb, lam_vecs, subln_g, layer_idx, with_ctx_out):
    n = h_lat.shape[1]
    scale = HEAD_DIM ** -0.5
    kv_off = A_W + B_QK_W
    kv_splits = [A_W, 2 * A_W, 2 * A_W + B_QK_W]
    p_lat = jnp.einsum('btd,de->bte', h_lat, w_in)
    q_a, q_b, kv_lat = jnp.split(p_lat, [A_W, kv_off], axis=-1)
    if with_ctx_out:
        p_ctx = jnp.einsum('btd,de->bte', h_ctx, w_in)
        q_ac, q_bc, kv_ctx = jnp.split(p_ctx, [A_W, kv_off], axis=-1)
    else:
        kv_ctx = jnp.einsum('btd,de->bte', h_ctx, w_in[:, kv_off:])
    k_a, v_a, k_b, v_b = jnp.split(kv_lat, kv_splits, axis=-1)
    k_ac, v_ac, k_bc, v_bc = jnp.split(kv_ctx, kv_splits, axis=-1)
    k_ac = _split_heads(k_ac, NA_HEADS)
    v_ac = _split_heads(v_ac, NA_HEADS)

    o_a = neighbourhood_attention(_split_heads(q_a, NA_HEADS), _split_heads(k_a, NA_HEADS),
                                  _split_heads(v_a, NA_HEADS), k_ac, v_ac, rpb)

    lam_init = 0.8 - 0.6 * math.exp(-0.3 * layer_idx)
    lv = lam_vecs.astype(jnp.float32)
    lam = jnp.exp(jnp.sum(lv[0] * lv[1])) - jnp.exp(jnp.sum(lv[2] * lv[3])) + lam_init
    cos, sin = axial_rope(n, HEAD_DIM)
    k12_ctx = _split_pair_heads(k_bc)
    v_ctx_b = _split_heads(v_bc, DIFF_HEADS)
    q12 = apply_rope(_split_pair_heads(q_b), cos, sin)
    k12_all = jnp.concatenate([apply_rope(_split_pair_heads(k_b), cos, sin), k12_ctx], axis=3)
    v_all = jnp.concatenate([_split_heads(v_b, DIFF_HEADS), v_ctx_b], axis=2)
    o_b = rmsnorm(diff_attention_latent(q12, k12_all, v_all, lam, scale), subln_g) * (1.0 - lam_init)
    y_lat = jnp.einsum('bte,ed->btd', jnp.concatenate([_merge_heads(o_a), _merge_heads(o_b)], axis=-1), w_out)
    if not with_ctx_out:
        return y_lat, None
    o_ac = _softmax_attend(_split_heads(q_ac, NA_HEADS), k_ac, v_ac, scale)
    o_bc = rmsnorm(_diff_attend(_split_pair_heads(q_bc), k12_ctx, v_ctx_b, lam, scale), subln_g) * (1.0 - lam_init)
    y_ctx = jnp.einsum('bte,ed->btd', jnp.concatenate([_merge_heads(o_ac), _merge_heads(o_bc)], axis=-1), w_out)
    return y_lat, y_ctx


def _hgrn_gates(fx, lb):
    lbb = lb[None, :, None, :]
    logf = jnp.logaddexp(jnp.log(lbb), jnp.log1p(-lbb) + jax.nn.log_sigmoid(fx))
    k = (1.0 - lbb) * jax.nn.sigmoid(-fx)
    return k, logf


def hgrn2_chunk_scan(k, v, logf, s0, q):
    b, h, t, dk = k.shape
    nc = t // REC_CHUNK
    tri = jnp.asarray(np.tril(np.ones((REC_CHUNK, REC_CHUNK), dtype=bool)))

    def chunks(a):
        return jnp.moveaxis(a.reshape(b, h, nc, REC_CHUNK, a.shape[-1]), 2, 0)

    def body(s, xs):
        kc, vc, gc = xs[0], xs[1], xs[2]
        bcum = jnp.cumsum(gc, axis=2)
        blast = bcum[:, :, -1:, :]
        s_new = jnp.exp(blast[:, :, 0])[..., None] * s + jnp.einsum('bhcd,bhce->bhde', kc * jnp.exp(blast - bcum), vc)
        if q is None:
            return s_new, None
        qc = xs[3]
        rel = jnp.where(tri[None, None, :, :, None], bcum[:, :, :, None, :] - bcum[:, :, None, :, :], -jnp.inf)
        scores = jnp.einsum('bhtsd,bhsd->bhts', qc[:, :, :, None, :] * jnp.exp(rel), kc)
        o = jnp.einsum('bhtd,bhde->bhte', qc * jnp.exp(bcum), s) + jnp.einsum('bhts,bhse->bhte', scores, vc)
        return s_new, o

    xs = (chunks(k), chunks(v), chunks(logf)) + ((chunks(q),) if q is not None else ())
    s_fin, o = lax.scan(body, s0, xs)
    if q is not None:
        o = jnp.moveaxis(o, 0, 2).reshape(b, h, t, -1)
    return o, s_fin


def _hgrn_out(o, g, gnorm_g, w_out, dtype):
    o = _merge_heads(rmsnorm(o, gnorm_g)).astype(dtype) * jax.nn.silu(g)
    return jnp.einsum('bte,ed->btd', o, w_out)


def odd_layer_mixer(h_lat, h_ctx, w_in, w_out, lb_fwd, lb_bwd, gnorm_g, with_ctx_out):
    f32 = jnp.float32
    heads = lambda a: _split_heads(a, REC_HEADS).astype(f32)
    flip = lambda a: jnp.flip(a, axis=2)
    lb_f = lb_fwd.reshape(REC_HEADS, REC_DK)
    lb_b = lb_bwd.reshape(REC_HEADS, REC_DK)
    q, ff, fb, inp, g = jnp.split(jnp.einsum('btd,de->bte', h_lat, w_in), 5, axis=-1)
    if with_ctx_out:
        qc, ffc, fbc, inpc, gc = jnp.split(jnp.einsum('btd,de->bte', h_ctx, w_in), 5, axis=-1)
        qc = jax.nn.silu(heads(qc))
    else:
        ffc, fbc, inpc = jnp.split(jnp.einsum('btd,de->bte', h_ctx, w_in[:, D_MODEL:4 * D_MODEL]), 3, axis=-1)
        qc = None
    vc = heads(inpc)
    kcf, gcf = _hgrn_gates(heads(ffc), lb_f)
    kcb, gcb = _hgrn_gates(heads(fbc), lb_b)
    s0 = jnp.zeros((h_ctx.shape[0], REC_HEADS, REC_DK, REC_DV), f32)
    o_cf, s_cf = hgrn2_chunk_scan(kcf, vc, gcf, s0, qc)
    o_cb, s_cb = hgrn2_chunk_scan(flip(kcb), flip(vc), flip(gcb), s0, None if qc is None else flip(qc))
    ql = jax.nn.silu(heads(q))
    vl = heads(inp)
    klf, glf = _hgrn_gates(heads(ff), lb_f)
    klb, glb = _hgrn_gates(heads(fb), lb_b)
    o_lf, _ = hgrn2_chunk_scan(klf, vl, glf, s_cf, ql)
    o_lb, _ = hgrn2_chunk_scan(flip(klb), flip(vl), flip(glb), s_cb, flip(ql))
    y_lat = _hgrn_out(o_lf + flip(o_lb), g, gnorm_g, w_out, h_lat.dtype)
    if not with_ctx_out:
        return y_lat, None
    y_ctx = _hgrn_out(o_cf + flip(o_cb), gc, gnorm_g, w_out, h_ctx.dtype)
    return y_lat, y_ctx


def expert_choice_ffn(h, w_router, w_gate, w_up, w_down):
    b, t, d = h.shape
    cap = EC_CAPACITY_FACTOR * t // N_EXPERTS
    aff = jax.nn.softmax(jnp.einsum('btd,de->bte', h, w_router).astype(jnp.float32), axis=-1)
    gate, idx = lax.top_k(jnp.swapaxes(aff, 1, 2), cap)
    x_sel = jax.vmap(lambda hb, ib: hb[ib])(h, idx)
    hid = jax.nn.silu(jnp.einsum('becd,edf->becf', x_sel, w_gate)) * jnp.einsum('becd,edf->becf', x_sel, w_up)
    y_sel = jnp.einsum('becf,efd->becd', hid, w_down) * gate[..., None].astype(h.dtype)
    return jax.vmap(lambda ib, yb: jnp.zeros((t, d), h.dtype).at[ib.reshape(-1)].add(yb.reshape(-1, d)))(idx, y_sel)


def setup_inputs(seed: int = 0) -> dict:
    key = jax.random.key(seed)
    ks = jax.random.split(key, 21)
    f32 = jnp.float32

    def nrm(k, shape, std):
        return jax.random.normal(k, shape, f32) * std

    def gain(k, shape):
        return 1.0 + 0.02 * jax.random.normal(k, shape, f32)

    return {
        'x': nrm(ks[0], (BATCH, SEQ, D_MODEL), 1.0),
        'c': nrm(ks[1], (BATCH, D_MODEL), 1.0),
        'ctx': nrm(ks[2], (BATCH, CTX_LEN, D_MODEL), 1.0),
        'c_ctx': nrm(ks[3], (D_MODEL,), 1.0),
        'w_mod': nrm(ks[4], (DEPTH, D_MODEL, 6 * D_MODEL), 0.5 * D_MODEL ** -0.5),
        'b_mod': nrm(ks[5], (DEPTH, 6 * D_MODEL), 0.02),
        'norm_g': gain(ks[6], (DEPTH, 2, D_MODEL)),
        'att_w_in': nrm(ks[7], (N_EVEN, D_MODEL, ATT_IN_W), D_MODEL ** -0.5),
        'att_w_out': nrm(ks[8], (N_EVEN, ATT_OUT_W, D_MODEL), ATT_OUT_W ** -0.5),
        'na_rpb': nrm(ks[9], (N_EVEN, NA_HEADS, 2 * NA_WIN_H - 1, 2 * NA_WIN_W - 1), 0.1),
        'diff_lambda': nrm(ks[10], (N_EVEN, 4, HEAD_DIM), 0.1),
        'diff_subln_g': gain(ks[11], (N_EVEN, DIFF_DV)),
        'rec_w_in': nrm(ks[12], (N_ODD, D_MODEL, REC_IN_W), D_MODEL ** -0.5),
        'rec_w_out': nrm(ks[13], (N_ODD, D_MODEL, D_MODEL), D_MODEL ** -0.5),
        'rec_lb_logits': 1.0 + 0.1 * jax.random.normal(ks[14], (2, DEPTH, D_MODEL), f32),
        'rec_gnorm_g': gain(ks[15], (N_ODD, REC_DV)),
        'moe_router': nrm(ks[16], (DEPTH, D_MODEL, N_EXPERTS), D_MODEL ** -0.5),
        'moe_w_gate': nrm(ks[17], (DEPTH, N_EXPERTS, D_MODEL, EXPERT_FF), D_MODEL ** -0.5),
        'moe_w_up': nrm(ks[18], (DEPTH, N_EXPERTS, D_MODEL, EXPERT_FF), D_MODEL ** -0.5),
        'moe_w_down': nrm(ks[19], (DEPTH, N_EXPERTS, EXPERT_FF, D_MODEL), EXPERT_FF ** -0.5),
        'final_g': gain(ks[20], (D_MODEL,)),
    }


def reference(x, c, ctx, c_ctx, w_mod, b_mod, norm_g, att_w_in, att_w_out, na_rpb, diff_lambda,
              diff_subln_g, rec_w_in, rec_w_out, rec_lb_logits, rec_gnorm_g, moe_router, moe_w_gate,
              moe_w_up, moe_w_down, final_g):
    sm = jax.nn.softmax(rec_lb_logits.astype(jnp.float32), axis=1)
    lower_bounds = jnp.cumsum(sm, axis=1) - sm[:, :1]
    for l in range(DEPTH):
        last = l == DEPTH - 1
        sh1, sc1, g1, sh2, sc2, g2 = [m[:, None, :] for m in modulation(c, w_mod[l], b_mod[l])]
        csh1, csc1, cg1, csh2, csc2, cg2 = modulation(c_ctx, w_mod[l], b_mod[l])
        h_lat = rmsnorm(x, norm_g[l, 0]) * (1.0 + sc1) + sh1
        h_ctx = rmsnorm(ctx, norm_g[l, 0]) * (1.0 + csc1) + csh1
        if l % 2 == 0:
            e = l // 2
            y_lat, y_ctx = even_layer_mixer(h_lat, h_ctx, att_w_in[e], att_w_out[e], na_rpb[e],
                                            diff_lambda[e], diff_subln_g[e], l, not last)
        else:
            o = l // 2
            y_lat, y_ctx = odd_layer_mixer(h_lat, h_ctx, rec_w_in[o], rec_w_out[o], lower_bounds[0, l],
                                           lower_bounds[1, l], rec_gnorm_g[o], not last)
        x = x + g1 * y_lat
        x = x + g2 * expert_choice_ffn(rmsnorm(x, norm_g[l, 1]) * (1.0 + sc2) + sh2,
                                       moe_router[l], moe_w_gate[l], moe_w_up[l], moe_w_down[l])
        if not last:
            ctx = ctx + cg1 * y_ctx
            ctx = ctx + cg2 * expert_choice_ffn(rmsnorm(ctx, norm_g[l, 1]) * (1.0 + csc2) + csh2,
                                                moe_router[l], moe_w_gate[l], moe_w_up[l], moe_w_down[l])
    return rmsnorm(x, final_g)
```

```python
import math
import numpy as np
import concourse.bass as bass
import concourse.mybir as mybir
from concourse.bass_utils import run_bass_kernel_spmd

F32 = mybir.dt.float32
BF16 = mybir.dt.bfloat16
AF = mybir.ActivationFunctionType
ALU = mybir.AluOpType

EPOCH = 28800


class Stream:
    def __init__(self, prog, name):
        self.prog = prog
        self.name = name
        self.sems = []
        self.n = 0

    def sem_for(self, e):
        while len(self.sems) <= e:
            self.sems.append(self.prog.nc.alloc_semaphore(f"{self.name}_{len(self.sems)}"))
        return self.sems[e]

    def bump(self, inc):
        e = self.n // EPOCH
        assert (self.n + inc - 1) // EPOCH == e
        self.n += inc
        return self.sem_for(e), (self, self.n)

    def loc(self, n):
        e = (n - 1) // EPOCH
        return self.sem_for(e), n - e * EPOCH


class Prog:
    ENGS = ("pe", "act", "dve", "pool", "sp")

    def __init__(self, nc, n_dma_sems=4):
        self.nc = nc
        self.q = {e: [] for e in self.ENGS}
        self.stream = {e: Stream(self, "s_" + e) for e in self.ENGS}
        self.seen = {e: {} for e in self.ENGS}
        self.last_w = {}
        self.readers = {}
        self.dma_streams = {}
        self.dma_rr = {}
        self.n_dma_sems = n_dma_sems
        self.ninst = 0

    def _deps(self, reads, writes):
        deps = []
        for k in reads:
            t = self.last_w.get(k)
            if t is not None:
                deps.append(t)
        for k in writes:
            t = self.last_w.get(k)
            if t is not None:
                deps.append(t)
            deps.extend(self.readers.get(k, ()))
        return deps

    def _commit(self, tok, reads, writes):
        for k in writes:
            self.last_w[k] = tok
            self.readers[k] = []
        for k in reads:
            if k in writes:
                continue
            self.readers.setdefault(k, []).append(tok)

    def _waits(self, eng, deps, skip_self=False):
        seen = self.seen[eng]
        best = {}
        for (st, n) in deps:
            if skip_self and st is self.stream[eng]:
                continue
            if seen.get(st, 0) >= n:
                continue
            if best.get(st, 0) < n:
                best[st] = n
        waits = []
        for st, n in best.items():
            seen[st] = n
            waits.append(st.loc(n))
        return waits

    def op(self, eng, fn, reads=(), writes=()):
        reads = tuple(reads)
        writes = tuple(writes) + tuple(k for k in reads if isinstance(k, str) and k.startswith("ps") and k[2:].isdigit())
        deps = self._deps(reads, writes)
        waits = self._waits(eng, deps, skip_self=(eng == "pe"))
        sem, tok = self.stream[eng].bump(1)

        def emit(E, waits=waits, fn=fn, sem=sem):
            for (s, v) in waits:
                E.wait_ge(s, v)
            fn(E).then_inc(sem, 1)

        self.q[eng].append(emit)
        self._commit(tok, reads, writes)
        self.ninst += 1
        return tok

    def dma(self, queue, out, in_, reads=(), writes=()):
        reads = tuple(reads)
        writes = tuple(writes)
        deps = self._deps(reads, writes)
        if queue not in self.dma_streams:
            self.dma_streams[queue] = [Stream(self, f"d_{queue}{i}") for i in range(self.n_dma_sems)]
            self.dma_rr[queue] = 0
        i = self.dma_rr[queue]
        self.dma_rr[queue] = (i + 1) % self.n_dma_sems
        st = self.dma_streams[queue][i]
        if st.n > 0:
            deps.append((st, st.n))
        waits = self._waits(queue, deps)
        sem, tok = st.bump(16)

        def emit(E, waits=waits, sem=sem, out=out, in_=in_):
            for (s, v) in waits:
                E.wait_ge(s, v)
            E.dma_start(out=out, in_=in_).then_inc(sem, 16)

        self.q[queue].append(emit)
        self._commit(tok, reads, writes)
        self.ninst += 1
        return tok

    def all_tokens(self):
        toks = []
        for e in self.ENGS:
            if self.stream[e].n:
                toks.append((self.stream[e], self.stream[e].n))
        for q, sts in self.dma_streams.items():
            for st in sts:
                if st.n:
                    toks.append((st, st.n))
        return toks

    def barrier(self):
        toks = self.all_tokens()
        for eng in self.ENGS:
            waits = self._waits(eng, toks)

            def emit(E, waits=waits):
                for (s, v) in waits:
                    E.wait_ge(s, v)

            self.q[eng].append(emit)

    def wait_dmas(self, eng):
        toks = [(st, st.n) for q, sts in self.dma_streams.items() for st in sts if st.n]
        waits = self._waits(eng, toks)

        def emit(E, waits=waits):
            for (s, v) in waits:
                E.wait_ge(s, v)

        self.q[eng].append(emit)

    def run(self):
        nc = self.nc
        with nc.Block() as block:
            @block.tensor
            def _(E):
                for f in self.q["pe"]:
                    f(E)

            @block.scalar
            def _(E):
                for f in self.q["act"]:
                    f(E)

            @block.vector
            def _(E):
                for f in self.q["dve"]:
                    f(E)

            @block.gpsimd
            def _(E):
                for f in self.q["pool"]:
                    f(E)

            @block.sync
            def _(E):
                for f in self.q["sp"]:
                    f(E)


DBG = {'na': 1, 'diff': 1, 'mix': 1, 'moe': 1, 'experts': 16, 'nagroups': 2, 'nqr': 32}
D = 1024
T = 2048
L = 256
TT = T + L
EPS = 1e-6
NE = 16
FF = 2048


def build(S, layers, final=True, debug_ctx=False):
    nc = bass.Bass("TRN2", target_bir_lowering=False)
    P = Prog(nc)
    V = S + 1

    def din(name, shape):
        return nc.dram_tensor(name, list(shape), F32, kind="ExternalInput").ap()

    x_d = din("x", [S, T, D])
    ctx_d = din("ctx", [S, L, D])
    cvT_d = din("cvT", [128, 8, V])
    NL = max(1, len(layers))
    LI = {l: i for i, l in enumerate(layers)}
    wmod_d = din("w_mod", [NL, D, 6 * D])
    bmT_d = din("b_modT", [128, 4, 48])
    ngT_d = din("norm_gT", [128, 4, 2, 8])
    attw_d = din("att_w", [2, D, 4096])
    attwo_d = din("att_wo", [2, D, D])
    nab_d = din("na_bias", [2, 128, 8, 16, 64])
    lam_d = din("lam_row", [2, 1, 256])
    subln_d = din("subln_bc", [2, 128, 128])
    cos_d = din("rope_cos", [128, T])
    sin_d = din("rope_sin", [128, T])
    recw_d = din("rec_w_in", [2, D, 5 * D])
    recwo_d = din("rec_wo", [2, D, D])
    lbT_d = din("rec_lbT", [128, 2, 4, 8])
    gn_d = din("rec_gn", [128, 2])
    router_d = din("router", [NL, D, NE])
    NEX = max(1, DBG["experts"])
    wg_d = din("wg", [NL, NEX, D, FF])
    wu_d = din("wu", [NL, NEX, D, FF])
    wd_d = din("wd", [NL, NEX, FF, D])
    fg_d = din("finalg_bc", [128, D])
    cid_d = din("c_ident", [128, 128])
    cij_d = din("c_iota_j", [128, 256])
    cip_d = din("c_iota_p", [128, 4])
    cmf_d = din("c_maskf", [128, 128])
    cmb_d = din("c_maskb", [128, 128])
    cil_d = din("c_ident_lo", [128, 128])
    cih_d = din("c_ident_hi", [128, 128])
    out_d = nc.dram_tensor("out", [S, T, D], F32, kind="ExternalOutput").ap()
    outc_d = nc.dram_tensor("out_ctx", [S, L, D], F32, kind="ExternalOutput").ap() if debug_ctx else None
    dbg_d = nc.dram_tensor("dbg", [16, 128, 512], F32, kind="ExternalOutput").ap() if debug_ctx else None

    base = (nc.sbuf_base + 63) // 64 * 64
    lim = nc.sbuf_top
    mem = {"p": base, "n": 0}

    def sb(name, shape, dt=F32):
        nb = int(np.prod(shape[1:])) * (2 if dt == BF16 else 4)
        nb = (nb + 63) // 64 * 64
        off = mem["p"]
        assert off + nb <= lim, f"SBUF overflow at {name}: {off + nb} > {lim}"
        mem["p"] = off + nb
        mem["n"] += 1
        return nc.alloc_sbuf_tensor_at(f"{name}_{mem['n']}", list(shape), dt, offset=off)

    PS = [nc.alloc_psum_tensor(f"ps{i}", [128, 512], F32) for i in range(8)]
    PSB = [p[:].bitcast(BF16) for p in PS]

    def MM(out, lhsT, rhs, st, sp, r, w):
        P.op("pe", lambda E: E.matmul(out, lhsT=lhsT, rhs=rhs, start=st, stop=sp), r, w)

    def TR(out, in_, idn, r, w):
        P.op("pe", lambda E: E.transpose(out=out, in_=in_, identity=idn), r, w)

    def ACT(out, in_, func, r, w, scale=None, bias=None, accum=None):
        kw = {}
        if scale is not None:
            kw["scale"] = scale
        if bias is not None:
            kw["bias"] = bias
        if accum is not None:
            kw["accum_out"] = accum
        P.op("act", lambda E: E.activation(out=out, in_=in_, func=func, **kw), r, w)

    def TS(eng, out, in0, s1, s2, op0, op1, r, w):
        if s2 is None:
            P.op(eng, lambda E: E.tensor_scalar(out=out, in0=in0, scalar1=s1, scalar2=None, op0=op0), r, w)
        else:
            P.op(eng, lambda E: E.tensor_scalar(out=out, in0=in0, scalar1=s1, scalar2=s2, op0=op0, op1=op1), r, w)

    def TTo(eng, out, in0, in1, op, r, w):
        P.op(eng, lambda E: E.tensor_tensor(out=out, in0=in0, in1=in1, op=op), r, w)

    def STT(eng, out, in0, sc, in1, op0, op1, r, w):
        P.op(eng, lambda E: E.scalar_tensor_tensor(out=out, in0=in0, scalar=sc, in1=in1, op0=op0, op1=op1), r, w)

    def CP(eng, out, in_, r, w):
        if eng == "act":
            P.op("act", lambda E: E.copy(out=out, in_=in_), r, w)
        else:
            P.op(eng, lambda E: E.tensor_copy(out=out, in_=in_), r, w)

    def MS(eng, ap, v, w):
        P.op(eng, lambda E: E.memset(ap, v), [], w)

    dbgbuf = {}

    def dump(i, ap, keys, n):
        if dbg_d is None:
            return
        if "t" not in dbgbuf:
            dbgbuf["t"] = nc.alloc_sbuf_tensor_at("dbgt", [128, 512], F32, offset=(lim - 4096) // 64 * 64)
        t = dbgbuf["t"]
        rows = ap.shape[0]
        P.op("dve", lambda E: E.memset(t[:], 0.0), [], ["dbgt"])
        P.op("dve", lambda E: E.tensor_copy(out=t[0:rows, 0:n], in_=ap), keys, ["dbgt"])
        P.dma("sp", dbg_d[i], t[:], ["dbgt"], [])

    def RCP(out, in_, r, w):
        P.op("dve", lambda E: E.reciprocal(out=out, in_=in_), r, w)

    def wview(src2d):
        return src2d.rearrange("(k p) n -> p k n", p=128)

    X = sb("X", [128, 16, D])
    XC = sb("XC", [128, 2, D])
    ident_f = sb("ident_f", [128, 128])
    ident_b = sb("ident_b", [128, 128], BF16)
    ones_f = sb("ones_f", [128, 128])
    ident_lo = sb("ident_lo", [128, 128], BF16)
    ident_hi = sb("ident_hi", [128, 128], BF16)
    iota_j = sb("iota_j", [128, 256])
    iota_p = sb("iota_p", [128, 4])
    maskf = sb("maskf", [128, 128])
    maskb = sb("maskb", [128, 128])
    modT = sb("modT", [128, 4, V, 48])
    bmT = sb("bmT", [128, 4, 48])
    ngT = sb("ngT", [128, 4, 2, 8])
    scT = sb("scT", [128, 8, V])
    lbT = sb("lbT", [128, 2, 4, 8])
    omlT = sb("omlT", [128, 2, 4, 8])
    nomlT = sb("nomlT", [128, 2, 4, 8])
    gnT = sb("gnT", [128, 2])
    am = sb("am", [128, 2, 2, 8])
    ssq = sb("ssq", [128, 1])
    rt = sb("rt", [128, 1])
    rstd = sb("rstd", [128, 1])
    neglam = sb("neglam", [128, 1])
    small = sb("small", [128, 16])
    lsum = sb("lsum", [128, 2, 8])
    phase_base = mem["p"]

    P.dma("sp", ident_f[:], cid_d, [], ["ident_f"])
    P.dma("pool", ident_b[:], cid_d, [], ["ident_b"])
    P.dma("pool", ident_lo[:], cil_d, [], ["ident_b"])
    P.dma("pool", ident_hi[:], cih_d, [], ["ident_b"])
    P.dma("sp", iota_j[:], cij_d, [], ["iota_j"])
    P.dma("sp", iota_p[:], cip_d, [], ["iota_p"])
    P.dma("sp", maskf[:], cmf_d, [], ["maskf"])
    P.dma("sp", maskb[:], cmb_d, [], ["maskb"])
    P.dma("sp", bmT[:], bmT_d, [], ["bmT"])
    P.dma("sp", ngT[:], ngT_d, [], ["ngT"])
    P.dma("sp", scT[:], cvT_d, [], ["scT"])
    P.dma("sp", lbT[:], lbT_d, [], ["lbT"])
    P.dma("sp", gnT[:], gn_d, [], ["gnT"])
    MS("pool", ones_f[:], 1.0, ["ones_f"])
    ACT(scT[:], scT[:], AF.Silu, ["scT"], ["scT"])

    ACT(lbT[:], lbT[:], AF.Exp, ["lbT"], ["lbT"])
    TTo("dve", lsum[:], lbT[:, :, 0, :], lbT[:, :, 1, :], ALU.add, ["lbT"], ["lsum"])
    TTo("dve", lsum[:], lsum[:], lbT[:, :, 2, :], ALU.add, ["lbT", "lsum"], ["lsum"])
    TTo("dve", lsum[:], lsum[:], lbT[:, :, 3, :], ALU.add, ["lbT", "lsum"], ["lsum"])
    RCP(lsum[:], lsum[:], ["lsum"], ["lsum"])
    for j in range(4):
        TTo("dve", lbT[:, :, j, :], lbT[:, :, j, :], lsum[:], ALU.mult, ["lbT", "lsum"], ["lbT"])
    TTo("dve", lbT[:, :, 2, :], lbT[:, :, 2, :], lbT[:, :, 1, :], ALU.add, ["lbT"], ["lbT"])
    TTo("dve", lbT[:, :, 3, :], lbT[:, :, 3, :], lbT[:, :, 2, :], ALU.add, ["lbT"], ["lbT"])
    MS("dve", lbT[:, :, 0, :], 0.0, ["lbT"])
    TS("dve", omlT[:], lbT[:], -1.0, 1.0, ALU.mult, ALU.add, ["lbT"], ["omlT"])
    TS("dve", nomlT[:], omlT[:], -1.0, None, ALU.mult, None, ["omlT"], ["nomlT"])

    mem["p"] = phase_base
    wm = [sb("wm0", [128, 8, 512]), sb("wm1", [128, 8, 512])]
    it = 0
    for l in layers:
        for jb in list(range(12)) + [0]:
            w_ = wm[it % 2]
            wk = f"wm{it % 2}"
            P.dma("sp", w_[:], wview(wmod_d[LI[l], :, jb * 512:(jb + 1) * 512]), [], [wk])
            pb = PS[it % 2]
            pk = f"ps{it % 2}"
            for j in range(4):
                for k in range(8):
                    MM(pb[:, j * V:(j + 1) * V], w_[:, k, j * 128:(j + 1) * 128], scT[:, k, :], k == 0, k == 7,
                       [wk, "scT"], [pk])
            TTo("dve", modT[:, l, :, jb * 4:(jb + 1) * 4],
                pb[:, 0:4 * V].rearrange("p (j v) -> p v j", v=V),
                bmT[:, l, jb * 4:(jb + 1) * 4].unsqueeze(1).to_broadcast([128, V, 4]),
                ALU.add, [pk, "bmT"], ["modT"])
            it += 1
    P.barrier()

    def mslice(l, v, i):
        return modT[:, l, v, i * 8:(i + 1) * 8]

    def rms_rstd(src, srckey, junk, d):
        ACT(junk, src, AF.Square, [srckey], ["junk", "ssq"], accum=ssq[:])
        ACT(rt[:], ssq[:], AF.Sqrt, ["ssq"], ["rt"], scale=1.0 / d, bias=EPS)
        RCP(rstd[:], rt[:], ["rt"], ["rstd"])

    def xsrc(tile):
        if tile < 16:
            return X[:, tile, :], ("X", tile)
        return XC[:, tile - 16, :], ("XC", tile - 16)

    def build_G(dst, gcol, gtmp, dkey):
        for c in range(8):
            TS("dve", gtmp[:, c, :], ones_f[:], gcol[:, c:c + 1], None, ALU.mult, None, ["ones_f", "modT"], [("gtmp", c)])
            MM(PS[6 + c // 4][:, (c % 4) * 128:(c % 4 + 1) * 128], gtmp[:, c, :], ident_f[:], True, True,
               [("gtmp", c), "ident_f"], [f"ps{6 + c // 4}"])
        CP("act", dst[:, 0:512], PS[6][:, :], ["ps6"], [dkey])
        CP("act", dst[:, 512:1024], PS[7][:, :], ["ps7"], [dkey])

    def residual_add(tile, half, pbank, pkey, G, gkey, tmp, tmpkey):
        dst, dk = xsrc(tile)
        TTo("dve", tmp[:], pbank[:, 0:512], G[:, half * 512:(half + 1) * 512], ALU.mult, [pkey, gkey], [tmpkey])
        TTo("pool", dst[:, half * 512:(half + 1) * 512], dst[:, half * 512:(half + 1) * 512], tmp[:], ALU.add,
            [tmpkey, dk], [dk])

    def compute_hT(s, l, hT, xnb, junk):
        for vi, v in enumerate((s, S)):
            STT("dve", am[:, 0, vi, :], mslice(l, v, 1), 1.0, ngT[:, l, 0, :], ALU.add, ALU.mult, ["modT", "ngT"], ["am"])
        for tile in range(18):
            vi = 0 if tile < 16 else 1
            v = s if tile < 16 else S
            src, sk = xsrc(tile)
            rms_rstd(src, sk, junk[:], D)
            TS("dve", xnb[:], src, rstd[:, 0:1], None, ALU.mult, None, [sk, "rstd"], ["xnb"])
            pb = PSB[tile % 2]
            pk = f"ps{tile % 2}"
            for c in range(8):
                TR(pb[:, c * 128:(c + 1) * 128], xnb[:, c * 128:(c + 1) * 128], ident_b[:], ["xnb", "ident_b"], [pk])
            for c in range(8):
                dst = hT[:, c, tile * 128:(tile + 1) * 128]
                if False:
                    pass
                else:
                    TS("dve", dst, pb[:, c * 128:(c + 1) * 128], am[:, 0, vi, c:c + 1], mslice(l, v, 0)[:, c:c + 1],
                       ALU.mult, ALU.add, [pk, "am", "modT"], [("hT", tile)])

    def hkeys(t0, t1):
        return [("hT", t) for t in range(t0, t1)]

    def proj_fm(dst_fn, W, wkey, wcols, hT, evac):
        for tb in range(5):
            n = 512 if tb < 4 else 256
            c0 = tb * 512
            pb = PS[tb % 2]
            pk = f"ps{tb % 2}"
            for k in range(8):
                MM(pb[:, 0:n], W[:, k, wcols], hT[:, k, c0:c0 + n], k == 0, k == 7,
                   [wkey] + hkeys(c0 // 128, (c0 + n) // 128), [pk])
            evac(tb, c0, n, pb, pk)

    def even_mixer(s, l, with_ctx):
        e = l // 2
        lam_init = 0.8 - 0.6 * math.exp(-0.3 * l)
        mem["p"] = phase_base
        hT = sb("hT", [128, 8, TT], BF16)
        xnb = sb("xnb", [128, D], BF16)
        junk = sb("junk", [128, D], BF16)
        G1 = sb("G1", [128, D])
        G1c = sb("G1c", [128, D])
        gtmp = sb("gtmp", [128, 8, 128])
        tmpy = sb("tmpy", [128, 512])
        sg_bc = sb("sg_bc", [128, 128])
        lr = sb("lr", [1, 256])
        grp_base = mem["p"]
        build_G(G1, mslice(l, s, 2), gtmp, "G1")
        build_G(G1c, mslice(l, S, 2), gtmp, "G1c")
        compute_hT(s, l, hT, xnb, junk)
        P.dma("sp", lr[:], lam_d[e], [], ["lr"])
        TTo("dve", lr[0:1, 0:64], lr[0:1, 0:64], lr[0:1, 64:128], ALU.mult, ["lr"], ["lr"])
        TTo("dve", lr[0:1, 128:192], lr[0:1, 128:192], lr[0:1, 192:256], ALU.mult, ["lr"], ["lr"])
        ACT(lr[0:1, 64:128], lr[0:1, 0:64], AF.Identity, ["lr"], ["lr", "small"], accum=small[0:1, 0:1])
        ACT(lr[0:1, 192:256], lr[0:1, 128:192], AF.Identity, ["lr"], ["lr", "small"], accum=small[0:1, 1:2])
        ACT(small[0:1, 0:2], small[0:1, 0:2], AF.Exp, ["small"], ["small"])
        TTo("dve", small[0:1, 2:3], small[0:1, 1:2], small[0:1, 0:1], ALU.subtract, ["small"], ["small"])
        TS("dve", small[0:1, 3:4], small[0:1, 2:3], -lam_init, None, ALU.add, None, ["small"], ["small"])
        MM(PS[7][:, 0:1], ones_f[0:1, :], small[0:1, 3:4], True, True, ["ones_f", "small"], ["ps7"])
        CP("dve", neglam[:], PS[7][:, 0:1], ["ps7"], ["neglam"])
        P.dma("sp", sg_bc[:], subln_d[e], [], ["sg_bc"])
        TS("dve", sg_bc[:], sg_bc[:], 1.0 - lam_init, None, ALU.mult, None, ["sg_bc"], ["sg_bc"])

        ntile_out = 18 if with_ctx else 16

        def out_proj(oT, nch, Wo, wokey):
            for tile in range(ntile_out):
                G, gk = (G1, "G1") if tile < 16 else (G1c, "G1c")
                for half in range(2):
                    pb = PS[5 + half]
                    pk = f"ps{5 + half}"
                    for ci in range(nch):
                        MM(pb[:, 0:512], oT[:, ci, tile * 128:(tile + 1) * 128], Wo[:, ci, half * 512:(half + 1) * 512],
                           ci == 0, ci == nch - 1, [("oT", tile), wokey], [pk])
                    residual_add(tile, half, pb, pk, G, gk, tmpy, "tmpy")

        for g in range(DBG['nagroups'] if DBG['na'] else 0):
            P.barrier()
            mem["p"] = grp_base
            Wq = sb("Wq", [128, 8, 256], BF16)
            Wk = sb("Wk", [128, 8, 256], BF16)
            Wv = sb("Wv", [128, 8, 256], BF16)
            Wo = sb("Wo", [128, 2, D], BF16)
            Ball = sb("Ball", [128, 4, 16, 64], BF16)
            qaT = sb("qaT", [128, 2, TT], BF16)
            kaT = sb("kaT", [128, 2, TT], BF16)
            va = sb("va", [128, 18, 4, 65], BF16)
            oT = sb("oT", [128, 2, TT], BF16)
            Eb = [sb("E0", [128, 512], BF16), sb("E1", [128, 512], BF16)]
            rc = sb("rc", [128, 4])
            otile = sb("otile", [128, 256], BF16)
            P.dma("pool", Wq[:], wview(attw_d[e, :, g * 256:(g + 1) * 256]), [], ["Wq"])
            P.dma("pool", Wk[:], wview(attw_d[e, :, 1024 + g * 256:1024 + (g + 1) * 256]), [], ["Wk"])
            P.dma("pool", Wv[:], wview(attw_d[e, :, 1536 + g * 256:1536 + (g + 1) * 256]), [], ["Wv"])
            P.dma("pool", Wo[:], attwo_d[e, g * 256:(g + 1) * 256, :].rearrange("(c p) n -> p c n", p=128), [], ["Wo"])
            P.dma("pool", Ball[:], nab_d[e, :, 4 * g:4 * g + 4, :, :], [], ["Ball"])
            ACT(Ball[:], Ball[:], AF.Copy, ["Ball"], ["Ball"], scale=8.0)
            MS("pool", va[:, :, :, 64:65], 1.0, ["va1"])
            for ci in range(2):
                def ev_q(tb, c0, n, pb, pk, ci=ci):
                    CP("act", qaT[:, ci, c0:c0 + n], pb[:, 0:n], [pk], [("qaT", ci, tb)])

                def ev_k(tb, c0, n, pb, pk, ci=ci):
                    CP("dve", kaT[:, ci, c0:c0 + n], pb[:, 0:n], [pk], [("kaT", ci, tb)])
                proj_fm(None, Wq, "Wq", slice(ci * 128, (ci + 1) * 128), hT, ev_q)
                proj_fm(None, Wk, "Wk", slice(ci * 128, (ci + 1) * 128), hT, ev_k)
            for tile in range(18):
                pb = PS[tile % 2]
                pk = f"ps{tile % 2}"
                for k in range(8):
                    MM(pb[:, 0:256], hT[:, k, tile * 128:(tile + 1) * 128], Wv[:, k, :], k == 0, k == 7,
                       ["Wv", ("hT", tile)], [pk])
                CP("act" if tile % 2 else "dve", va[:, tile, :, 0:64], pb[:, 0:256].rearrange("p (h d) -> p h d", d=64),
                   [pk], [("va", tile)])
            if g == 0 and DBG.get('dump'):
                dump(9, modT[:, l, s, :], ["modT"], 48)
                dump(10, am[:, :, :, :].rearrange("p a b c -> p (a b c)"), ["am"], 32)
                dump(11, bmT[:, l, :], ["bmT"], 48)
                dump(0, hT[:, 0, 0:512], hkeys(0, 4), 512)
                dump(1, G1[:, 0:512], ["G1"], 512)
                dump(2, qaT[:, 0, 0:512], [("qaT", 0, 0)], 512)
                dump(3, kaT[:, 0, 0:512], [("kaT", 0, 0)], 512)
                dump(4, va[:, 0, :, :].rearrange("p h d -> p (h d)"), [("va", 0), "va1"], 260)
                dump(5, Ball[:, 0, 3, :], ["Ball"], 64)
            qkeys = lambda ci: [("qaT", ci, tb) for tb in range(5)]
            kkeys = lambda ci: [("kaT", ci, tb) for tb in range(5)]
            for qr in range(DBG['nqr']):
                rs = min(max(qr - 4, 0), 24)
                t0 = rs // 2
                tiles = list(range(t0, t0 + (4 if rs % 2 == 0 else 5)))
                ob = PS[2 + qr % 2]
                ok = f"ps{2 + qr % 2}"
                for hh in range(4):
                    ci, hf = hh // 2, hh % 2
                    pr = slice(hf * 64, (hf + 1) * 64)
                    sbk = PS[hh % 2]
                    sk = f"ps{hh % 2}"
                    q_ap = qaT[pr, ci, qr * 64:(qr + 1) * 64]
                    slots = []
                    for si, tl in enumerate(tiles):
                        idx = []
                        for kr in (2 * tl, 2 * tl + 1):
                            idx.append(kr - qr + 7 if rs <= kr < rs + 8 else 15)
                        so = sbk[:, si * 64:(si + 1) * 64]
                        MM(so, kaT[pr, ci, tl * 128:(tl + 1) * 128], q_ap, True, False, qkeys(ci) + kkeys(ci), [sk])
                        MM(so, ident_lo[:, :], Ball[:, hh, idx[0], :], False, False, ["ident_b", "Ball"], [sk])
                        MM(so, ident_hi[:, :], Ball[:, hh, idx[1], :], False, True, ["ident_b", "Ball"], [sk])
                        slots.append(tl)
                    for cj in range(2):
                        si = len(tiles) + cj
                        MM(sbk[:, si * 64:(si + 1) * 64], kaT[pr, ci, T + cj * 128:T + (cj + 1) * 128], q_ap, True, True,
                           qkeys(ci) + kkeys(ci), [sk])
                        slots.append(16 + cj)
                    ns = len(slots)
                    E_ = Eb[hh % 2]
                    ek = f"E{hh % 2}"
                    ACT(E_[:, 0:ns * 64], sbk[:, 0:ns * 64], AF.Exp, [sk], [ek], scale=0.125)
                    for si, tl in enumerate(slots):
                        MM(ob[0:64, hh * 65:(hh + 1) * 65], E_[:, si * 64:(si + 1) * 64], va[:, tl, hh, :], si == 0, si == ns - 1,
                           [ek, ("va", tl), "va1"], [ok])
                ov = ob[0:64, 0:260].rearrange("p (h d) -> p h d", d=65)
                RCP(rc[0:64, :].unsqueeze(2), ov[:, :, 64:65], [ok], ["rc"])
                TTo("dve", otile[0:64, :].rearrange("p (h d) -> p h d", d=64), ov[:, :, 0:64],
                    rc[0:64, :].unsqueeze(2).to_broadcast([64, 4, 64]), ALU.mult, [ok, "rc"], ["otile"])
                for ci in range(2):
                    TR(PSB[4][:, ci * 64:(ci + 1) * 64], otile[0:64, ci * 128:(ci + 1) * 128], ident_b[0:64, 0:64],
                       ["otile", "ident_b"], ["ps4"])
                for ci in range(2):
                    CP("dve", oT[:, ci, qr * 64:(qr + 1) * 64], PSB[4][:, ci * 64:(ci + 1) * 64], ["ps4"], [("oT", qr // 2)])
            if with_ctx:
                for hh in range(4):
                    ci, hf = hh // 2, hh % 2
                    pr = slice(hf * 64, (hf + 1) * 64)
                    sbk = PS[hh % 2]
                    sk = f"ps{hh % 2}"
                    for cj in range(2):
                        MM(sbk[:, cj * 256:(cj + 1) * 256], kaT[pr, ci, T + cj * 128:T + (cj + 1) * 128], qaT[pr, ci, T:TT],
                           True, True, qkeys(ci) + kkeys(ci), [sk])
                    E_ = Eb[hh % 2]
                    ek = f"E{hh % 2}"
                    ACT(E_[:, 0:512], sbk[:, 0:512], AF.Exp, [sk], [ek], scale=0.125)
                    for qt in range(2):
                        for cj in range(2):
                            MM(PS[2 + qt][:, hh * 65:(hh + 1) * 65], E_[:, cj * 256 + qt * 128:cj * 256 + (qt + 1) * 128],
                               va[:, 16 + cj, hh, :], cj == 0, cj == 1, [ek, ("va", 16 + cj), "va1"], [f"ps{2 + qt}"])
                for qt in range(2):
                    ob = PS[2 + qt]
                    ok = f"ps{2 + qt}"
                    ov = ob[:, 0:260].rearrange("p (h d) -> p h d", d=65)
                    RCP(rc[:, :].unsqueeze(2), ov[:, :, 64:65], [ok], ["rc"])
                    TTo("dve", otile[:, :].rearrange("p (h d) -> p h d", d=64), ov[:, :, 0:64],
                        rc[:, :].unsqueeze(2).to_broadcast([128, 4, 64]), ALU.mult, [ok, "rc"], ["otile"])
                    for ci in range(2):
                        TR(PSB[4][:, ci * 128:(ci + 1) * 128], otile[:, ci * 128:(ci + 1) * 128], ident_b[:],
                           ["otile", "ident_b"], ["ps4"])
                    CP("act", oT[:, :, T + qt * 128:T + (qt + 1) * 128], PSB[4][:, 0:256].rearrange("p (c t) -> p c t", t=128),
                       ["ps4"], [("oT", 16 + qt)])
            if g == 0 and DBG.get('dump'):
                dump(6, oT[:, 0, 0:512], [("oT", t) for t in range(4)], 512)
                dump(7, Eb[0][:, 0:512], ["E0"], 512)
                dump(8, otile[:, :], ["otile"], 256)
            out_proj(oT, 2, Wo, "Wo")

        for hb in range(4 if DBG['diff'] else 0):
            P.barrier()
            mem["p"] = grp_base
            W5 = sb("W5", [128, 8, 5, 128], BF16)
            Wo = sb("Wo1", [128, 1, D], BF16)
            cosT = sb("cosT", [128, T], BF16)
            sinT = sb("sinT", [128, T], BF16)
            qbT = sb("qbT", [128, TT], BF16)
            kbT = sb("kbT", [128, TT], BF16)
            vb = sb("vb", [128, 18, 129], BF16)
            oT = sb("oTb", [128, 1, TT], BF16)
            Eb = [sb("E0", [128, 512], BF16), sb("E1", [128, 512], BF16)]
            t1 = sb("t1", [128, 512])
            t2 = sb("t2", [128, 512])
            dd = sb("dd", [128, 128])
            obt = sb("obt", [128, 128], BF16)
            r12 = sb("r12", [128, 4])
            cols = [512 + hb * 128, 3072 + hb * 128, 2048 + hb * 128, 3584 + hb * 128, 2560 + hb * 128]
            for i, c0 in enumerate(cols):
                P.dma("pool", W5[:, :, i, :], wview(attw_d[e, :, c0:c0 + 128]), [], [("W5", i)])
            P.dma("pool", Wo[:, 0, :], attwo_d[e, 512 + hb * 128:512 + (hb + 1) * 128, :], [], ["Wo1"])
            P.dma("pool", cosT[:], cos_d, [], ["cosT"])
            P.dma("pool", sinT[:], sin_d, [], ["sinT"])
            MS("pool", vb[:, :, 128:129], 1.0, ["vb1"])
            for (dstT, dname, i_raw, i_sw) in ((qbT, "qbT", 0, 1), (kbT, "kbT", 2, 3)):
                for tb in range(5):
                    n = 512 if tb < 4 else 256
                    c0 = tb * 512
                    hk = hkeys(c0 // 128, (c0 + n) // 128)
                    for k in range(8):
                        MM(PS[0][:, 0:n], W5[:, k, i_raw, :], hT[:, k, c0:c0 + n], k == 0, k == 7, [("W5", i_raw)] + hk, ["ps0"])
                    if tb < 4:
                        for k in range(8):
                            MM(PS[1][:, 0:n], W5[:, k, i_sw, :], hT[:, k, c0:c0 + n], k == 0, k == 7, [("W5", i_sw)] + hk, ["ps1"])
                        TTo("dve", t1[:], PS[0][:, 0:n], cosT[:, c0:c0 + n], ALU.mult, ["ps0", "cosT"], ["t1"])
                        TTo("dve", t2[:], PS[1][:, 0:n], sinT[:, c0:c0 + n], ALU.mult, ["ps1", "sinT"], ["t2"])
                        TTo("pool", dstT[:, c0:c0 + n], t1[:], t2[:], ALU.add, ["t1", "t2"], [(dname, tb)])
                    else:
                        CP("act", dstT[:, c0:c0 + n], PS[0][:, 0:n], ["ps0"], [(dname, tb)])
            for tile in range(18):
                pb = PS[tile % 2]
                pk = f"ps{tile % 2}"
                for k in range(8):
                    MM(pb[:, 0:128], hT[:, k, tile * 128:(tile + 1) * 128], W5[:, k, 4, :], k == 0, k == 7,
                       [("W5", 4), ("hT", tile)], [pk])
                CP("act" if tile % 2 else "dve", vb[:, tile, 0:128], pb[:, 0:128], [pk], [("vb", tile)])
            qk_all = [("qbT", tb) for tb in range(5)] + [("kbT", tb) for tb in range(5)]

            def diff_block(qc0, nq, kts):
                nsub = nq // 128
                acc = {}
                for sub in range(nsub):
                    for m in range(2):
                        a = sub * 2 + m
                        acc[(sub, m)] = (PS[2 + a // 3][:, (a % 3) * 129:(a % 3 + 1) * 129], f"ps{2 + a // 3}")
                for bnk in sorted(set(2 + (sub * 2 + m) // 3 for sub in range(nsub) for m in range(2))):
                    MS("dve", PS[bnk][:, :], 0.0, [f"ps{bnk}"])
                cnt = 0
                for ki, kt in enumerate(kts):
                    for m in range(2):
                        pr = slice(m * 64, (m + 1) * 64)
                        sbk = PS[cnt % 2]
                        sk = f"ps{cnt % 2}"
                        E_ = Eb[cnt % 2]
                        ek = f"E{cnt % 2}"
                        cnt += 1
                        MM(sbk[:, 0:nq], kbT[pr, kt * 128:(kt + 1) * 128], qbT[pr, qc0:qc0 + nq], True, True, qk_all, [sk])
                        ACT(E_[:, 0:nq], sbk[:, 0:nq], AF.Exp, [sk], [ek], scale=0.125)
                        for sub in range(nsub):
                            ap, akey = acc[(sub, m)]
                            MM(ap, E_[:, sub * 128:(sub + 1) * 128], vb[:, kt, :], False, ki == len(kts) - 1,
                               [ek, ("vb", kt), "vb1"], [akey])
                for sub in range(nsub):
                    (o1, k1), (o2, k2) = acc[(sub, 0)], acc[(sub, 1)]
                    RCP(r12[:, 0:1], o1[:, 128:129], [k1], ["r12"])
                    RCP(r12[:, 1:2], o2[:, 128:129], [k2], ["r12"])
                    TTo("dve", r12[:, 2:3], r12[:, 1:2], neglam[:, 0:1], ALU.mult, ["r12", "neglam"], ["r12"])
                    TS("dve", t1[:, 0:128], o1[:, 0:128], r12[:, 0:1], None, ALU.mult, None, [k1, "r12"], ["t1"])
                    STT("dve", dd[:], o2[:, 0:128], r12[:, 2:3], t1[:, 0:128], ALU.mult, ALU.add, [k2, "r12", "t1"], ["dd"])
                    rms_rstd(dd[:], "dd", t2[:, 0:128], 128)
                    STT("dve", obt[:], dd[:], rstd[:, 0:1], sg_bc[:], ALU.mult, ALU.mult, ["dd", "rstd", "sg_bc"], ["obt"])
                    TR(PSB[5][:, 0:128], obt[:], ident_b[:], ["obt", "ident_b"], ["ps5"])
                    tcol = qc0 + sub * 128
                    CP("act", oT[:, 0, tcol:tcol + 128], PSB[5][:, 0:128], ["ps5"], [("oT", tcol // 128)])

            for qb_ in range(4):
                diff_block(qb_ * 512, 512, list(range(18)))
            if with_ctx:
                diff_block(T, 256, [16, 17])
            out_proj(oT, 1, Wo, "Wo1")
        P.barrier()

    def odd_mixer(s, l, with_ctx):
        o = l // 2
        mem["p"] = phase_base
        hT = sb("hT", [128, 8, TT], BF16)
        G1 = sb("G1", [128, D])
        G1c = sb("G1c", [128, D])
        tmpy = sb("tmpy", [128, 512])
        tmp_mark = mem["p"]
        xnb = sb("xnb", [128, D], BF16)
        junk = sb("junk", [128, D], BF16)
        gtmp = sb("gtmp", [128, 8, 128])
        build_G(G1, mslice(l, s, 2), gtmp, "G1")
        build_G(G1c, mslice(l, S, 2), gtmp, "G1c")
        compute_hT(s, l, hT, xnb, junk)
        P.barrier()
        mem["p"] = tmp_mark
        W5 = sb("W5", [128, 8, 5, 128], BF16)
        Wo = sb("Wo", [128, D], BF16)
        A = sb("A", [128, TT])
        B = sb("B", [128, TT])
        kk = sb("kk", [128, TT], BF16)
        sq = sb("sq", [128, TT], BF16)
        sgt = sb("sgt", [128, TT], BF16)
        qt_ = sb("qt", [128, TT], BF16)
        qh = sb("qh", [128, TT], BF16)
        kt_ = sb("kt", [128, TT], BF16)
        vv = sb("vv", [128, 18, 128], BF16)
        oTs = sb("oTs", [128, TT])
        tot = sb("tot", [128, 36])
        Ee = sb("Ee", [128, 36])
        Ep = sb("Ep", [128, 36])
        ATm = sb("ATm", [128, 128], BF16)
        ktok = sb("ktok", [128, 128], BF16)
        R32 = [sb("R32a", [128, 128]), sb("R32b", [128, 128])]
        Rb = [sb("Rba", [128, 128], BF16), sb("Rbb", [128, 128], BF16)]
        ntile_out = 18 if with_ctx else 16
        NCH = 36
        for hd in range(8):
            P.barrier()
            cols = [hd * 128, 1024 + hd * 128, 2048 + hd * 128, 3072 + hd * 128, 4096 + hd * 128]
            for i, c0 in enumerate(cols):
                P.dma("pool", W5[:, :, i, :], wview(recw_d[o, :, c0:c0 + 128]), [], [("W5", i)])
            P.dma("pool", Wo[:], recwo_d[o, hd * 128:(hd + 1) * 128, :], [], ["Wo"])

            def ev_q(tb, c0, n, pb, pk):
                ACT(sq[:, c0:c0 + n], pb[:, 0:n], AF.Silu, [pk], ["sq"])

            def ev_g(tb, c0, n, pb, pk):
                ACT(sgt[:, c0:c0 + n], pb[:, 0:n], AF.Silu, [pk], ["sgt"])
            proj_fm(None, W5[:, :, 0, :], ("W5", 0), slice(0, 128), hT, ev_q)
            proj_fm(None, W5[:, :, 4, :], ("W5", 4), slice(0, 128), hT, ev_g)
            for tile in range(18):
                pb = PS[tile % 2]
                pk = f"ps{tile % 2}"
                for k in range(8):
                    MM(pb[:, 0:128], hT[:, k, tile * 128:(tile + 1) * 128], W5[:, k, 3, :], k == 0, k == 7,
                       [("W5", 3), ("hT", tile)], [pk])
                CP("dve", vv[:, tile, :], pb[:, 0:128], [pk], [("vv", tile)])
            vkeys = [("vv", t) for t in range(18)]
            for dr in range(2):
                lbc = lbT[:, dr, l, hd:hd + 1]
                omc = omlT[:, dr, l, hd:hd + 1]
                nomc = nomlT[:, dr, l, hd:hd + 1]

                def ev_f(tb, c0, n, pb, pk):
                    ACT(A[:, c0:c0 + n], pb[:, 0:n], AF.Sigmoid, [pk], ["A"])
                proj_fm(None, W5[:, :, 1 + dr, :], ("W5", 1 + dr), slice(0, 128), hT, ev_f)
                ACT(B[:], A[:], AF.Ln, ["A", "lbT", "omlT"], ["B"], scale=omc, bias=lbc)
                TS("dve", kk[:], A[:], nomc, omc, ALU.mult, ALU.add, ["A", "nomlT", "omlT"], ["kk"])
                for ch in range(NCH):
                    P.op("dve", (lambda a, b: (lambda E: E.tensor_tensor_scan(out=a, data0=ones_f[:, 0:64], data1=b, initial=0.0,
                                                                              op0=ALU.mult, op1=ALU.add)))(
                        A[:, ch * 64:(ch + 1) * 64], B[:, ch * 64:(ch + 1) * 64]), ["B", "ones_f", "kk"], ["A"])
                Av = A[:].rearrange("p (c k) -> p c k", k=64)
                Bv = B[:].rearrange("p (c k) -> p c k", k=64)
                CP("dve", tot[:].unsqueeze(2), Av[:, :, 63:64], ["A"], ["tot"])
                if dr == 1:
                    TTo("dve", A[:], B[:], A[:], ALU.subtract, ["A", "B"], ["A"])
                    TTo("dve", Av, Av, tot[:].unsqueeze(2).to_broadcast([128, NCH, 64]), ALU.add, ["A", "tot"], ["A"])
                ACT(Ee[:], tot[:], AF.Exp, ["tot"], ["Ee"])
                MS("dve", Ep[:], 0.0, ["Ep"])
                if dr == 0:
                    CP("dve", Ep[:, 1:32], Ee[:, 0:31], ["Ee"], ["Ep"])
                    CP("dve", Ep[:, 0:1], Ee[:, 35:36], ["Ee"], ["Ep"])
                    CP("dve", Ep[:, 33:36], Ee[:, 32:35], ["Ee"], ["Ep"])
                    order = [32, 33, 34, 35] + list(range(32))
                    msk = maskf
                else:
                    CP("dve", Ep[:, 0:31], Ee[:, 1:32], ["Ee"], ["Ep"])
                    CP("dve", Ep[:, 31:32], Ee[:, 32:33], ["Ee"], ["Ep"])
                    CP("dve", Ep[:, 32:35], Ee[:, 33:36], ["Ee"], ["Ep"])
                    order = [35, 34, 33, 32] + list(range(31, -1, -1))
                    msk = maskb
                ACT(qt_[:], A[:], AF.Exp, ["A"], ["qt"])
                ACT(kt_[:], A[:], AF.Exp, ["A"], ["kt"], scale=-1.0)
                TTo("pool", qt_[:], qt_[:], sq[:], ALU.mult, ["qt", "sq"], ["qt"])
                TTo("dve", kt_[:], kt_[:], kk[:], ALU.mult, ["kt", "kk"], ["kt"])
                TTo("pool", qh[:].rearrange("p (c k) -> p c k", k=64), qt_[:].rearrange("p (c k) -> p c k", k=64),
                    Ep[:].unsqueeze(2).to_broadcast([128, NCH, 64]), ALU.mult, ["qt", "Ep"], ["qh"])
                if hd == 0 and dr == 0 and DBG.get('dump'):
                    dump(0, B[:, 0:512], ["B"], 512)
                    dump(1, A[:, 0:512], ["A"], 512)
                    dump(2, kk[:, 0:512], ["kk"], 512)
                    dump(3, qt_[:, 0:512], ["qt"], 512)
                    dump(4, kt_[:, 0:512], ["kt"], 512)
                    dump(5, Ee[:, :], ["Ee"], 36)
                    dump(6, Ep[:, :], ["Ep"], 36)
                    dump(7, tot[:, :], ["tot"], 36)
                    dump(8, lbT[:].rearrange("p a b c -> p (a b c)"), ["lbT"], 64)
                    dump(9, vv[:, 0:4, :].rearrange("p a b -> p (a b)"), vkeys, 512)
                pp = 0
                first = True
                for ti in range(18):
                    ch_a, ch_b = order[2 * ti], order[2 * ti + 1]
                    tl = ch_a // 2
                    assert ch_b // 2 == tl
                    tc = slice(tl * 128, (tl + 1) * 128)
                    MM(PS[2][:, 0:128], kt_[:, tc], qt_[:, tc], True, True, ["kt", "qt"], ["ps2"])
                    TTo("dve", ATm[:], PS[2][:, 0:128], msk[:], ALU.mult, ["ps2", "maskf", "maskb"], ["ATm"])
                    TR(PSB[3][:, 0:128], kt_[:, tc], ident_b[:], ["kt", "ident_b"], ["ps3"])
                    CP("act", ktok[:], PSB[3][:, 0:128], ["ps3"], ["ktok"])
                    ob = PS[4 + ti % 2]
                    ok = f"ps{4 + ti % 2}"
                    chs = [ch_a, ch_b]
                    n_inter = sum(1 for ch in chs if not (first and ch == chs[0]))
                    MM(ob[:, 0:128], vv[:, tl, :], ATm[:], True, n_inter == 0, ["ATm"] + vkeys, [ok])
                    done = 0
                    for ch in chs:
                        hf = ch % 2
                        if not first:
                            done += 1
                            MM(ob[:, hf * 64:(hf + 1) * 64], Rb[pp][:], qh[:, ch * 64:(ch + 1) * 64], False, done == n_inter,
                               [("Rb", pp), "qh"], [ok])
                        MM(PS[6][:, 0:128], ktok[hf * 64:(hf + 1) * 64, :], vv[hf * 64:(hf + 1) * 64, tl, :], True, True,
                           ["ktok"] + vkeys, ["ps6"])
                        if first:
                            CP("dve", Rb[pp][:], PS[6][:, 0:128], ["ps6"], [("Rb", pp)])
                            CP("dve", R32[pp][:], PS[6][:, 0:128], ["ps6"], [("R32", pp)])
                            first = False
                        else:
                            STT("dve", Rb[1 - pp][:], R32[pp][:], Ep[:, ch:ch + 1], PS[6][:, 0:128], ALU.mult, ALU.add,
                                [("R32", pp), "Ep", "ps6"], [("Rb", 1 - pp)])
                            STT("dve", R32[1 - pp][:], R32[pp][:], Ep[:, ch:ch + 1], PS[6][:, 0:128], ALU.mult, ALU.add,
                                [("R32", pp), "Ep", "ps6"], [("R32", 1 - pp)])
                            pp = 1 - pp
                    if dr == 0:
                        CP("act", oTs[:, tc], ob[:, 0:128], [ok], [("oTs", tl)])
                    else:
                        TTo("dve", oTs[:, tc], oTs[:, tc], ob[:, 0:128], ALU.add, [ok, ("oTs", tl)], [("oTs", tl)])
            if hd == 0 and DBG.get('dump'):
                dump(10, oTs[:, 0:512], [("oTs", t) for t in range(4)], 512)
                dump(11, oTs[:, T:TT], [("oTs", t) for t in (16, 17)], 256)
                dump(12, R32[0][:], [("R32", 0)], 128)
            oTh = kk
            for tb in range(5):
                n = 512 if tb < 4 else 256
                c0 = tb * 512
                ok_ = [("oTs", t) for t in range(c0 // 128, (c0 + n) // 128)]
                ACT(A[:, c0:c0 + n], oTs[:, c0:c0 + n], AF.Square, ok_, ["A"])
                MM(PS[7][:, 0:n], ones_f[:], A[:, c0:c0 + n], True, True, ["ones_f", "A"], ["ps7"])
                ACT(B[:, c0:c0 + n], PS[7][:, 0:n], AF.Sqrt, ["ps7"], ["B"], scale=1.0 / 128, bias=EPS)
                RCP(B[:, c0:c0 + n], B[:, c0:c0 + n], ["B"], ["B"])
                TTo("dve", A[:, c0:c0 + n], oTs[:, c0:c0 + n], B[:, c0:c0 + n], ALU.mult, ok_ + ["B", "A"], ["A"])
                STT("dve", oTh[:, c0:c0 + n], A[:, c0:c0 + n], gnT[:, o:o + 1], sgt[:, c0:c0 + n], ALU.mult, ALU.mult,
                    ["A", "gnT", "sgt"], ["kk"])
            for tile in range(ntile_out):
                G, gk = (G1, "G1") if tile < 16 else (G1c, "G1c")
                for half in range(2):
                    pb = PS[half]
                    pk = f"ps{half}"
                    MM(pb[:, 0:512], oTh[:, tile * 128:(tile + 1) * 128], Wo[:, half * 512:(half + 1) * 512], True, True,
                       ["kk", "Wo"], [pk])
                    residual_add(tile, half, pb, pk, G, gk, tmpy, "tmpy")
        P.barrier()

    def moe(s, l, with_ctx):
        mem["p"] = phase_base
        ntl = 18 if with_ctx else 16
        NW = 288 if with_ctx else 256
        njt = 3 if with_ctx else 2
        hn = sb("hn", [128, 18, D], BF16)
        G2 = sb("G2", [128, D])
        G2c = sb("G2c", [128, D])
        posm_b = sb("posm_b", [16, TT], BF16)
        w_b = sb("w_b", [16, TT], BF16)
        pos_tok = sb("pos_tok", [128, 18, NE])
        wr = sb("wr", [128, 8, NE])
        selT = sb("selT", [16, 128], BF16)
        loop_base = mem["p"]
        gtmp = sb("gtmp", [128, 8, 128])
        xn32 = sb("xn32", [128, D])
        junk = sb("junk", [128, D], BF16)
        hT32 = sb("hT32", [128, 8, 128])
        afft = sb("afft", [128, NE])
        affT = sb("affT", [16, TT])
        work = sb("work", [16, TT])
        msk = sb("msk", [16, TT])
        pos = sb("pos", [16, TT])
        mx = sb("mx", [16, 8])
        build_G(G2, mslice(l, s, 5), gtmp, "G2")
        build_G(G2c, mslice(l, S, 5), gtmp, "G2c")
        P.dma("sp", wr[:], wview(router_d[LI[l]]), [], ["wr"])
        for vi, v in enumerate((s, S)):
            STT("dve", am[:, 1, vi, :], mslice(l, v, 4), 1.0, ngT[:, l, 1, :], ALU.add, ALU.mult, ["modT", "ngT"], ["am"])
        for tile in range(ntl):
            vi = 0 if tile < 16 else 1
            v = s if tile < 16 else S
            src, sk = xsrc(tile)
            rms_rstd(src, sk, junk[:], D)
            TS("dve", xn32[:], src, rstd[:, 0:1], None, ALU.mult, None, [sk, "rstd"], ["xn32"])
            CP("act" if DBG.get("nopool") else "pool", hn[:, tile, :], xn32[:], ["xn32"], [("hn", tile)])
            if DBG.get('sub', 9) < 1:
                continue
            pbk = f"ps{tile % 2}"
            for c in range(8):
                TR(PSB[tile % 2][:, c * 128:(c + 1) * 128], hn[:, tile, c * 128:(c + 1) * 128], ident_b[:],
                   [("hn", tile), "ident_b"], [pbk])
            for c in range(8):
                if DBG.get('noevac'):
                    continue
                src_p = PSB[tile % 2][:, c * 128:(c + 1) * 128]
                if c % 2 == 0:
                    TS("dve", hT32[:, c, :], src_p, am[:, 1, vi, c:c + 1], mslice(l, v, 3)[:, c:c + 1], ALU.mult, ALU.add,
                       [pbk, "am", "modT"], [("hT32", c)])
                    continue
                if c % 2 == 0:
                    ACT(hT32[:, c, :], src_p, AF.Identity, [pbk, "am", "modT"], [("hT32", c)],
                        scale=am[:, 1, vi, c:c + 1], bias=mslice(l, v, 3)[:, c:c + 1])
                else:
                    TS("dve", hT32[:, c, :], src_p, am[:, 1, vi, c:c + 1], mslice(l, v, 3)[:, c:c + 1], ALU.mult, ALU.add,
                       [pbk, "am", "modT"], [("hT32", c)])
            if DBG.get('sub', 9) < 2:
                continue
            for c in range(8):
                MM(PS[2][:, 0:NE], hT32[:, c, :], wr[:, c, :], c == 0, c == 7, [("hT32", c), "wr"], ["ps2"])
            if DBG.get('sub', 9) < 3:
                continue
            ACT(afft[:], PS[2][:, 0:NE], AF.Exp, ["ps2"], ["afft", "ssq"], accum=ssq[:])
            RCP(rt[:], ssq[:], ["ssq"], ["rt"])
            TS("dve", afft[:], afft[:], rt[:, 0:1], None, ALU.mult, None, ["afft", "rt"], ["afft"])
            if not DBG.get("notr"):
                TR(PS[3][0:16, 0:128], afft[:], ident_f[:], ["afft", "ident_f"], ["ps3"])
                CP("act", affT[0:16, tile * 128:(tile + 1) * 128], PS[3][0:16, 0:128], ["ps3"], ["affT"])

        if DBG.get('stage', 9) < 1:
            P.barrier()
            return

        def route(c0, n, cap):
            CP("dve", work[:, c0:c0 + n], affT[:, c0:c0 + n], ["affT"], ["work"])
            nit = cap // 8
            for it_ in range(nit):
                P.op("dve", (lambda a, b: (lambda E: E.max(out=a, in_=b)))(mx[:], work[:, c0:c0 + n]), ["work"], ["mx"])
                if it_ < nit - 1:
                    P.op("dve", (lambda a, b, c_: (lambda E: E.match_replace(out=a, in_to_replace=b, in_values=c_, imm_value=-1.0)))(
                        work[:, c0:c0 + n], mx[:], work[:, c0:c0 + n]), ["work", "mx"], ["work"])
            TS("dve", msk[:, c0:c0 + n], affT[:, c0:c0 + n], mx[:, 7:8], None, ALU.is_ge, None, ["affT", "mx"], ["msk"])
            MS("dve", work[:, c0:c0 + n], 1.0, ["work"])
            P.op("dve", (lambda a, b, c_: (lambda E: E.tensor_tensor_scan(out=a, data0=b, data1=c_, initial=0.0, op0=ALU.mult, op1=ALU.add)))(
                pos[:, c0:c0 + n], work[:, c0:c0 + n], msk[:, c0:c0 + n]), ["work", "msk"], ["pos"])
            TTo("dve", pos[:, c0:c0 + n], pos[:, c0:c0 + n], msk[:, c0:c0 + n], ALU.mult, ["pos", "msk"], ["pos"])
            TS("dve", pos[:, c0:c0 + n], pos[:, c0:c0 + n], -1.0, None, ALU.add, None, ["pos"], ["pos"])
            CP("dve", posm_b[:, c0:c0 + n], pos[:, c0:c0 + n], ["pos"], ["posm_b"])
            TTo("dve", w_b[:, c0:c0 + n], affT[:, c0:c0 + n], msk[:, c0:c0 + n], ALU.mult, ["affT", "msk"], ["w_b"])

        route(0, T, 256)
        if with_ctx:
            route(T, L, 32)
        if DBG.get('stage', 9) < 2:
            P.barrier()
            return
        for tile in range(ntl):
            TR(PS[3][:, 0:NE], pos[0:16, tile * 128:(tile + 1) * 128], ident_f[0:16, 0:16], ["pos", "ident_f"], ["ps3"])
            CP("act", pos_tok[:, tile, :], PS[3][:, 0:NE], ["ps3"], ["pos_tok"])
        P.barrier()
        mem["p"] = loop_base
        Pe = sb("Pe", [128, 16, 256], BF16)
        Pce = sb("Pce", [128, 2, 32], BF16)
        PT = sb("PT", [128, 2, T], BF16)
        PTc = sb("PTc", [32, L], BF16)
        xsel = sb("xsel", [128, 8, 288], BF16)
        hid = [sb("hid0", [128, 288], BF16), sb("hid1", [128, 288], BF16)]
        sgl = sb("sgl", [128, 288])
        yy = sb("yy", [128, 3, D], BF16)
        wsb = sb("wsb", [128, 512])
        Wg = [sb("Wg0", [128, 8, 256], BF16), sb("Wg1", [128, 8, 256], BF16)]
        Wu = [sb("Wu0", [128, 8, 256], BF16), sb("Wu1", [128, 8, 256], BF16)]
        Wd = [sb("Wd0", [128, 2, D], BF16), sb("Wd1", [128, 2, D], BF16)]
        hnk = [("hn", t) for t in range(ntl)]
        wcnt = 0
        for e in range(DBG['experts']):
            TS("dve", selT[:], ones_f[0:16, :], ident_f[0:16, e:e + 1], None, ALU.mult, None, ["ones_f", "ident_f"], ["selT"])
            TTo("dve", Pe[:], iota_j[:].unsqueeze(1).to_broadcast([128, 16, 256]),
                pos_tok[:, 0:16, e:e + 1].to_broadcast([128, 16, 256]), ALU.is_equal, ["iota_j", "pos_tok"], ["Pe"])
            if with_ctx:
                TTo("dve", Pce[:], iota_j[:, 0:32].unsqueeze(1).to_broadcast([128, 2, 32]),
                    pos_tok[:, 16:18, e:e + 1].to_broadcast([128, 2, 32]), ALU.is_equal, ["iota_j", "pos_tok"], ["Pce"])
            for blk in range(4):
                bc = slice(blk * 512, (blk + 1) * 512)
                MM(PS[6][:, 0:512], selT[0:16, :], posm_b[0:16, bc], True, True, ["selT", "posm_b"], ["ps6"])
                MM(PS[7][:, 0:512], selT[0:16, :], w_b[0:16, bc], True, True, ["selT", "w_b"], ["ps7"])
                CP("act", wsb[:], PS[7][:, 0:512], ["ps7"], ["wsb"])
                for jt in range(2):
                    STT("dve", PT[:, jt, bc], PS[6][:, 0:512], iota_p[:, jt:jt + 1], wsb[:], ALU.is_equal, ALU.mult,
                        ["ps6", "iota_p", "wsb"], ["PT"])
            if with_ctx:
                MM(PS[6][0:32, 0:L], selT[0:16, 0:32], posm_b[0:16, T:TT], True, True, ["selT", "posm_b"], ["ps6"])
                MM(PS[7][0:32, 0:L], selT[0:16, 0:32], w_b[0:16, T:TT], True, True, ["selT", "w_b"], ["ps7"])
                CP("act", wsb[0:32, 0:L], PS[7][0:32, 0:L], ["ps7"], ["wsb"])
                STT("dve", PTc[:], PS[6][0:32, 0:L], iota_p[0:32, 0:1], wsb[0:32, 0:L], ALU.is_equal, ALU.mult,
                    ["ps6", "iota_p", "wsb"], ["PTc"])
            for c in range(8):
                pb = PS[6 + c % 2]
                pk = f"ps{6 + c % 2}"
                for tile in range(16):
                    MM(pb[:, 0:256], hn[:, tile, c * 128:(c + 1) * 128], Pe[:, tile, :], tile == 0, tile == 15, ["Pe"] + hnk, [pk])
                if with_ctx:
                    for ct in range(2):
                        MM(pb[:, 256:288], hn[:, 16 + ct, c * 128:(c + 1) * 128], Pce[:, ct, :], ct == 0, ct == 1, ["Pce"] + hnk, [pk])
                TS("dve", xsel[:, c, 0:256], pb[:, 0:256], am[:, 1, 0, c:c + 1], mslice(l, s, 3)[:, c:c + 1], ALU.mult, ALU.add,
                   [pk, "am", "modT"], [("xsel", c)])
                if with_ctx:
                    TS("dve", xsel[:, c, 256:288], pb[:, 256:288], am[:, 1, 1, c:c + 1], mslice(l, S, 3)[:, c:c + 1], ALU.mult, ALU.add,
                       [pk, "am", "modT"], [("xsel", c)])
            xk = [("xsel", c) for c in range(8)]
            pend = None

            def down(fc, wi, f2):
                for jt in range(njt):
                    rows = 128 if jt < 2 else 32
                    for half in range(2):
                        b = jt * 2 + half
                        MM(PS[b][0:rows, 0:512], hid[fc % 2][:, jt * 128:jt * 128 + rows], Wd[wi][:, f2, half * 512:(half + 1) * 512],
                           fc == 0, fc == 15, [f"hid{fc % 2}", f"Wd{wi}"], [f"ps{b}"])

            for fb in range(8):
                wi = wcnt % 2
                wcnt += 1
                P.dma("pool", Wg[wi][:], wview(wg_d[LI[l], e, :, fb * 256:(fb + 1) * 256]), [], [f"Wg{wi}"])
                P.dma("pool", Wu[wi][:], wview(wu_d[LI[l], e, :, fb * 256:(fb + 1) * 256]), [], [f"Wu{wi}"])
                P.dma("pool", Wd[wi][:], wd_d[LI[l], e, fb * 256:(fb + 1) * 256, :].rearrange("(c p) n -> p c n", p=128), [], [f"Wd{wi}"])
                for f2 in range(2):
                    fc = fb * 2 + f2
                    for c in range(8):
                        MM(PS[6][:, 0:NW], Wg[wi][:, c, f2 * 128:(f2 + 1) * 128], xsel[:, c, 0:NW], c == 0, c == 7, [f"Wg{wi}"] + xk, ["ps6"])
                    for c in range(8):
                        MM(PS[7][:, 0:NW], Wu[wi][:, c, f2 * 128:(f2 + 1) * 128], xsel[:, c, 0:NW], c == 0, c == 7, [f"Wu{wi}"] + xk, ["ps7"])
                    ACT(sgl[:, 0:NW], PS[6][:, 0:NW], AF.Silu, ["ps6"], ["sgl"])
                    TTo("dve", hid[fc % 2][:, 0:NW], sgl[:, 0:NW], PS[7][:, 0:NW], ALU.mult, ["sgl", "ps7"], [f"hid{fc % 2}"])
                    if pend is not None:
                        down(*pend)
                    pend = (fc, wi, f2)
            down(*pend)
            for jt in range(njt):
                rows = 128 if jt < 2 else 32
                G = G2 if jt < 2 else G2c
                gk = "G2" if jt < 2 else "G2c"
                for half in range(2):
                    b = jt * 2 + half
                    TTo("dve", yy[0:rows, jt, half * 512:(half + 1) * 512], PS[b][0:rows, 0:512], G[0:rows, half * 512:(half + 1) * 512],
                        ALU.mult, [f"ps{b}", gk], [("yy", jt)])
            for tile in range(16):
                for half in range(2):
                    b = (tile % 3) * 2 + half
                    for jt in range(2):
                        MM(PS[b][:, 0:512], PT[:, jt, tile * 128:(tile + 1) * 128], yy[:, jt, half * 512:(half + 1) * 512], jt == 0, jt == 1,
                           ["PT", ("yy", jt)], [f"ps{b}"])
                    TTo("dve", X[:, tile, half * 512:(half + 1) * 512], X[:, tile, half * 512:(half + 1) * 512], PS[b][:, 0:512], ALU.add,
                        [f"ps{b}", ("X", tile)], [("X", tile)])
            if with_ctx:
                for ct in range(2):
                    for half in range(2):
                        b = ct * 2 + half
                        MM(PS[b][:, 0:512], PTc[0:32, ct * 128:(ct + 1) * 128], yy[0:32, 2, half * 512:(half + 1) * 512], True, True,
                           ["PTc", ("yy", 2)], [f"ps{b}"])
                        TTo("dve", XC[:, ct, half * 512:(half + 1) * 512], XC[:, ct, half * 512:(half + 1) * 512], PS[b][:, 0:512], ALU.add,
                            [f"ps{b}", ("XC", ct)], [("XC", ct)])
        P.barrier()

    for s in range(S):
        for tile in range(16):
            P.dma("sp", X[:, tile, :], x_d[s, tile * 128:(tile + 1) * 128, :], [], [("X", tile)])
        for ct in range(2):
            P.dma("sp", XC[:, ct, :], ctx_d[s, ct * 128:(ct + 1) * 128, :], [], [("XC", ct)])
        for l in layers:
            last = (l == 3)
            if DBG['mix']:
                if l % 2 == 0:
                    even_mixer(s, l, not last)
                else:
                    odd_mixer(s, l, not last)
            if DBG['moe']:
                moe(s, l, not last)
        P.barrier()
        mem["p"] = phase_base
        fg = sb("fg", [128, D])
        junk = sb("junk", [128, D], BF16)
        ot = [sb("ot0", [128, D]), sb("ot1", [128, D])]
        if final:
            P.dma("sp", fg[:], fg_d, [], ["fg"])
        for tile in range(16):
            src, sk = xsrc(tile)
            o_ = ot[tile % 2]
            okey = f"ot{tile % 2}"
            if final:
                rms_rstd(src, sk, junk[:], D)
                STT("dve", o_[:], src, rstd[:, 0:1], fg[:], ALU.mult, ALU.mult, [sk, "rstd", "fg"], [okey])
            else:
                CP("dve", o_[:], src, [sk], [okey])
            P.dma("sp", out_d[s, tile * 128:(tile + 1) * 128, :], o_[:], [okey], [])
        if debug_ctx:
            for ct in range(2):
                P.dma("sp", outc_d[s, ct * 128:(ct + 1) * 128, :], XC[:, ct, :], [("XC", ct)], [])
        P.barrier()
    P.wait_dmas("sp")
    P.run()
    return nc, P


def _consts():
    c = {}
    c["c_ident"] = np.eye(128, dtype=np.float32)
    lo = np.eye(128, dtype=np.float32); lo[64:, :] = 0
    hi = np.eye(128, dtype=np.float32); hi[:64, :] = 0
    c["c_ident_lo"] = lo
    c["c_ident_hi"] = hi
    c["c_iota_j"] = np.broadcast_to(np.arange(256, dtype=np.float32)[None, :], (128, 256)).copy()
    p = np.arange(128, dtype=np.float32)
    c["c_iota_p"] = np.stack([p, p + 128, p, p], axis=1).copy()
    s_ = np.arange(128)[:, None]
    t_ = np.arange(128)[None, :]
    same = (s_ // 64) == (t_ // 64)
    c["c_maskf"] = (same & (s_ <= t_)).astype(np.float32)
    c["c_maskb"] = (same & (s_ >= t_)).astype(np.float32)
    t = np.arange(T)
    rows = (t // 64).astype(np.float32)
    colsf = (t % 64).astype(np.float32)
    inv = (10000.0 ** (-np.arange(16, dtype=np.float32) / 16)).astype(np.float32)
    ang = np.concatenate([rows[:, None] * inv, colsf[:, None] * inv], axis=-1).astype(np.float32)
    cos = np.cos(ang).astype(np.float32).T
    sin = np.sin(ang).astype(np.float32).T
    cos64 = np.concatenate([cos, cos], axis=0)
    sin64 = np.concatenate([-sin, sin], axis=0)
    c["rope_cos"] = np.concatenate([cos64, cos64], axis=0).astype(np.float32)
    c["rope_sin"] = np.concatenate([sin64, sin64], axis=0).astype(np.float32)
    return c


def _swap_perm(base):
    idx = []
    for m in range(8):
        o = base + m * 64
        idx += list(range(o + 32, o + 64)) + list(range(o, o + 32))
    return np.array(idx)


def _na_bias_table(rpb):
    kc = np.arange(64)[:, None]
    qc = np.arange(64)[None, :]
    cstart = np.clip(qc - 8, 0, 48)
    valid = (kc >= cstart) & (kc < cstart + 16)
    dcol = np.clip(kc - qc + 15, 0, 30)
    tab = np.full((64, 8, 16, 64), -3750.0, dtype=np.float32)
    g = rpb[:, :, dcol]
    g = np.transpose(g, (2, 0, 1, 3))
    vm = np.broadcast_to(valid[:, None, None, :], g.shape)
    tab[:, :, 0:15, :] = np.where(vm, g, np.float32(-3750.0))
    return np.concatenate([tab, tab], axis=0)


def prep_shared(inp, layers=(0, 1, 2, 3)):
    layers = list(layers) if len(layers) else [0]
    f = lambda a: np.ascontiguousarray(np.asarray(a, dtype=np.float32))
    d = dict(_consts())
    d["w_mod"] = f(np.asarray(inp["w_mod"])[layers])
    d["b_modT"] = f(np.transpose(np.asarray(inp["b_mod"]).reshape(4, 48, 128), (2, 0, 1)))
    d["norm_gT"] = f(np.transpose(np.asarray(inp["norm_g"]).reshape(4, 2, 8, 128), (3, 0, 1, 2)))
    w_in = np.asarray(inp["att_w_in"])
    d["att_w"] = f(np.concatenate([w_in, w_in[:, :, _swap_perm(512)], w_in[:, :, _swap_perm(2048)]], axis=2))
    d["att_wo"] = f(inp["att_w_out"])
    d["na_bias"] = f(np.stack([_na_bias_table(np.asarray(inp["na_rpb"])[e]) for e in range(2)], axis=0))
    d["lam_row"] = f(np.asarray(inp["diff_lambda"]).reshape(2, 1, 256))
    d["subln_bc"] = f(np.broadcast_to(np.asarray(inp["diff_subln_g"])[:, None, :], (2, 128, 128)))
    d["rec_w_in"] = f(inp["rec_w_in"])
    d["rec_wo"] = f(inp["rec_w_out"])
    d["rec_lbT"] = f(np.transpose(np.asarray(inp["rec_lb_logits"]).reshape(2, 4, 8, 128), (3, 0, 1, 2)))
    d["rec_gn"] = f(np.asarray(inp["rec_gnorm_g"]).T)
    d["router"] = f(np.asarray(inp["moe_router"])[layers])
    nex = max(1, DBG["experts"])
    d["wg"] = f(np.asarray(inp["moe_w_gate"])[layers][:, :nex])
    d["wu"] = f(np.asarray(inp["moe_w_up"])[layers][:, :nex])
    d["wd"] = f(np.asarray(inp["moe_w_down"])[layers][:, :nex])
    d["finalg_bc"] = f(np.broadcast_to(np.asarray(inp["final_g"])[None, :], (128, D)))
    return d


def prep_core(inp, samples):
    f = lambda a: np.ascontiguousarray(np.asarray(a, dtype=np.float32))
    x = np.asarray(inp["x"])
    ctx = np.asarray(inp["ctx"])
    c = np.asarray(inp["c"])
    cv = np.concatenate([c[samples], np.asarray(inp["c_ctx"])[None, :]], axis=0)
    V = cv.shape[0]
    return {
        "x": f(x[samples]),
        "ctx": f(ctx[samples]),
        "cvT": f(np.transpose(cv.reshape(V, 8, 128), (2, 1, 0))),
    }


_CACHE = {}


def kernel(**inputs):
    n_cores = 8
    S = 2
    if "nc" not in _CACHE:
        _CACHE["nc"] = build(S, [0, 1, 2, 3], final=True)[0]
    nc = _CACHE["nc"]
    shared = prep_shared(inputs)
    in_maps = []
    for i in range(n_cores):
        m = dict(shared)
        m.update(prep_core(inputs, list(range(i * S, (i + 1) * S))))
        in_maps.append(m)
    res = run_bass_kernel_spmd(nc, in_maps, core_ids=list(range(n_cores)))
    return np.concatenate([r["out"] for r in res.results], axis=0).astype(np.float32)
```

```python
import math
import numpy as np
import concourse.bass as bass
import concourse.mybir as mybir
from concourse.bass_utils import run_bass_kernel_spmd

F32 = mybir.dt.float32
BF16 = mybir.dt.bfloat16
AF = mybir.ActivationFunctionType
ALU = mybir.AluOpType

EPOCH = 28800


class Stream:
    def __init__(self, prog, name):
        self.prog = prog
        self.name = name
        self.sems = []
        self.n = 0

    def sem_for(self, e):
        while len(self.sems) <= e:
            self.sems.append(self.prog.nc.alloc_semaphore(f"{self.name}_{len(self.sems)}"))
        return self.sems[e]

    def bump(self, inc):
        e = self.n // EPOCH
        assert (self.n + inc - 1) // EPOCH == e
        self.n += inc
        return self.sem_for(e), (self, self.n)

    def loc(self, n):
        e = (n - 1) // EPOCH
        return self.sem_for(e), n - e * EPOCH


class Prog:
    ENGS = ("pe", "act", "dve", "pool", "sp")

    def __init__(self, nc, n_dma_sems=4):
        self.nc = nc
        self.q = {e: [] for e in self.ENGS}
        self.stream = {e: Stream(self, "s_" + e) for e in self.ENGS}
        self.seen = {e: {} for e in self.ENGS}
        self.last_w = {}
        self.readers = {}
        self.dma_streams = {}
        self.dma_rr = {}
        self.n_dma_sems = n_dma_sems
        self.ninst = 0

    def _deps(self, reads, writes):
        deps = []
        for k in reads:
            t = self.last_w.get(k)
            if t is not None:
                deps.append(t)
        for k in writes:
            t = self.last_w.get(k)
            if t is not None:
                deps.append(t)
            deps.extend(self.readers.get(k, ()))
        return deps

    def _commit(self, tok, reads, writes):
        for k in writes:
            self.last_w[k] = tok
            self.readers[k] = []
        for k in reads:
            if k in writes:
                continue
            self.readers.setdefault(k, []).append(tok)

    def _waits(self, eng, deps, skip_self=False):
        seen = self.seen[eng]
        best = {}
        for (st, n) in deps:
            if skip_self and st is self.stream[eng]:
                continue
            if seen.get(st, 0) >= n:
                continue
            if best.get(st, 0) < n:
                best[st] = n
        waits = []
        for st, n in best.items():
            seen[st] = n
            waits.append(st.loc(n))
        return waits

    def op(self, eng, fn, reads=(), writes=()):
        reads = tuple(reads)
        writes = tuple(writes) + tuple(k for k in reads if isinstance(k, str) and k.startswith("ps") and k[2:].isdigit())
        deps = self._deps(reads, writes)
        waits = self._waits(eng, deps, skip_self=(eng == "pe"))
        sem, tok = self.stream[eng].bump(1)

        def emit(E, waits=waits, fn=fn, sem=sem):
            for (s, v) in waits:
                E.wait_ge(s, v)
            fn(E).then_inc(sem, 1)

        self.q[eng].append(emit)
        self._commit(tok, reads, writes)
        self.ninst += 1
        return tok

    def dma(self, queue, out, in_, reads=(), writes=()):
        reads = tuple(reads)
        writes = tuple(writes)
        deps = self._deps(reads, writes)
        if queue not in self.dma_streams:
            self.dma_streams[queue] = [Stream(self, f"d_{queue}{i}") for i in range(self.n_dma_sems)]
            self.dma_rr[queue] = 0
        i = self.dma_rr[queue]
        self.dma_rr[queue] = (i + 1) % self.n_dma_sems
        st = self.dma_streams[queue][i]
        if st.n > 0:
            deps.append((st, st.n))
        waits = self._waits(queue, deps)
        sem, tok = st.bump(16)

        def emit(E, waits=waits, sem=sem, out=out, in_=in_):
            for (s, v) in waits:
                E.wait_ge(s, v)
            E.dma_start(out=out, in_=in_).then_inc(sem, 16)

        self.q[queue].append(emit)
        self._commit(tok, reads, writes)
        self.ninst += 1
        return tok

    def all_tokens(self):
        toks = []
        for e in self.ENGS:
            if self.stream[e].n:
                toks.append((self.stream[e], self.stream[e].n))
        for q, sts in self.dma_streams.items():
            for st in sts:
                if st.n:
                    toks.append((st, st.n))
        return toks

    def barrier(self):
        toks = self.all_tokens()
        for eng in self.ENGS:
            waits = self._waits(eng, toks)

            def emit(E, waits=waits):
                for (s, v) in waits:
                    E.wait_ge(s, v)

            self.q[eng].append(emit)

    def wait_dmas(self, eng):
        toks = [(st, st.n) for q, sts in self.dma_streams.items() for st in sts if st.n]
        waits = self._waits(eng, toks)

        def emit(E, waits=waits):
            for (s, v) in waits:
                E.wait_ge(s, v)

        self.q[eng].append(emit)

    def run(self):
        nc = self.nc
        with nc.Block() as block:
            @block.tensor
            def _(E):
                for f in self.q["pe"]:
                    f(E)

            @block.scalar
            def _(E):
                for f in self.q["act"]:
                    f(E)

            @block.vector
            def _(E):
                for f in self.q["dve"]:
                    f(E)

            @block.gpsimd
            def _(E):
                for f in self.q["pool"]:
                    f(E)

            @block.sync
            def _(E):
                for f in self.q["sp"]:
                    f(E)


DBG = {'na': 1, 'diff': 1, 'mix': 1, 'moe': 1, 'experts': 16, 'nagroups': 2, 'nqr': 32}
D = 1024
T = 2048
L = 256
TT = T + L
EPS = 1e-6
NE = 16
FF = 2048


def build(S, layers, final=True, debug_ctx=False):
    nc = bass.Bass("TRN2", target_bir_lowering=False)
    P = Prog(nc)
    V = S + 1

    def din(name, shape):
        return nc.dram_tensor(name, list(shape), F32, kind="ExternalInput").ap()

    x_d = din("x", [S, T, D])
    ctx_d = din("ctx", [S, L, D])
    cvT_d = din("cvT", [128, 8, V])
    NL = max(1, len(layers))
    LI = {l: i for i, l in enumerate(layers)}
    wmod_d = din("w_mod", [NL, D, 6 * D])
    bmT_d = din("b_modT", [128, 4, 48])
    ngT_d = din("norm_gT", [128, 4, 2, 8])
    attw_d = din("att_w", [2, D, 4096])
    attwo_d = din("att_wo", [2, D, D])
    nab_d = din("na_bias", [2, 128, 8, 16, 64])
    lam_d = din("lam_row", [2, 1, 256])
    subln_d = din("subln_bc", [2, 128, 128])
    cos_d = din("rope_cos", [128, T])
    sin_d = din("rope_sin", [128, T])
    recw_d = din("rec_w_in", [2, D, 5 * D])
    recwo_d = din("rec_wo", [2, D, D])
    lbT_d = din("rec_lbT", [128, 2, 4, 8])
    gn_d = din("rec_gn", [128, 2])
    router_d = din("router", [NL, D, NE])
    NEX = max(1, DBG["experts"])
    wg_d = din("wg", [NL, NEX, D, FF])
    wu_d = din("wu", [NL, NEX, D, FF])
    wd_d = din("wd", [NL, NEX, FF, D])
    fg_d = din("finalg_bc", [128, D])
    cid_d = din("c_ident", [128, 128])
    cij_d = din("c_iota_j", [128, 256])
    cip_d = din("c_iota_p", [128, 4])
    cmf_d = din("c_maskf", [128, 128])
    cmb_d = din("c_maskb", [128, 128])
    cil_d = din("c_ident_lo", [128, 128])
    cih_d = din("c_ident_hi", [128, 128])
    out_d = nc.dram_tensor("out", [S, T, D], F32, kind="ExternalOutput").ap()
    outc_d = nc.dram_tensor("out_ctx", [S, L, D], F32, kind="ExternalOutput").ap() if debug_ctx else None
    dbg_d = nc.dram_tensor("dbg", [16, 128, 512], F32, kind="ExternalOutput").ap() if debug_ctx else None

    base = (nc.sbuf_base + 63) // 64 * 64
    lim = nc.sbuf_top
    mem = {"p": base, "n": 0}

    def sb(name, shape, dt=F32):
        nb = int(np.prod(shape[1:])) * (2 if dt == BF16 else 4)
        nb = (nb + 63) // 64 * 64
        off = mem["p"]
        assert off + nb <= lim, f"SBUF overflow at {name}: {off + nb} > {lim}"
        mem["p"] = off + nb
        mem["n"] += 1
        return nc.alloc_sbuf_tensor_at(f"{name}_{mem['n']}", list(shape), dt, offset=off)

    PS = [nc.alloc_psum_tensor(f"ps{i}", [128, 512], F32) for i in range(8)]
    PSB = [p[:].bitcast(BF16) for p in PS]

    def MM(out, lhsT, rhs, st, sp, r, w):
        P.op("pe", lambda E: E.matmul(out, lhsT=lhsT, rhs=rhs, start=st, stop=sp), r, w)

    def TR(out, in_, idn, r, w):
        P.op("pe", lambda E: E.transpose(out=out, in_=in_, identity=idn), r, w)

    def ACT(out, in_, func, r, w, scale=None, bias=None, accum=None):
        kw = {}
        if scale is not None:
            kw["scale"] = scale
        if bias is not None:
            kw["bias"] = bias
        if accum is not None:
            kw["accum_out"] = accum
        P.op("act", lambda E: E.activation(out=out, in_=in_, func=func, **kw), r, w)

    def TS(eng, out, in0, s1, s2, op0, op1, r, w):
        if s2 is None:
            P.op(eng, lambda E: E.tensor_scalar(out=out, in0=in0, scalar1=s1, scalar2=None, op0=op0), r, w)
        else:
            P.op(eng, lambda E: E.tensor_scalar(out=out, in0=in0, scalar1=s1, scalar2=s2, op0=op0, op1=op1), r, w)

    def TTo(eng, out, in0, in1, op, r, w):
        P.op(eng, lambda E: E.tensor_tensor(out=out, in0=in0, in1=in1, op=op), r, w)

    def STT(eng, out, in0, sc, in1, op0, op1, r, w):
        P.op(eng, lambda E: E.scalar_tensor_tensor(out=out, in0=in0, scalar=sc, in1=in1, op0=op0, op1=op1), r, w)

    def CP(eng, out, in_, r, w):
        if eng == "act":
            P.op("act", lambda E: E.copy(out=out, in_=in_), r, w)
        else:
            P.op(eng, lambda E: E.tensor_copy(out=out, in_=in_), r, w)

    def MS(eng, ap, v, w):
        P.op(eng, lambda E: E.memset(ap, v), [], w)

    dbgbuf = {}

    def dump(i, ap, keys, n):
        if dbg_d is None:
            return
        if "t" not in dbgbuf:
            dbgbuf["t"] = nc.alloc_sbuf_tensor_at("dbgt", [128, 512], F32, offset=(lim - 4096) // 64 * 64)
        t = dbgbuf["t"]
        rows = ap.shape[0]
        P.op("dve", lambda E: E.memset(t[:], 0.0), [], ["dbgt"])
        P.op("dve", lambda E: E.tensor_copy(out=t[0:rows, 0:n], in_=ap), keys, ["dbgt"])
        P.dma("sp", dbg_d[i], t[:], ["dbgt"], [])

    def RCP(out, in_, r, w):
        P.op("dve", lambda E: E.reciprocal(out=out, in_=in_), r, w)

    def wview(src2d):
        return src2d.rearrange("(k p) n -> p k n", p=128)

    X = sb("X", [128, 16, D])
    XC = sb("XC", [128, 2, D])
    ident_f = sb("ident_f", [128, 128])
    ident_b = sb("ident_b", [128, 128], BF16)
    ones_f = sb("ones_f", [128, 128])
    ident_lo = sb("ident_lo", [128, 128], BF16)
    ident_hi = sb("ident_hi", [128, 128], BF16)
    iota_j = sb("iota_j", [128, 256])
    iota_p = sb("iota_p", [128, 4])
    maskf = sb("maskf", [128, 128])
    maskb = sb("maskb", [128, 128])
    modT = sb("modT", [128, 4, V, 48])
    bmT = sb("bmT", [128, 4, 48])
    ngT = sb("ngT", [128, 4, 2, 8])
    scT = sb("scT", [128, 8, V])
    lbT = sb("lbT", [128, 2, 4, 8])
    omlT = sb("omlT", [128, 2, 4, 8])
    nomlT = sb("nomlT", [128, 2, 4, 8])
    gnT = sb("gnT", [128, 2])
    am = sb("am", [128, 2, 2, 8])
    ssq = sb("ssq", [128, 1])
    rt = sb("rt", [128, 1])
    rstd = sb("rstd", [128, 1])
    neglam = sb("neglam", [128, 1])
    small = sb("small", [128, 16])
    lsum = sb("lsum", [128, 2, 8])
    phase_base = mem["p"]

    P.dma("sp", ident_f[:], cid_d, [], ["ident_f"])
    P.dma("pool", ident_b[:], cid_d, [], ["ident_b"])
    P.dma("pool", ident_lo[:], cil_d, [], ["ident_b"])
    P.dma("pool", ident_hi[:], cih_d, [], ["ident_b"])
    P.dma("sp", iota_j[:], cij_d, [], ["iota_j"])
    P.dma("sp", iota_p[:], cip_d, [], ["iota_p"])
    P.dma("sp", maskf[:], cmf_d, [], ["maskf"])
    P.dma("sp", maskb[:], cmb_d, [], ["maskb"])
    P.dma("sp", bmT[:], bmT_d, [], ["bmT"])
    P.dma("sp", ngT[:], ngT_d, [], ["ngT"])
    P.dma("sp", scT[:], cvT_d, [], ["scT"])
    P.dma("sp", lbT[:], lbT_d, [], ["lbT"])
    P.dma("sp", gnT[:], gn_d, [], ["gnT"])
    MS("pool", ones_f[:], 1.0, ["ones_f"])
    ACT(scT[:], scT[:], AF.Silu, ["scT"], ["scT"])

    ACT(lbT[:], lbT[:], AF.Exp, ["lbT"], ["lbT"])
    TTo("dve", lsum[:], lbT[:, :, 0, :], lbT[:, :, 1, :], ALU.add, ["lbT"], ["lsum"])
    TTo("dve", lsum[:], lsum[:], lbT[:, :, 2, :], ALU.add, ["lbT", "lsum"], ["lsum"])
    TTo("dve", lsum[:], lsum[:], lbT[:, :, 3, :], ALU.add, ["lbT", "lsum"], ["lsum"])
    RCP(lsum[:], lsum[:], ["lsum"], ["lsum"])
    for j in range(4):
        TTo("dve", lbT[:, :, j, :], lbT[:, :, j, :], lsum[:], ALU.mult, ["lbT", "lsum"], ["lbT"])
    TTo("dve", lbT[:, :, 2, :], lbT[:, :, 2, :], lbT[:, :, 1, :], ALU.add, ["lbT"], ["lbT"])
    TTo("dve", lbT[:, :, 3, :], lbT[:, :, 3, :], lbT[:, :, 2, :], ALU.add, ["lbT"], ["lbT"])
    MS("dve", lbT[:, :, 0, :], 0.0, ["lbT"])
    TS("dve", omlT[:], lbT[:], -1.0, 1.0, ALU.mult, ALU.add, ["lbT"], ["omlT"])
    TS("dve", nomlT[:], omlT[:], -1.0, None, ALU.mult, None, ["omlT"], ["nomlT"])

    mem["p"] = phase_base
    wm = [sb("wm0", [128, 8, 512]), sb("wm1", [128, 8, 512])]
    it = 0
    for l in layers:
        for jb in list(range(12)) + [0]:
            w_ = wm[it % 2]
            wk = f"wm{it % 2}"
            P.dma("sp", w_[:], wview(wmod_d[LI[l], :, jb * 512:(jb + 1) * 512]), [], [wk])
            pb = PS[it % 2]
            pk = f"ps{it % 2}"
            for j in range(4):
                for k in range(8):
                    MM(pb[:, j * V:(j + 1) * V], w_[:, k, j * 128:(j + 1) * 128], scT[:, k, :], k == 0, k == 7,
                       [wk, "scT"], [pk])
            TTo("dve", modT[:, l, :, jb * 4:(jb + 1) * 4],
                pb[:, 0:4 * V].rearrange("p (j v) -> p v j", v=V),
                bmT[:, l, jb * 4:(jb + 1) * 4].unsqueeze(1).to_broadcast([128, V, 4]),
                ALU.add, [pk, "bmT"], ["modT"])
            it += 1
    P.barrier()

    def mslice(l, v, i):
        return modT[:, l, v, i * 8:(i + 1) * 8]

    def rms_rstd(src, srckey, junk, d):
        ACT(junk, src, AF.Square, [srckey], ["junk", "ssq"], accum=ssq[:])
        ACT(rt[:], ssq[:], AF.Sqrt, ["ssq"], ["rt"], scale=1.0 / d, bias=EPS)
        RCP(rstd[:], rt[:], ["rt"], ["rstd"])

    def xsrc(tile):
        if tile < 16:
            return X[:, tile, :], ("X", tile)
        return XC[:, tile - 16, :], ("XC", tile - 16)

    def build_G(dst, gcol, gtmp, dkey):
        for c in range(8):
            TS("dve", gtmp[:, c, :], ones_f[:], gcol[:, c:c + 1], None, ALU.mult, None, ["ones_f", "modT"], [("gtmp", c)])
            MM(PS[6 + c // 4][:, (c % 4) * 128:(c % 4 + 1) * 128], gtmp[:, c, :], ident_f[:], True, True,
               [("gtmp", c), "ident_f"], [f"ps{6 + c // 4}"])
        CP("act", dst[:, 0:512], PS[6][:, :], ["ps6"], [dkey])
        CP("act", dst[:, 512:1024], PS[7][:, :], ["ps7"], [dkey])

    def residual_add(tile, half, pbank, pkey, G, gkey, tmp, tmpkey):
        dst, dk = xsrc(tile)
        TTo("dve", tmp[:], pbank[:, 0:512], G[:, half * 512:(half + 1) * 512], ALU.mult, [pkey, gkey], [tmpkey])
        TTo("pool", dst[:, half * 512:(half + 1) * 512], dst[:, half * 512:(half + 1) * 512], tmp[:], ALU.add,
            [tmpkey, dk], [dk])

    def compute_hT(s, l, hT, xnb, junk):
        for vi, v in enumerate((s, S)):
            STT("dve", am[:, 0, vi, :], mslice(l, v, 1), 1.0, ngT[:, l, 0, :], ALU.add, ALU.mult, ["modT", "ngT"], ["am"])
        for tile in range(18):
            vi = 0 if tile < 16 else 1
            v = s if tile < 16 else S
            src, sk = xsrc(tile)
            rms_rstd(src, sk, junk[:], D)
            TS("dve", xnb[:], src, rstd[:, 0:1], None, ALU.mult, None, [sk, "rstd"], ["xnb"])
            pb = PSB[tile % 2]
            pk = f"ps{tile % 2}"
            for c in range(8):
                TR(pb[:, c * 128:(c + 1) * 128], xnb[:, c * 128:(c + 1) * 128], ident_b[:], ["xnb", "ident_b"], [pk])
            for c in range(8):
                dst = hT[:, c, tile * 128:(tile + 1) * 128]
                if False:
                    pass
                else:
                    TS("dve", dst, pb[:, c * 128:(c + 1) * 128], am[:, 0, vi, c:c + 1], mslice(l, v, 0)[:, c:c + 1],
                       ALU.mult, ALU.add, [pk, "am", "modT"], [("hT", tile)])

    def hkeys(t0, t1):
        return [("hT", t) for t in range(t0, t1)]

    def proj_fm(dst_fn, W, wkey, wcols, hT, evac):
        for tb in range(5):
            n = 512 if tb < 4 else 256
            c0 = tb * 512
            pb = PS[tb % 2]
            pk = f"ps{tb % 2}"
            for k in range(8):
                MM(pb[:, 0:n], W[:, k, wcols], hT[:, k, c0:c0 + n], k == 0, k == 7,
                   [wkey] + hkeys(c0 // 128, (c0 + n) // 128), [pk])
            evac(tb, c0, n, pb, pk)

    def even_mixer(s, l, with_ctx):
        e = l // 2
        lam_init = 0.8 - 0.6 * math.exp(-0.3 * l)
        mem["p"] = phase_base
        hT = sb("hT", [128, 8, TT], BF16)
        xnb = sb("xnb", [128, D], BF16)
        junk = sb("junk", [128, D], BF16)
        G1 = sb("G1", [128, D])
        G1c = sb("G1c", [128, D])
        gtmp = sb("gtmp", [128, 8, 128])
        tmpy = sb("tmpy", [128, 512])
        sg_bc = sb("sg_bc", [128, 128])
        lr = sb("lr", [1, 256])
        grp_base = mem["p"]
        build_G(G1, mslice(l, s, 2), gtmp, "G1")
        build_G(G1c, mslice(l, S, 2), gtmp, "G1c")
        compute_hT(s, l, hT, xnb, junk)
        P.dma("sp", lr[:], lam_d[e], [], ["lr"])
        TTo("dve", lr[0:1, 0:64], lr[0:1, 0:64], lr[0:1, 64:128], ALU.mult, ["lr"], ["lr"])
        TTo("dve", lr[0:1, 128:192], lr[0:1, 128:192], lr[0:1, 192:256], ALU.mult, ["lr"], ["lr"])
        ACT(lr[0:1, 64:128], lr[0:1, 0:64], AF.Identity, ["lr"], ["lr", "small"], accum=small[0:1, 0:1])
        ACT(lr[0:1, 192:256], lr[0:1, 128:192], AF.Identity, ["lr"], ["lr", "small"], accum=small[0:1, 1:2])
        ACT(small[0:1, 0:2], small[0:1, 0:2], AF.Exp, ["small"], ["small"])
        TTo("dve", small[0:1, 2:3], small[0:1, 1:2], small[0:1, 0:1], ALU.subtract, ["small"], ["small"])
        TS("dve", small[0:1, 3:4], small[0:1, 2:3], -lam_init, None, ALU.add, None, ["small"], ["small"])
        MM(PS[7][:, 0:1], ones_f[0:1, :], small[0:1, 3:4], True, True, ["ones_f", "small"], ["ps7"])
        CP("dve", neglam[:], PS[7][:, 0:1], ["ps7"], ["neglam"])
        P.dma("sp", sg_bc[:], subln_d[e], [], ["sg_bc"])
        TS("dve", sg_bc[:], sg_bc[:], 1.0 - lam_init, None, ALU.mult, None, ["sg_bc"], ["sg_bc"])

        ntile_out = 18 if with_ctx else 16

        def out_proj(oT, nch, Wo, wokey):
            for tile in range(ntile_out):
                G, gk = (G1, "G1") if tile < 16 else (G1c, "G1c")
                for half in range(2):
                    pb = PS[5 + half]
                    pk = f"ps{5 + half}"
                    for ci in range(nch):
                        MM(pb[:, 0:512], oT[:, ci, tile * 128:(tile + 1) * 128], Wo[:, ci, half * 512:(half + 1) * 512],
                           ci == 0, ci == nch - 1, [("oT", tile), wokey], [pk])
                    residual_add(tile, half, pb, pk, G, gk, tmpy, "tmpy")

        for g in range(DBG['nagroups'] if DBG['na'] else 0):
            P.barrier()
            mem["p"] = grp_base
            Wq = sb("Wq", [128, 8, 256], BF16)
            Wk = sb("Wk", [128, 8, 256], BF16)
            Wv = sb("Wv", [128, 8, 256], BF16)
            Wo = sb("Wo", [128, 2, D], BF16)
            Ball = sb("Ball", [128, 4, 16, 64], BF16)
            qaT = sb("qaT", [128, 2, TT], BF16)
            kaT = sb("kaT", [128, 2, TT], BF16)
            va = sb("va", [128, 18, 4, 65], BF16)
            oT = sb("oT", [128, 2, TT], BF16)
            Eb = [sb("E0", [128, 512], BF16), sb("E1", [128, 512], BF16)]
            rc = sb("rc", [128, 4])
            otile = sb("otile", [128, 256], BF16)
            P.dma("pool", Wq[:], wview(attw_d[e, :, g * 256:(g + 1) * 256]), [], ["Wq"])
            P.dma("pool", Wk[:], wview(attw_d[e, :, 1024 + g * 256:1024 + (g + 1) * 256]), [], ["Wk"])
            P.dma("pool", Wv[:], wview(attw_d[e, :, 1536 + g * 256:1536 + (g + 1) * 256]), [], ["Wv"])
            P.dma("pool", Wo[:], attwo_d[e, g * 256:(g + 1) * 256, :].rearrange("(c p) n -> p c n", p=128), [], ["Wo"])
            P.dma("pool", Ball[:], nab_d[e, :, 4 * g:4 * g + 4, :, :], [], ["Ball"])
            ACT(Ball[:], Ball[:], AF.Copy, ["Ball"], ["Ball"], scale=8.0)
            MS("pool", va[:, :, :, 64:65], 1.0, ["va1"])
            for ci in range(2):
                def ev_q(tb, c0, n, pb, pk, ci=ci):
                    CP("act", qaT[:, ci, c0:c0 + n], pb[:, 0:n], [pk], [("qaT", ci, tb)])

                def ev_k(tb, c0, n, pb, pk, ci=ci):
                    CP("dve", kaT[:, ci, c0:c0 + n], pb[:, 0:n], [pk], [("kaT", ci, tb)])
                proj_fm(None, Wq, "Wq", slice(ci * 128, (ci + 1) * 128), hT, ev_q)
                proj_fm(None, Wk, "Wk", slice(ci * 128, (ci + 1) * 128), hT, ev_k)
            for tile in range(18):
                pb = PS[tile % 2]
                pk = f"ps{tile % 2}"
                for k in range(8):
                    MM(pb[:, 0:256], hT[:, k, tile * 128:(tile + 1) * 128], Wv[:, k, :], k == 0, k == 7,
                       ["Wv", ("hT", tile)], [pk])
                CP("act" if tile % 2 else "dve", va[:, tile, :, 0:64], pb[:, 0:256].rearrange("p (h d) -> p h d", d=64),
                   [pk], [("va", tile)])
            if g == 0 and DBG.get('dump'):
                dump(9, modT[:, l, s, :], ["modT"], 48)
                dump(10, am[:, :, :, :].rearrange("p a b c -> p (a b c)"), ["am"], 32)
                dump(11, bmT[:, l, :], ["bmT"], 48)
                dump(0, hT[:, 0, 0:512], hkeys(0, 4), 512)
                dump(1, G1[:, 0:512], ["G1"], 512)
                dump(2, qaT[:, 0, 0:512], [("qaT", 0, 0)], 512)
                dump(3, kaT[:, 0, 0:512], [("kaT", 0, 0)], 512)
                dump(4, va[:, 0, :, :].rearrange("p h d -> p (h d)"), [("va", 0), "va1"], 260)
                dump(5, Ball[:, 0, 3, :], ["Ball"], 64)
            qkeys = lambda ci: [("qaT", ci, tb) for tb in range(5)]
            kkeys = lambda ci: [("kaT", ci, tb) for tb in range(5)]
            for qr in range(DBG['nqr']):
                rs = min(max(qr - 4, 0), 24)
                t0 = rs // 2
                tiles = list(range(t0, t0 + (4 if rs % 2 == 0 else 5)))
                ob = PS[2 + qr % 2]
                ok = f"ps{2 + qr % 2}"
                for hh in range(4):
                    ci, hf = hh // 2, hh % 2
                    pr = slice(hf * 64, (hf + 1) * 64)
                    sbk = PS[hh % 2]
                    sk = f"ps{hh % 2}"
                    q_ap = qaT[pr, ci, qr * 64:(qr + 1) * 64]
                    slots = []
                    for si, tl in enumerate(tiles):
                        idx = []
                        for kr in (2 * tl, 2 * tl + 1):
                            idx.append(kr - qr + 7 if rs <= kr < rs + 8 else 15)
                        so = sbk[:, si * 64:(si + 1) * 64]
                        MM(so, kaT[pr, ci, tl * 128:(tl + 1) * 128], q_ap, True, False, qkeys(ci) + kkeys(ci), [sk])
                        MM(so, ident_lo[:, :], Ball[:, hh, idx[0], :], False, False, ["ident_b", "Ball"], [sk])
                        MM(so, ident_hi[:, :], Ball[:, hh, idx[1], :], False, True, ["ident_b", "Ball"], [sk])
                        slots.append(tl)
                    for cj in range(2):
                        si = len(tiles) + cj
                        MM(sbk[:, si * 64:(si + 1) * 64], kaT[pr, ci, T + cj * 128:T + (cj + 1) * 128], q_ap, True, True,
                           qkeys(ci) + kkeys(ci), [sk])
                        slots.append(16 + cj)
                    ns = len(slots)
                    E_ = Eb[hh % 2]
                    ek = f"E{hh % 2}"
                    ACT(E_[:, 0:ns * 64], sbk[:, 0:ns * 64], AF.Exp, [sk], [ek], scale=0.125)
                    for si, tl in enumerate(slots):
                        MM(ob[0:64, hh * 65:(hh + 1) * 65], E_[:, si * 64:(si + 1) * 64], va[:, tl, hh, :], si == 0, si == ns - 1,
                           [ek, ("va", tl), "va1"], [ok])
                ov = ob[0:64, 0:260].rearrange("p (h d) -> p h d", d=65)
                RCP(rc[0:64, :].unsqueeze(2), ov[:, :, 64:65], [ok], ["rc"])
                TTo("dve", otile[0:64, :].rearrange("p (h d) -> p h d", d=64), ov[:, :, 0:64],
                    rc[0:64, :].unsqueeze(2).to_broadcast([64, 4, 64]), ALU.mult, [ok, "rc"], ["otile"])
                for ci in range(2):
                    TR(PSB[4][:, ci * 64:(ci + 1) * 64], otile[0:64, ci * 128:(ci + 1) * 128], ident_b[0:64, 0:64],
                       ["otile", "ident_b"], ["ps4"])
                for ci in range(2):
                    CP("dve", oT[:, ci, qr * 64:(qr + 1) * 64], PSB[4][:, ci * 64:(ci + 1) * 64], ["ps4"], [("oT", qr // 2)])
            if with_ctx:
                for hh in range(4):
                    ci, hf = hh // 2, hh % 2
                    pr = slice(hf * 64, (hf + 1) * 64)
                    sbk = PS[hh % 2]
                    sk = f"ps{hh % 2}"
                    for cj in range(2):
                        MM(sbk[:, cj * 256:(cj + 1) * 256], kaT[pr, ci, T + cj * 128:T + (cj + 1) * 128], qaT[pr, ci, T:TT],
                           True, True, qkeys(ci) + kkeys(ci), [sk])
                    E_ = Eb[hh % 2]
                    ek = f"E{hh % 2}"
                    ACT(E_[:, 0:512], sbk[:, 0:512], AF.Exp, [sk], [ek], scale=0.125)
                    for qt in range(2):
                        for cj in range(2):
                            MM(PS[2 + qt][:, hh * 65:(hh + 1) * 65], E_[:, cj * 256 + qt * 128:cj * 256 + (qt + 1) * 128],
                               va[:, 16 + cj, hh, :], cj == 0, cj == 1, [ek, ("va", 16 + cj), "va1"], [f"ps{2 + qt}"])
                for qt in range(2):
                    ob = PS[2 + qt]
                    ok = f"ps{2 + qt}"
                    ov = ob[:, 0:260].rearrange("p (h d) -> p h d", d=65)
                    RCP(rc[:, :].unsqueeze(2), ov[:, :, 64:65], [ok], ["rc"])
                    TTo("dve", otile[:, :].rearrange("p (h d) -> p h d", d=64), ov[:, :, 0:64],
                        rc[:, :].unsqueeze(2).to_broadcast([128, 4, 64]), ALU.mult, [ok, "rc"], ["otile"])
                    for ci in range(2):
                        TR(PSB[4][:, ci * 128:(ci + 1) * 128], otile[:, ci * 128:(ci + 1) * 128], ident_b[:],
                           ["otile", "ident_b"], ["ps4"])
                    CP("act", oT[:, :, T + qt * 128:T + (qt + 1) * 128], PSB[4][:, 0:256].rearrange("p (c t) -> p c t", t=128),
                       ["ps4"], [("oT", 16 + qt)])
            if g == 0 and DBG.get('dump'):
                dump(6, oT[:, 0, 0:512], [("oT", t) for t in range(4)], 512)
                dump(7, Eb[0][:, 0:512], ["E0"], 512)
                dump(8, otile[:, :], ["otile"], 256)
            out_proj(oT, 2, Wo, "Wo")

        for hb in range(4 if DBG['diff'] else 0):
            P.barrier()
            mem["p"] = grp_base
            W5 = sb("W5", [128, 8, 5, 128], BF16)
            Wo = sb("Wo1", [128, 1, D], BF16)
            cosT = sb("cosT", [128, T], BF16)
            sinT = sb("sinT", [128, T], BF16)
            qbT = sb("qbT", [128, TT], BF16)
            kbT = sb("kbT", [128, TT], BF16)
            vb = sb("vb", [128, 18, 129], BF16)
            oT = sb("oTb", [128, 1, TT], BF16)
            Eb = [sb(f"E{i}", [128, 512], BF16) for i in range(4)]
            SBK = [0, 1, 6, 7]
            t1 = sb("t1", [128, 512])
            t2 = sb("t2", [128, 512])
            dd = sb("dd", [128, 128])
            obt = sb("obt", [128, 128], BF16)
            r12 = sb("r12", [128, 4])
            cols = [512 + hb * 128, 3072 + hb * 128, 2048 + hb * 128, 3584 + hb * 128, 2560 + hb * 128]
            for i, c0 in enumerate(cols):
                P.dma("pool", W5[:, :, i, :], wview(attw_d[e, :, c0:c0 + 128]), [], [("W5", i)])
            P.dma("pool", Wo[:, 0, :], attwo_d[e, 512 + hb * 128:512 + (hb + 1) * 128, :], [], ["Wo1"])
            P.dma("pool", cosT[:], cos_d, [], ["cosT"])
            P.dma("pool", sinT[:], sin_d, [], ["sinT"])
            MS("pool", vb[:, :, 128:129], 1.0, ["vb1"])
            for (dstT, dname, i_raw, i_sw) in ((qbT, "qbT", 0, 1), (kbT, "kbT", 2, 3)):
                for tb in range(5):
                    n = 512 if tb < 4 else 256
                    c0 = tb * 512
                    hk = hkeys(c0 // 128, (c0 + n) // 128)
                    for k in range(8):
                        MM(PS[0][:, 0:n], W5[:, k, i_raw, :], hT[:, k, c0:c0 + n], k == 0, k == 7, [("W5", i_raw)] + hk, ["ps0"])
                    if tb < 4:
                        for k in range(8):
                            MM(PS[1][:, 0:n], W5[:, k, i_sw, :], hT[:, k, c0:c0 + n], k == 0, k == 7, [("W5", i_sw)] + hk, ["ps1"])
                        TTo("dve", t1[:], PS[0][:, 0:n], cosT[:, c0:c0 + n], ALU.mult, ["ps0", "cosT"], ["t1"])
                        TTo("dve", t2[:], PS[1][:, 0:n], sinT[:, c0:c0 + n], ALU.mult, ["ps1", "sinT"], ["t2"])
                        TTo("pool", dstT[:, c0:c0 + n], t1[:], t2[:], ALU.add, ["t1", "t2"], [(dname, tb)])
                    else:
                        CP("act", dstT[:, c0:c0 + n], PS[0][:, 0:n], ["ps0"], [(dname, tb)])
            for tile in range(18):
                pb = PS[tile % 2]
                pk = f"ps{tile % 2}"
                for k in range(8):
                    MM(pb[:, 0:128], hT[:, k, tile * 128:(tile + 1) * 128], W5[:, k, 4, :], k == 0, k == 7,
                       [("W5", 4), ("hT", tile)], [pk])
                CP("act" if tile % 2 else "dve", vb[:, tile, 0:128], pb[:, 0:128], [pk], [("vb", tile)])
            qk_all = [("qbT", tb) for tb in range(5)] + [("kbT", tb) for tb in range(5)]

            def diff_block(qc0, nq, kts):
                nsub = nq // 128
                acc = {}
                for sub in range(nsub):
                    for m in range(2):
                        a = sub * 2 + m
                        acc[(sub, m)] = (PS[2 + a // 3][:, (a % 3) * 129:(a % 3 + 1) * 129], f"ps{2 + a // 3}")
                for bnk in sorted(set(2 + (sub * 2 + m) // 3 for sub in range(nsub) for m in range(2))):
                    MS("dve", PS[bnk][:, :], 0.0, [f"ps{bnk}"])
                cnt = 0
                for ki, kt in enumerate(kts):
                    for m in range(2):
                        pr = slice(m * 64, (m + 1) * 64)
                        sbk = PS[SBK[cnt % 4]]
                        sk = f"ps{SBK[cnt % 4]}"
                        E_ = Eb[cnt % 4]
                        ek = f"E{cnt % 4}"
                        cnt += 1
                        MM(sbk[:, 0:nq], kbT[pr, kt * 128:(kt + 1) * 128], qbT[pr, qc0:qc0 + nq], True, True, qk_all, [sk])
                        ACT(E_[:, 0:nq], sbk[:, 0:nq], AF.Exp, [sk], [ek], scale=0.125)
                        for sub in range(nsub):
                            ap, akey = acc[(sub, m)]
                            MM(ap, E_[:, sub * 128:(sub + 1) * 128], vb[:, kt, :], False, ki == len(kts) - 1,
                               [ek, ("vb", kt), "vb1"], [akey])
                for sub in range(nsub):
                    (o1, k1), (o2, k2) = acc[(sub, 0)], acc[(sub, 1)]
                    RCP(r12[:, 0:1], o1[:, 128:129], [k1], ["r12"])
                    RCP(r12[:, 1:2], o2[:, 128:129], [k2], ["r12"])
                    TTo("dve", r12[:, 2:3], r12[:, 1:2], neglam[:, 0:1], ALU.mult, ["r12", "neglam"], ["r12"])
                    TS("dve", t1[:, 0:128], o1[:, 0:128], r12[:, 0:1], None, ALU.mult, None, [k1, "r12"], ["t1"])
                    STT("dve", dd[:], o2[:, 0:128], r12[:, 2:3], t1[:, 0:128], ALU.mult, ALU.add, [k2, "r12", "t1"], ["dd"])
                    rms_rstd(dd[:], "dd", t2[:, 0:128], 128)
                    STT("dve", obt[:], dd[:], rstd[:, 0:1], sg_bc[:], ALU.mult, ALU.mult, ["dd", "rstd", "sg_bc"], ["obt"])
                    TR(PSB[5][:, 0:128], obt[:], ident_b[:], ["obt", "ident_b"], ["ps5"])
                    tcol = qc0 + sub * 128
                    CP("act", oT[:, 0, tcol:tcol + 128], PSB[5][:, 0:128], ["ps5"], [("oT", tcol // 128)])

            for qb_ in range(4):
                diff_block(qb_ * 512, 512, list(range(18)))
            if with_ctx:
                diff_block(T, 256, [16, 17])
            out_proj(oT, 1, Wo, "Wo1")
        P.barrier()

    def odd_mixer(s, l, with_ctx):
        o = l // 2
        mem["p"] = phase_base
        hT = sb("hT", [128, 8, TT], BF16)
        G1 = sb("G1", [128, D])
        G1c = sb("G1c", [128, D])
        tmpy = sb("tmpy", [128, 512])
        tmp_mark = mem["p"]
        xnb = sb("xnb", [128, D], BF16)
        junk = sb("junk", [128, D], BF16)
        gtmp = sb("gtmp", [128, 8, 128])
        build_G(G1, mslice(l, s, 2), gtmp, "G1")
        build_G(G1c, mslice(l, S, 2), gtmp, "G1c")
        compute_hT(s, l, hT, xnb, junk)
        P.barrier()
        mem["p"] = tmp_mark
        W5 = sb("W5", [128, 8, 5, 128], BF16)
        Wo = sb("Wo", [128, D], BF16)
        A = sb("A", [128, TT])
        B = sb("B", [128, TT])
        kk = sb("kk", [128, TT], BF16)
        sq = sb("sq", [128, TT], BF16)
        sgt = sb("sgt", [128, TT], BF16)
        qt_ = sb("qt", [128, TT], BF16)
        qh = sb("qh", [128, TT], BF16)
        kt_ = sb("kt", [128, TT], BF16)
        vv = sb("vv", [128, 18, 128], BF16)
        oTs = sb("oTs", [128, TT])
        tot = sb("tot", [128, 36])
        Ee = sb("Ee", [128, 36])
        Ep = sb("Ep", [128, 36])
        ATm2 = [sb("ATm0", [128, 128], BF16), sb("ATm1", [128, 128], BF16)]
        ktok2 = [sb("ktok0", [128, 128], BF16), sb("ktok1", [128, 128], BF16)]
        rmask = sb("rmask", [128, TT], BF16)
        MS("pool", rmask[:], 1.0, ["rmask"])
        MS("pool", rmask[:].rearrange("p (c k) -> p c k", k=64)[:, :, 0:1], 0.0, ["rmask"])
        R32 = [sb("R32a", [128, 128]), sb("R32b", [128, 128])]
        Rb = [sb("Rba", [128, 128], BF16), sb("Rbb", [128, 128], BF16)]
        ntile_out = 18 if with_ctx else 16
        NCH = 36
        for hd in range(8):
            P.barrier()
            cols = [hd * 128, 1024 + hd * 128, 2048 + hd * 128, 3072 + hd * 128, 4096 + hd * 128]
            for i, c0 in enumerate(cols):
                P.dma("pool", W5[:, :, i, :], wview(recw_d[o, :, c0:c0 + 128]), [], [("W5", i)])
            P.dma("pool", Wo[:], recwo_d[o, hd * 128:(hd + 1) * 128, :], [], ["Wo"])

            def ev_q(tb, c0, n, pb, pk):
                ACT(sq[:, c0:c0 + n], pb[:, 0:n], AF.Silu, [pk], ["sq"])

            def ev_g(tb, c0, n, pb, pk):
                ACT(sgt[:, c0:c0 + n], pb[:, 0:n], AF.Silu, [pk], ["sgt"])
            proj_fm(None, W5[:, :, 0, :], ("W5", 0), slice(0, 128), hT, ev_q)
            proj_fm(None, W5[:, :, 4, :], ("W5", 4), slice(0, 128), hT, ev_g)
            for tile in range(18):
                pb = PS[tile % 2]
                pk = f"ps{tile % 2}"
                for k in range(8):
                    MM(pb[:, 0:128], hT[:, k, tile * 128:(tile + 1) * 128], W5[:, k, 3, :], k == 0, k == 7,
                       [("W5", 3), ("hT", tile)], [pk])
                CP("dve", vv[:, tile, :], pb[:, 0:128], [pk], [("vv", tile)])
            vkeys = [("vv", t) for t in range(18)]
            for dr in range(2):
                lbc = lbT[:, dr, l, hd:hd + 1]
                omc = omlT[:, dr, l, hd:hd + 1]
                nomc = nomlT[:, dr, l, hd:hd + 1]

                def ev_f(tb, c0, n, pb, pk):
                    ACT(A[:, c0:c0 + n], pb[:, 0:n], AF.Sigmoid, [pk], ["A"])
                proj_fm(None, W5[:, :, 1 + dr, :], ("W5", 1 + dr), slice(0, 128), hT, ev_f)
                ACT(B[:], A[:], AF.Ln, ["A", "lbT", "omlT"], ["B"], scale=omc, bias=lbc)
                TS("dve", kk[:], A[:], nomc, omc, ALU.mult, ALU.add, ["A", "nomlT", "omlT"], ["kk"])
                P.op("dve", lambda E: E.tensor_tensor_scan(out=A[:], data0=rmask[:], data1=B[:], initial=0.0,
                                                            op0=ALU.mult, op1=ALU.add), ["B", "rmask", "kk"], ["A"])
                Av = A[:].rearrange("p (c k) -> p c k", k=64)
                Bv = B[:].rearrange("p (c k) -> p c k", k=64)
                CP("dve", tot[:].unsqueeze(2), Av[:, :, 63:64], ["A"], ["tot"])
                if dr == 1:
                    TTo("dve", A[:], B[:], A[:], ALU.subtract, ["A", "B"], ["A"])
                    TTo("dve", Av, Av, tot[:].unsqueeze(2).to_broadcast([128, NCH, 64]), ALU.add, ["A", "tot"], ["A"])
                ACT(Ee[:], tot[:], AF.Exp, ["tot"], ["Ee"])
                MS("dve", Ep[:], 0.0, ["Ep"])
                if dr == 0:
                    CP("dve", Ep[:, 1:32], Ee[:, 0:31], ["Ee"], ["Ep"])
                    CP("dve", Ep[:, 0:1], Ee[:, 35:36], ["Ee"], ["Ep"])
                    CP("dve", Ep[:, 33:36], Ee[:, 32:35], ["Ee"], ["Ep"])
                    order = [32, 33, 34, 35] + list(range(32))
                    msk = maskf
                else:
                    CP("dve", Ep[:, 0:31], Ee[:, 1:32], ["Ee"], ["Ep"])
                    CP("dve", Ep[:, 31:32], Ee[:, 32:33], ["Ee"], ["Ep"])
                    CP("dve", Ep[:, 32:35], Ee[:, 33:36], ["Ee"], ["Ep"])
                    order = [35, 34, 33, 32] + list(range(31, -1, -1))
                    msk = maskb
                ACT(qt_[:], A[:], AF.Exp, ["A"], ["qt"])
                ACT(kt_[:], A[:], AF.Exp, ["A"], ["kt"], scale=-1.0)
                TTo("pool", qt_[:], qt_[:], sq[:], ALU.mult, ["qt", "sq"], ["qt"])
                TTo("dve", kt_[:], kt_[:], kk[:], ALU.mult, ["kt", "kk"], ["kt"])
                TTo("pool", qh[:].rearrange("p (c k) -> p c k", k=64), qt_[:].rearrange("p (c k) -> p c k", k=64),
                    Ep[:].unsqueeze(2).to_broadcast([128, NCH, 64]), ALU.mult, ["qt", "Ep"], ["qh"])
                if hd == 0 and dr == 0 and DBG.get('dump'):
                    dump(0, B[:, 0:512], ["B"], 512)
                    dump(1, A[:, 0:512], ["A"], 512)
                    dump(2, kk[:, 0:512], ["kk"], 512)
                    dump(3, qt_[:, 0:512], ["qt"], 512)
                    dump(4, kt_[:, 0:512], ["kt"], 512)
                    dump(5, Ee[:, :], ["Ee"], 36)
                    dump(6, Ep[:, :], ["Ep"], 36)
                    dump(7, tot[:, :], ["tot"], 36)
                    dump(8, lbT[:].rearrange("p a b c -> p (a b c)"), ["lbT"], 64)
                    dump(9, vv[:, 0:4, :].rearrange("p a b -> p (a b)"), vkeys, 512)
                st_ = {"pp": 0, "first": True, "ucnt": 0}
                ABK = [2, 0]
                TBK = [3, 1]
                UBK = [6, 7]

                def stageA(ti):
                    tl = order[2 * ti] // 2
                    tc = slice(tl * 128, (tl + 1) * 128)
                    i2 = ti % 2
                    ab, tb_ = ABK[i2], TBK[i2]
                    MM(PS[ab][:, 0:128], kt_[:, tc], qt_[:, tc], True, True, ["kt", "qt"], [f"ps{ab}"])
                    TTo("dve", ATm2[i2][:], PS[ab][:, 0:128], msk[:], ALU.mult, [f"ps{ab}", "maskf", "maskb"], [("ATm", i2)])
                    TR(PSB[tb_][:, 0:128], kt_[:, tc], ident_b[:], ["kt", "ident_b"], [f"ps{tb_}"])
                    CP("act", ktok2[i2][:], PSB[tb_][:, 0:128], [f"ps{tb_}"], [("ktok", i2)])

                def stageB(ti):
                    ch_a, ch_b = order[2 * ti], order[2 * ti + 1]
                    tl = ch_a // 2
                    assert ch_b // 2 == tl
                    tc = slice(tl * 128, (tl + 1) * 128)
                    i2 = ti % 2
                    ob = PS[4 + ti % 2]
                    ok = f"ps{4 + ti % 2}"
                    chs = [ch_a, ch_b]
                    n_inter = sum(1 for ch in chs if not (st_["first"] and ch == chs[0]))
                    MM(ob[:, 0:128], vv[:, tl, :], ATm2[i2][:], True, n_inter == 0, [("ATm", i2)] + vkeys, [ok])
                    done = 0
                    for ch in chs:
                        hf = ch % 2
                        pp = st_["pp"]
                        if not st_["first"]:
                            done += 1
                            MM(ob[:, hf * 64:(hf + 1) * 64], Rb[pp][:], qh[:, ch * 64:(ch + 1) * 64], False, done == n_inter,
                               [("Rb", pp), "qh"], [ok])
                        ub = UBK[st_["ucnt"] % 2]
                        st_["ucnt"] += 1
                        uk = f"ps{ub}"
                        MM(PS[ub][:, 0:128], ktok2[i2][hf * 64:(hf + 1) * 64, :], vv[hf * 64:(hf + 1) * 64, tl, :], True, True,
                           [("ktok", i2)] + vkeys, [uk])
                        if st_["first"]:
                            CP("dve", Rb[pp][:], PS[ub][:, 0:128], [uk], [("Rb", pp)])
                            CP("dve", R32[pp][:], PS[ub][:, 0:128], [uk], [("R32", pp)])
                            st_["first"] = False
                        else:
                            STT("dve", Rb[1 - pp][:], R32[pp][:], Ep[:, ch:ch + 1], PS[ub][:, 0:128], ALU.mult, ALU.add,
                                [("R32", pp), "Ep", uk], [("Rb", 1 - pp)])
                            STT("dve", R32[1 - pp][:], R32[pp][:], Ep[:, ch:ch + 1], PS[ub][:, 0:128], ALU.mult, ALU.add,
                                [("R32", pp), "Ep", uk], [("R32", 1 - pp)])
                            st_["pp"] = 1 - pp
                    if dr == 0:
                        CP("act", oTs[:, tc], ob[:, 0:128], [ok], [("oTs", tl)])
                    else:
                        TTo("dve", oTs[:, tc], oTs[:, tc], ob[:, 0:128], ALU.add, [ok, ("oTs", tl)], [("oTs", tl)])

                stageA(0)
                for ti in range(18):
                    if ti + 1 < 18:
                        stageA(ti + 1)
                    stageB(ti)
            if hd == 0 and DBG.get('dump'):
                dump(10, oTs[:, 0:512], [("oTs", t) for t in range(4)], 512)
                dump(11, oTs[:, T:TT], [("oTs", t) for t in (16, 17)], 256)
                dump(12, R32[0][:], [("R32", 0)], 128)
            oTh = kk
            for tb in range(5):
                n = 512 if tb < 4 else 256
                c0 = tb * 512
                ok_ = [("oTs", t) for t in range(c0 // 128, (c0 + n) // 128)]
                ACT(A[:, c0:c0 + n], oTs[:, c0:c0 + n], AF.Square, ok_, ["A"])
                MM(PS[7][:, 0:n], ones_f[:], A[:, c0:c0 + n], True, True, ["ones_f", "A"], ["ps7"])
                ACT(B[:, c0:c0 + n], PS[7][:, 0:n], AF.Sqrt, ["ps7"], ["B"], scale=1.0 / 128, bias=EPS)
                RCP(B[:, c0:c0 + n], B[:, c0:c0 + n], ["B"], ["B"])
                TTo("dve", A[:, c0:c0 + n], oTs[:, c0:c0 + n], B[:, c0:c0 + n], ALU.mult, ok_ + ["B", "A"], ["A"])
                STT("dve", oTh[:, c0:c0 + n], A[:, c0:c0 + n], gnT[:, o:o + 1], sgt[:, c0:c0 + n], ALU.mult, ALU.mult,
                    ["A", "gnT", "sgt"], ["kk"])
            for tile in range(ntile_out):
                G, gk = (G1, "G1") if tile < 16 else (G1c, "G1c")
                for half in range(2):
                    pb = PS[half]
                    pk = f"ps{half}"
                    MM(pb[:, 0:512], oTh[:, tile * 128:(tile + 1) * 128], Wo[:, half * 512:(half + 1) * 512], True, True,
                       ["kk", "Wo"], [pk])
                    residual_add(tile, half, pb, pk, G, gk, tmpy, "tmpy")
        P.barrier()

    def moe(s, l, with_ctx):
        mem["p"] = phase_base
        ntl = 18 if with_ctx else 16
        NW = 288 if with_ctx else 256
        njt = 3 if with_ctx else 2
        hn = sb("hn", [128, 18, D], BF16)
        G2 = sb("G2", [128, D])
        G2c = sb("G2c", [128, D])
        posm_b = sb("posm_b", [16, TT], BF16)
        w_b = sb("w_b", [16, TT], BF16)
        pos_tok = sb("pos_tok", [128, 18, NE])
        wr = sb("wr", [128, 8, NE])
        selT = sb("selT", [16, 128], BF16)
        loop_base = mem["p"]
        gtmp = sb("gtmp", [128, 8, 128])
        xn32 = sb("xn32", [128, D])
        junk = sb("junk", [128, D], BF16)
        hT32 = sb("hT32", [128, 8, 128])
        afft = sb("afft", [128, NE])
        affT = sb("affT", [16, TT])
        work = sb("work", [16, TT])
        msk = sb("msk", [16, TT])
        pos = sb("pos", [16, TT])
        mx = sb("mx", [16, 8])
        build_G(G2, mslice(l, s, 5), gtmp, "G2")
        build_G(G2c, mslice(l, S, 5), gtmp, "G2c")
        P.dma("sp", wr[:], wview(router_d[LI[l]]), [], ["wr"])
        for vi, v in enumerate((s, S)):
            STT("dve", am[:, 1, vi, :], mslice(l, v, 4), 1.0, ngT[:, l, 1, :], ALU.add, ALU.mult, ["modT", "ngT"], ["am"])
        for tile in range(ntl):
            vi = 0 if tile < 16 else 1
            v = s if tile < 16 else S
            src, sk = xsrc(tile)
            rms_rstd(src, sk, junk[:], D)
            TS("dve", xn32[:], src, rstd[:, 0:1], None, ALU.mult, None, [sk, "rstd"], ["xn32"])
            CP("act" if DBG.get("nopool") else "pool", hn[:, tile, :], xn32[:], ["xn32"], [("hn", tile)])
            if DBG.get('sub', 9) < 1:
                continue
            pbk = f"ps{tile % 2}"
            for c in range(8):
                TR(PSB[tile % 2][:, c * 128:(c + 1) * 128], hn[:, tile, c * 128:(c + 1) * 128], ident_b[:],
                   [("hn", tile), "ident_b"], [pbk])
            for c in range(8):
                if DBG.get('noevac'):
                    continue
                src_p = PSB[tile % 2][:, c * 128:(c + 1) * 128]
                if c % 2 == 0:
                    TS("dve", hT32[:, c, :], src_p, am[:, 1, vi, c:c + 1], mslice(l, v, 3)[:, c:c + 1], ALU.mult, ALU.add,
                       [pbk, "am", "modT"], [("hT32", c)])
                    continue
                if c % 2 == 0:
                    ACT(hT32[:, c, :], src_p, AF.Identity, [pbk, "am", "modT"], [("hT32", c)],
                        scale=am[:, 1, vi, c:c + 1], bias=mslice(l, v, 3)[:, c:c + 1])
                else:
                    TS("dve", hT32[:, c, :], src_p, am[:, 1, vi, c:c + 1], mslice(l, v, 3)[:, c:c + 1], ALU.mult, ALU.add,
                       [pbk, "am", "modT"], [("hT32", c)])
            if DBG.get('sub', 9) < 2:
                continue
            for c in range(8):
                MM(PS[2][:, 0:NE], hT32[:, c, :], wr[:, c, :], c == 0, c == 7, [("hT32", c), "wr"], ["ps2"])
            if DBG.get('sub', 9) < 3:
                continue
            ACT(afft[:], PS[2][:, 0:NE], AF.Exp, ["ps2"], ["afft", "ssq"], accum=ssq[:])
            RCP(rt[:], ssq[:], ["ssq"], ["rt"])
            TS("dve", afft[:], afft[:], rt[:, 0:1], None, ALU.mult, None, ["afft", "rt"], ["afft"])
            if not DBG.get("notr"):
                TR(PS[3][0:16, 0:128], afft[:], ident_f[:], ["afft", "ident_f"], ["ps3"])
                CP("act", affT[0:16, tile * 128:(tile + 1) * 128], PS[3][0:16, 0:128], ["ps3"], ["affT"])

        if DBG.get('stage', 9) < 1:
            P.barrier()
            return

        def route(c0, n, cap):
            CP("dve", work[:, c0:c0 + n], affT[:, c0:c0 + n], ["affT"], ["work"])
            nit = cap // 8
            for it_ in range(nit):
                P.op("dve", (lambda a, b: (lambda E: E.max(out=a, in_=b)))(mx[:], work[:, c0:c0 + n]), ["work"], ["mx"])
                if it_ < nit - 1:
                    P.op("dve", (lambda a, b, c_: (lambda E: E.match_replace(out=a, in_to_replace=b, in_values=c_, imm_value=-1.0)))(
                        work[:, c0:c0 + n], mx[:], work[:, c0:c0 + n]), ["work", "mx"], ["work"])
            TS("dve", msk[:, c0:c0 + n], affT[:, c0:c0 + n], mx[:, 7:8], None, ALU.is_ge, None, ["affT", "mx"], ["msk"])
            MS("dve", work[:, c0:c0 + n], 1.0, ["work"])
            P.op("dve", (lambda a, b, c_: (lambda E: E.tensor_tensor_scan(out=a, data0=b, data1=c_, initial=0.0, op0=ALU.mult, op1=ALU.add)))(
                pos[:, c0:c0 + n], work[:, c0:c0 + n], msk[:, c0:c0 + n]), ["work", "msk"], ["pos"])
            TTo("dve", pos[:, c0:c0 + n], pos[:, c0:c0 + n], msk[:, c0:c0 + n], ALU.mult, ["pos", "msk"], ["pos"])
            TS("dve", pos[:, c0:c0 + n], pos[:, c0:c0 + n], -1.0, None, ALU.add, None, ["pos"], ["pos"])
            CP("dve", posm_b[:, c0:c0 + n], pos[:, c0:c0 + n], ["pos"], ["posm_b"])
            TTo("dve", w_b[:, c0:c0 + n], affT[:, c0:c0 + n], msk[:, c0:c0 + n], ALU.mult, ["affT", "msk"], ["w_b"])

        route(0, T, 256)
        if with_ctx:
            route(T, L, 32)
        if DBG.get('stage', 9) < 2:
            P.barrier()
            return
        for tile in range(ntl):
            TR(PS[3][:, 0:NE], pos[0:16, tile * 128:(tile + 1) * 128], ident_f[0:16, 0:16], ["pos", "ident_f"], ["ps3"])
            CP("act", pos_tok[:, tile, :], PS[3][:, 0:NE], ["ps3"], ["pos_tok"])
        P.barrier()
        mem["p"] = loop_base
        Pe = sb("Pe", [128, 16, 256], BF16)
        Pce = sb("Pce", [128, 2, 32], BF16)
        PT = sb("PT", [128, 2, T], BF16)
        PTc = sb("PTc", [32, L], BF16)
        xsel = sb("xsel", [128, 8, 288], BF16)
        hid = [sb("hid0", [128, 288], BF16), sb("hid1", [128, 288], BF16)]
        sgl = sb("sgl", [128, 288])
        yy = sb("yy", [128, 3, D], BF16)
        wsb = sb("wsb", [128, 512])
        Wg = [sb("Wg0", [128, 8, 256], BF16), sb("Wg1", [128, 8, 256], BF16)]
        Wu = [sb("Wu0", [128, 8, 256], BF16), sb("Wu1", [128, 8, 256], BF16)]
        Wd = [sb("Wd0", [128, 2, D], BF16), sb("Wd1", [128, 2, D], BF16)]
        hnk = [("hn", t) for t in range(ntl)]
        wcnt = 0
        for e in range(DBG['experts']):
            TS("dve", selT[:], ones_f[0:16, :], ident_f[0:16, e:e + 1], None, ALU.mult, None, ["ones_f", "ident_f"], ["selT"])
            TTo("dve", Pe[:], iota_j[:].unsqueeze(1).to_broadcast([128, 16, 256]),
                pos_tok[:, 0:16, e:e + 1].to_broadcast([128, 16, 256]), ALU.is_equal, ["iota_j", "pos_tok"], ["Pe"])
            if with_ctx:
                TTo("dve", Pce[:], iota_j[:, 0:32].unsqueeze(1).to_broadcast([128, 2, 32]),
                    pos_tok[:, 16:18, e:e + 1].to_broadcast([128, 2, 32]), ALU.is_equal, ["iota_j", "pos_tok"], ["Pce"])
            for blk in range(4):
                bc = slice(blk * 512, (blk + 1) * 512)
                MM(PS[6][:, 0:512], selT[0:16, :], posm_b[0:16, bc], True, True, ["selT", "posm_b"], ["ps6"])
                MM(PS[7][:, 0:512], selT[0:16, :], w_b[0:16, bc], True, True, ["selT", "w_b"], ["ps7"])
                CP("act", wsb[:], PS[7][:, 0:512], ["ps7"], ["wsb"])
                for jt in range(2):
                    STT("dve", PT[:, jt, bc], PS[6][:, 0:512], iota_p[:, jt:jt + 1], wsb[:], ALU.is_equal, ALU.mult,
                        ["ps6", "iota_p", "wsb"], ["PT"])
            if with_ctx:
                MM(PS[6][0:32, 0:L], selT[0:16, 0:32], posm_b[0:16, T:TT], True, True, ["selT", "posm_b"], ["ps6"])
                MM(PS[7][0:32, 0:L], selT[0:16, 0:32], w_b[0:16, T:TT], True, True, ["selT", "w_b"], ["ps7"])
                CP("act", wsb[0:32, 0:L], PS[7][0:32, 0:L], ["ps7"], ["wsb"])
                STT("dve", PTc[:], PS[6][0:32, 0:L], iota_p[0:32, 0:1], wsb[0:32, 0:L], ALU.is_equal, ALU.mult,
                    ["ps6", "iota_p", "wsb"], ["PTc"])
            for c in range(8):
                pb = PS[6 + c % 2]
                pk = f"ps{6 + c % 2}"
                for tile in range(16):
                    MM(pb[:, 0:256], hn[:, tile, c * 128:(c + 1) * 128], Pe[:, tile, :], tile == 0, tile == 15, ["Pe"] + hnk, [pk])
                if with_ctx:
                    for ct in range(2):
                        MM(pb[:, 256:288], hn[:, 16 + ct, c * 128:(c + 1) * 128], Pce[:, ct, :], ct == 0, ct == 1, ["Pce"] + hnk, [pk])
                TS("dve", xsel[:, c, 0:256], pb[:, 0:256], am[:, 1, 0, c:c + 1], mslice(l, s, 3)[:, c:c + 1], ALU.mult, ALU.add,
                   [pk, "am", "modT"], [("xsel", c)])
                if with_ctx:
                    TS("dve", xsel[:, c, 256:288], pb[:, 256:288], am[:, 1, 1, c:c + 1], mslice(l, S, 3)[:, c:c + 1], ALU.mult, ALU.add,
                       [pk, "am", "modT"], [("xsel", c)])
            xk = [("xsel", c) for c in range(8)]
            pend = None

            def down(fc, wi, f2):
                for jt in range(njt):
                    rows = 128 if jt < 2 else 32
                    for half in range(2):
                        b = jt * 2 + half
                        MM(PS[b][0:rows, 0:512], hid[fc % 2][:, jt * 128:jt * 128 + rows], Wd[wi][:, f2, half * 512:(half + 1) * 512],
                           fc == 0, fc == 15, [f"hid{fc % 2}", f"Wd{wi}"], [f"ps{b}"])

            for fb in range(8):
                wi = wcnt % 2
                wcnt += 1
                P.dma("pool", Wg[wi][:], wview(wg_d[LI[l], e, :, fb * 256:(fb + 1) * 256]), [], [f"Wg{wi}"])
                P.dma("pool", Wu[wi][:], wview(wu_d[LI[l], e, :, fb * 256:(fb + 1) * 256]), [], [f"Wu{wi}"])
                P.dma("pool", Wd[wi][:], wd_d[LI[l], e, fb * 256:(fb + 1) * 256, :].rearrange("(c p) n -> p c n", p=128), [], [f"Wd{wi}"])
                for f2 in range(2):
                    fc = fb * 2 + f2
                    for c in range(8):
                        MM(PS[6][:, 0:NW], Wg[wi][:, c, f2 * 128:(f2 + 1) * 128], xsel[:, c, 0:NW], c == 0, c == 7, [f"Wg{wi}"] + xk, ["ps6"])
                    for c in range(8):
                        MM(PS[7][:, 0:NW], Wu[wi][:, c, f2 * 128:(f2 + 1) * 128], xsel[:, c, 0:NW], c == 0, c == 7, [f"Wu{wi}"] + xk, ["ps7"])
                    ACT(sgl[:, 0:NW], PS[6][:, 0:NW], AF.Silu, ["ps6"], ["sgl"])
                    TTo("dve", hid[fc % 2][:, 0:NW], sgl[:, 0:NW], PS[7][:, 0:NW], ALU.mult, ["sgl", "ps7"], [f"hid{fc % 2}"])
                    if pend is not None:
                        down(*pend)
                    pend = (fc, wi, f2)
            down(*pend)
            for jt in range(njt):
                rows = 128 if jt < 2 else 32
                G = G2 if jt < 2 else G2c
                gk = "G2" if jt < 2 else "G2c"
                for half in range(2):
                    b = jt * 2 + half
                    TTo("dve", yy[0:rows, jt, half * 512:(half + 1) * 512], PS[b][0:rows, 0:512], G[0:rows, half * 512:(half + 1) * 512],
                        ALU.mult, [f"ps{b}", gk], [("yy", jt)])
            for tile in range(16):
                for half in range(2):
                    b = (tile % 3) * 2 + half
                    for jt in range(2):
                        MM(PS[b][:, 0:512], PT[:, jt, tile * 128:(tile + 1) * 128], yy[:, jt, half * 512:(half + 1) * 512], jt == 0, jt == 1,
                           ["PT", ("yy", jt)], [f"ps{b}"])
                    TTo("dve", X[:, tile, half * 512:(half + 1) * 512], X[:, tile, half * 512:(half + 1) * 512], PS[b][:, 0:512], ALU.add,
                        [f"ps{b}", ("X", tile)], [("X", tile)])
            if with_ctx:
                for ct in range(2):
                    for half in range(2):
                        b = ct * 2 + half
                        MM(PS[b][:, 0:512], PTc[0:32, ct * 128:(ct + 1) * 128], yy[0:32, 2, half * 512:(half + 1) * 512], True, True,
                           ["PTc", ("yy", 2)], [f"ps{b}"])
                        TTo("dve", XC[:, ct, half * 512:(half + 1) * 512], XC[:, ct, half * 512:(half + 1) * 512], PS[b][:, 0:512], ALU.add,
                            [f"ps{b}", ("XC", ct)], [("XC", ct)])
        P.barrier()

    for s in range(S):
        for tile in range(16):
            P.dma("sp", X[:, tile, :], x_d[s, tile * 128:(tile + 1) * 128, :], [], [("X", tile)])
        for ct in range(2):
            P.dma("sp", XC[:, ct, :], ctx_d[s, ct * 128:(ct + 1) * 128, :], [], [("XC", ct)])
        for l in layers:
            last = (l == 3)
            if DBG['mix']:
                if l % 2 == 0:
                    even_mixer(s, l, not last)
                else:
                    odd_mixer(s, l, not last)
            if DBG['moe']:
                moe(s, l, not last)
        P.barrier()
        mem["p"] = phase_base
        fg = sb("fg", [128, D])
        junk = sb("junk", [128, D], BF16)
        ot = [sb("ot0", [128, D]), sb("ot1", [128, D])]
        if final:
            P.dma("sp", fg[:], fg_d, [], ["fg"])
        for tile in range(16):
            src, sk = xsrc(tile)
            o_ = ot[tile % 2]
            okey = f"ot{tile % 2}"
            if final:
                rms_rstd(src, sk, junk[:], D)
                STT("dve", o_[:], src, rstd[:, 0:1], fg[:], ALU.mult, ALU.mult, [sk, "rstd", "fg"], [okey])
            else:
                CP("dve", o_[:], src, [sk], [okey])
            P.dma("sp", out_d[s, tile * 128:(tile + 1) * 128, :], o_[:], [okey], [])
        if debug_ctx:
            for ct in range(2):
                P.dma("sp", outc_d[s, ct * 128:(ct + 1) * 128, :], XC[:, ct, :], [("XC", ct)], [])
        P.barrier()
    P.wait_dmas("sp")
    P.run()
    return nc, P


def _consts():
    c = {}
    c["c_ident"] = np.eye(128, dtype=np.float32)
    lo = np.eye(128, dtype=np.float32); lo[64:, :] = 0
    hi = np.eye(128, dtype=np.float32); hi[:64, :] = 0
    c["c_ident_lo"] = lo
    c["c_ident_hi"] = hi
    c["c_iota_j"] = np.broadcast_to(np.arange(256, dtype=np.float32)[None, :], (128, 256)).copy()
    p = np.arange(128, dtype=np.float32)
    c["c_iota_p"] = np.stack([p, p + 128, p, p], axis=1).copy()
    s_ = np.arange(128)[:, None]
    t_ = np.arange(128)[None, :]
    same = (s_ // 64) == (t_ // 64)
    c["c_maskf"] = (same & (s_ <= t_)).astype(np.float32)
    c["c_maskb"] = (same & (s_ >= t_)).astype(np.float32)
    t = np.arange(T)
    rows = (t // 64).astype(np.float32)
    colsf = (t % 64).astype(np.float32)
    inv = (10000.0 ** (-np.arange(16, dtype=np.float32) / 16)).astype(np.float32)
    ang = np.concatenate([rows[:, None] * inv, colsf[:, None] * inv], axis=-1).astype(np.float32)
    cos = np.cos(ang).astype(np.float32).T
    sin = np.sin(ang).astype(np.float32).T
    cos64 = np.concatenate([cos, cos], axis=0)
    sin64 = np.concatenate([-sin, sin], axis=0)
    c["rope_cos"] = np.concatenate([cos64, cos64], axis=0).astype(np.float32)
    c["rope_sin"] = np.concatenate([sin64, sin64], axis=0).astype(np.float32)
    return c


def _swap_perm(base):
    idx = []
    for m in range(8):
        o = base + m * 64
        idx += list(range(o + 32, o + 64)) + list(range(o, o + 32))
    return np.array(idx)


def _na_bias_table(rpb):
    kc = np.arange(64)[:, None]
    qc = np.arange(64)[None, :]
    cstart = np.clip(qc - 8, 0, 48)
    valid = (kc >= cstart) & (kc < cstart + 16)
    dcol = np.clip(kc - qc + 15, 0, 30)
    tab = np.full((64, 8, 16, 64), -3750.0, dtype=np.float32)
    g = rpb[:, :, dcol]
    g = np.transpose(g, (2, 0, 1, 3))
    vm = np.broadcast_to(valid[:, None, None, :], g.shape)
    tab[:, :, 0:15, :] = np.where(vm, g, np.float32(-3750.0))
    return np.concatenate([tab, tab], axis=0)


def prep_shared(inp, layers=(0, 1, 2, 3)):
    layers = list(layers) if len(layers) else [0]
    f = lambda a: np.ascontiguousarray(np.asarray(a, dtype=np.float32))
    d = dict(_consts())
    d["w_mod"] = f(np.asarray(inp["w_mod"])[layers])
    d["b_modT"] = f(np.transpose(np.asarray(inp["b_mod"]).reshape(4, 48, 128), (2, 0, 1)))
    d["norm_gT"] = f(np.transpose(np.asarray(inp["norm_g"]).reshape(4, 2, 8, 128), (3, 0, 1, 2)))
    w_in = np.asarray(inp["att_w_in"])
    d["att_w"] = f(np.concatenate([w_in, w_in[:, :, _swap_perm(512)], w_in[:, :, _swap_perm(2048)]], axis=2))
    d["att_wo"] = f(inp["att_w_out"])
    d["na_bias"] = f(np.stack([_na_bias_table(np.asarray(inp["na_rpb"])[e]) for e in range(2)], axis=0))
    d["lam_row"] = f(np.asarray(inp["diff_lambda"]).reshape(2, 1, 256))
    d["subln_bc"] = f(np.broadcast_to(np.asarray(inp["diff_subln_g"])[:, None, :], (2, 128, 128)))
    d["rec_w_in"] = f(inp["rec_w_in"])
    d["rec_wo"] = f(inp["rec_w_out"])
    d["rec_lbT"] = f(np.transpose(np.asarray(inp["rec_lb_logits"]).reshape(2, 4, 8, 128), (3, 0, 1, 2)))
    d["rec_gn"] = f(np.asarray(inp["rec_gnorm_g"]).T)
    d["router"] = f(np.asarray(inp["moe_router"])[layers])
    nex = max(1, DBG["experts"])
    d["wg"] = f(np.asarray(inp["moe_w_gate"])[layers][:, :nex])
    d["wu"] = f(np.asarray(inp["moe_w_up"])[layers][:, :nex])
    d["wd"] = f(np.asarray(inp["moe_w_down"])[layers][:, :nex])
    d["finalg_bc"] = f(np.broadcast_to(np.asarray(inp["final_g"])[None, :], (128, D)))
    return d


def prep_core(inp, samples):
    f = lambda a: np.ascontiguousarray(np.asarray(a, dtype=np.float32))
    x = np.asarray(inp["x"])
    ctx = np.asarray(inp["ctx"])
    c = np.asarray(inp["c"])
    cv = np.concatenate([c[samples], np.asarray(inp["c_ctx"])[None, :]], axis=0)
    V = cv.shape[0]
    return {
        "x": f(x[samples]),
        "ctx": f(ctx[samples]),
        "cvT": f(np.transpose(cv.reshape(V, 8, 128), (2, 1, 0))),
    }


_CACHE = {}


def kernel(**inputs):
    n_cores = 8
    S = 2
    if "nc" not in _CACHE:
        _CACHE["nc"] = build(S, [0, 1, 2, 3], final=True)[0]
    nc = _CACHE["nc"]
    shared = prep_shared(inputs)
    in_maps = []
    for i in range(n_cores):
        m = dict(shared)
        m.update(prep_core(inputs, list(range(i * S, (i + 1) * S))))
        in_maps.append(m)
    res = run_bass_kernel_spmd(nc, in_maps, core_ids=list(range(n_cores)))
    return np.concatenate([r["out"] for r in res.results], axis=0).astype(np.float32)
```

```python
import math
import numpy as np
import concourse.bass as bass
import concourse.mybir as mybir
from concourse.bass_utils import run_bass_kernel_spmd

F32 = mybir.dt.float32
BF16 = mybir.dt.bfloat16
AF = mybir.ActivationFunctionType
ALU = mybir.AluOpType

EPOCH = 28800


class Stream:
    def __init__(self, prog, name):
        self.prog = prog
        self.name = name
        self.sems = []
        self.n = 0

    def sem_for(self, e):
        while len(self.sems) <= e:
            self.sems.append(self.prog.nc.alloc_semaphore(f"{self.name}_{len(self.sems)}"))
        return self.sems[e]

    def bump(self, inc):
        e = self.n // EPOCH
        assert (self.n + inc - 1) // EPOCH == e
        self.n += inc
        return self.sem_for(e), (self, self.n)

    def loc(self, n):
        e = (n - 1) // EPOCH
        return self.sem_for(e), n - e * EPOCH


class Prog:
    ENGS = ("pe", "act", "dve", "pool", "sp")

    def __init__(self, nc, n_dma_sems=4):
        self.nc = nc
        self.q = {e: [] for e in self.ENGS}
        self.stream = {e: Stream(self, "s_" + e) for e in self.ENGS}
        self.seen = {e: {} for e in self.ENGS}
        self.last_w = {}
        self.readers = {}
        self.dma_streams = {}
        self.dma_rr = {}
        self.n_dma_sems = n_dma_sems
        self.ninst = 0

    def _deps(self, reads, writes):
        deps = []
        for k in reads:
            t = self.last_w.get(k)
            if t is not None:
                deps.append(t)
        for k in writes:
            t = self.last_w.get(k)
            if t is not None:
                deps.append(t)
            deps.extend(self.readers.get(k, ()))
        return deps

    def _commit(self, tok, reads, writes):
        for k in writes:
            self.last_w[k] = tok
            self.readers[k] = []
        for k in reads:
            if k in writes:
                continue
            self.readers.setdefault(k, []).append(tok)

    def _waits(self, eng, deps, skip_self=False):
        seen = self.seen[eng]
        best = {}
        for (st, n) in deps:
            if skip_self and st is self.stream[eng]:
                continue
            if seen.get(st, 0) >= n:
                continue
            if best.get(st, 0) < n:
                best[st] = n
        waits = []
        for st, n in best.items():
            seen[st] = n
            waits.append(st.loc(n))
        return waits

    def op(self, eng, fn, reads=(), writes=()):
        reads = tuple(reads)
        writes = tuple(writes) + tuple(k for k in reads if isinstance(k, str) and k.startswith("ps") and k[2:].isdigit())
        deps = self._deps(reads, writes)
        waits = self._waits(eng, deps, skip_self=(eng == "pe"))
        sem, tok = self.stream[eng].bump(1)

        def emit(E, waits=waits, fn=fn, sem=sem):
            for (s, v) in waits:
                E.wait_ge(s, v)
            fn(E).then_inc(sem, 1)

        self.q[eng].append(emit)
        self._commit(tok, reads, writes)
        self.ninst += 1
        return tok

    def dma(self, queue, out, in_, reads=(), writes=()):
        reads = tuple(reads)
        writes = tuple(writes)
        deps = self._deps(reads, writes)
        if queue not in self.dma_streams:
            self.dma_streams[queue] = [Stream(self, f"d_{queue}{i}") for i in range(self.n_dma_sems)]
            self.dma_rr[queue] = 0
        i = self.dma_rr[queue]
        self.dma_rr[queue] = (i + 1) % self.n_dma_sems
        st = self.dma_streams[queue][i]
        if st.n > 0:
            deps.append((st, st.n))
        waits = self._waits(queue, deps)
        sem, tok = st.bump(16)

        def emit(E, waits=waits, sem=sem, out=out, in_=in_):
            for (s, v) in waits:
                E.wait_ge(s, v)
            E.dma_start(out=out, in_=in_).then_inc(sem, 16)

        self.q[queue].append(emit)
        self._commit(tok, reads, writes)
        self.ninst += 1
        return tok

    def all_tokens(self):
        toks = []
        for e in self.ENGS:
            if self.stream[e].n:
                toks.append((self.stream[e], self.stream[e].n))
        for q, sts in self.dma_streams.items():
            for st in sts:
                if st.n:
                    toks.append((st, st.n))
        return toks

    def barrier(self):
        toks = self.all_tokens()
        for eng in self.ENGS:
            waits = self._waits(eng, toks)

            def emit(E, waits=waits):
                for (s, v) in waits:
                    E.wait_ge(s, v)

            self.q[eng].append(emit)

    def wait_dmas(self, eng):
        toks = [(st, st.n) for q, sts in self.dma_streams.items() for st in sts if st.n]
        waits = self._waits(eng, toks)

        def emit(E, waits=waits):
            for (s, v) in waits:
                E.wait_ge(s, v)

        self.q[eng].append(emit)

    def run(self):
        nc = self.nc
        with nc.Block() as block:
            @block.tensor
            def _(E):
                for f in self.q["pe"]:
                    f(E)

            @block.scalar
            def _(E):
                for f in self.q["act"]:
                    f(E)

            @block.vector
            def _(E):
                for f in self.q["dve"]:
                    f(E)

            @block.gpsimd
            def _(E):
                for f in self.q["pool"]:
                    f(E)

            @block.sync
            def _(E):
                for f in self.q["sp"]:
                    f(E)


DBG = {'na': 1, 'diff': 1, 'mix': 1, 'moe': 1, 'experts': 16, 'nagroups': 2, 'nqr': 32}
D = 1024
T = 2048
L = 256
TT = T + L
EPS = 1e-6
NE = 16
FF = 2048


def build(S, layers, final=True, debug_ctx=False):
    nc = bass.Bass("TRN2", target_bir_lowering=False)
    P = Prog(nc)
    V = S + 1

    def din(name, shape):
        return nc.dram_tensor(name, list(shape), F32, kind="ExternalInput").ap()

    x_d = din("x", [S, T, D])
    ctx_d = din("ctx", [S, L, D])
    cvT_d = din("cvT", [128, 8, V])
    NL = max(1, len(layers))
    LI = {l: i for i, l in enumerate(layers)}
    wmod_d = din("w_mod", [NL, D, 6 * D])
    bmT_d = din("b_modT", [128, 4, 48])
    ngT_d = din("norm_gT", [128, 4, 2, 8])
    attw_d = din("att_w", [2, D, 4096])
    attwo_d = din("att_wo", [2, D, D])
    nab_d = din("na_bias", [2, 128, 8, 16, 64])
    lam_d = din("lam_row", [2, 1, 256])
    subln_d = din("subln_bc", [2, 128, 128])
    cos_d = din("rope_cos", [128, T])
    sin_d = din("rope_sin", [128, T])
    recw_d = din("rec_w_in", [2, D, 5 * D])
    recwo_d = din("rec_wo", [2, D, D])
    lbT_d = din("rec_lbT", [128, 2, 4, 8])
    gn_d = din("rec_gn", [128, 2])
    router_d = din("router", [NL, D, NE])
    NEX = max(1, DBG["experts"])
    wg_d = din("wg", [NL, NEX, D, FF])
    wu_d = din("wu", [NL, NEX, D, FF])
    wd_d = din("wd", [NL, NEX, FF, D])
    fg_d = din("finalg_bc", [128, D])
    cid_d = din("c_ident", [128, 128])
    cij_d = din("c_iota_j", [128, 256])
    cip_d = din("c_iota_p", [128, 4])
    cmf_d = din("c_maskf", [128, 128])
    cmb_d = din("c_maskb", [128, 128])
    cil_d = din("c_ident_lo", [128, 128])
    cih_d = din("c_ident_hi", [128, 128])
    out_d = nc.dram_tensor("out", [S, T, D], F32, kind="ExternalOutput").ap()
    outc_d = nc.dram_tensor("out_ctx", [S, L, D], F32, kind="ExternalOutput").ap() if debug_ctx else None
    dbg_d = nc.dram_tensor("dbg", [16, 128, 512], F32, kind="ExternalOutput").ap() if debug_ctx else None

    base = (nc.sbuf_base + 63) // 64 * 64
    lim = nc.sbuf_top
    mem = {"p": base, "n": 0}

    def sb(name, shape, dt=F32):
        nb = int(np.prod(shape[1:])) * (2 if dt == BF16 else 4)
        nb = (nb + 63) // 64 * 64
        off = mem["p"]
        assert off + nb <= lim, f"SBUF overflow at {name}: {off + nb} > {lim}"
        mem["p"] = off + nb
        mem["n"] += 1
        return nc.alloc_sbuf_tensor_at(f"{name}_{mem['n']}", list(shape), dt, offset=off)

    PS = [nc.alloc_psum_tensor(f"ps{i}", [128, 512], F32) for i in range(8)]
    PSB = [p[:].bitcast(BF16) for p in PS]

    def MM(out, lhsT, rhs, st, sp, r, w):
        P.op("pe", lambda E: E.matmul(out, lhsT=lhsT, rhs=rhs, start=st, stop=sp), r, w)

    def TR(out, in_, idn, r, w):
        P.op("pe", lambda E: E.transpose(out=out, in_=in_, identity=idn), r, w)

    def ACT(out, in_, func, r, w, scale=None, bias=None, accum=None):
        kw = {}
        if scale is not None:
            kw["scale"] = scale
        if bias is not None:
            kw["bias"] = bias
        if accum is not None:
            kw["accum_out"] = accum
        P.op("act", lambda E: E.activation(out=out, in_=in_, func=func, **kw), r, w)

    def TS(eng, out, in0, s1, s2, op0, op1, r, w):
        if s2 is None:
            P.op(eng, lambda E: E.tensor_scalar(out=out, in0=in0, scalar1=s1, scalar2=None, op0=op0), r, w)
        else:
            P.op(eng, lambda E: E.tensor_scalar(out=out, in0=in0, scalar1=s1, scalar2=s2, op0=op0, op1=op1), r, w)

    def TTo(eng, out, in0, in1, op, r, w):
        P.op(eng, lambda E: E.tensor_tensor(out=out, in0=in0, in1=in1, op=op), r, w)

    def STT(eng, out, in0, sc, in1, op0, op1, r, w):
        P.op(eng, lambda E: E.scalar_tensor_tensor(out=out, in0=in0, scalar=sc, in1=in1, op0=op0, op1=op1), r, w)

    def CP(eng, out, in_, r, w):
        if eng == "act":
            P.op("act", lambda E: E.copy(out=out, in_=in_), r, w)
        else:
            P.op(eng, lambda E: E.tensor_copy(out=out, in_=in_), r, w)

    def MS(eng, ap, v, w):
        P.op(eng, lambda E: E.memset(ap, v), [], w)

    dbgbuf = {}

    def dump(i, ap, keys, n):
        if dbg_d is None:
            return
        if "t" not in dbgbuf:
            dbgbuf["t"] = nc.alloc_sbuf_tensor_at("dbgt", [128, 512], F32, offset=(lim - 4096) // 64 * 64)
        t = dbgbuf["t"]
        rows = ap.shape[0]
        P.op("dve", lambda E: E.memset(t[:], 0.0), [], ["dbgt"])
        P.op("dve", lambda E: E.tensor_copy(out=t[0:rows, 0:n], in_=ap), keys, ["dbgt"])
        P.dma("sp", dbg_d[i], t[:], ["dbgt"], [])

    def RCP(out, in_, r, w):
        P.op("dve", lambda E: E.reciprocal(out=out, in_=in_), r, w)

    def wview(src2d):
        return src2d.rearrange("(k p) n -> p k n", p=128)

    X = sb("X", [128, 16, D])
    XC = sb("XC", [128, 2, D])
    ident_f = sb("ident_f", [128, 128])
    ident_b = sb("ident_b", [128, 128], BF16)
    ones_f = sb("ones_f", [128, 128])
    ident_lo = sb("ident_lo", [128, 128], BF16)
    ident_hi = sb("ident_hi", [128, 128], BF16)
    iota_j = sb("iota_j", [128, 256])
    iota_p = sb("iota_p", [128, 4])
    maskf = sb("maskf", [128, 128])
    maskb = sb("maskb", [128, 128])
    modT = sb("modT", [128, 4, V, 48])
    bmT = sb("bmT", [128, 4, 48])
    ngT = sb("ngT", [128, 4, 2, 8])
    scT = sb("scT", [128, 8, V])
    lbT = sb("lbT", [128, 2, 4, 8])
    omlT = sb("omlT", [128, 2, 4, 8])
    nomlT = sb("nomlT", [128, 2, 4, 8])
    gnT = sb("gnT", [128, 2])
    am = sb("am", [128, 2, 2, 8])
    ssq = sb("ssq", [128, 1])
    rt = sb("rt", [128, 1])
    rstd = sb("rstd", [128, 1])
    neglam = sb("neglam", [128, 1])
    small = sb("small", [128, 16])
    lsum = sb("lsum", [128, 2, 8])
    phase_base = mem["p"]

    P.dma("sp", ident_f[:], cid_d, [], ["ident_f"])
    P.dma("pool", ident_b[:], cid_d, [], ["ident_b"])
    P.dma("pool", ident_lo[:], cil_d, [], ["ident_b"])
    P.dma("pool", ident_hi[:], cih_d, [], ["ident_b"])
    P.dma("sp", iota_j[:], cij_d, [], ["iota_j"])
    P.dma("sp", iota_p[:], cip_d, [], ["iota_p"])
    P.dma("sp", maskf[:], cmf_d, [], ["maskf"])
    P.dma("sp", maskb[:], cmb_d, [], ["maskb"])
    P.dma("sp", bmT[:], bmT_d, [], ["bmT"])
    P.dma("sp", ngT[:], ngT_d, [], ["ngT"])
    P.dma("sp", scT[:], cvT_d, [], ["scT"])
    P.dma("sp", lbT[:], lbT_d, [], ["lbT"])
    P.dma("sp", gnT[:], gn_d, [], ["gnT"])
    MS("pool", ones_f[:], 1.0, ["ones_f"])
    ACT(scT[:], scT[:], AF.Silu, ["scT"], ["scT"])

    ACT(lbT[:], lbT[:], AF.Exp, ["lbT"], ["lbT"])
    TTo("dve", lsum[:], lbT[:, :, 0, :], lbT[:, :, 1, :], ALU.add, ["lbT"], ["lsum"])
    TTo("dve", lsum[:], lsum[:], lbT[:, :, 2, :], ALU.add, ["lbT", "lsum"], ["lsum"])
    TTo("dve", lsum[:], lsum[:], lbT[:, :, 3, :], ALU.add, ["lbT", "lsum"], ["lsum"])
    RCP(lsum[:], lsum[:], ["lsum"], ["lsum"])
    for j in range(4):
        TTo("dve", lbT[:, :, j, :], lbT[:, :, j, :], lsum[:], ALU.mult, ["lbT", "lsum"], ["lbT"])
    TTo("dve", lbT[:, :, 2, :], lbT[:, :, 2, :], lbT[:, :, 1, :], ALU.add, ["lbT"], ["lbT"])
    TTo("dve", lbT[:, :, 3, :], lbT[:, :, 3, :], lbT[:, :, 2, :], ALU.add, ["lbT"], ["lbT"])
    MS("dve", lbT[:, :, 0, :], 0.0, ["lbT"])
    TS("dve", omlT[:], lbT[:], -1.0, 1.0, ALU.mult, ALU.add, ["lbT"], ["omlT"])
    TS("dve", nomlT[:], omlT[:], -1.0, None, ALU.mult, None, ["omlT"], ["nomlT"])

    mem["p"] = phase_base
    wm = [sb("wm0", [128, 8, 512]), sb("wm1", [128, 8, 512])]
    it = 0
    for l in layers:
        for jb in list(range(12)) + [0]:
            w_ = wm[it % 2]
            wk = f"wm{it % 2}"
            P.dma("sp", w_[:], wview(wmod_d[LI[l], :, jb * 512:(jb + 1) * 512]), [], [wk])
            pb = PS[it % 2]
            pk = f"ps{it % 2}"
            for j in range(4):
                for k in range(8):
                    MM(pb[:, j * V:(j + 1) * V], w_[:, k, j * 128:(j + 1) * 128], scT[:, k, :], k == 0, k == 7,
                       [wk, "scT"], [pk])
            TTo("dve", modT[:, l, :, jb * 4:(jb + 1) * 4],
                pb[:, 0:4 * V].rearrange("p (j v) -> p v j", v=V),
                bmT[:, l, jb * 4:(jb + 1) * 4].unsqueeze(1).to_broadcast([128, V, 4]),
                ALU.add, [pk, "bmT"], ["modT"])
            it += 1
    P.barrier()

    def mslice(l, v, i):
        return modT[:, l, v, i * 8:(i + 1) * 8]

    def rms_rstd(src, srckey, junk, d):
        ACT(junk, src, AF.Square, [srckey], ["junk", "ssq"], accum=ssq[:])
        ACT(rt[:], ssq[:], AF.Sqrt, ["ssq"], ["rt"], scale=1.0 / d, bias=EPS)
        RCP(rstd[:], rt[:], ["rt"], ["rstd"])

    def xsrc(tile):
        if tile < 16:
            return X[:, tile, :], ("X", tile)
        return XC[:, tile - 16, :], ("XC", tile - 16)

    def build_G(dst, gcol, gtmp, dkey):
        for c in range(8):
            TS("dve", gtmp[:, c, :], ones_f[:], gcol[:, c:c + 1], None, ALU.mult, None, ["ones_f", "modT"], [("gtmp", c)])
            MM(PS[6 + c // 4][:, (c % 4) * 128:(c % 4 + 1) * 128], gtmp[:, c, :], ident_f[:], True, True,
               [("gtmp", c), "ident_f"], [f"ps{6 + c // 4}"])
        CP("act", dst[:, 0:512], PS[6][:, :], ["ps6"], [dkey])
        CP("act", dst[:, 512:1024], PS[7][:, :], ["ps7"], [dkey])

    def residual_add(tile, half, pbank, pkey, G, gkey, tmp, tmpkey):
        dst, dk = xsrc(tile)
        TTo("dve", tmp[:], pbank[:, 0:512], G[:, half * 512:(half + 1) * 512], ALU.mult, [pkey, gkey], [tmpkey])
        TTo("pool", dst[:, half * 512:(half + 1) * 512], dst[:, half * 512:(half + 1) * 512], tmp[:], ALU.add,
            [tmpkey, dk], [dk])

    def compute_hT(s, l, hT, xnb, junk):
        for vi, v in enumerate((s, S)):
            STT("dve", am[:, 0, vi, :], mslice(l, v, 1), 1.0, ngT[:, l, 0, :], ALU.add, ALU.mult, ["modT", "ngT"], ["am"])
        for tile in range(18):
            vi = 0 if tile < 16 else 1
            v = s if tile < 16 else S
            src, sk = xsrc(tile)
            rms_rstd(src, sk, junk[:], D)
            TS("dve", xnb[:], src, rstd[:, 0:1], None, ALU.mult, None, [sk, "rstd"], ["xnb"])
            pb = PSB[tile % 2]
            pk = f"ps{tile % 2}"
            for c in range(8):
                TR(pb[:, c * 128:(c + 1) * 128], xnb[:, c * 128:(c + 1) * 128], ident_b[:], ["xnb", "ident_b"], [pk])
            for c in range(8):
                dst = hT[:, c, tile * 128:(tile + 1) * 128]
                if False:
                    pass
                else:
                    TS("dve", dst, pb[:, c * 128:(c + 1) * 128], am[:, 0, vi, c:c + 1], mslice(l, v, 0)[:, c:c + 1],
                       ALU.mult, ALU.add, [pk, "am", "modT"], [("hT", tile)])

    def hkeys(t0, t1):
        return [("hT", t) for t in range(t0, t1)]

    def proj_fm(dst_fn, W, wkey, wcols, hT, evac):
        for tb in range(5):
            n = 512 if tb < 4 else 256
            c0 = tb * 512
            pb = PS[tb % 2]
            pk = f"ps{tb % 2}"
            for k in range(8):
                MM(pb[:, 0:n], W[:, k, wcols], hT[:, k, c0:c0 + n], k == 0, k == 7,
                   [wkey] + hkeys(c0 // 128, (c0 + n) // 128), [pk])
            evac(tb, c0, n, pb, pk)

    def even_mixer(s, l, with_ctx):
        e = l // 2
        lam_init = 0.8 - 0.6 * math.exp(-0.3 * l)
        mem["p"] = phase_base
        hT = sb("hT", [128, 8, TT], BF16)
        xnb = sb("xnb", [128, D], BF16)
        junk = sb("junk", [128, D], BF16)
        G1 = sb("G1", [128, D])
        G1c = sb("G1c", [128, D])
        gtmp = sb("gtmp", [128, 8, 128])
        tmpy = sb("tmpy", [128, 512])
        sg_bc = sb("sg_bc", [128, 128])
        lr = sb("lr", [1, 256])
        grp_base = mem["p"]
        build_G(G1, mslice(l, s, 2), gtmp, "G1")
        build_G(G1c, mslice(l, S, 2), gtmp, "G1c")
        compute_hT(s, l, hT, xnb, junk)
        P.dma("sp", lr[:], lam_d[e], [], ["lr"])
        TTo("dve", lr[0:1, 0:64], lr[0:1, 0:64], lr[0:1, 64:128], ALU.mult, ["lr"], ["lr"])
        TTo("dve", lr[0:1, 128:192], lr[0:1, 128:192], lr[0:1, 192:256], ALU.mult, ["lr"], ["lr"])
        ACT(lr[0:1, 64:128], lr[0:1, 0:64], AF.Identity, ["lr"], ["lr", "small"], accum=small[0:1, 0:1])
        ACT(lr[0:1, 192:256], lr[0:1, 128:192], AF.Identity, ["lr"], ["lr", "small"], accum=small[0:1, 1:2])
        ACT(small[0:1, 0:2], small[0:1, 0:2], AF.Exp, ["small"], ["small"])
        TTo("dve", small[0:1, 2:3], small[0:1, 1:2], small[0:1, 0:1], ALU.subtract, ["small"], ["small"])
        TS("dve", small[0:1, 3:4], small[0:1, 2:3], -lam_init, None, ALU.add, None, ["small"], ["small"])
        MM(PS[7][:, 0:1], ones_f[0:1, :], small[0:1, 3:4], True, True, ["ones_f", "small"], ["ps7"])
        CP("dve", neglam[:], PS[7][:, 0:1], ["ps7"], ["neglam"])
        P.dma("sp", sg_bc[:], subln_d[e], [], ["sg_bc"])
        TS("dve", sg_bc[:], sg_bc[:], 1.0 - lam_init, None, ALU.mult, None, ["sg_bc"], ["sg_bc"])

        ntile_out = 18 if with_ctx else 16

        def out_proj(oT, nch, Wo, wokey):
            for tile in range(ntile_out):
                G, gk = (G1, "G1") if tile < 16 else (G1c, "G1c")
                for half in range(2):
                    pb = PS[5 + half]
                    pk = f"ps{5 + half}"
                    for ci in range(nch):
                        MM(pb[:, 0:512], oT[:, ci, tile * 128:(tile + 1) * 128], Wo[:, ci, half * 512:(half + 1) * 512],
                           ci == 0, ci == nch - 1, [("oT", tile), wokey], [pk])
                    residual_add(tile, half, pb, pk, G, gk, tmpy, "tmpy")

        for g in range(DBG['nagroups'] if DBG['na'] else 0):
            P.barrier()
            mem["p"] = grp_base
            Wq = sb("Wq", [128, 8, 256], BF16)
            Wk = sb("Wk", [128, 8, 256], BF16)
            Wv = sb("Wv", [128, 8, 256], BF16)
            Wo = sb("Wo", [128, 2, D], BF16)
            Ball = sb("Ball", [128, 4, 16, 64], BF16)
            qaT = sb("qaT", [128, 2, TT], BF16)
            kaT = sb("kaT", [128, 2, TT], BF16)
            va = sb("va", [128, 18, 4, 65], BF16)
            oT = sb("oT", [128, 2, TT], BF16)
            Eb = [sb("E0", [128, 512], BF16), sb("E1", [128, 512], BF16)]
            rc = sb("rc", [128, 4])
            otile = sb("otile", [128, 256], BF16)
            P.dma("pool", Wq[:], wview(attw_d[e, :, g * 256:(g + 1) * 256]), [], ["Wq"])
            P.dma("pool", Wk[:], wview(attw_d[e, :, 1024 + g * 256:1024 + (g + 1) * 256]), [], ["Wk"])
            P.dma("pool", Wv[:], wview(attw_d[e, :, 1536 + g * 256:1536 + (g + 1) * 256]), [], ["Wv"])
            P.dma("pool", Wo[:], attwo_d[e, g * 256:(g + 1) * 256, :].rearrange("(c p) n -> p c n", p=128), [], ["Wo"])
            P.dma("pool", Ball[:], nab_d[e, :, 4 * g:4 * g + 4, :, :], [], ["Ball"])
            ACT(Ball[:], Ball[:], AF.Copy, ["Ball"], ["Ball"], scale=8.0)
            MS("pool", va[:, :, :, 64:65], 1.0, ["va1"])
            for ci in range(2):
                def ev_q(tb, c0, n, pb, pk, ci=ci):
                    CP("act", qaT[:, ci, c0:c0 + n], pb[:, 0:n], [pk], [("qaT", ci, tb)])

                def ev_k(tb, c0, n, pb, pk, ci=ci):
                    CP("dve", kaT[:, ci, c0:c0 + n], pb[:, 0:n], [pk], [("kaT", ci, tb)])
                proj_fm(None, Wq, "Wq", slice(ci * 128, (ci + 1) * 128), hT, ev_q)
                proj_fm(None, Wk, "Wk", slice(ci * 128, (ci + 1) * 128), hT, ev_k)
            for tile in range(18):
                pb = PS[tile % 2]
                pk = f"ps{tile % 2}"
                for k in range(8):
                    MM(pb[:, 0:256], hT[:, k, tile * 128:(tile + 1) * 128], Wv[:, k, :], k == 0, k == 7,
                       ["Wv", ("hT", tile)], [pk])
                CP("act" if tile % 2 else "dve", va[:, tile, :, 0:64], pb[:, 0:256].rearrange("p (h d) -> p h d", d=64),
                   [pk], [("va", tile)])
            if g == 0 and DBG.get('dump'):
                dump(9, modT[:, l, s, :], ["modT"], 48)
                dump(10, am[:, :, :, :].rearrange("p a b c -> p (a b c)"), ["am"], 32)
                dump(11, bmT[:, l, :], ["bmT"], 48)
                dump(0, hT[:, 0, 0:512], hkeys(0, 4), 512)
                dump(1, G1[:, 0:512], ["G1"], 512)
                dump(2, qaT[:, 0, 0:512], [("qaT", 0, 0)], 512)
                dump(3, kaT[:, 0, 0:512], [("kaT", 0, 0)], 512)
                dump(4, va[:, 0, :, :].rearrange("p h d -> p (h d)"), [("va", 0), "va1"], 260)
                dump(5, Ball[:, 0, 3, :], ["Ball"], 64)
            qkeys = lambda ci: [("qaT", ci, tb) for tb in range(5)]
            kkeys = lambda ci: [("kaT", ci, tb) for tb in range(5)]
            na_steps = []
            for qr in range(DBG['nqr']):
                rs = min(max(qr - 4, 0), 24)
                t0 = rs // 2
                tiles = list(range(t0, t0 + (4 if rs % 2 == 0 else 5)))
                for hh in range(4):
                    na_steps.append((qr, hh, rs, tiles))

            def na_S(n):
                qr, hh, rs, tiles = na_steps[n]
                ci, hf = hh // 2, hh % 2
                pr = slice(hf * 64, (hf + 1) * 64)
                sbk = PS[n % 2]
                sk = f"ps{n % 2}"
                q_ap = qaT[pr, ci, qr * 64:(qr + 1) * 64]
                slots = []
                for si, tl in enumerate(tiles):
                    idx = []
                    for kr in (2 * tl, 2 * tl + 1):
                        idx.append(kr - qr + 7 if rs <= kr < rs + 8 else 15)
                    so = sbk[:, si * 64:(si + 1) * 64]
                    MM(so, kaT[pr, ci, tl * 128:(tl + 1) * 128], q_ap, True, False, qkeys(ci) + kkeys(ci), [sk])
                    MM(so, ident_lo[:, :], Ball[:, hh, idx[0], :], False, False, ["ident_b", "Ball"], [sk])
                    MM(so, ident_hi[:, :], Ball[:, hh, idx[1], :], False, True, ["ident_b", "Ball"], [sk])
                    slots.append(tl)
                for cj in range(2):
                    si = len(tiles) + cj
                    MM(sbk[:, si * 64:(si + 1) * 64], kaT[pr, ci, T + cj * 128:T + (cj + 1) * 128], q_ap, True, True,
                       qkeys(ci) + kkeys(ci), [sk])
                    slots.append(16 + cj)
                ns = len(slots)
                ACT(Eb[n % 2][:, 0:ns * 64], sbk[:, 0:ns * 64], AF.Exp, [sk], [f"E{n % 2}"], scale=0.125)
                return slots

            def na_PV(n, slots):
                qr, hh, rs, tiles = na_steps[n]
                ob = PS[2 + qr % 2]
                ok = f"ps{2 + qr % 2}"
                E_ = Eb[n % 2]
                ek = f"E{n % 2}"
                ns = len(slots)
                for si, tl in enumerate(slots):
                    MM(ob[0:64, hh * 65:(hh + 1) * 65], E_[:, si * 64:(si + 1) * 64], va[:, tl, hh, :], si == 0, si == ns - 1,
                       [ek, ("va", tl), "va1"], [ok])
                if hh == 3:
                    ov = ob[0:64, 0:260].rearrange("p (h d) -> p h d", d=65)
                    RCP(rc[0:64, :].unsqueeze(2), ov[:, :, 64:65], [ok], ["rc"])
                    TTo("dve", otile[0:64, :].rearrange("p (h d) -> p h d", d=64), ov[:, :, 0:64],
                        rc[0:64, :].unsqueeze(2).to_broadcast([64, 4, 64]), ALU.mult, [ok, "rc"], ["otile"])
                    for ci in range(2):
                        TR(PSB[4][:, ci * 64:(ci + 1) * 64], otile[0:64, ci * 128:(ci + 1) * 128], ident_b[0:64, 0:64],
                           ["otile", "ident_b"], ["ps4"])
                    for ci in range(2):
                        CP("dve", oT[:, ci, qr * 64:(qr + 1) * 64], PSB[4][:, ci * 64:(ci + 1) * 64], ["ps4"], [("oT", qr // 2)])

            pend_slots = {}
            if na_steps:
                pend_slots[0] = na_S(0)
            for n in range(len(na_steps)):
                if n + 1 < len(na_steps):
                    pend_slots[n + 1] = na_S(n + 1)
                na_PV(n, pend_slots.pop(n))
            if with_ctx:
                for hh in range(4):
                    ci, hf = hh // 2, hh % 2
                    pr = slice(hf * 64, (hf + 1) * 64)
                    sbk = PS[hh % 2]
                    sk = f"ps{hh % 2}"
                    for cj in range(2):
                        MM(sbk[:, cj * 256:(cj + 1) * 256], kaT[pr, ci, T + cj * 128:T + (cj + 1) * 128], qaT[pr, ci, T:TT],
                           True, True, qkeys(ci) + kkeys(ci), [sk])
                    E_ = Eb[hh % 2]
                    ek = f"E{hh % 2}"
                    ACT(E_[:, 0:512], sbk[:, 0:512], AF.Exp, [sk], [ek], scale=0.125)
                    for qt in range(2):
                        for cj in range(2):
                            MM(PS[2 + qt][:, hh * 65:(hh + 1) * 65], E_[:, cj * 256 + qt * 128:cj * 256 + (qt + 1) * 128],
                               va[:, 16 + cj, hh, :], cj == 0, cj == 1, [ek, ("va", 16 + cj), "va1"], [f"ps{2 + qt}"])
                for qt in range(2):
                    ob = PS[2 + qt]
                    ok = f"ps{2 + qt}"
                    ov = ob[:, 0:260].rearrange("p (h d) -> p h d", d=65)
                    RCP(rc[:, :].unsqueeze(2), ov[:, :, 64:65], [ok], ["rc"])
                    TTo("dve", otile[:, :].rearrange("p (h d) -> p h d", d=64), ov[:, :, 0:64],
                        rc[:, :].unsqueeze(2).to_broadcast([128, 4, 64]), ALU.mult, [ok, "rc"], ["otile"])
                    for ci in range(2):
                        TR(PSB[4][:, ci * 128:(ci + 1) * 128], otile[:, ci * 128:(ci + 1) * 128], ident_b[:],
                           ["otile", "ident_b"], ["ps4"])
                    CP("act", oT[:, :, T + qt * 128:T + (qt + 1) * 128], PSB[4][:, 0:256].rearrange("p (c t) -> p c t", t=128),
                       ["ps4"], [("oT", 16 + qt)])
            if g == 0 and DBG.get('dump'):
                dump(6, oT[:, 0, 0:512], [("oT", t) for t in range(4)], 512)
                dump(7, Eb[0][:, 0:512], ["E0"], 512)
                dump(8, otile[:, :], ["otile"], 256)
            out_proj(oT, 2, Wo, "Wo")

        for hb in range(4 if DBG['diff'] else 0):
            P.barrier()
            mem["p"] = grp_base
            W5 = sb("W5", [128, 8, 5, 128], BF16)
            Wo = sb("Wo1", [128, 1, D], BF16)
            cosT = sb("cosT", [128, T], BF16)
            sinT = sb("sinT", [128, T], BF16)
            qbT = sb("qbT", [128, TT], BF16)
            kbT = sb("kbT", [128, TT], BF16)
            vb = sb("vb", [128, 18, 129], BF16)
            oT = sb("oTb", [128, 1, TT], BF16)
            Eb = [sb(f"E{i}", [128, 512], BF16) for i in range(4)]
            SBK = [0, 1, 6, 7]
            t1 = sb("t1", [128, 512])
            t2 = sb("t2", [128, 512])
            dd = sb("dd", [128, 128])
            obt = sb("obt", [128, 128], BF16)
            r12 = sb("r12", [128, 4])
            cols = [512 + hb * 128, 3072 + hb * 128, 2048 + hb * 128, 3584 + hb * 128, 2560 + hb * 128]
            for i, c0 in enumerate(cols):
                P.dma("pool", W5[:, :, i, :], wview(attw_d[e, :, c0:c0 + 128]), [], [("W5", i)])
            P.dma("pool", Wo[:, 0, :], attwo_d[e, 512 + hb * 128:512 + (hb + 1) * 128, :], [], ["Wo1"])
            P.dma("pool", cosT[:], cos_d, [], ["cosT"])
            P.dma("pool", sinT[:], sin_d, [], ["sinT"])
            MS("pool", vb[:, :, 128:129], 1.0, ["vb1"])
            for (dstT, dname, i_raw, i_sw) in ((qbT, "qbT", 0, 1), (kbT, "kbT", 2, 3)):
                for tb in range(5):
                    n = 512 if tb < 4 else 256
                    c0 = tb * 512
                    hk = hkeys(c0 // 128, (c0 + n) // 128)
                    for k in range(8):
                        MM(PS[0][:, 0:n], W5[:, k, i_raw, :], hT[:, k, c0:c0 + n], k == 0, k == 7, [("W5", i_raw)] + hk, ["ps0"])
                    if tb < 4:
                        for k in range(8):
                            MM(PS[1][:, 0:n], W5[:, k, i_sw, :], hT[:, k, c0:c0 + n], k == 0, k == 7, [("W5", i_sw)] + hk, ["ps1"])
                        TTo("dve", t1[:], PS[0][:, 0:n], cosT[:, c0:c0 + n], ALU.mult, ["ps0", "cosT"], ["t1"])
                        TTo("dve", t2[:], PS[1][:, 0:n], sinT[:, c0:c0 + n], ALU.mult, ["ps1", "sinT"], ["t2"])
                        TTo("pool", dstT[:, c0:c0 + n], t1[:], t2[:], ALU.add, ["t1", "t2"], [(dname, tb)])
                    else:
                        CP("act", dstT[:, c0:c0 + n], PS[0][:, 0:n], ["ps0"], [(dname, tb)])
            for tile in range(18):
                pb = PS[tile % 2]
                pk = f"ps{tile % 2}"
                for k in range(8):
                    MM(pb[:, 0:128], hT[:, k, tile * 128:(tile + 1) * 128], W5[:, k, 4, :], k == 0, k == 7,
                       [("W5", 4), ("hT", tile)], [pk])
                CP("act" if tile % 2 else "dve", vb[:, tile, 0:128], pb[:, 0:128], [pk], [("vb", tile)])
            qk_all = [("qbT", tb) for tb in range(5)] + [("kbT", tb) for tb in range(5)]

            def diff_block(qc0, nq, kts):
                nsub = nq // 128
                acc = {}
                for sub in range(nsub):
                    for m in range(2):
                        a = sub * 2 + m
                        acc[(sub, m)] = (PS[2 + a // 3][:, (a % 3) * 129:(a % 3 + 1) * 129], f"ps{2 + a // 3}")
                for bnk in sorted(set(2 + (sub * 2 + m) // 3 for sub in range(nsub) for m in range(2))):
                    MS("dve", PS[bnk][:, :], 0.0, [f"ps{bnk}"])
                steps = [(ki, kt, m) for ki, kt in enumerate(kts) for m in range(2)]

                def emit_S(i):
                    ki, kt, m = steps[i]
                    pr = slice(m * 64, (m + 1) * 64)
                    sbk = PS[SBK[i % 4]]
                    sk = f"ps{SBK[i % 4]}"
                    E_ = Eb[i % 4]
                    ek = f"E{i % 4}"
                    MM(sbk[:, 0:nq], kbT[pr, kt * 128:(kt + 1) * 128], qbT[pr, qc0:qc0 + nq], True, True, qk_all, [sk])
                    ACT(E_[:, 0:nq], sbk[:, 0:nq], AF.Exp, [sk], [ek], scale=0.125)

                def emit_PV(i):
                    ki, kt, m = steps[i]
                    E_ = Eb[i % 4]
                    ek = f"E{i % 4}"
                    for sub in range(nsub):
                        ap, akey = acc[(sub, m)]
                        MM(ap, E_[:, sub * 128:(sub + 1) * 128], vb[:, kt, :], False, ki == len(kts) - 1,
                           [ek, ("vb", kt), "vb1"], [akey])

                LA = 2
                for i in range(len(steps) + LA):
                    if i < len(steps):
                        emit_S(i)
                    if i - LA >= 0:
                        emit_PV(i - LA)
                for sub in range(nsub):
                    (o1, k1), (o2, k2) = acc[(sub, 0)], acc[(sub, 1)]
                    RCP(r12[:, 0:1], o1[:, 128:129], [k1], ["r12"])
                    RCP(r12[:, 1:2], o2[:, 128:129], [k2], ["r12"])
                    TTo("dve", r12[:, 2:3], r12[:, 1:2], neglam[:, 0:1], ALU.mult, ["r12", "neglam"], ["r12"])
                    TS("dve", t1[:, 0:128], o1[:, 0:128], r12[:, 0:1], None, ALU.mult, None, [k1, "r12"], ["t1"])
                    STT("dve", dd[:], o2[:, 0:128], r12[:, 2:3], t1[:, 0:128], ALU.mult, ALU.add, [k2, "r12", "t1"], ["dd"])
                    rms_rstd(dd[:], "dd", t2[:, 0:128], 128)
                    STT("dve", obt[:], dd[:], rstd[:, 0:1], sg_bc[:], ALU.mult, ALU.mult, ["dd", "rstd", "sg_bc"], ["obt"])
                    TR(PSB[5][:, 0:128], obt[:], ident_b[:], ["obt", "ident_b"], ["ps5"])
                    tcol = qc0 + sub * 128
                    CP("act", oT[:, 0, tcol:tcol + 128], PSB[5][:, 0:128], ["ps5"], [("oT", tcol // 128)])

            for qb_ in range(4):
                diff_block(qb_ * 512, 512, list(range(18)))
            if with_ctx:
                diff_block(T, 256, [16, 17])
            out_proj(oT, 1, Wo, "Wo1")
        P.barrier()

    def odd_mixer(s, l, with_ctx):
        o = l // 2
        mem["p"] = phase_base
        hT = sb("hT", [128, 8, TT], BF16)
        G1 = sb("G1", [128, D])
        G1c = sb("G1c", [128, D])
        tmpy = sb("tmpy", [128, 512])
        tmp_mark = mem["p"]
        xnb = sb("xnb", [128, D], BF16)
        junk = sb("junk", [128, D], BF16)
        gtmp = sb("gtmp", [128, 8, 128])
        build_G(G1, mslice(l, s, 2), gtmp, "G1")
        build_G(G1c, mslice(l, S, 2), gtmp, "G1c")
        compute_hT(s, l, hT, xnb, junk)
        P.barrier()
        mem["p"] = tmp_mark
        W5 = sb("W5", [128, 8, 5, 128], BF16)
        Wo = sb("Wo", [128, D], BF16)
        A = sb("A", [128, TT])
        B = sb("B", [128, TT])
        kk = sb("kk", [128, TT], BF16)
        sq = sb("sq", [128, TT], BF16)
        sgt = sb("sgt", [128, TT], BF16)
        qt_ = sb("qt", [128, TT], BF16)
        qh = sb("qh", [128, TT], BF16)
        kt_ = sb("kt", [128, TT], BF16)
        vv = sb("vv", [128, 18, 128], BF16)
        oTs = sb("oTs", [128, TT])
        tot = sb("tot", [128, 36])
        Ee = sb("Ee", [128, 36])
        Ep = sb("Ep", [128, 36])
        ATm2 = [sb("ATm0", [128, 128], BF16), sb("ATm1", [128, 128], BF16)]
        ktok2 = [sb("ktok0", [128, 128], BF16), sb("ktok1", [128, 128], BF16)]
        rmask = sb("rmask", [128, TT], BF16)
        MS("pool", rmask[:], 1.0, ["rmask"])
        MS("pool", rmask[:].rearrange("p (c k) -> p c k", k=64)[:, :, 0:1], 0.0, ["rmask"])
        R32 = [sb("R32a", [128, 128]), sb("R32b", [128, 128])]
        Rb = [sb("Rba", [128, 128], BF16), sb("Rbb", [128, 128], BF16)]
        ntile_out = 18 if with_ctx else 16
        NCH = 36
        for hd in range(8):
            P.barrier()
            cols = [hd * 128, 1024 + hd * 128, 2048 + hd * 128, 3072 + hd * 128, 4096 + hd * 128]
            for i, c0 in enumerate(cols):
                P.dma("pool", W5[:, :, i, :], wview(recw_d[o, :, c0:c0 + 128]), [], [("W5", i)])
            P.dma("pool", Wo[:], recwo_d[o, hd * 128:(hd + 1) * 128, :], [], ["Wo"])

            def ev_q(tb, c0, n, pb, pk):
                ACT(sq[:, c0:c0 + n], pb[:, 0:n], AF.Silu, [pk], ["sq"])

            def ev_g(tb, c0, n, pb, pk):
                ACT(sgt[:, c0:c0 + n], pb[:, 0:n], AF.Silu, [pk], ["sgt"])
            proj_fm(None, W5[:, :, 0, :], ("W5", 0), slice(0, 128), hT, ev_q)
            proj_fm(None, W5[:, :, 4, :], ("W5", 4), slice(0, 128), hT, ev_g)
            for tile in range(18):
                pb = PS[tile % 2]
                pk = f"ps{tile % 2}"
                for k in range(8):
                    MM(pb[:, 0:128], hT[:, k, tile * 128:(tile + 1) * 128], W5[:, k, 3, :], k == 0, k == 7,
                       [("W5", 3), ("hT", tile)], [pk])
                CP("dve", vv[:, tile, :], pb[:, 0:128], [pk], [("vv", tile)])
            vkeys = [("vv", t) for t in range(18)]
            for dr in range(2):
                lbc = lbT[:, dr, l, hd:hd + 1]
                omc = omlT[:, dr, l, hd:hd + 1]
                nomc = nomlT[:, dr, l, hd:hd + 1]

                def ev_f(tb, c0, n, pb, pk):
                    ACT(A[:, c0:c0 + n], pb[:, 0:n], AF.Sigmoid, [pk], ["A"])
                proj_fm(None, W5[:, :, 1 + dr, :], ("W5", 1 + dr), slice(0, 128), hT, ev_f)
                ACT(B[:], A[:], AF.Ln, ["A", "lbT", "omlT"], ["B"], scale=omc, bias=lbc)
                TS("dve", kk[:], A[:], nomc, omc, ALU.mult, ALU.add, ["A", "nomlT", "omlT"], ["kk"])
                P.op("dve", lambda E: E.tensor_tensor_scan(out=A[:], data0=rmask[:], data1=B[:], initial=0.0,
                                                            op0=ALU.mult, op1=ALU.add), ["B", "rmask", "kk"], ["A"])
                Av = A[:].rearrange("p (c k) -> p c k", k=64)
                Bv = B[:].rearrange("p (c k) -> p c k", k=64)
                CP("dve", tot[:].unsqueeze(2), Av[:, :, 63:64], ["A"], ["tot"])
                if dr == 1:
                    TTo("dve", A[:], B[:], A[:], ALU.subtract, ["A", "B"], ["A"])
                    TTo("dve", Av, Av, tot[:].unsqueeze(2).to_broadcast([128, NCH, 64]), ALU.add, ["A", "tot"], ["A"])
                ACT(Ee[:], tot[:], AF.Exp, ["tot"], ["Ee"])
                MS("dve", Ep[:], 0.0, ["Ep"])
                if dr == 0:
                    CP("dve", Ep[:, 1:32], Ee[:, 0:31], ["Ee"], ["Ep"])
                    CP("dve", Ep[:, 0:1], Ee[:, 35:36], ["Ee"], ["Ep"])
                    CP("dve", Ep[:, 33:36], Ee[:, 32:35], ["Ee"], ["Ep"])
                    order = [32, 33, 34, 35] + list(range(32))
                    msk = maskf
                else:
                    CP("dve", Ep[:, 0:31], Ee[:, 1:32], ["Ee"], ["Ep"])
                    CP("dve", Ep[:, 31:32], Ee[:, 32:33], ["Ee"], ["Ep"])
                    CP("dve", Ep[:, 32:35], Ee[:, 33:36], ["Ee"], ["Ep"])
                    order = [35, 34, 33, 32] + list(range(31, -1, -1))
                    msk = maskb
                ACT(qt_[:], A[:], AF.Exp, ["A"], ["qt"])
                ACT(kt_[:], A[:], AF.Exp, ["A"], ["kt"], scale=-1.0)
                TTo("pool", qt_[:], qt_[:], sq[:], ALU.mult, ["qt", "sq"], ["qt"])
                TTo("dve", kt_[:], kt_[:], kk[:], ALU.mult, ["kt", "kk"], ["kt"])
                TTo("pool", qh[:].rearrange("p (c k) -> p c k", k=64), qt_[:].rearrange("p (c k) -> p c k", k=64),
                    Ep[:].unsqueeze(2).to_broadcast([128, NCH, 64]), ALU.mult, ["qt", "Ep"], ["qh"])
                if hd == 0 and dr == 0 and DBG.get('dump'):
                    dump(0, B[:, 0:512], ["B"], 512)
                    dump(1, A[:, 0:512], ["A"], 512)
                    dump(2, kk[:, 0:512], ["kk"], 512)
                    dump(3, qt_[:, 0:512], ["qt"], 512)
                    dump(4, kt_[:, 0:512], ["kt"], 512)
                    dump(5, Ee[:, :], ["Ee"], 36)
                    dump(6, Ep[:, :], ["Ep"], 36)
                    dump(7, tot[:, :], ["tot"], 36)
                    dump(8, lbT[:].rearrange("p a b c -> p (a b c)"), ["lbT"], 64)
                    dump(9, vv[:, 0:4, :].rearrange("p a b -> p (a b)"), vkeys, 512)
                st_ = {"pp": 0, "first": True, "ucnt": 0}
                ABK = [2, 0]
                TBK = [3, 1]
                UBK = [6, 7]

                def stageA(ti):
                    tl = order[2 * ti] // 2
                    tc = slice(tl * 128, (tl + 1) * 128)
                    i2 = ti % 2
                    ab, tb_ = ABK[i2], TBK[i2]
                    MM(PS[ab][:, 0:128], kt_[:, tc], qt_[:, tc], True, True, ["kt", "qt"], [f"ps{ab}"])
                    TTo("dve", ATm2[i2][:], PS[ab][:, 0:128], msk[:], ALU.mult, [f"ps{ab}", "maskf", "maskb"], [("ATm", i2)])
                    TR(PSB[tb_][:, 0:128], kt_[:, tc], ident_b[:], ["kt", "ident_b"], [f"ps{tb_}"])
                    CP("act", ktok2[i2][:], PSB[tb_][:, 0:128], [f"ps{tb_}"], [("ktok", i2)])

                def stageB(ti):
                    ch_a, ch_b = order[2 * ti], order[2 * ti + 1]
                    tl = ch_a // 2
                    assert ch_b // 2 == tl
                    tc = slice(tl * 128, (tl + 1) * 128)
                    i2 = ti % 2
                    ob = PS[4 + ti % 2]
                    ok = f"ps{4 + ti % 2}"
                    chs = [ch_a, ch_b]
                    n_inter = sum(1 for ch in chs if not (st_["first"] and ch == chs[0]))
                    MM(ob[:, 0:128], vv[:, tl, :], ATm2[i2][:], True, n_inter == 0, [("ATm", i2)] + vkeys, [ok])
                    done = 0
                    for ch in chs:
                        hf = ch % 2
                        pp = st_["pp"]
                        if not st_["first"]:
                            done += 1
                            MM(ob[:, hf * 64:(hf + 1) * 64], Rb[pp][:], qh[:, ch * 64:(ch + 1) * 64], False, done == n_inter,
                               [("Rb", pp), "qh"], [ok])
                        ub = UBK[st_["ucnt"] % 2]
                        st_["ucnt"] += 1
                        uk = f"ps{ub}"
                        MM(PS[ub][:, 0:128], ktok2[i2][hf * 64:(hf + 1) * 64, :], vv[hf * 64:(hf + 1) * 64, tl, :], True, True,
                           [("ktok", i2)] + vkeys, [uk])
                        if st_["first"]:
                            CP("dve", Rb[pp][:], PS[ub][:, 0:128], [uk], [("Rb", pp)])
                            CP("dve", R32[pp][:], PS[ub][:, 0:128], [uk], [("R32", pp)])
                            st_["first"] = False
                        else:
                            STT("dve", Rb[1 - pp][:], R32[pp][:], Ep[:, ch:ch + 1], PS[ub][:, 0:128], ALU.mult, ALU.add,
                                [("R32", pp), "Ep", uk], [("Rb", 1 - pp)])
                            STT("dve", R32[1 - pp][:], R32[pp][:], Ep[:, ch:ch + 1], PS[ub][:, 0:128], ALU.mult, ALU.add,
                                [("R32", pp), "Ep", uk], [("R32", 1 - pp)])
                            st_["pp"] = 1 - pp
                    if dr == 0:
                        CP("act", oTs[:, tc], ob[:, 0:128], [ok], [("oTs", tl)])
                    else:
                        TTo("dve", oTs[:, tc], oTs[:, tc], ob[:, 0:128], ALU.add, [ok, ("oTs", tl)], [("oTs", tl)])

                stageA(0)
                for ti in range(18):
                    if ti + 1 < 18:
                        stageA(ti + 1)
                    stageB(ti)
            if hd == 0 and DBG.get('dump'):
                dump(10, oTs[:, 0:512], [("oTs", t) for t in range(4)], 512)
                dump(11, oTs[:, T:TT], [("oTs", t) for t in (16, 17)], 256)
                dump(12, R32[0][:], [("R32", 0)], 128)
            oTh = kk
            for tb in range(5):
                n = 512 if tb < 4 else 256
                c0 = tb * 512
                ok_ = [("oTs", t) for t in range(c0 // 128, (c0 + n) // 128)]
                ACT(A[:, c0:c0 + n], oTs[:, c0:c0 + n], AF.Square, ok_, ["A"])
                MM(PS[7][:, 0:n], ones_f[:], A[:, c0:c0 + n], True, True, ["ones_f", "A"], ["ps7"])
                ACT(B[:, c0:c0 + n], PS[7][:, 0:n], AF.Sqrt, ["ps7"], ["B"], scale=1.0 / 128, bias=EPS)
                RCP(B[:, c0:c0 + n], B[:, c0:c0 + n], ["B"], ["B"])
                TTo("dve", A[:, c0:c0 + n], oTs[:, c0:c0 + n], B[:, c0:c0 + n], ALU.mult, ok_ + ["B", "A"], ["A"])
                STT("dve", oTh[:, c0:c0 + n], A[:, c0:c0 + n], gnT[:, o:o + 1], sgt[:, c0:c0 + n], ALU.mult, ALU.mult,
                    ["A", "gnT", "sgt"], ["kk"])
            for tile in range(ntile_out):
                G, gk = (G1, "G1") if tile < 16 else (G1c, "G1c")
                for half in range(2):
                    pb = PS[half]
                    pk = f"ps{half}"
                    MM(pb[:, 0:512], oTh[:, tile * 128:(tile + 1) * 128], Wo[:, half * 512:(half + 1) * 512], True, True,
                       ["kk", "Wo"], [pk])
                    residual_add(tile, half, pb, pk, G, gk, tmpy, "tmpy")
        P.barrier()

    def moe(s, l, with_ctx):
        mem["p"] = phase_base
        ntl = 18 if with_ctx else 16
        NW = 288 if with_ctx else 256
        njt = 3 if with_ctx else 2
        hn = sb("hn", [128, 18, D], BF16)
        G2 = sb("G2", [128, D])
        G2c = sb("G2c", [128, D])
        posm_b = sb("posm_b", [16, TT], BF16)
        w_b = sb("w_b", [16, TT], BF16)
        pos_tok = sb("pos_tok", [128, 18, NE])
        wr = sb("wr", [128, 8, NE])
        selT = sb("selT", [16, 128], BF16)
        loop_base = mem["p"]
        gtmp = sb("gtmp", [128, 8, 128])
        xn32 = sb("xn32", [128, D])
        junk = sb("junk", [128, D], BF16)
        hT32 = sb("hT32", [128, 8, 128])
        afft = sb("afft", [128, NE])
        affT = sb("affT", [16, TT])
        work = sb("work", [16, TT])
        msk = sb("msk", [16, TT])
        pos = sb("pos", [16, TT])
        mx = sb("mx", [16, 8])
        build_G(G2, mslice(l, s, 5), gtmp, "G2")
        build_G(G2c, mslice(l, S, 5), gtmp, "G2c")
        P.dma("sp", wr[:], wview(router_d[LI[l]]), [], ["wr"])
        for vi, v in enumerate((s, S)):
            STT("dve", am[:, 1, vi, :], mslice(l, v, 4), 1.0, ngT[:, l, 1, :], ALU.add, ALU.mult, ["modT", "ngT"], ["am"])
        for tile in range(ntl):
            vi = 0 if tile < 16 else 1
            v = s if tile < 16 else S
            src, sk = xsrc(tile)
            rms_rstd(src, sk, junk[:], D)
            TS("dve", xn32[:], src, rstd[:, 0:1], None, ALU.mult, None, [sk, "rstd"], ["xn32"])
            CP("act" if DBG.get("nopool") else "pool", hn[:, tile, :], xn32[:], ["xn32"], [("hn", tile)])
            if DBG.get('sub', 9) < 1:
                continue
            pbk = f"ps{tile % 2}"
            for c in range(8):
                TR(PSB[tile % 2][:, c * 128:(c + 1) * 128], hn[:, tile, c * 128:(c + 1) * 128], ident_b[:],
                   [("hn", tile), "ident_b"], [pbk])
            for c in range(8):
                if DBG.get('noevac'):
                    continue
                src_p = PSB[tile % 2][:, c * 128:(c + 1) * 128]
                if c % 2 == 0:
                    TS("dve", hT32[:, c, :], src_p, am[:, 1, vi, c:c + 1], mslice(l, v, 3)[:, c:c + 1], ALU.mult, ALU.add,
                       [pbk, "am", "modT"], [("hT32", c)])
                    continue
                if c % 2 == 0:
                    ACT(hT32[:, c, :], src_p, AF.Identity, [pbk, "am", "modT"], [("hT32", c)],
                        scale=am[:, 1, vi, c:c + 1], bias=mslice(l, v, 3)[:, c:c + 1])
                else:
                    TS("dve", hT32[:, c, :], src_p, am[:, 1, vi, c:c + 1], mslice(l, v, 3)[:, c:c + 1], ALU.mult, ALU.add,
                       [pbk, "am", "modT"], [("hT32", c)])
            if DBG.get('sub', 9) < 2:
                continue
            for c in range(8):
                MM(PS[2][:, 0:NE], hT32[:, c, :], wr[:, c, :], c == 0, c == 7, [("hT32", c), "wr"], ["ps2"])
            if DBG.get('sub', 9) < 3:
                continue
            ACT(afft[:], PS[2][:, 0:NE], AF.Exp, ["ps2"], ["afft", "ssq"], accum=ssq[:])
            RCP(rt[:], ssq[:], ["ssq"], ["rt"])
            TS("dve", afft[:], afft[:], rt[:, 0:1], None, ALU.mult, None, ["afft", "rt"], ["afft"])
            if not DBG.get("notr"):
                TR(PS[3][0:16, 0:128], afft[:], ident_f[:], ["afft", "ident_f"], ["ps3"])
                CP("act", affT[0:16, tile * 128:(tile + 1) * 128], PS[3][0:16, 0:128], ["ps3"], ["affT"])

        if DBG.get('stage', 9) < 1:
            P.barrier()
            return

        def route(c0, n, cap):
            CP("dve", work[:, c0:c0 + n], affT[:, c0:c0 + n], ["affT"], ["work"])
            nit = cap // 8
            for it_ in range(nit):
                P.op("dve", (lambda a, b: (lambda E: E.max(out=a, in_=b)))(mx[:], work[:, c0:c0 + n]), ["work"], ["mx"])
                if it_ < nit - 1:
                    P.op("dve", (lambda a, b, c_: (lambda E: E.match_replace(out=a, in_to_replace=b, in_values=c_, imm_value=-1.0)))(
                        work[:, c0:c0 + n], mx[:], work[:, c0:c0 + n]), ["work", "mx"], ["work"])
            TS("dve", msk[:, c0:c0 + n], affT[:, c0:c0 + n], mx[:, 7:8], None, ALU.is_ge, None, ["affT", "mx"], ["msk"])
            MS("dve", work[:, c0:c0 + n], 1.0, ["work"])
            P.op("dve", (lambda a, b, c_: (lambda E: E.tensor_tensor_scan(out=a, data0=b, data1=c_, initial=0.0, op0=ALU.mult, op1=ALU.add)))(
                pos[:, c0:c0 + n], work[:, c0:c0 + n], msk[:, c0:c0 + n]), ["work", "msk"], ["pos"])
            TTo("dve", pos[:, c0:c0 + n], pos[:, c0:c0 + n], msk[:, c0:c0 + n], ALU.mult, ["pos", "msk"], ["pos"])
            TS("dve", pos[:, c0:c0 + n], pos[:, c0:c0 + n], -1.0, None, ALU.add, None, ["pos"], ["pos"])
            CP("dve", posm_b[:, c0:c0 + n], pos[:, c0:c0 + n], ["pos"], ["posm_b"])
            TTo("dve", w_b[:, c0:c0 + n], affT[:, c0:c0 + n], msk[:, c0:c0 + n], ALU.mult, ["affT", "msk"], ["w_b"])

        route(0, T, 256)
        if with_ctx:
            route(T, L, 32)
        if DBG.get('stage', 9) < 2:
            P.barrier()
            return
        for tile in range(ntl):
            TR(PS[3][:, 0:NE], pos[0:16, tile * 128:(tile + 1) * 128], ident_f[0:16, 0:16], ["pos", "ident_f"], ["ps3"])
            CP("act", pos_tok[:, tile, :], PS[3][:, 0:NE], ["ps3"], ["pos_tok"])
        P.barrier()
        mem["p"] = loop_base
        Pe = sb("Pe", [128, 16, 256], BF16)
        Pce = sb("Pce", [128, 2, 32], BF16)
        PT = sb("PT", [128, 2, T], BF16)
        PTc = sb("PTc", [32, L], BF16)
        xsel = sb("xsel", [128, 8, 288], BF16)
        hid = [sb("hid0", [128, 288], BF16), sb("hid1", [128, 288], BF16)]
        sgl = sb("sgl", [128, 288])
        yy = sb("yy", [128, 3, D], BF16)
        wsb = sb("wsb", [128, 512])
        Wg = [sb("Wg0", [128, 8, 256], BF16), sb("Wg1", [128, 8, 256], BF16)]
        Wu = [sb("Wu0", [128, 8, 256], BF16), sb("Wu1", [128, 8, 256], BF16)]
        Wd = [sb("Wd0", [128, 2, D], BF16), sb("Wd1", [128, 2, D], BF16)]
        hnk = [("hn", t) for t in range(ntl)]
        wcnt = 0
        for e in range(DBG['experts']):
            TS("dve", selT[:], ones_f[0:16, :], ident_f[0:16, e:e + 1], None, ALU.mult, None, ["ones_f", "ident_f"], ["selT"])
            TTo("dve", Pe[:], iota_j[:].unsqueeze(1).to_broadcast([128, 16, 256]),
                pos_tok[:, 0:16, e:e + 1].to_broadcast([128, 16, 256]), ALU.is_equal, ["iota_j", "pos_tok"], ["Pe"])
            if with_ctx:
                TTo("dve", Pce[:], iota_j[:, 0:32].unsqueeze(1).to_broadcast([128, 2, 32]),
                    pos_tok[:, 16:18, e:e + 1].to_broadcast([128, 2, 32]), ALU.is_equal, ["iota_j", "pos_tok"], ["Pce"])
            for blk in range(4):
                bc = slice(blk * 512, (blk + 1) * 512)
                MM(PS[6][:, 0:512], selT[0:16, :], posm_b[0:16, bc], True, True, ["selT", "posm_b"], ["ps6"])
                MM(PS[7][:, 0:512], selT[0:16, :], w_b[0:16, bc], True, True, ["selT", "w_b"], ["ps7"])
                CP("act", wsb[:], PS[7][:, 0:512], ["ps7"], ["wsb"])
                for jt in range(2):
                    STT("dve", PT[:, jt, bc], PS[6][:, 0:512], iota_p[:, jt:jt + 1], wsb[:], ALU.is_equal, ALU.mult,
                        ["ps6", "iota_p", "wsb"], ["PT"])
            if with_ctx:
                MM(PS[6][0:32, 0:L], selT[0:16, 0:32], posm_b[0:16, T:TT], True, True, ["selT", "posm_b"], ["ps6"])
                MM(PS[7][0:32, 0:L], selT[0:16, 0:32], w_b[0:16, T:TT], True, True, ["selT", "w_b"], ["ps7"])
                CP("act", wsb[0:32, 0:L], PS[7][0:32, 0:L], ["ps7"], ["wsb"])
                STT("dve", PTc[:], PS[6][0:32, 0:L], iota_p[0:32, 0:1], wsb[0:32, 0:L], ALU.is_equal, ALU.mult,
                    ["ps6", "iota_p", "wsb"], ["PTc"])
            for c in range(8):
                pb = PS[6 + c % 2]
                pk = f"ps{6 + c % 2}"
                for tile in range(16):
                    MM(pb[:, 0:256], hn[:, tile, c * 128:(c + 1) * 128], Pe[:, tile, :], tile == 0, tile == 15, ["Pe"] + hnk, [pk])
                if with_ctx:
                    for ct in range(2):
                        MM(pb[:, 256:288], hn[:, 16 + ct, c * 128:(c + 1) * 128], Pce[:, ct, :], ct == 0, ct == 1, ["Pce"] + hnk, [pk])
                TS("dve", xsel[:, c, 0:256], pb[:, 0:256], am[:, 1, 0, c:c + 1], mslice(l, s, 3)[:, c:c + 1], ALU.mult, ALU.add,
                   [pk, "am", "modT"], [("xsel", c)])
                if with_ctx:
                    TS("dve", xsel[:, c, 256:288], pb[:, 256:288], am[:, 1, 1, c:c + 1], mslice(l, S, 3)[:, c:c + 1], ALU.mult, ALU.add,
                       [pk, "am", "modT"], [("xsel", c)])
            xk = [("xsel", c) for c in range(8)]
            pend = None

            def down(fc, wi, f2):
                for jt in range(njt):
                    rows = 128 if jt < 2 else 32
                    for half in range(2):
                        b = jt * 2 + half
                        MM(PS[b][0:rows, 0:512], hid[fc % 2][:, jt * 128:jt * 128 + rows], Wd[wi][:, f2, half * 512:(half + 1) * 512],
                           fc == 0, fc == 15, [f"hid{fc % 2}", f"Wd{wi}"], [f"ps{b}"])

            for fb in range(8):
                wi = wcnt % 2
                wcnt += 1
                P.dma("pool", Wg[wi][:], wview(wg_d[LI[l], e, :, fb * 256:(fb + 1) * 256]), [], [f"Wg{wi}"])
                P.dma("pool", Wu[wi][:], wview(wu_d[LI[l], e, :, fb * 256:(fb + 1) * 256]), [], [f"Wu{wi}"])
                P.dma("pool", Wd[wi][:], wd_d[LI[l], e, fb * 256:(fb + 1) * 256, :].rearrange("(c p) n -> p c n", p=128), [], [f"Wd{wi}"])
                for f2 in range(2):
                    fc = fb * 2 + f2
                    for c in range(8):
                        MM(PS[6][:, 0:NW], Wg[wi][:, c, f2 * 128:(f2 + 1) * 128], xsel[:, c, 0:NW], c == 0, c == 7, [f"Wg{wi}"] + xk, ["ps6"])
                    for c in range(8):
                        MM(PS[7][:, 0:NW], Wu[wi][:, c, f2 * 128:(f2 + 1) * 128], xsel[:, c, 0:NW], c == 0, c == 7, [f"Wu{wi}"] + xk, ["ps7"])
                    ACT(sgl[:, 0:NW], PS[6][:, 0:NW], AF.Silu, ["ps6"], ["sgl"])
                    TTo("dve", hid[fc % 2][:, 0:NW], sgl[:, 0:NW], PS[7][:, 0:NW], ALU.mult, ["sgl", "ps7"], [f"hid{fc % 2}"])
                    if pend is not None:
                        down(*pend)
                    pend = (fc, wi, f2)
            down(*pend)
            for jt in range(njt):
                rows = 128 if jt < 2 else 32
                G = G2 if jt < 2 else G2c
                gk = "G2" if jt < 2 else "G2c"
                for half in range(2):
                    b = jt * 2 + half
                    TTo("dve", yy[0:rows, jt, half * 512:(half + 1) * 512], PS[b][0:rows, 0:512], G[0:rows, half * 512:(half + 1) * 512],
                        ALU.mult, [f"ps{b}", gk], [("yy", jt)])
            for tile in range(16):
                for half in range(2):
                    b = (tile % 3) * 2 + half
                    for jt in range(2):
                        MM(PS[b][:, 0:512], PT[:, jt, tile * 128:(tile + 1) * 128], yy[:, jt, half * 512:(half + 1) * 512], jt == 0, jt == 1,
                           ["PT", ("yy", jt)], [f"ps{b}"])
                    TTo("dve", X[:, tile, half * 512:(half + 1) * 512], X[:, tile, half * 512:(half + 1) * 512], PS[b][:, 0:512], ALU.add,
                        [f"ps{b}", ("X", tile)], [("X", tile)])
            if with_ctx:
                for ct in range(2):
                    for half in range(2):
                        b = ct * 2 + half
                        MM(PS[b][:, 0:512], PTc[0:32, ct * 128:(ct + 1) * 128], yy[0:32, 2, half * 512:(half + 1) * 512], True, True,
                           ["PTc", ("yy", 2)], [f"ps{b}"])
                        TTo("dve", XC[:, ct, half * 512:(half + 1) * 512], XC[:, ct, half * 512:(half + 1) * 512], PS[b][:, 0:512], ALU.add,
                            [f"ps{b}", ("XC", ct)], [("XC", ct)])
        P.barrier()

    for s in range(S):
        for tile in range(16):
            P.dma("sp", X[:, tile, :], x_d[s, tile * 128:(tile + 1) * 128, :], [], [("X", tile)])
        for ct in range(2):
            P.dma("sp", XC[:, ct, :], ctx_d[s, ct * 128:(ct + 1) * 128, :], [], [("XC", ct)])
        for l in layers:
            last = (l == 3)
            if DBG['mix']:
                if l % 2 == 0:
                    even_mixer(s, l, not last)
                else:
                    odd_mixer(s, l, not last)
            if DBG['moe']:
                moe(s, l, not last)
        P.barrier()
        mem["p"] = phase_base
        fg = sb("fg", [128, D])
        junk = sb("junk", [128, D], BF16)
        ot = [sb("ot0", [128, D]), sb("ot1", [128, D])]
        if final:
            P.dma("sp", fg[:], fg_d, [], ["fg"])
        for tile in range(16):
            src, sk = xsrc(tile)
            o_ = ot[tile % 2]
            okey = f"ot{tile % 2}"
            if final:
                rms_rstd(src, sk, junk[:], D)
                STT("dve", o_[:], src, rstd[:, 0:1], fg[:], ALU.mult, ALU.mult, [sk, "rstd", "fg"], [okey])
            else:
                CP("dve", o_[:], src, [sk], [okey])
            P.dma("sp", out_d[s, tile * 128:(tile + 1) * 128, :], o_[:], [okey], [])
        if debug_ctx:
            for ct in range(2):
                P.dma("sp", outc_d[s, ct * 128:(ct + 1) * 128, :], XC[:, ct, :], [("XC", ct)], [])
        P.barrier()
    P.wait_dmas("sp")
    P.run()
    return nc, P


def _consts():
    c = {}
    c["c_ident"] = np.eye(128, dtype=np.float32)
    lo = np.eye(128, dtype=np.float32); lo[64:, :] = 0
    hi = np.eye(128, dtype=np.float32); hi[:64, :] = 0
    c["c_ident_lo"] = lo
    c["c_ident_hi"] = hi
    c["c_iota_j"] = np.broadcast_to(np.arange(256, dtype=np.float32)[None, :], (128, 256)).copy()
    p = np.arange(128, dtype=np.float32)
    c["c_iota_p"] = np.stack([p, p + 128, p, p], axis=1).copy()
    s_ = np.arange(128)[:, None]
    t_ = np.arange(128)[None, :]
    same = (s_ // 64) == (t_ // 64)
    c["c_maskf"] = (same & (s_ <= t_)).astype(np.float32)
    c["c_maskb"] = (same & (s_ >= t_)).astype(np.float32)
    t = np.arange(T)
    rows = (t // 64).astype(np.float32)
    colsf = (t % 64).astype(np.float32)
    inv = (10000.0 ** (-np.arange(16, dtype=np.float32) / 16)).astype(np.float32)
    ang = np.concatenate([rows[:, None] * inv, colsf[:, None] * inv], axis=-1).astype(np.float32)
    cos = np.cos(ang).astype(np.float32).T
    sin = np.sin(ang).astype(np.float32).T
    cos64 = np.concatenate([cos, cos], axis=0)
    sin64 = np.concatenate([-sin, sin], axis=0)
    c["rope_cos"] = np.concatenate([cos64, cos64], axis=0).astype(np.float32)
    c["rope_sin"] = np.concatenate([sin64, sin64], axis=0).astype(np.float32)
    return c


def _swap_perm(base):
    idx = []
    for m in range(8):
        o = base + m * 64
        idx += list(range(o + 32, o + 64)) + list(range(o, o + 32))
    return np.array(idx)


def _na_bias_table(rpb):
    kc = np.arange(64)[:, None]
    qc = np.arange(64)[None, :]
    cstart = np.clip(qc - 8, 0, 48)
    valid = (kc >= cstart) & (kc < cstart + 16)
    dcol = np.clip(kc - qc + 15, 0, 30)
    tab = np.full((64, 8, 16, 64), -3750.0, dtype=np.float32)
    g = rpb[:, :, dcol]
    g = np.transpose(g, (2, 0, 1, 3))
    vm = np.broadcast_to(valid[:, None, None, :], g.shape)
    tab[:, :, 0:15, :] = np.where(vm, g, np.float32(-3750.0))
    return np.concatenate([tab, tab], axis=0)


def prep_shared(inp, layers=(0, 1, 2, 3)):
    layers = list(layers) if len(layers) else [0]
    f = lambda a: np.ascontiguousarray(np.asarray(a, dtype=np.float32))
    d = dict(_consts())
    d["w_mod"] = f(np.asarray(inp["w_mod"])[layers])
    d["b_modT"] = f(np.transpose(np.asarray(inp["b_mod"]).reshape(4, 48, 128), (2, 0, 1)))
    d["norm_gT"] = f(np.transpose(np.asarray(inp["norm_g"]).reshape(4, 2, 8, 128), (3, 0, 1, 2)))
    w_in = np.asarray(inp["att_w_in"])
    d["att_w"] = f(np.concatenate([w_in, w_in[:, :, _swap_perm(512)], w_in[:, :, _swap_perm(2048)]], axis=2))
    d["att_wo"] = f(inp["att_w_out"])
    d["na_bias"] = f(np.stack([_na_bias_table(np.asarray(inp["na_rpb"])[e]) for e in range(2)], axis=0))
    d["lam_row"] = f(np.asarray(inp["diff_lambda"]).reshape(2, 1, 256))
    d["subln_bc"] = f(np.broadcast_to(np.asarray(inp["diff_subln_g"])[:, None, :], (2, 128, 128)))
    d["rec_w_in"] = f(inp["rec_w_in"])
    d["rec_wo"] = f(inp["rec_w_out"])
    d["rec_lbT"] = f(np.transpose(np.asarray(inp["rec_lb_logits"]).reshape(2, 4, 8, 128), (3, 0, 1, 2)))
    d["rec_gn"] = f(np.asarray(inp["rec_gnorm_g"]).T)
    d["router"] = f(np.asarray(inp["moe_router"])[layers])
    nex = max(1, DBG["experts"])
    d["wg"] = f(np.asarray(inp["moe_w_gate"])[layers][:, :nex])
    d["wu"] = f(np.asarray(inp["moe_w_up"])[layers][:, :nex])
    d["wd"] = f(np.asarray(inp["moe_w_down"])[layers][:, :nex])
    d["finalg_bc"] = f(np.broadcast_to(np.asarray(inp["final_g"])[None, :], (128, D)))
    return d


def prep_core(inp, samples):
    f = lambda a: np.ascontiguousarray(np.asarray(a, dtype=np.float32))
    x = np.asarray(inp["x"])
    ctx = np.asarray(inp["ctx"])
    c = np.asarray(inp["c"])
    cv = np.concatenate([c[samples], np.asarray(inp["c_ctx"])[None, :]], axis=0)
    V = cv.shape[0]
    return {
        "x": f(x[samples]),
        "ctx": f(ctx[samples]),
        "cvT": f(np.transpose(cv.reshape(V, 8, 128), (2, 1, 0))),
    }


_CACHE = {}


def kernel(**inputs):
    n_cores = 8
    S = 2
    if "nc" not in _CACHE:
        _CACHE["nc"] = build(S, [0, 1, 2, 3], final=True)[0]
    nc = _CACHE["nc"]
    shared = prep_shared(inputs)
    in_maps = []
    for i in range(n_cores):
        m = dict(shared)
        m.update(prep_core(inputs, list(range(i * S, (i + 1) * S))))
        in_maps.append(m)
    res = run_bass_kernel_spmd(nc, in_maps, core_ids=list(range(n_cores)))
    return np.concatenate([r["out"] for r in res.results], axis=0).astype(np.float32)
```

```python
import math
import numpy as np
import concourse.bass as bass
import concourse.mybir as mybir
from concourse.bass_utils import run_bass_kernel_spmd

F32 = mybir.dt.float32
BF16 = mybir.dt.bfloat16
AF = mybir.ActivationFunctionType
ALU = mybir.AluOpType

EPOCH = 28800


class Stream:
    def __init__(self, prog, name):
        self.prog = prog
        self.name = name
        self.sems = []
        self.n = 0

    def sem_for(self, e):
        while len(self.sems) <= e:
            self.sems.append(self.prog.nc.alloc_semaphore(f"{self.name}_{len(self.sems)}"))
        return self.sems[e]

    def bump(self, inc):
        e = self.n // EPOCH
        assert (self.n + inc - 1) // EPOCH == e
        self.n += inc
        return self.sem_for(e), (self, self.n)

    def loc(self, n):
        e = (n - 1) // EPOCH
        return self.sem_for(e), n - e * EPOCH


class Prog:
    ENGS = ("pe", "act", "dve", "pool", "sp")

    def __init__(self, nc, n_dma_sems=4):
        self.nc = nc
        self.q = {e: [] for e in self.ENGS}
        self.stream = {e: Stream(self, "s_" + e) for e in self.ENGS}
        self.seen = {e: {} for e in self.ENGS}
        self.last_w = {}
        self.readers = {}
        self.dma_streams = {}
        self.dma_rr = {}
        self.n_dma_sems = n_dma_sems
        self.ninst = 0

    def _deps(self, reads, writes):
        deps = []
        for k in reads:
            t = self.last_w.get(k)
            if t is not None:
                deps.append(t)
        for k in writes:
            t = self.last_w.get(k)
            if t is not None:
                deps.append(t)
            deps.extend(self.readers.get(k, ()))
        return deps

    def _commit(self, tok, reads, writes):
        for k in writes:
            self.last_w[k] = tok
            self.readers[k] = []
        for k in reads:
            if k in writes:
                continue
            self.readers.setdefault(k, []).append(tok)

    def _waits(self, eng, deps, skip_self=False):
        seen = self.seen[eng]
        best = {}
        for (st, n) in deps:
            if skip_self and st is self.stream[eng]:
                continue
            if seen.get(st, 0) >= n:
                continue
            if best.get(st, 0) < n:
                best[st] = n
        waits = []
        for st, n in best.items():
            seen[st] = n
            waits.append(st.loc(n))
        return waits

    def op(self, eng, fn, reads=(), writes=()):
        reads = tuple(reads)
        writes = tuple(writes) + tuple(k for k in reads if isinstance(k, str) and k.startswith("ps") and k[2:].isdigit())
        deps = self._deps(reads, writes)
        waits = self._waits(eng, deps, skip_self=(eng == "pe"))
        sem, tok = self.stream[eng].bump(1)

        def emit(E, waits=waits, fn=fn, sem=sem):
            for (s, v) in waits:
                E.wait_ge(s, v)
            fn(E).then_inc(sem, 1)

        self.q[eng].append(emit)
        self._commit(tok, reads, writes)
        self.ninst += 1
        return tok

    def dma(self, queue, out, in_, reads=(), writes=()):
        reads = tuple(reads)
        writes = tuple(writes)
        deps = self._deps(reads, writes)
        if queue not in self.dma_streams:
            self.dma_streams[queue] = [Stream(self, f"d_{queue}{i}") for i in range(self.n_dma_sems)]
            self.dma_rr[queue] = 0
        i = self.dma_rr[queue]
        self.dma_rr[queue] = (i + 1) % self.n_dma_sems
        st = self.dma_streams[queue][i]
        if st.n > 0:
            deps.append((st, st.n))
        waits = self._waits(queue, deps)
        sem, tok = st.bump(16)

        def emit(E, waits=waits, sem=sem, out=out, in_=in_):
            for (s, v) in waits:
                E.wait_ge(s, v)
            E.dma_start(out=out, in_=in_).then_inc(sem, 16)

        self.q[queue].append(emit)
        self._commit(tok, reads, writes)
        self.ninst += 1
        return tok

    def all_tokens(self):
        toks = []
        for e in self.ENGS:
            if self.stream[e].n:
                toks.append((self.stream[e], self.stream[e].n))
        for q, sts in self.dma_streams.items():
            for st in sts:
                if st.n:
                    toks.append((st, st.n))
        return toks

    def barrier(self):
        toks = self.all_tokens()
        for eng in self.ENGS:
            waits = self._waits(eng, toks)

            def emit(E, waits=waits):
                for (s, v) in waits:
                    E.wait_ge(s, v)

            self.q[eng].append(emit)

    def wait_dmas(self, eng):
        toks = [(st, st.n) for q, sts in self.dma_streams.items() for st in sts if st.n]
        waits = self._waits(eng, toks)

        def emit(E, waits=waits):
            for (s, v) in waits:
                E.wait_ge(s, v)

        self.q[eng].append(emit)

    def run(self):
        nc = self.nc
        with nc.Block() as block:
            @block.tensor
            def _(E):
                for f in self.q["pe"]:
                    f(E)

            @block.scalar
            def _(E):
                for f in self.q["act"]:
                    f(E)

            @block.vector
            def _(E):
                for f in self.q["dve"]:
                    f(E)

            @block.gpsimd
            def _(E):
                for f in self.q["pool"]:
                    f(E)

            @block.sync
            def _(E):
                for f in self.q["sp"]:
                    f(E)


DBG = {'na': 1, 'diff': 1, 'mix': 1, 'moe': 1, 'experts': 16, 'nagroups': 2, 'nqr': 32}
D = 1024
T = 2048
L = 256
TT = T + L
EPS = 1e-6
NE = 16
FF = 2048


def build(S, layers, final=True, debug_ctx=False):
    nc = bass.Bass("TRN2", target_bir_lowering=False)
    P = Prog(nc)
    V = S + 1

    def din(name, shape):
        return nc.dram_tensor(name, list(shape), F32, kind="ExternalInput").ap()

    x_d = din("x", [S, T, D])
    ctx_d = din("ctx", [S, L, D])
    cvT_d = din("cvT", [128, 8, V])
    NL = max(1, len(layers))
    LI = {l: i for i, l in enumerate(layers)}
    wmod_d = din("w_mod", [NL, D, 6 * D])
    bmT_d = din("b_modT", [128, 4, 48])
    ngT_d = din("norm_gT", [128, 4, 2, 8])
    attw_d = din("att_w", [2, D, 4096])
    attwo_d = din("att_wo", [2, D, D])
    nab_d = din("na_bias", [2, 128, 8, 16, 64])
    lam_d = din("lam_row", [2, 1, 256])
    subln_d = din("subln_bc", [2, 128, 128])
    cos_d = din("rope_cos", [128, T])
    sin_d = din("rope_sin", [128, T])
    recw_d = din("rec_w_in", [2, D, 5 * D])
    recwo_d = din("rec_wo", [2, D, D])
    lbT_d = din("rec_lbT", [128, 2, 4, 8])
    gn_d = din("rec_gn", [128, 2])
    router_d = din("router", [NL, D, NE])
    NEX = max(1, DBG["experts"])
    wg_d = din("wg", [NL, NEX, D, FF])
    wu_d = din("wu", [NL, NEX, D, FF])
    wd_d = din("wd", [NL, NEX, FF, D])
    fg_d = din("finalg_bc", [128, D])
    cid_d = din("c_ident", [128, 128])
    cij_d = din("c_iota_j", [128, 256])
    cip_d = din("c_iota_p", [128, 4])
    cmf_d = din("c_maskf", [128, 128])
    cmb_d = din("c_maskb", [128, 128])
    cil_d = din("c_ident_lo", [128, 128])
    cih_d = din("c_ident_hi", [128, 128])
    out_d = nc.dram_tensor("out", [S, T, D], F32, kind="ExternalOutput").ap()
    outc_d = nc.dram_tensor("out_ctx", [S, L, D], F32, kind="ExternalOutput").ap() if debug_ctx else None
    dbg_d = nc.dram_tensor("dbg", [16, 128, 512], F32, kind="ExternalOutput").ap() if debug_ctx else None

    base = (nc.sbuf_base + 63) // 64 * 64
    lim = nc.sbuf_top
    mem = {"p": base, "n": 0}

    def sb(name, shape, dt=F32):
        nb = int(np.prod(shape[1:])) * (2 if dt == BF16 else 4)
        nb = (nb + 63) // 64 * 64
        off = mem["p"]
        assert off + nb <= lim, f"SBUF overflow at {name}: {off + nb} > {lim}"
        mem["p"] = off + nb
        mem["n"] += 1
        return nc.alloc_sbuf_tensor_at(f"{name}_{mem['n']}", list(shape), dt, offset=off)

    PS = [nc.alloc_psum_tensor(f"ps{i}", [128, 512], F32) for i in range(8)]
    PSB = [p[:].bitcast(BF16) for p in PS]

    def MM(out, lhsT, rhs, st, sp, r, w):
        P.op("pe", lambda E: E.matmul(out, lhsT=lhsT, rhs=rhs, start=st, stop=sp), r, w)

    def TR(out, in_, idn, r, w):
        P.op("pe", lambda E: E.transpose(out=out, in_=in_, identity=idn), r, w)

    def ACT(out, in_, func, r, w, scale=None, bias=None, accum=None):
        kw = {}
        if scale is not None:
            kw["scale"] = scale
        if bias is not None:
            kw["bias"] = bias
        if accum is not None:
            kw["accum_out"] = accum
        P.op("act", lambda E: E.activation(out=out, in_=in_, func=func, **kw), r, w)

    def TS(eng, out, in0, s1, s2, op0, op1, r, w):
        if s2 is None:
            P.op(eng, lambda E: E.tensor_scalar(out=out, in0=in0, scalar1=s1, scalar2=None, op0=op0), r, w)
        else:
            P.op(eng, lambda E: E.tensor_scalar(out=out, in0=in0, scalar1=s1, scalar2=s2, op0=op0, op1=op1), r, w)

    def TTo(eng, out, in0, in1, op, r, w):
        P.op(eng, lambda E: E.tensor_tensor(out=out, in0=in0, in1=in1, op=op), r, w)

    def STT(eng, out, in0, sc, in1, op0, op1, r, w):
        P.op(eng, lambda E: E.scalar_tensor_tensor(out=out, in0=in0, scalar=sc, in1=in1, op0=op0, op1=op1), r, w)

    def CP(eng, out, in_, r, w):
        if eng == "act":
            P.op("act", lambda E: E.copy(out=out, in_=in_), r, w)
        else:
            P.op(eng, lambda E: E.tensor_copy(out=out, in_=in_), r, w)

    def MS(eng, ap, v, w):
        P.op(eng, lambda E: E.memset(ap, v), [], w)

    dbgbuf = {}

    def dump(i, ap, keys, n):
        if dbg_d is None:
            return
        if "t" not in dbgbuf:
            dbgbuf["t"] = nc.alloc_sbuf_tensor_at("dbgt", [128, 512], F32, offset=(lim - 4096) // 64 * 64)
        t = dbgbuf["t"]
        rows = ap.shape[0]
        P.op("dve", lambda E: E.memset(t[:], 0.0), [], ["dbgt"])
        P.op("dve", lambda E: E.tensor_copy(out=t[0:rows, 0:n], in_=ap), keys, ["dbgt"])
        P.dma("sp", dbg_d[i], t[:], ["dbgt"], [])

    def RCP(out, in_, r, w):
        P.op("dve", lambda E: E.reciprocal(out=out, in_=in_), r, w)

    def wview(src2d):
        return src2d.rearrange("(k p) n -> p k n", p=128)

    X = sb("X", [128, 16, D])
    XC = sb("XC", [128, 2, D])
    ident_f = sb("ident_f", [128, 128])
    ident_b = sb("ident_b", [128, 128], BF16)
    ones_f = sb("ones_f", [128, 128])
    ident_lo = sb("ident_lo", [128, 128], BF16)
    ident_hi = sb("ident_hi", [128, 128], BF16)
    iota_j = sb("iota_j", [128, 256])
    iota_p = sb("iota_p", [128, 4])
    maskf = sb("maskf", [128, 128])
    maskb = sb("maskb", [128, 128])
    modT = sb("modT", [128, 4, V, 48])
    bmT = sb("bmT", [128, 4, 48])
    ngT = sb("ngT", [128, 4, 2, 8])
    scT = sb("scT", [128, 8, V])
    lbT = sb("lbT", [128, 2, 4, 8])
    omlT = sb("omlT", [128, 2, 4, 8])
    nomlT = sb("nomlT", [128, 2, 4, 8])
    gnT = sb("gnT", [128, 2])
    am = sb("am", [128, 2, 2, 8])
    ssq = sb("ssq", [128, 1])
    rt = sb("rt", [128, 1])
    rstd = sb("rstd", [128, 1])
    neglam = sb("neglam", [128, 1])
    small = sb("small", [128, 16])
    lsum = sb("lsum", [128, 2, 8])
    phase_base = mem["p"]

    P.dma("sp", ident_f[:], cid_d, [], ["ident_f"])
    P.dma("pool", ident_b[:], cid_d, [], ["ident_b"])
    P.dma("pool", ident_lo[:], cil_d, [], ["ident_b"])
    P.dma("pool", ident_hi[:], cih_d, [], ["ident_b"])
    P.dma("sp", iota_j[:], cij_d, [], ["iota_j"])
    P.dma("sp", iota_p[:], cip_d, [], ["iota_p"])
    P.dma("sp", maskf[:], cmf_d, [], ["maskf"])
    P.dma("sp", maskb[:], cmb_d, [], ["maskb"])
    P.dma("sp", bmT[:], bmT_d, [], ["bmT"])
    P.dma("sp", ngT[:], ngT_d, [], ["ngT"])
    P.dma("sp", scT[:], cvT_d, [], ["scT"])
    P.dma("sp", lbT[:], lbT_d, [], ["lbT"])
    P.dma("sp", gnT[:], gn_d, [], ["gnT"])
    MS("pool", ones_f[:], 1.0, ["ones_f"])
    ACT(scT[:], scT[:], AF.Silu, ["scT"], ["scT"])

    ACT(lbT[:], lbT[:], AF.Exp, ["lbT"], ["lbT"])
    TTo("dve", lsum[:], lbT[:, :, 0, :], lbT[:, :, 1, :], ALU.add, ["lbT"], ["lsum"])
    TTo("dve", lsum[:], lsum[:], lbT[:, :, 2, :], ALU.add, ["lbT", "lsum"], ["lsum"])
    TTo("dve", lsum[:], lsum[:], lbT[:, :, 3, :], ALU.add, ["lbT", "lsum"], ["lsum"])
    RCP(lsum[:], lsum[:], ["lsum"], ["lsum"])
    for j in range(4):
        TTo("dve", lbT[:, :, j, :], lbT[:, :, j, :], lsum[:], ALU.mult, ["lbT", "lsum"], ["lbT"])
    TTo("dve", lbT[:, :, 2, :], lbT[:, :, 2, :], lbT[:, :, 1, :], ALU.add, ["lbT"], ["lbT"])
    TTo("dve", lbT[:, :, 3, :], lbT[:, :, 3, :], lbT[:, :, 2, :], ALU.add, ["lbT"], ["lbT"])
    MS("dve", lbT[:, :, 0, :], 0.0, ["lbT"])
    TS("dve", omlT[:], lbT[:], -1.0, 1.0, ALU.mult, ALU.add, ["lbT"], ["omlT"])
    TS("dve", nomlT[:], omlT[:], -1.0, None, ALU.mult, None, ["omlT"], ["nomlT"])

    mem["p"] = phase_base
    wm = [sb("wm0", [128, 8, 512]), sb("wm1", [128, 8, 512])]
    it = 0
    for l in layers:
        for jb in list(range(12)) + [0]:
            w_ = wm[it % 2]
            wk = f"wm{it % 2}"
            P.dma("sp", w_[:], wview(wmod_d[LI[l], :, jb * 512:(jb + 1) * 512]), [], [wk])
            pb = PS[it % 2]
            pk = f"ps{it % 2}"
            for j in range(4):
                for k in range(8):
                    MM(pb[:, j * V:(j + 1) * V], w_[:, k, j * 128:(j + 1) * 128], scT[:, k, :], k == 0, k == 7,
                       [wk, "scT"], [pk])
            TTo("dve", modT[:, l, :, jb * 4:(jb + 1) * 4],
                pb[:, 0:4 * V].rearrange("p (j v) -> p v j", v=V),
                bmT[:, l, jb * 4:(jb + 1) * 4].unsqueeze(1).to_broadcast([128, V, 4]),
                ALU.add, [pk, "bmT"], ["modT"])
            it += 1
    P.barrier()

    def mslice(l, v, i):
        return modT[:, l, v, i * 8:(i + 1) * 8]

    def rms_rstd(src, srckey, junk, d):
        ACT(junk, src, AF.Square, [srckey], ["junk", "ssq"], accum=ssq[:])
        ACT(rt[:], ssq[:], AF.Sqrt, ["ssq"], ["rt"], scale=1.0 / d, bias=EPS)
        RCP(rstd[:], rt[:], ["rt"], ["rstd"])

    def xsrc(tile):
        if tile < 16:
            return X[:, tile, :], ("X", tile)
        return XC[:, tile - 16, :], ("XC", tile - 16)

    def build_G(dst, gcol, gtmp, dkey):
        for c in range(8):
            TS("dve", gtmp[:, c, :], ones_f[:], gcol[:, c:c + 1], None, ALU.mult, None, ["ones_f", "modT"], [("gtmp", c)])
            MM(PS[6 + c // 4][:, (c % 4) * 128:(c % 4 + 1) * 128], gtmp[:, c, :], ident_f[:], True, True,
               [("gtmp", c), "ident_f"], [f"ps{6 + c // 4}"])
        CP("act", dst[:, 0:512], PS[6][:, :], ["ps6"], [dkey])
        CP("act", dst[:, 512:1024], PS[7][:, :], ["ps7"], [dkey])

    def residual_add(tile, half, pbank, pkey, G, gkey, tmp, tmpkey):
        dst, dk = xsrc(tile)
        TTo("dve", tmp[:], pbank[:, 0:512], G[:, half * 512:(half + 1) * 512], ALU.mult, [pkey, gkey], [tmpkey])
        TTo("pool", dst[:, half * 512:(half + 1) * 512], dst[:, half * 512:(half + 1) * 512], tmp[:], ALU.add,
            [tmpkey, dk], [dk])

    def compute_hT(s, l, hT, xnb, junk):
        for vi, v in enumerate((s, S)):
            STT("dve", am[:, 0, vi, :], mslice(l, v, 1), 1.0, ngT[:, l, 0, :], ALU.add, ALU.mult, ["modT", "ngT"], ["am"])
        for tile in range(18):
            vi = 0 if tile < 16 else 1
            v = s if tile < 16 else S
            src, sk = xsrc(tile)
            rms_rstd(src, sk, junk[:], D)
            TS("dve", xnb[:], src, rstd[:, 0:1], None, ALU.mult, None, [sk, "rstd"], ["xnb"])
            pb = PSB[tile % 2]
            pk = f"ps{tile % 2}"
            for c in range(8):
                TR(pb[:, c * 128:(c + 1) * 128], xnb[:, c * 128:(c + 1) * 128], ident_b[:], ["xnb", "ident_b"], [pk])
            for c in range(8):
                dst = hT[:, c, tile * 128:(tile + 1) * 128]
                if False:
                    pass
                else:
                    TS("dve", dst, pb[:, c * 128:(c + 1) * 128], am[:, 0, vi, c:c + 1], mslice(l, v, 0)[:, c:c + 1],
                       ALU.mult, ALU.add, [pk, "am", "modT"], [("hT", tile)])

    def hkeys(t0, t1):
        return [("hT", t) for t in range(t0, t1)]

    def proj_fm(dst_fn, W, wkey, wcols, hT, evac):
        for tb in range(5):
            n = 512 if tb < 4 else 256
            c0 = tb * 512
            pb = PS[tb % 2]
            pk = f"ps{tb % 2}"
            for k in range(8):
                MM(pb[:, 0:n], W[:, k, wcols], hT[:, k, c0:c0 + n], k == 0, k == 7,
                   [wkey] + hkeys(c0 // 128, (c0 + n) // 128), [pk])
            evac(tb, c0, n, pb, pk)

    def even_mixer(s, l, with_ctx):
        e = l // 2
        lam_init = 0.8 - 0.6 * math.exp(-0.3 * l)
        mem["p"] = phase_base
        hT = sb("hT", [128, 8, TT], BF16)
        xnb = sb("xnb", [128, D], BF16)
        junk = sb("junk", [128, D], BF16)
        G1 = sb("G1", [128, D])
        G1c = sb("G1c", [128, D])
        gtmp = sb("gtmp", [128, 8, 128])
        tmpy = sb("tmpy", [128, 512])
        sg_bc = sb("sg_bc", [128, 128])
        lr = sb("lr", [1, 256])
        grp_base = mem["p"]
        build_G(G1, mslice(l, s, 2), gtmp, "G1")
        build_G(G1c, mslice(l, S, 2), gtmp, "G1c")
        compute_hT(s, l, hT, xnb, junk)
        P.dma("sp", lr[:], lam_d[e], [], ["lr"])
        TTo("dve", lr[0:1, 0:64], lr[0:1, 0:64], lr[0:1, 64:128], ALU.mult, ["lr"], ["lr"])
        TTo("dve", lr[0:1, 128:192], lr[0:1, 128:192], lr[0:1, 192:256], ALU.mult, ["lr"], ["lr"])
        ACT(lr[0:1, 64:128], lr[0:1, 0:64], AF.Identity, ["lr"], ["lr", "small"], accum=small[0:1, 0:1])
        ACT(lr[0:1, 192:256], lr[0:1, 128:192], AF.Identity, ["lr"], ["lr", "small"], accum=small[0:1, 1:2])
        ACT(small[0:1, 0:2], small[0:1, 0:2], AF.Exp, ["small"], ["small"])
        TTo("dve", small[0:1, 2:3], small[0:1, 1:2], small[0:1, 0:1], ALU.subtract, ["small"], ["small"])
        TS("dve", small[0:1, 3:4], small[0:1, 2:3], -lam_init, None, ALU.add, None, ["small"], ["small"])
        MM(PS[7][:, 0:1], ones_f[0:1, :], small[0:1, 3:4], True, True, ["ones_f", "small"], ["ps7"])
        CP("dve", neglam[:], PS[7][:, 0:1], ["ps7"], ["neglam"])
        P.dma("sp", sg_bc[:], subln_d[e], [], ["sg_bc"])
        TS("dve", sg_bc[:], sg_bc[:], 1.0 - lam_init, None, ALU.mult, None, ["sg_bc"], ["sg_bc"])

        ntile_out = 18 if with_ctx else 16

        def out_proj(oT, nch, Wo, wokey):
            for tile in range(ntile_out):
                G, gk = (G1, "G1") if tile < 16 else (G1c, "G1c")
                for half in range(2):
                    pb = PS[5 + half]
                    pk = f"ps{5 + half}"
                    for ci in range(nch):
                        MM(pb[:, 0:512], oT[:, ci, tile * 128:(tile + 1) * 128], Wo[:, ci, half * 512:(half + 1) * 512],
                           ci == 0, ci == nch - 1, [("oT", tile), wokey], [pk])
                    residual_add(tile, half, pb, pk, G, gk, tmpy, "tmpy")

        for g in range(DBG['nagroups'] if DBG['na'] else 0):
            P.barrier()
            mem["p"] = grp_base
            Wq = sb("Wq", [128, 8, 256], BF16)
            Wk = sb("Wk", [128, 8, 256], BF16)
            Wv = sb("Wv", [128, 8, 256], BF16)
            Wo = sb("Wo", [128, 2, D], BF16)
            Ball = sb("Ball", [128, 4, 16, 64], BF16)
            qaT = sb("qaT", [128, 2, TT], BF16)
            kaT = sb("kaT", [128, 2, TT], BF16)
            va = sb("va", [128, 18, 4, 65], BF16)
            oT = sb("oT", [128, 2, TT], BF16)
            Eb = [sb("E0", [128, 512], BF16), sb("E1", [128, 512], BF16)]
            rc = sb("rc", [128, 4])
            otile = sb("otile", [128, 256], BF16)
            P.dma("pool", Wq[:], wview(attw_d[e, :, g * 256:(g + 1) * 256]), [], ["Wq"])
            P.dma("pool", Wk[:], wview(attw_d[e, :, 1024 + g * 256:1024 + (g + 1) * 256]), [], ["Wk"])
            P.dma("pool", Wv[:], wview(attw_d[e, :, 1536 + g * 256:1536 + (g + 1) * 256]), [], ["Wv"])
            P.dma("pool", Wo[:], attwo_d[e, g * 256:(g + 1) * 256, :].rearrange("(c p) n -> p c n", p=128), [], ["Wo"])
            P.dma("pool", Ball[:], nab_d[e, :, 4 * g:4 * g + 4, :, :], [], ["Ball"])
            ACT(Ball[:], Ball[:], AF.Copy, ["Ball"], ["Ball"], scale=8.0)
            MS("pool", va[:, :, :, 64:65], 1.0, ["va1"])
            for ci in range(2):
                def ev_q(tb, c0, n, pb, pk, ci=ci):
                    CP("act", qaT[:, ci, c0:c0 + n], pb[:, 0:n], [pk], [("qaT", ci, tb)])

                def ev_k(tb, c0, n, pb, pk, ci=ci):
                    CP("dve", kaT[:, ci, c0:c0 + n], pb[:, 0:n], [pk], [("kaT", ci, tb)])
                proj_fm(None, Wq, "Wq", slice(ci * 128, (ci + 1) * 128), hT, ev_q)
                proj_fm(None, Wk, "Wk", slice(ci * 128, (ci + 1) * 128), hT, ev_k)
            for tile in range(18):
                pb = PS[tile % 2]
                pk = f"ps{tile % 2}"
                for k in range(8):
                    MM(pb[:, 0:256], hT[:, k, tile * 128:(tile + 1) * 128], Wv[:, k, :], k == 0, k == 7,
                       ["Wv", ("hT", tile)], [pk])
                CP("act" if tile % 2 else "dve", va[:, tile, :, 0:64], pb[:, 0:256].rearrange("p (h d) -> p h d", d=64),
                   [pk], [("va", tile)])
            if g == 0 and DBG.get('dump'):
                dump(9, modT[:, l, s, :], ["modT"], 48)
                dump(10, am[:, :, :, :].rearrange("p a b c -> p (a b c)"), ["am"], 32)
                dump(11, bmT[:, l, :], ["bmT"], 48)
                dump(0, hT[:, 0, 0:512], hkeys(0, 4), 512)
                dump(1, G1[:, 0:512], ["G1"], 512)
                dump(2, qaT[:, 0, 0:512], [("qaT", 0, 0)], 512)
                dump(3, kaT[:, 0, 0:512], [("kaT", 0, 0)], 512)
                dump(4, va[:, 0, :, :].rearrange("p h d -> p (h d)"), [("va", 0), "va1"], 260)
                dump(5, Ball[:, 0, 3, :], ["Ball"], 64)
            qkeys = lambda ci: [("qaT", ci, tb) for tb in range(5)]
            kkeys = lambda ci: [("kaT", ci, tb) for tb in range(5)]
            na_steps = []
            for qr in range(DBG['nqr']):
                rs = min(max(qr - 4, 0), 24)
                t0 = rs // 2
                tiles = list(range(t0, t0 + (4 if rs % 2 == 0 else 5)))
                for hh in range(4):
                    na_steps.append((qr, hh, rs, tiles))

            def na_S(n):
                qr, hh, rs, tiles = na_steps[n]
                ci, hf = hh // 2, hh % 2
                pr = slice(hf * 64, (hf + 1) * 64)
                sbk = PS[n % 2]
                sk = f"ps{n % 2}"
                q_ap = qaT[pr, ci, qr * 64:(qr + 1) * 64]
                slots = []
                for si, tl in enumerate(tiles):
                    idx = []
                    for kr in (2 * tl, 2 * tl + 1):
                        idx.append(kr - qr + 7 if rs <= kr < rs + 8 else 15)
                    so = sbk[:, si * 64:(si + 1) * 64]
                    MM(so, kaT[pr, ci, tl * 128:(tl + 1) * 128], q_ap, True, False, qkeys(ci) + kkeys(ci), [sk])
                    MM(so, ident_lo[:, :], Ball[:, hh, idx[0], :], False, False, ["ident_b", "Ball"], [sk])
                    MM(so, ident_hi[:, :], Ball[:, hh, idx[1], :], False, True, ["ident_b", "Ball"], [sk])
                    slots.append(tl)
                for cj in range(2):
                    si = len(tiles) + cj
                    MM(sbk[:, si * 64:(si + 1) * 64], kaT[pr, ci, T + cj * 128:T + (cj + 1) * 128], q_ap, True, True,
                       qkeys(ci) + kkeys(ci), [sk])
                    slots.append(16 + cj)
                ns = len(slots)
                ACT(Eb[n % 2][:, 0:ns * 64], sbk[:, 0:ns * 64], AF.Exp, [sk], [f"E{n % 2}"], scale=0.125)
                return slots

            def na_PV(n, slots):
                qr, hh, rs, tiles = na_steps[n]
                ob = PS[2 + qr % 2]
                ok = f"ps{2 + qr % 2}"
                E_ = Eb[n % 2]
                ek = f"E{n % 2}"
                ns = len(slots)
                for si, tl in enumerate(slots):
                    MM(ob[0:64, hh * 65:(hh + 1) * 65], E_[:, si * 64:(si + 1) * 64], va[:, tl, hh, :], si == 0, si == ns - 1,
                       [ek, ("va", tl), "va1"], [ok])
                if hh == 3:
                    ov = ob[0:64, 0:260].rearrange("p (h d) -> p h d", d=65)
                    RCP(rc[0:64, :].unsqueeze(2), ov[:, :, 64:65], [ok], ["rc"])
                    TTo("dve", otile[0:64, :].rearrange("p (h d) -> p h d", d=64), ov[:, :, 0:64],
                        rc[0:64, :].unsqueeze(2).to_broadcast([64, 4, 64]), ALU.mult, [ok, "rc"], ["otile"])
                    for ci in range(2):
                        TR(PSB[4][:, ci * 64:(ci + 1) * 64], otile[0:64, ci * 128:(ci + 1) * 128], ident_b[0:64, 0:64],
                           ["otile", "ident_b"], ["ps4"])
                    for ci in range(2):
                        CP("dve", oT[:, ci, qr * 64:(qr + 1) * 64], PSB[4][:, ci * 64:(ci + 1) * 64], ["ps4"], [("oT", qr // 2)])

            pend_slots = {}
            if na_steps:
                pend_slots[0] = na_S(0)
            for n in range(len(na_steps)):
                if n + 1 < len(na_steps):
                    pend_slots[n + 1] = na_S(n + 1)
                na_PV(n, pend_slots.pop(n))
            if with_ctx:
                for hh in range(4):
                    ci, hf = hh // 2, hh % 2
                    pr = slice(hf * 64, (hf + 1) * 64)
                    sbk = PS[hh % 2]
                    sk = f"ps{hh % 2}"
                    for cj in range(2):
                        MM(sbk[:, cj * 256:(cj + 1) * 256], kaT[pr, ci, T + cj * 128:T + (cj + 1) * 128], qaT[pr, ci, T:TT],
                           True, True, qkeys(ci) + kkeys(ci), [sk])
                    E_ = Eb[hh % 2]
                    ek = f"E{hh % 2}"
                    ACT(E_[:, 0:512], sbk[:, 0:512], AF.Exp, [sk], [ek], scale=0.125)
                    for qt in range(2):
                        for cj in range(2):
                            MM(PS[2 + qt][:, hh * 65:(hh + 1) * 65], E_[:, cj * 256 + qt * 128:cj * 256 + (qt + 1) * 128],
                               va[:, 16 + cj, hh, :], cj == 0, cj == 1, [ek, ("va", 16 + cj), "va1"], [f"ps{2 + qt}"])
                for qt in range(2):
                    ob = PS[2 + qt]
                    ok = f"ps{2 + qt}"
                    ov = ob[:, 0:260].rearrange("p (h d) -> p h d", d=65)
                    RCP(rc[:, :].unsqueeze(2), ov[:, :, 64:65], [ok], ["rc"])
                    TTo("dve", otile[:, :].rearrange("p (h d) -> p h d", d=64), ov[:, :, 0:64],
                        rc[:, :].unsqueeze(2).to_broadcast([128, 4, 64]), ALU.mult, [ok, "rc"], ["otile"])
                    for ci in range(2):
                        TR(PSB[4][:, ci * 128:(ci + 1) * 128], otile[:, ci * 128:(ci + 1) * 128], ident_b[:],
                           ["otile", "ident_b"], ["ps4"])
                    CP("act", oT[:, :, T + qt * 128:T + (qt + 1) * 128], PSB[4][:, 0:256].rearrange("p (c t) -> p c t", t=128),
                       ["ps4"], [("oT", 16 + qt)])
            if g == 0 and DBG.get('dump'):
                dump(6, oT[:, 0, 0:512], [("oT", t) for t in range(4)], 512)
                dump(7, Eb[0][:, 0:512], ["E0"], 512)
                dump(8, otile[:, :], ["otile"], 256)
            out_proj(oT, 2, Wo, "Wo")

        for hb in range(4 if DBG['diff'] else 0):
            P.barrier()
            mem["p"] = grp_base
            W5 = sb("W5", [128, 8, 5, 128], BF16)
            Wo = sb("Wo1", [128, 1, D], BF16)
            cosT = sb("cosT", [128, T], BF16)
            sinT = sb("sinT", [128, T], BF16)
            qbT = sb("qbT", [128, TT], BF16)
            kbT = sb("kbT", [128, TT], BF16)
            vb = sb("vb", [128, 18, 129], BF16)
            oT = sb("oTb", [128, 1, TT], BF16)
            Eb = [sb(f"E{i}", [128, 512], BF16) for i in range(4)]
            SBK = [0, 1, 6, 7]
            t1 = sb("t1", [128, 512])
            t2 = sb("t2", [128, 512])
            dd = sb("dd", [128, 128])
            obt = sb("obt", [128, 128], BF16)
            r12 = sb("r12", [128, 4])
            cols = [512 + hb * 128, 3072 + hb * 128, 2048 + hb * 128, 3584 + hb * 128, 2560 + hb * 128]
            for i, c0 in enumerate(cols):
                P.dma("pool", W5[:, :, i, :], wview(attw_d[e, :, c0:c0 + 128]), [], [("W5", i)])
            P.dma("pool", Wo[:, 0, :], attwo_d[e, 512 + hb * 128:512 + (hb + 1) * 128, :], [], ["Wo1"])
            P.dma("pool", cosT[:], cos_d, [], ["cosT"])
            P.dma("pool", sinT[:], sin_d, [], ["sinT"])
            MS("pool", vb[:, :, 128:129], 1.0, ["vb1"])
            for (dstT, dname, i_raw, i_sw) in ((qbT, "qbT", 0, 1), (kbT, "kbT", 2, 3)):
                for tb in range(5):
                    n = 512 if tb < 4 else 256
                    c0 = tb * 512
                    hk = hkeys(c0 // 128, (c0 + n) // 128)
                    for k in range(8):
                        MM(PS[0][:, 0:n], W5[:, k, i_raw, :], hT[:, k, c0:c0 + n], k == 0, k == 7, [("W5", i_raw)] + hk, ["ps0"])
                    if tb < 4:
                        for k in range(8):
                            MM(PS[1][:, 0:n], W5[:, k, i_sw, :], hT[:, k, c0:c0 + n], k == 0, k == 7, [("W5", i_sw)] + hk, ["ps1"])
                        TTo("dve", t1[:], PS[0][:, 0:n], cosT[:, c0:c0 + n], ALU.mult, ["ps0", "cosT"], ["t1"])
                        TTo("dve", t2[:], PS[1][:, 0:n], sinT[:, c0:c0 + n], ALU.mult, ["ps1", "sinT"], ["t2"])
                        TTo("pool", dstT[:, c0:c0 + n], t1[:], t2[:], ALU.add, ["t1", "t2"], [(dname, tb)])
                    else:
                        CP("act", dstT[:, c0:c0 + n], PS[0][:, 0:n], ["ps0"], [(dname, tb)])
            for tile in range(18):
                pb = PS[tile % 2]
                pk = f"ps{tile % 2}"
                for k in range(8):
                    MM(pb[:, 0:128], hT[:, k, tile * 128:(tile + 1) * 128], W5[:, k, 4, :], k == 0, k == 7,
                       [("W5", 4), ("hT", tile)], [pk])
                CP("act" if tile % 2 else "dve", vb[:, tile, 0:128], pb[:, 0:128], [pk], [("vb", tile)])
            qk_all = [("qbT", tb) for tb in range(5)] + [("kbT", tb) for tb in range(5)]

            def diff_block(qc0, nq, kts):
                nsub = nq // 128
                acc = {}
                for sub in range(nsub):
                    for m in range(2):
                        a = sub * 2 + m
                        acc[(sub, m)] = (PS[2 + a // 3][:, (a % 3) * 129:(a % 3 + 1) * 129], f"ps{2 + a // 3}")
                for bnk in sorted(set(2 + (sub * 2 + m) // 3 for sub in range(nsub) for m in range(2))):
                    MS("dve", PS[bnk][:, :], 0.0, [f"ps{bnk}"])
                steps = [(ki, kt, m) for ki, kt in enumerate(kts) for m in range(2)]

                def emit_S(i):
                    ki, kt, m = steps[i]
                    pr = slice(m * 64, (m + 1) * 64)
                    sbk = PS[SBK[i % 4]]
                    sk = f"ps{SBK[i % 4]}"
                    E_ = Eb[i % 4]
                    ek = f"E{i % 4}"
                    MM(sbk[:, 0:nq], kbT[pr, kt * 128:(kt + 1) * 128], qbT[pr, qc0:qc0 + nq], True, True, qk_all, [sk])
                    ACT(E_[:, 0:nq], sbk[:, 0:nq], AF.Exp, [sk], [ek], scale=0.125)

                def emit_PV(i):
                    ki, kt, m = steps[i]
                    E_ = Eb[i % 4]
                    ek = f"E{i % 4}"
                    for sub in range(nsub):
                        ap, akey = acc[(sub, m)]
                        MM(ap, E_[:, sub * 128:(sub + 1) * 128], vb[:, kt, :], False, ki == len(kts) - 1,
                           [ek, ("vb", kt), "vb1"], [akey])

                LA = 2
                for i in range(len(steps) + LA):
                    if i < len(steps):
                        emit_S(i)
                    if i - LA >= 0:
                        emit_PV(i - LA)
                for sub in range(nsub):
                    (o1, k1), (o2, k2) = acc[(sub, 0)], acc[(sub, 1)]
                    RCP(r12[:, 0:1], o1[:, 128:129], [k1], ["r12"])
                    RCP(r12[:, 1:2], o2[:, 128:129], [k2], ["r12"])
                    TTo("dve", r12[:, 2:3], r12[:, 1:2], neglam[:, 0:1], ALU.mult, ["r12", "neglam"], ["r12"])
                    TS("dve", t1[:, 0:128], o1[:, 0:128], r12[:, 0:1], None, ALU.mult, None, [k1, "r12"], ["t1"])
                    STT("dve", dd[:], o2[:, 0:128], r12[:, 2:3], t1[:, 0:128], ALU.mult, ALU.add, [k2, "r12", "t1"], ["dd"])
                    rms_rstd(dd[:], "dd", t2[:, 0:128], 128)
                    STT("dve", obt[:], dd[:], rstd[:, 0:1], sg_bc[:], ALU.mult, ALU.mult, ["dd", "rstd", "sg_bc"], ["obt"])
                    TR(PSB[5][:, 0:128], obt[:], ident_b[:], ["obt", "ident_b"], ["ps5"])
                    tcol = qc0 + sub * 128
                    CP("act", oT[:, 0, tcol:tcol + 128], PSB[5][:, 0:128], ["ps5"], [("oT", tcol // 128)])

            for qb_ in range(4):
                diff_block(qb_ * 512, 512, list(range(18)))
            if with_ctx:
                diff_block(T, 256, [16, 17])
            out_proj(oT, 1, Wo, "Wo1")
        P.barrier()

    def odd_mixer(s, l, with_ctx):
        o = l // 2
        mem["p"] = phase_base
        hT = sb("hT", [128, 8, TT], BF16)
        G1 = sb("G1", [128, D])
        G1c = sb("G1c", [128, D])
        tmpy = sb("tmpy", [128, 512])
        tmp_mark = mem["p"]
        xnb = sb("xnb", [128, D], BF16)
        junk = sb("junk", [128, D], BF16)
        gtmp = sb("gtmp", [128, 8, 128])
        build_G(G1, mslice(l, s, 2), gtmp, "G1")
        build_G(G1c, mslice(l, S, 2), gtmp, "G1c")
        compute_hT(s, l, hT, xnb, junk)
        P.barrier()
        mem["p"] = tmp_mark
        W5 = sb("W5", [128, 8, 5, 128], BF16)
        Wo = sb("Wo", [128, D], BF16)
        A = sb("A", [128, TT])
        B = sb("B", [128, TT])
        kk = sb("kk", [128, TT], BF16)
        sq = sb("sq", [128, TT], BF16)
        sgt = sb("sgt", [128, TT], BF16)
        qt_ = sb("qt", [128, TT], BF16)
        qh = sb("qh", [128, TT], BF16)
        kt_ = sb("kt", [128, TT], BF16)
        vv = sb("vv", [128, 18, 128], BF16)
        oTs = sb("oTs", [128, TT])
        tot = sb("tot", [128, 36])
        Ee = sb("Ee", [128, 36])
        Ep = sb("Ep", [128, 36])
        ATm2 = [sb("ATm0", [128, 128], BF16), sb("ATm1", [128, 128], BF16)]
        ktok2 = [sb("ktok0", [128, 128], BF16), sb("ktok1", [128, 128], BF16)]
        rmask = sb("rmask", [128, TT], BF16)
        MS("pool", rmask[:], 1.0, ["rmask"])
        MS("pool", rmask[:].rearrange("p (c k) -> p c k", k=64)[:, :, 0:1], 0.0, ["rmask"])
        R32 = [sb("R32a", [128, 128]), sb("R32b", [128, 128])]
        Rb = [sb("Rba", [128, 128], BF16), sb("Rbb", [128, 128], BF16)]
        ntile_out = 18 if with_ctx else 16
        NCH = 36
        for hd in range(8):
            P.barrier()
            cols = [hd * 128, 1024 + hd * 128, 2048 + hd * 128, 3072 + hd * 128, 4096 + hd * 128]
            for i, c0 in enumerate(cols):
                P.dma("pool", W5[:, :, i, :], wview(recw_d[o, :, c0:c0 + 128]), [], [("W5", i)])
            P.dma("pool", Wo[:], recwo_d[o, hd * 128:(hd + 1) * 128, :], [], ["Wo"])

            def ev_q(tb, c0, n, pb, pk):
                ACT(sq[:, c0:c0 + n], pb[:, 0:n], AF.Silu, [pk], ["sq"])

            def ev_g(tb, c0, n, pb, pk):
                ACT(sgt[:, c0:c0 + n], pb[:, 0:n], AF.Silu, [pk], ["sgt"])
            proj_fm(None, W5[:, :, 0, :], ("W5", 0), slice(0, 128), hT, ev_q)
            proj_fm(None, W5[:, :, 4, :], ("W5", 4), slice(0, 128), hT, ev_g)
            for tile in range(18):
                pb = PS[tile % 2]
                pk = f"ps{tile % 2}"
                for k in range(8):
                    MM(pb[:, 0:128], hT[:, k, tile * 128:(tile + 1) * 128], W5[:, k, 3, :], k == 0, k == 7,
                       [("W5", 3), ("hT", tile)], [pk])
                CP("dve", vv[:, tile, :], pb[:, 0:128], [pk], [("vv", tile)])
            vkeys = [("vv", t) for t in range(18)]
            for dr in range(2):
                lbc = lbT[:, dr, l, hd:hd + 1]
                omc = omlT[:, dr, l, hd:hd + 1]
                nomc = nomlT[:, dr, l, hd:hd + 1]

                def ev_f(tb, c0, n, pb, pk):
                    ACT(A[:, c0:c0 + n], pb[:, 0:n], AF.Sigmoid, [pk], ["A"])
                proj_fm(None, W5[:, :, 1 + dr, :], ("W5", 1 + dr), slice(0, 128), hT, ev_f)
                ACT(B[:], A[:], AF.Ln, ["A", "lbT", "omlT"], ["B"], scale=omc, bias=lbc)
                TS("dve", kk[:], A[:], nomc, omc, ALU.mult, ALU.add, ["A", "nomlT", "omlT"], ["kk"])
                P.op("dve", lambda E: E.tensor_tensor_scan(out=A[:], data0=rmask[:], data1=B[:], initial=0.0,
                                                            op0=ALU.mult, op1=ALU.add), ["B", "rmask", "kk"], ["A"])
                Av = A[:].rearrange("p (c k) -> p c k", k=64)
                Bv = B[:].rearrange("p (c k) -> p c k", k=64)
                CP("dve", tot[:].unsqueeze(2), Av[:, :, 63:64], ["A"], ["tot"])
                if dr == 1:
                    TTo("dve", A[:], B[:], A[:], ALU.subtract, ["A", "B"], ["A"])
                    TTo("dve", Av, Av, tot[:].unsqueeze(2).to_broadcast([128, NCH, 64]), ALU.add, ["A", "tot"], ["A"])
                ACT(Ee[:], tot[:], AF.Exp, ["tot"], ["Ee"])
                MS("dve", Ep[:], 0.0, ["Ep"])
                if dr == 0:
                    CP("dve", Ep[:, 1:32], Ee[:, 0:31], ["Ee"], ["Ep"])
                    CP("dve", Ep[:, 0:1], Ee[:, 35:36], ["Ee"], ["Ep"])
                    CP("dve", Ep[:, 33:36], Ee[:, 32:35], ["Ee"], ["Ep"])
                    order = [32, 33, 34, 35] + list(range(32))
                    msk = maskf
                else:
                    CP("dve", Ep[:, 0:31], Ee[:, 1:32], ["Ee"], ["Ep"])
                    CP("dve", Ep[:, 31:32], Ee[:, 32:33], ["Ee"], ["Ep"])
                    CP("dve", Ep[:, 32:35], Ee[:, 33:36], ["Ee"], ["Ep"])
                    order = [35, 34, 33, 32] + list(range(31, -1, -1))
                    msk = maskb
                ACT(qt_[:], A[:], AF.Exp, ["A"], ["qt"])
                ACT(kt_[:], A[:], AF.Exp, ["A"], ["kt"], scale=-1.0)
                TTo("pool", qt_[:], qt_[:], sq[:], ALU.mult, ["qt", "sq"], ["qt"])
                TTo("dve", kt_[:], kt_[:], kk[:], ALU.mult, ["kt", "kk"], ["kt"])
                TTo("pool", qh[:].rearrange("p (c k) -> p c k", k=64), qt_[:].rearrange("p (c k) -> p c k", k=64),
                    Ep[:].unsqueeze(2).to_broadcast([128, NCH, 64]), ALU.mult, ["qt", "Ep"], ["qh"])
                if hd == 0 and dr == 0 and DBG.get('dump'):
                    dump(0, B[:, 0:512], ["B"], 512)
                    dump(1, A[:, 0:512], ["A"], 512)
                    dump(2, kk[:, 0:512], ["kk"], 512)
                    dump(3, qt_[:, 0:512], ["qt"], 512)
                    dump(4, kt_[:, 0:512], ["kt"], 512)
                    dump(5, Ee[:, :], ["Ee"], 36)
                    dump(6, Ep[:, :], ["Ep"], 36)
                    dump(7, tot[:, :], ["tot"], 36)
                    dump(8, lbT[:].rearrange("p a b c -> p (a b c)"), ["lbT"], 64)
                    dump(9, vv[:, 0:4, :].rearrange("p a b -> p (a b)"), vkeys, 512)
                st_ = {"pp": 0, "first": True, "ucnt": 0}
                ABK = [2, 0]
                TBK = [3, 1]
                UBK = [6, 7]

                def stageA(ti):
                    tl = order[2 * ti] // 2
                    tc = slice(tl * 128, (tl + 1) * 128)
                    i2 = ti % 2
                    ab, tb_ = ABK[i2], TBK[i2]
                    MM(PS[ab][:, 0:128], kt_[:, tc], qt_[:, tc], True, True, ["kt", "qt"], [f"ps{ab}"])
                    TTo("dve", ATm2[i2][:], PS[ab][:, 0:128], msk[:], ALU.mult, [f"ps{ab}", "maskf", "maskb"], [("ATm", i2)])
                    TR(PSB[tb_][:, 0:128], kt_[:, tc], ident_b[:], ["kt", "ident_b"], [f"ps{tb_}"])
                    CP("act", ktok2[i2][:], PSB[tb_][:, 0:128], [f"ps{tb_}"], [("ktok", i2)])

                def stageB(ti):
                    ch_a, ch_b = order[2 * ti], order[2 * ti + 1]
                    tl = ch_a // 2
                    assert ch_b // 2 == tl
                    tc = slice(tl * 128, (tl + 1) * 128)
                    i2 = ti % 2
                    ob = PS[4 + ti % 2]
                    ok = f"ps{4 + ti % 2}"
                    chs = [ch_a, ch_b]
                    n_inter = sum(1 for ch in chs if not (st_["first"] and ch == chs[0]))
                    MM(ob[:, 0:128], vv[:, tl, :], ATm2[i2][:], True, n_inter == 0, [("ATm", i2)] + vkeys, [ok])
                    done = 0
                    for ch in chs:
                        hf = ch % 2
                        pp = st_["pp"]
                        ub = UBK[st_["ucnt"] % 2]
                        st_["ucnt"] += 1
                        uk = f"ps{ub}"
                        MM(PS[ub][:, 0:128], ktok2[i2][hf * 64:(hf + 1) * 64, :], vv[hf * 64:(hf + 1) * 64, tl, :], True, True,
                           [("ktok", i2)] + vkeys, [uk])
                        if not st_["first"]:
                            done += 1
                            MM(ob[:, hf * 64:(hf + 1) * 64], Rb[pp][:], qh[:, ch * 64:(ch + 1) * 64], False, done == n_inter,
                               [("Rb", pp), "qh"], [ok])
                        if st_["first"]:
                            CP("dve", Rb[pp][:], PS[ub][:, 0:128], [uk], [("Rb", pp)])
                            CP("dve", R32[pp][:], PS[ub][:, 0:128], [uk], [("R32", pp)])
                            st_["first"] = False
                        else:
                            STT("dve", Rb[1 - pp][:], R32[pp][:], Ep[:, ch:ch + 1], PS[ub][:, 0:128], ALU.mult, ALU.add,
                                [("R32", pp), "Ep", uk], [("Rb", 1 - pp)])
                            STT("dve", R32[1 - pp][:], R32[pp][:], Ep[:, ch:ch + 1], PS[ub][:, 0:128], ALU.mult, ALU.add,
                                [("R32", pp), "Ep", uk], [("R32", 1 - pp)])
                            st_["pp"] = 1 - pp
                    if dr == 0:
                        CP("act", oTs[:, tc], ob[:, 0:128], [ok], [("oTs", tl)])
                    else:
                        TTo("dve", oTs[:, tc], oTs[:, tc], ob[:, 0:128], ALU.add, [ok, ("oTs", tl)], [("oTs", tl)])

                stageA(0)
                for ti in range(18):
                    if ti + 1 < 18:
                        stageA(ti + 1)
                    stageB(ti)
            if hd == 0 and DBG.get('dump'):
                dump(10, oTs[:, 0:512], [("oTs", t) for t in range(4)], 512)
                dump(11, oTs[:, T:TT], [("oTs", t) for t in (16, 17)], 256)
                dump(12, R32[0][:], [("R32", 0)], 128)
            oTh = kk
            for tb in range(5):
                n = 512 if tb < 4 else 256
                c0 = tb * 512
                ok_ = [("oTs", t) for t in range(c0 // 128, (c0 + n) // 128)]
                ACT(A[:, c0:c0 + n], oTs[:, c0:c0 + n], AF.Square, ok_, ["A"])
                MM(PS[7][:, 0:n], ones_f[:], A[:, c0:c0 + n], True, True, ["ones_f", "A"], ["ps7"])
                ACT(B[:, c0:c0 + n], PS[7][:, 0:n], AF.Sqrt, ["ps7"], ["B"], scale=1.0 / 128, bias=EPS)
                RCP(B[:, c0:c0 + n], B[:, c0:c0 + n], ["B"], ["B"])
                TTo("dve", A[:, c0:c0 + n], oTs[:, c0:c0 + n], B[:, c0:c0 + n], ALU.mult, ok_ + ["B", "A"], ["A"])
                STT("dve", oTh[:, c0:c0 + n], A[:, c0:c0 + n], gnT[:, o:o + 1], sgt[:, c0:c0 + n], ALU.mult, ALU.mult,
                    ["A", "gnT", "sgt"], ["kk"])
            for tile in range(ntile_out):
                G, gk = (G1, "G1") if tile < 16 else (G1c, "G1c")
                for half in range(2):
                    pb = PS[half]
                    pk = f"ps{half}"
                    MM(pb[:, 0:512], oTh[:, tile * 128:(tile + 1) * 128], Wo[:, half * 512:(half + 1) * 512], True, True,
                       ["kk", "Wo"], [pk])
                    residual_add(tile, half, pb, pk, G, gk, tmpy, "tmpy")
        P.barrier()

    def moe(s, l, with_ctx):
        mem["p"] = phase_base
        ntl = 18 if with_ctx else 16
        NW = 288 if with_ctx else 256
        njt = 3 if with_ctx else 2
        hn = sb("hn", [128, 18, D], BF16)
        G2 = sb("G2", [128, D])
        G2c = sb("G2c", [128, D])
        posm_b = sb("posm_b", [16, TT], BF16)
        w_b = sb("w_b", [16, TT], BF16)
        pos_tok = sb("pos_tok", [128, 18, NE])
        wr = sb("wr", [128, 8, NE])
        selT = sb("selT", [16, 128], BF16)
        loop_base = mem["p"]
        gtmp = sb("gtmp", [128, 8, 128])
        xn32 = sb("xn32", [128, D])
        junk = sb("junk", [128, D], BF16)
        hT32 = sb("hT32", [128, 8, 128])
        afft = sb("afft", [128, NE])
        affT = sb("affT", [16, TT])
        work = sb("work", [16, TT])
        msk = sb("msk", [16, TT])
        pos = sb("pos", [16, TT])
        mx = sb("mx", [16, 8])
        build_G(G2, mslice(l, s, 5), gtmp, "G2")
        build_G(G2c, mslice(l, S, 5), gtmp, "G2c")
        P.dma("sp", wr[:], wview(router_d[LI[l]]), [], ["wr"])
        for vi, v in enumerate((s, S)):
            STT("dve", am[:, 1, vi, :], mslice(l, v, 4), 1.0, ngT[:, l, 1, :], ALU.add, ALU.mult, ["modT", "ngT"], ["am"])
        for tile in range(ntl):
            vi = 0 if tile < 16 else 1
            v = s if tile < 16 else S
            src, sk = xsrc(tile)
            rms_rstd(src, sk, junk[:], D)
            TS("dve", xn32[:], src, rstd[:, 0:1], None, ALU.mult, None, [sk, "rstd"], ["xn32"])
            CP("act" if DBG.get("nopool") else "pool", hn[:, tile, :], xn32[:], ["xn32"], [("hn", tile)])
            if DBG.get('sub', 9) < 1:
                continue
            pbk = f"ps{tile % 2}"
            for c in range(8):
                TR(PSB[tile % 2][:, c * 128:(c + 1) * 128], hn[:, tile, c * 128:(c + 1) * 128], ident_b[:],
                   [("hn", tile), "ident_b"], [pbk])
            for c in range(8):
                if DBG.get('noevac'):
                    continue
                src_p = PSB[tile % 2][:, c * 128:(c + 1) * 128]
                if c % 2 == 0:
                    TS("dve", hT32[:, c, :], src_p, am[:, 1, vi, c:c + 1], mslice(l, v, 3)[:, c:c + 1], ALU.mult, ALU.add,
                       [pbk, "am", "modT"], [("hT32", c)])
                    continue
                if c % 2 == 0:
                    ACT(hT32[:, c, :], src_p, AF.Identity, [pbk, "am", "modT"], [("hT32", c)],
                        scale=am[:, 1, vi, c:c + 1], bias=mslice(l, v, 3)[:, c:c + 1])
                else:
                    TS("dve", hT32[:, c, :], src_p, am[:, 1, vi, c:c + 1], mslice(l, v, 3)[:, c:c + 1], ALU.mult, ALU.add,
                       [pbk, "am", "modT"], [("hT32", c)])
            if DBG.get('sub', 9) < 2:
                continue
            for c in range(8):
                MM(PS[2][:, 0:NE], hT32[:, c, :], wr[:, c, :], c == 0, c == 7, [("hT32", c), "wr"], ["ps2"])
            if DBG.get('sub', 9) < 3:
                continue
            ACT(afft[:], PS[2][:, 0:NE], AF.Exp, ["ps2"], ["afft", "ssq"], accum=ssq[:])
            RCP(rt[:], ssq[:], ["ssq"], ["rt"])
            TS("dve", afft[:], afft[:], rt[:, 0:1], None, ALU.mult, None, ["afft", "rt"], ["afft"])
            if not DBG.get("notr"):
                TR(PS[3][0:16, 0:128], afft[:], ident_f[:], ["afft", "ident_f"], ["ps3"])
                CP("act", affT[0:16, tile * 128:(tile + 1) * 128], PS[3][0:16, 0:128], ["ps3"], ["affT"])

        if DBG.get('stage', 9) < 1:
            P.barrier()
            return

        def route(c0, n, cap):
            CP("dve", work[:, c0:c0 + n], affT[:, c0:c0 + n], ["affT"], ["work"])
            nit = cap // 8
            for it_ in range(nit):
                P.op("dve", (lambda a, b: (lambda E: E.max(out=a, in_=b)))(mx[:], work[:, c0:c0 + n]), ["work"], ["mx"])
                if it_ < nit - 1:
                    P.op("dve", (lambda a, b, c_: (lambda E: E.match_replace(out=a, in_to_replace=b, in_values=c_, imm_value=-1.0)))(
                        work[:, c0:c0 + n], mx[:], work[:, c0:c0 + n]), ["work", "mx"], ["work"])
            TS("dve", msk[:, c0:c0 + n], affT[:, c0:c0 + n], mx[:, 7:8], None, ALU.is_ge, None, ["affT", "mx"], ["msk"])
            MS("dve", work[:, c0:c0 + n], 1.0, ["work"])
            P.op("dve", (lambda a, b, c_: (lambda E: E.tensor_tensor_scan(out=a, data0=b, data1=c_, initial=0.0, op0=ALU.mult, op1=ALU.add)))(
                pos[:, c0:c0 + n], work[:, c0:c0 + n], msk[:, c0:c0 + n]), ["work", "msk"], ["pos"])
            TTo("dve", pos[:, c0:c0 + n], pos[:, c0:c0 + n], msk[:, c0:c0 + n], ALU.mult, ["pos", "msk"], ["pos"])
            TS("dve", pos[:, c0:c0 + n], pos[:, c0:c0 + n], -1.0, None, ALU.add, None, ["pos"], ["pos"])
            CP("dve", posm_b[:, c0:c0 + n], pos[:, c0:c0 + n], ["pos"], ["posm_b"])
            TTo("dve", w_b[:, c0:c0 + n], affT[:, c0:c0 + n], msk[:, c0:c0 + n], ALU.mult, ["affT", "msk"], ["w_b"])

        route(0, T, 256)
        if with_ctx:
            route(T, L, 32)
        if DBG.get('stage', 9) < 2:
            P.barrier()
            return
        for tile in range(ntl):
            TR(PS[3][:, 0:NE], pos[0:16, tile * 128:(tile + 1) * 128], ident_f[0:16, 0:16], ["pos", "ident_f"], ["ps3"])
            CP("act", pos_tok[:, tile, :], PS[3][:, 0:NE], ["ps3"], ["pos_tok"])
        P.barrier()
        mem["p"] = loop_base
        Pe = sb("Pe", [128, 16, 256], BF16)
        Pce = sb("Pce", [128, 2, 32], BF16)
        PT = sb("PT", [128, 2, T], BF16)
        PTc = sb("PTc", [32, L], BF16)
        xsel = sb("xsel", [128, 8, 288], BF16)
        hid = [sb("hid0", [128, 288], BF16), sb("hid1", [128, 288], BF16)]
        sgl = sb("sgl", [128, 288])
        yy = sb("yy", [128, 3, D], BF16)
        wsb = sb("wsb", [128, 512])
        Wg = [sb("Wg0", [128, 8, 256], BF16), sb("Wg1", [128, 8, 256], BF16)]
        Wu = [sb("Wu0", [128, 8, 256], BF16), sb("Wu1", [128, 8, 256], BF16)]
        Wd = [sb("Wd0", [128, 2, D], BF16), sb("Wd1", [128, 2, D], BF16)]
        hnk = [("hn", t) for t in range(ntl)]
        wcnt = 0
        for e in range(DBG['experts']):
            TS("dve", selT[:], ones_f[0:16, :], ident_f[0:16, e:e + 1], None, ALU.mult, None, ["ones_f", "ident_f"], ["selT"])
            TTo("dve", Pe[:], iota_j[:].unsqueeze(1).to_broadcast([128, 16, 256]),
                pos_tok[:, 0:16, e:e + 1].to_broadcast([128, 16, 256]), ALU.is_equal, ["iota_j", "pos_tok"], ["Pe"])
            if with_ctx:
                TTo("dve", Pce[:], iota_j[:, 0:32].unsqueeze(1).to_broadcast([128, 2, 32]),
                    pos_tok[:, 16:18, e:e + 1].to_broadcast([128, 2, 32]), ALU.is_equal, ["iota_j", "pos_tok"], ["Pce"])
            for blk in range(4):
                bc = slice(blk * 512, (blk + 1) * 512)
                MM(PS[6][:, 0:512], selT[0:16, :], posm_b[0:16, bc], True, True, ["selT", "posm_b"], ["ps6"])
                MM(PS[7][:, 0:512], selT[0:16, :], w_b[0:16, bc], True, True, ["selT", "w_b"], ["ps7"])
                CP("act", wsb[:], PS[7][:, 0:512], ["ps7"], ["wsb"])
                for jt in range(2):
                    STT("dve", PT[:, jt, bc], PS[6][:, 0:512], iota_p[:, jt:jt + 1], wsb[:], ALU.is_equal, ALU.mult,
                        ["ps6", "iota_p", "wsb"], ["PT"])
            if with_ctx:
                MM(PS[6][0:32, 0:L], selT[0:16, 0:32], posm_b[0:16, T:TT], True, True, ["selT", "posm_b"], ["ps6"])
                MM(PS[7][0:32, 0:L], selT[0:16, 0:32], w_b[0:16, T:TT], True, True, ["selT", "w_b"], ["ps7"])
                CP("act", wsb[0:32, 0:L], PS[7][0:32, 0:L], ["ps7"], ["wsb"])
                STT("dve", PTc[:], PS[6][0:32, 0:L], iota_p[0:32, 0:1], wsb[0:32, 0:L], ALU.is_equal, ALU.mult,
                    ["ps6", "iota_p", "wsb"], ["PTc"])
            for c in range(8):
                pb = PS[6 + c % 2]
                pk = f"ps{6 + c % 2}"
                for tile in range(16):
                    MM(pb[:, 0:256], hn[:, tile, c * 128:(c + 1) * 128], Pe[:, tile, :], tile == 0, tile == 15, ["Pe"] + hnk, [pk])
                if with_ctx:
                    for ct in range(2):
                        MM(pb[:, 256:288], hn[:, 16 + ct, c * 128:(c + 1) * 128], Pce[:, ct, :], ct == 0, ct == 1, ["Pce"] + hnk, [pk])
                TS("dve", xsel[:, c, 0:256], pb[:, 0:256], am[:, 1, 0, c:c + 1], mslice(l, s, 3)[:, c:c + 1], ALU.mult, ALU.add,
                   [pk, "am", "modT"], [("xsel", c)])
                if with_ctx:
                    TS("dve", xsel[:, c, 256:288], pb[:, 256:288], am[:, 1, 1, c:c + 1], mslice(l, S, 3)[:, c:c + 1], ALU.mult, ALU.add,
                       [pk, "am", "modT"], [("xsel", c)])
            xk = [("xsel", c) for c in range(8)]
            pend = None

            def down(fc, wi, f2):
                for jt in range(njt):
                    rows = 128 if jt < 2 else 32
                    for half in range(2):
                        b = jt * 2 + half
                        MM(PS[b][0:rows, 0:512], hid[fc % 2][:, jt * 128:jt * 128 + rows], Wd[wi][:, f2, half * 512:(half + 1) * 512],
                           fc == 0, fc == 15, [f"hid{fc % 2}", f"Wd{wi}"], [f"ps{b}"])

            for fb in range(8):
                wi = wcnt % 2
                wcnt += 1
                P.dma("pool", Wg[wi][:], wview(wg_d[LI[l], e, :, fb * 256:(fb + 1) * 256]), [], [f"Wg{wi}"])
                P.dma("pool", Wu[wi][:], wview(wu_d[LI[l], e, :, fb * 256:(fb + 1) * 256]), [], [f"Wu{wi}"])
                P.dma("pool", Wd[wi][:], wd_d[LI[l], e, fb * 256:(fb + 1) * 256, :].rearrange("(c p) n -> p c n", p=128), [], [f"Wd{wi}"])
                for f2 in range(2):
                    fc = fb * 2 + f2
                    for c in range(8):
                        MM(PS[6][:, 0:NW], Wg[wi][:, c, f2 * 128:(f2 + 1) * 128], xsel[:, c, 0:NW], c == 0, c == 7, [f"Wg{wi}"] + xk, ["ps6"])
                    for c in range(8):
                        MM(PS[7][:, 0:NW], Wu[wi][:, c, f2 * 128:(f2 + 1) * 128], xsel[:, c, 0:NW], c == 0, c == 7, [f"Wu{wi}"] + xk, ["ps7"])
                    ACT(sgl[:, 0:NW], PS[6][:, 0:NW], AF.Silu, ["ps6"], ["sgl"])
                    TTo("dve", hid[fc % 2][:, 0:NW], sgl[:, 0:NW], PS[7][:, 0:NW], ALU.mult, ["sgl", "ps7"], [f"hid{fc % 2}"])
                    if pend is not None:
                        down(*pend)
                    pend = (fc, wi, f2)
            down(*pend)
            for jt in range(njt):
                rows = 128 if jt < 2 else 32
                G = G2 if jt < 2 else G2c
                gk = "G2" if jt < 2 else "G2c"
                for half in range(2):
                    b = jt * 2 + half
                    TTo("dve", yy[0:rows, jt, half * 512:(half + 1) * 512], PS[b][0:rows, 0:512], G[0:rows, half * 512:(half + 1) * 512],
                        ALU.mult, [f"ps{b}", gk], [("yy", jt)])
            for tile in range(16):
                for half in range(2):
                    b = (tile % 3) * 2 + half
                    for jt in range(2):
                        MM(PS[b][:, 0:512], PT[:, jt, tile * 128:(tile + 1) * 128], yy[:, jt, half * 512:(half + 1) * 512], jt == 0, jt == 1,
                           ["PT", ("yy", jt)], [f"ps{b}"])
                    TTo("dve", X[:, tile, half * 512:(half + 1) * 512], X[:, tile, half * 512:(half + 1) * 512], PS[b][:, 0:512], ALU.add,
                        [f"ps{b}", ("X", tile)], [("X", tile)])
            if with_ctx:
                for ct in range(2):
                    for half in range(2):
                        b = ct * 2 + half
                        MM(PS[b][:, 0:512], PTc[0:32, ct * 128:(ct + 1) * 128], yy[0:32, 2, half * 512:(half + 1) * 512], True, True,
                           ["PTc", ("yy", 2)], [f"ps{b}"])
                        TTo("dve", XC[:, ct, half * 512:(half + 1) * 512], XC[:, ct, half * 512:(half + 1) * 512], PS[b][:, 0:512], ALU.add,
                            [f"ps{b}", ("XC", ct)], [("XC", ct)])
        P.barrier()

    for s in range(S):
        for tile in range(16):
            P.dma("sp", X[:, tile, :], x_d[s, tile * 128:(tile + 1) * 128, :], [], [("X", tile)])
        for ct in range(2):
            P.dma("sp", XC[:, ct, :], ctx_d[s, ct * 128:(ct + 1) * 128, :], [], [("XC", ct)])
        for l in layers:
            last = (l == 3)
            if DBG['mix']:
                if l % 2 == 0:
                    even_mixer(s, l, not last)
                else:
                    odd_mixer(s, l, not last)
            if DBG['moe']:
                moe(s, l, not last)
        P.barrier()
        mem["p"] = phase_base
        fg = sb("fg", [128, D])
        junk = sb("junk", [128, D], BF16)
        ot = [sb("ot0", [128, D]), sb("ot1", [128, D])]
        if final:
            P.dma("sp", fg[:], fg_d, [], ["fg"])
        for tile in range(16):
            src, sk = xsrc(tile)
            o_ = ot[tile % 2]
            okey = f"ot{tile % 2}"
            if final:
                rms_rstd(src, sk, junk[:], D)
                STT("dve", o_[:], src, rstd[:, 0:1], fg[:], ALU.mult, ALU.mult, [sk, "rstd", "fg"], [okey])
            else:
                CP("dve", o_[:], src, [sk], [okey])
            P.dma("sp", out_d[s, tile * 128:(tile + 1) * 128, :], o_[:], [okey], [])
        if debug_ctx:
            for ct in range(2):
                P.dma("sp", outc_d[s, ct * 128:(ct + 1) * 128, :], XC[:, ct, :], [("XC", ct)], [])
        P.barrier()
    P.wait_dmas("sp")
    P.run()
    return nc, P


def _consts():
    c = {}
    c["c_ident"] = np.eye(128, dtype=np.float32)
    lo = np.eye(128, dtype=np.float32); lo[64:, :] = 0
    hi = np.eye(128, dtype=np.float32); hi[:64, :] = 0
    c["c_ident_lo"] = lo
    c["c_ident_hi"] = hi
    c["c_iota_j"] = np.broadcast_to(np.arange(256, dtype=np.float32)[None, :], (128, 256)).copy()
    p = np.arange(128, dtype=np.float32)
    c["c_iota_p"] = np.stack([p, p + 128, p, p], axis=1).copy()
    s_ = np.arange(128)[:, None]
    t_ = np.arange(128)[None, :]
    same = (s_ // 64) == (t_ // 64)
    c["c_maskf"] = (same & (s_ <= t_)).astype(np.float32)
    c["c_maskb"] = (same & (s_ >= t_)).astype(np.float32)
    t = np.arange(T)
    rows = (t // 64).astype(np.float32)
    colsf = (t % 64).astype(np.float32)
    inv = (10000.0 ** (-np.arange(16, dtype=np.float32) / 16)).astype(np.float32)
    ang = np.concatenate([rows[:, None] * inv, colsf[:, None] * inv], axis=-1).astype(np.float32)
    cos = np.cos(ang).astype(np.float32).T
    sin = np.sin(ang).astype(np.float32).T
    cos64 = np.concatenate([cos, cos], axis=0)
    sin64 = np.concatenate([-sin, sin], axis=0)
    c["rope_cos"] = np.concatenate([cos64, cos64], axis=0).astype(np.float32)
    c["rope_sin"] = np.concatenate([sin64, sin64], axis=0).astype(np.float32)
    return c


def _swap_perm(base):
    idx = []
    for m in range(8):
        o = base + m * 64
        idx += list(range(o + 32, o + 64)) + list(range(o, o + 32))
    return np.array(idx)


def _na_bias_table(rpb):
    kc = np.arange(64)[:, None]
    qc = np.arange(64)[None, :]
    cstart = np.clip(qc - 8, 0, 48)
    valid = (kc >= cstart) & (kc < cstart + 16)
    dcol = np.clip(kc - qc + 15, 0, 30)
    tab = np.full((64, 8, 16, 64), -3750.0, dtype=np.float32)
    g = rpb[:, :, dcol]
    g = np.transpose(g, (2, 0, 1, 3))
    vm = np.broadcast_to(valid[:, None, None, :], g.shape)
    tab[:, :, 0:15, :] = np.where(vm, g, np.float32(-3750.0))
    return np.concatenate([tab, tab], axis=0)


def prep_shared(inp, layers=(0, 1, 2, 3)):
    layers = list(layers) if len(layers) else [0]
    f = lambda a: np.ascontiguousarray(np.asarray(a, dtype=np.float32))
    d = dict(_consts())
    d["w_mod"] = f(np.asarray(inp["w_mod"])[layers])
    d["b_modT"] = f(np.transpose(np.asarray(inp["b_mod"]).reshape(4, 48, 128), (2, 0, 1)))
    d["norm_gT"] = f(np.transpose(np.asarray(inp["norm_g"]).reshape(4, 2, 8, 128), (3, 0, 1, 2)))
    w_in = np.asarray(inp["att_w_in"])
    d["att_w"] = f(np.concatenate([w_in, w_in[:, :, _swap_perm(512)], w_in[:, :, _swap_perm(2048)]], axis=2))
    d["att_wo"] = f(inp["att_w_out"])
    d["na_bias"] = f(np.stack([_na_bias_table(np.asarray(inp["na_rpb"])[e]) for e in range(2)], axis=0))
    d["lam_row"] = f(np.asarray(inp["diff_lambda"]).reshape(2, 1, 256))
    d["subln_bc"] = f(np.broadcast_to(np.asarray(inp["diff_subln_g"])[:, None, :], (2, 128, 128)))
    d["rec_w_in"] = f(inp["rec_w_in"])
    d["rec_wo"] = f(inp["rec_w_out"])
    d["rec_lbT"] = f(np.transpose(np.asarray(inp["rec_lb_logits"]).reshape(2, 4, 8, 128), (3, 0, 1, 2)))
    d["rec_gn"] = f(np.asarray(inp["rec_gnorm_g"]).T)
    d["router"] = f(np.asarray(inp["moe_router"])[layers])
    nex = max(1, DBG["experts"])
    d["wg"] = f(np.asarray(inp["moe_w_gate"])[layers][:, :nex])
    d["wu"] = f(np.asarray(inp["moe_w_up"])[layers][:, :nex])
    d["wd"] = f(np.asarray(inp["moe_w_down"])[layers][:, :nex])
    d["finalg_bc"] = f(np.broadcast_to(np.asarray(inp["final_g"])[None, :], (128, D)))
    return d


def prep_core(inp, samples):
    f = lambda a: np.ascontiguousarray(np.asarray(a, dtype=np.float32))
    x = np.asarray(inp["x"])
    ctx = np.asarray(inp["ctx"])
    c = np.asarray(inp["c"])
    cv = np.concatenate([c[samples], np.asarray(inp["c_ctx"])[None, :]], axis=0)
    V = cv.shape[0]
    return {
        "x": f(x[samples]),
        "ctx": f(ctx[samples]),
        "cvT": f(np.transpose(cv.reshape(V, 8, 128), (2, 1, 0))),
    }


_CACHE = {}


def kernel(**inputs):
    n_cores = 8
    S = 2
    if "nc" not in _CACHE:
        _CACHE["nc"] = build(S, [0, 1, 2, 3], final=True)[0]
    nc = _CACHE["nc"]
    shared = prep_shared(inputs)
    in_maps = []
    for i in range(n_cores):
        m = dict(shared)
        m.update(prep_core(inputs, list(range(i * S, (i + 1) * S))))
        in_maps.append(m)
    res = run_bass_kernel_spmd(nc, in_maps, core_ids=list(range(n_cores)))
    return np.concatenate([r["out"] for r in res.results], axis=0).astype(np.float32)
```

```python
import math
import numpy as np
import concourse.bass as bass
import concourse.mybir as mybir
from concourse.bass_utils import run_bass_kernel_spmd

F32 = mybir.dt.float32
BF16 = mybir.dt.bfloat16
AF = mybir.ActivationFunctionType
ALU = mybir.AluOpType

EPOCH = 28800


class Stream:
    def __init__(self, prog, name):
        self.prog = prog
        self.name = name
        self.sems = []
        self.n = 0

    def sem_for(self, e):
        while len(self.sems) <= e:
            self.sems.append(self.prog.nc.alloc_semaphore(f"{self.name}_{len(self.sems)}"))
        return self.sems[e]

    def bump(self, inc):
        e = self.n // EPOCH
        assert (self.n + inc - 1) // EPOCH == e
        self.n += inc
        return self.sem_for(e), (self, self.n)

    def loc(self, n):
        e = (n - 1) // EPOCH
        return self.sem_for(e), n - e * EPOCH


class Prog:
    ENGS = ("pe", "act", "dve", "pool", "sp")

    def __init__(self, nc, n_dma_sems=4):
        self.nc = nc
        self.q = {e: [] for e in self.ENGS}
        self.stream = {e: Stream(self, "s_" + e) for e in self.ENGS}
        self.seen = {e: {} for e in self.ENGS}
        self.last_w = {}
        self.readers = {}
        self.dma_streams = {}
        self.dma_rr = {}
        self.n_dma_sems = n_dma_sems
        self.ninst = 0

    def _deps(self, reads, writes):
        deps = []
        for k in reads:
            t = self.last_w.get(k)
            if t is not None:
                deps.append(t)
        for k in writes:
            t = self.last_w.get(k)
            if t is not None:
                deps.append(t)
            deps.extend(self.readers.get(k, ()))
        return deps

    def _commit(self, tok, reads, writes):
        for k in writes:
            self.last_w[k] = tok
            self.readers[k] = []
        for k in reads:
            if k in writes:
                continue
            self.readers.setdefault(k, []).append(tok)

    def _waits(self, eng, deps, skip_self=False):
        seen = self.seen[eng]
        best = {}
        for (st, n) in deps:
            if skip_self and st is self.stream[eng]:
                continue
            if seen.get(st, 0) >= n:
                continue
            if best.get(st, 0) < n:
                best[st] = n
        waits = []
        for st, n in best.items():
            seen[st] = n
            waits.append(st.loc(n))
        return waits

    def op(self, eng, fn, reads=(), writes=()):
        reads = tuple(reads)
        writes = tuple(writes) + tuple(k for k in reads if isinstance(k, str) and k.startswith("ps") and k[2:].isdigit())
        deps = self._deps(reads, writes)
        waits = self._waits(eng, deps, skip_self=(eng == "pe"))
        sem, tok = self.stream[eng].bump(1)

        def emit(E, waits=waits, fn=fn, sem=sem):
            for (s, v) in waits:
                E.wait_ge(s, v)
            fn(E).then_inc(sem, 1)

        self.q[eng].append(emit)
        self._commit(tok, reads, writes)
        self.ninst += 1
        return tok

    def dma(self, queue, out, in_, reads=(), writes=()):
        reads = tuple(reads)
        writes = tuple(writes)
        deps = self._deps(reads, writes)
        if queue not in self.dma_streams:
            self.dma_streams[queue] = [Stream(self, f"d_{queue}{i}") for i in range(self.n_dma_sems)]
            self.dma_rr[queue] = 0
        i = self.dma_rr[queue]
        self.dma_rr[queue] = (i + 1) % self.n_dma_sems
        st = self.dma_streams[queue][i]
        if st.n > 0:
            deps.append((st, st.n))
        waits = self._waits(queue, deps)
        sem, tok = st.bump(16)

        def emit(E, waits=waits, sem=sem, out=out, in_=in_):
            for (s, v) in waits:
                E.wait_ge(s, v)
            E.dma_start(out=out, in_=in_).then_inc(sem, 16)

        self.q[queue].append(emit)
        self._commit(tok, reads, writes)
        self.ninst += 1
        return tok

    def all_tokens(self):
        toks = []
        for e in self.ENGS:
            if self.stream[e].n:
                toks.append((self.stream[e], self.stream[e].n))
        for q, sts in self.dma_streams.items():
            for st in sts:
                if st.n:
                    toks.append((st, st.n))
        return toks

    def barrier(self):
        toks = self.all_tokens()
        for eng in self.ENGS:
            waits = self._waits(eng, toks)

            def emit(E, waits=waits):
                for (s, v) in waits:
                    E.wait_ge(s, v)

            self.q[eng].append(emit)

    def wait_dmas(self, eng):
        toks = [(st, st.n) for q, sts in self.dma_streams.items() for st in sts if st.n]
        waits = self._waits(eng, toks)

        def emit(E, waits=waits):
            for (s, v) in waits:
                E.wait_ge(s, v)

        self.q[eng].append(emit)

    def run(self):
        nc = self.nc
        with nc.Block() as block:
            @block.tensor
            def _(E):
                for f in self.q["pe"]:
                    f(E)

            @block.scalar
            def _(E):
                for f in self.q["act"]:
                    f(E)

            @block.vector
            def _(E):
                for f in self.q["dve"]:
                    f(E)

            @block.gpsimd
            def _(E):
                for f in self.q["pool"]:
                    f(E)

            @block.sync
            def _(E):
                for f in self.q["sp"]:
                    f(E)


DBG = {'na': 1, 'diff': 1, 'mix': 1, 'moe': 1, 'experts': 16, 'nagroups': 2, 'nqr': 32}
D = 1024
T = 2048
L = 256
TT = T + L
EPS = 1e-6
NE = 16
FF = 2048


def build(S, layers, final=True, debug_ctx=False):
    nc = bass.Bass("TRN2", target_bir_lowering=False)
    P = Prog(nc)
    V = S + 1

    def din(name, shape):
        return nc.dram_tensor(name, list(shape), F32, kind="ExternalInput").ap()

    x_d = din("x", [S, T, D])
    ctx_d = din("ctx", [S, L, D])
    cvT_d = din("cvT", [128, 8, V])
    NL = max(1, len(layers))
    LI = {l: i for i, l in enumerate(layers)}
    wmod_d = din("w_mod", [NL, D, 6 * D])
    bmT_d = din("b_modT", [128, 4, 48])
    ngT_d = din("norm_gT", [128, 4, 2, 8])
    attw_d = din("att_w", [2, D, 4096])
    attwo_d = din("att_wo", [2, D, D])
    nab_d = din("na_bias", [2, 128, 8, 16, 64])
    lam_d = din("lam_row", [2, 1, 256])
    subln_d = din("subln_bc", [2, 128, 128])
    cos_d = din("rope_cos", [128, T])
    sin_d = din("rope_sin", [128, T])
    recw_d = din("rec_w_in", [2, D, 5 * D])
    recwo_d = din("rec_wo", [2, D, D])
    lbT_d = din("rec_lbT", [128, 2, 4, 8])
    gn_d = din("rec_gn", [128, 2])
    router_d = din("router", [NL, D, NE])
    NEX = max(1, DBG["experts"])
    wg_d = din("wg", [NL, NEX, D, FF])
    wu_d = din("wu", [NL, NEX, D, FF])
    wd_d = din("wd", [NL, NEX, FF, D])
    fg_d = din("finalg_bc", [128, D])
    cid_d = din("c_ident", [128, 128])
    cij_d = din("c_iota_j", [128, 256])
    cip_d = din("c_iota_p", [128, 4])
    cmf_d = din("c_maskf", [128, 128])
    cmb_d = din("c_maskb", [128, 128])
    cil_d = din("c_ident_lo", [128, 128])
    cih_d = din("c_ident_hi", [128, 128])
    out_d = nc.dram_tensor("out", [S, T, D], F32, kind="ExternalOutput").ap()
    outc_d = nc.dram_tensor("out_ctx", [S, L, D], F32, kind="ExternalOutput").ap() if debug_ctx else None
    dbg_d = nc.dram_tensor("dbg", [16, 128, 512], F32, kind="ExternalOutput").ap() if debug_ctx else None

    base = (nc.sbuf_base + 63) // 64 * 64
    lim = nc.sbuf_top
    mem = {"p": base, "n": 0}

    def sb(name, shape, dt=F32):
        nb = int(np.prod(shape[1:])) * (2 if dt == BF16 else 4)
        nb = (nb + 63) // 64 * 64
        off = mem["p"]
        assert off + nb <= lim, f"SBUF overflow at {name}: {off + nb} > {lim}"
        mem["p"] = off + nb
        mem["n"] += 1
        return nc.alloc_sbuf_tensor_at(f"{name}_{mem['n']}", list(shape), dt, offset=off)

    PS = [nc.alloc_psum_tensor(f"ps{i}", [128, 512], F32) for i in range(8)]
    PSB = [p[:].bitcast(BF16) for p in PS]

    def MM(out, lhsT, rhs, st, sp, r, w):
        P.op("pe", lambda E: E.matmul(out, lhsT=lhsT, rhs=rhs, start=st, stop=sp), r, w)

    def TR(out, in_, idn, r, w):
        P.op("pe", lambda E: E.transpose(out=out, in_=in_, identity=idn), r, w)

    def ACT(out, in_, func, r, w, scale=None, bias=None, accum=None):
        kw = {}
        if scale is not None:
            kw["scale"] = scale
        if bias is not None:
            kw["bias"] = bias
        if accum is not None:
            kw["accum_out"] = accum
        P.op("act", lambda E: E.activation(out=out, in_=in_, func=func, **kw), r, w)

    def TS(eng, out, in0, s1, s2, op0, op1, r, w):
        if s2 is None:
            P.op(eng, lambda E: E.tensor_scalar(out=out, in0=in0, scalar1=s1, scalar2=None, op0=op0), r, w)
        else:
            P.op(eng, lambda E: E.tensor_scalar(out=out, in0=in0, scalar1=s1, scalar2=s2, op0=op0, op1=op1), r, w)

    def TTo(eng, out, in0, in1, op, r, w):
        P.op(eng, lambda E: E.tensor_tensor(out=out, in0=in0, in1=in1, op=op), r, w)

    def STT(eng, out, in0, sc, in1, op0, op1, r, w):
        P.op(eng, lambda E: E.scalar_tensor_tensor(out=out, in0=in0, scalar=sc, in1=in1, op0=op0, op1=op1), r, w)

    def CP(eng, out, in_, r, w):
        if eng == "act":
            P.op("act", lambda E: E.copy(out=out, in_=in_), r, w)
        else:
            P.op(eng, lambda E: E.tensor_copy(out=out, in_=in_), r, w)

    def MS(eng, ap, v, w):
        P.op(eng, lambda E: E.memset(ap, v), [], w)

    dbgbuf = {}

    def dump(i, ap, keys, n):
        if dbg_d is None:
            return
        if "t" not in dbgbuf:
            dbgbuf["t"] = nc.alloc_sbuf_tensor_at("dbgt", [128, 512], F32, offset=(lim - 4096) // 64 * 64)
        t = dbgbuf["t"]
        rows = ap.shape[0]
        P.op("dve", lambda E: E.memset(t[:], 0.0), [], ["dbgt"])
        P.op("dve", lambda E: E.tensor_copy(out=t[0:rows, 0:n], in_=ap), keys, ["dbgt"])
        P.dma("sp", dbg_d[i], t[:], ["dbgt"], [])

    def RCP(out, in_, r, w):
        P.op("dve", lambda E: E.reciprocal(out=out, in_=in_), r, w)

    def wview(src2d):
        return src2d.rearrange("(k p) n -> p k n", p=128)

    X = sb("X", [128, 16, D])
    XC = sb("XC", [128, 2, D])
    ident_f = sb("ident_f", [128, 128])
    ident_b = sb("ident_b", [128, 128], BF16)
    ones_f = sb("ones_f", [128, 128])
    ident_lo = sb("ident_lo", [128, 128], BF16)
    ident_hi = sb("ident_hi", [128, 128], BF16)
    iota_j = sb("iota_j", [128, 256])
    iota_p = sb("iota_p", [128, 4])
    maskf = sb("maskf", [128, 128])
    maskb = sb("maskb", [128, 128])
    modT = sb("modT", [128, 4, V, 48])
    bmT = sb("bmT", [128, 4, 48])
    ngT = sb("ngT", [128, 4, 2, 8])
    scT = sb("scT", [128, 8, V])
    lbT = sb("lbT", [128, 2, 4, 8])
    omlT = sb("omlT", [128, 2, 4, 8])
    nomlT = sb("nomlT", [128, 2, 4, 8])
    gnT = sb("gnT", [128, 2])
    am = sb("am", [128, 2, 2, 8])
    ssq = sb("ssq", [128, 1])
    rt = sb("rt", [128, 1])
    rstd = sb("rstd", [128, 1])
    neglam = sb("neglam", [128, 1])
    small = sb("small", [128, 16])
    lsum = sb("lsum", [128, 2, 8])
    phase_base = mem["p"]

    P.dma("sp", ident_f[:], cid_d, [], ["ident_f"])
    P.dma("pool", ident_b[:], cid_d, [], ["ident_b"])
    P.dma("pool", ident_lo[:], cil_d, [], ["ident_b"])
    P.dma("pool", ident_hi[:], cih_d, [], ["ident_b"])
    P.dma("sp", iota_j[:], cij_d, [], ["iota_j"])
    P.dma("sp", iota_p[:], cip_d, [], ["iota_p"])
    P.dma("sp", maskf[:], cmf_d, [], ["maskf"])
    P.dma("sp", maskb[:], cmb_d, [], ["maskb"])
    P.dma("sp", bmT[:], bmT_d, [], ["bmT"])
    P.dma("sp", ngT[:], ngT_d, [], ["ngT"])
    P.dma("sp", scT[:], cvT_d, [], ["scT"])
    P.dma("sp", lbT[:], lbT_d, [], ["lbT"])
    P.dma("sp", gnT[:], gn_d, [], ["gnT"])
    MS("pool", ones_f[:], 1.0, ["ones_f"])
    ACT(scT[:], scT[:], AF.Silu, ["scT"], ["scT"])

    ACT(lbT[:], lbT[:], AF.Exp, ["lbT"], ["lbT"])
    TTo("dve", lsum[:], lbT[:, :, 0, :], lbT[:, :, 1, :], ALU.add, ["lbT"], ["lsum"])
    TTo("dve", lsum[:], lsum[:], lbT[:, :, 2, :], ALU.add, ["lbT", "lsum"], ["lsum"])
    TTo("dve", lsum[:], lsum[:], lbT[:, :, 3, :], ALU.add, ["lbT", "lsum"], ["lsum"])
    RCP(lsum[:], lsum[:], ["lsum"], ["lsum"])
    for j in range(4):
        TTo("dve", lbT[:, :, j, :], lbT[:, :, j, :], lsum[:], ALU.mult, ["lbT", "lsum"], ["lbT"])
    TTo("dve", lbT[:, :, 2, :], lbT[:, :, 2, :], lbT[:, :, 1, :], ALU.add, ["lbT"], ["lbT"])
    TTo("dve", lbT[:, :, 3, :], lbT[:, :, 3, :], lbT[:, :, 2, :], ALU.add, ["lbT"], ["lbT"])
    MS("dve", lbT[:, :, 0, :], 0.0, ["lbT"])
    TS("dve", omlT[:], lbT[:], -1.0, 1.0, ALU.mult, ALU.add, ["lbT"], ["omlT"])
    TS("dve", nomlT[:], omlT[:], -1.0, None, ALU.mult, None, ["omlT"], ["nomlT"])

    mem["p"] = phase_base
    wm = [sb("wm0", [128, 8, 512]), sb("wm1", [128, 8, 512])]
    it = 0
    for l in layers:
        for jb in list(range(12)) + [0]:
            w_ = wm[it % 2]
            wk = f"wm{it % 2}"
            P.dma("sp", w_[:], wview(wmod_d[LI[l], :, jb * 512:(jb + 1) * 512]), [], [wk])
            pb = PS[it % 2]
            pk = f"ps{it % 2}"
            for j in range(4):
                for k in range(8):
                    MM(pb[:, j * V:(j + 1) * V], w_[:, k, j * 128:(j + 1) * 128], scT[:, k, :], k == 0, k == 7,
                       [wk, "scT"], [pk])
            TTo("dve", modT[:, l, :, jb * 4:(jb + 1) * 4],
                pb[:, 0:4 * V].rearrange("p (j v) -> p v j", v=V),
                bmT[:, l, jb * 4:(jb + 1) * 4].unsqueeze(1).to_broadcast([128, V, 4]),
                ALU.add, [pk, "bmT"], ["modT"])
            it += 1
    P.barrier()

    def mslice(l, v, i):
        return modT[:, l, v, i * 8:(i + 1) * 8]

    def rms_rstd(src, srckey, junk, d):
        ACT(junk, src, AF.Square, [srckey], ["junk", "ssq"], accum=ssq[:])
        ACT(rt[:], ssq[:], AF.Sqrt, ["ssq"], ["rt"], scale=1.0 / d, bias=EPS)
        RCP(rstd[:], rt[:], ["rt"], ["rstd"])

    def xsrc(tile):
        if tile < 16:
            return X[:, tile, :], ("X", tile)
        return XC[:, tile - 16, :], ("XC", tile - 16)

    def build_G(dst, gcol, gtmp, dkey):
        for c in range(8):
            TS("dve", gtmp[:, c, :], ones_f[:], gcol[:, c:c + 1], None, ALU.mult, None, ["ones_f", "modT"], [("gtmp", c)])
            MM(PS[6 + c // 4][:, (c % 4) * 128:(c % 4 + 1) * 128], gtmp[:, c, :], ident_f[:], True, True,
               [("gtmp", c), "ident_f"], [f"ps{6 + c // 4}"])
        CP("act", dst[:, 0:512], PS[6][:, :], ["ps6"], [dkey])
        CP("act", dst[:, 512:1024], PS[7][:, :], ["ps7"], [dkey])

    def residual_add(tile, half, pbank, pkey, G, gkey, tmp, tmpkey):
        dst, dk = xsrc(tile)
        TTo("dve", tmp[:], pbank[:, 0:512], G[:, half * 512:(half + 1) * 512], ALU.mult, [pkey, gkey], [tmpkey])
        TTo("pool", dst[:, half * 512:(half + 1) * 512], dst[:, half * 512:(half + 1) * 512], tmp[:], ALU.add,
            [tmpkey, dk], [dk])

    def residual_add_direct(tile, half, pbank, pkey):
        dst, dk = xsrc(tile)
        TTo("dve", dst[:, half * 512:(half + 1) * 512], dst[:, half * 512:(half + 1) * 512], pbank[:, 0:512], ALU.add,
            [pkey, dk], [dk])

    def compute_hT(s, l, hT, xnb, junk):
        for vi, v in enumerate((s, S)):
            STT("dve", am[:, 0, vi, :], mslice(l, v, 1), 1.0, ngT[:, l, 0, :], ALU.add, ALU.mult, ["modT", "ngT"], ["am"])
        for tile in range(18):
            vi = 0 if tile < 16 else 1
            v = s if tile < 16 else S
            src, sk = xsrc(tile)
            rms_rstd(src, sk, junk[:], D)
            TS("dve", xnb[:], src, rstd[:, 0:1], None, ALU.mult, None, [sk, "rstd"], ["xnb"])
            pb = PSB[tile % 2]
            pk = f"ps{tile % 2}"
            for c in range(8):
                TR(pb[:, c * 128:(c + 1) * 128], xnb[:, c * 128:(c + 1) * 128], ident_b[:], ["xnb", "ident_b"], [pk])
            for c in range(8):
                dst = hT[:, c, tile * 128:(tile + 1) * 128]
                if False:
                    pass
                else:
                    TS("dve", dst, pb[:, c * 128:(c + 1) * 128], am[:, 0, vi, c:c + 1], mslice(l, v, 0)[:, c:c + 1],
                       ALU.mult, ALU.add, [pk, "am", "modT"], [("hT", tile)])

    def hkeys(t0, t1):
        return [("hT", t) for t in range(t0, t1)]

    def proj_fm(dst_fn, W, wkey, wcols, hT, evac):
        for tb in range(5):
            n = 512 if tb < 4 else 256
            c0 = tb * 512
            pb = PS[tb % 2]
            pk = f"ps{tb % 2}"
            for k in range(8):
                MM(pb[:, 0:n], W[:, k, wcols], hT[:, k, c0:c0 + n], k == 0, k == 7,
                   [wkey] + hkeys(c0 // 128, (c0 + n) // 128), [pk])
            evac(tb, c0, n, pb, pk)

    def even_mixer(s, l, with_ctx):
        e = l // 2
        lam_init = 0.8 - 0.6 * math.exp(-0.3 * l)
        mem["p"] = phase_base
        hT = sb("hT", [128, 8, TT], BF16)
        xnb = sb("xnb", [128, D], BF16)
        junk = sb("junk", [128, D], BF16)
        G1 = sb("G1", [128, D])
        G1c = sb("G1c", [128, D])
        gtmp = sb("gtmp", [128, 8, 128])
        tmpy = sb("tmpy", [128, 512])
        sg_bc = sb("sg_bc", [128, 128])
        lr = sb("lr", [1, 256])
        grp_base = mem["p"]
        build_G(G1, mslice(l, s, 2), gtmp, "G1")
        build_G(G1c, mslice(l, S, 2), gtmp, "G1c")
        compute_hT(s, l, hT, xnb, junk)
        P.dma("sp", lr[:], lam_d[e], [], ["lr"])
        TTo("dve", lr[0:1, 0:64], lr[0:1, 0:64], lr[0:1, 64:128], ALU.mult, ["lr"], ["lr"])
        TTo("dve", lr[0:1, 128:192], lr[0:1, 128:192], lr[0:1, 192:256], ALU.mult, ["lr"], ["lr"])
        ACT(lr[0:1, 64:128], lr[0:1, 0:64], AF.Identity, ["lr"], ["lr", "small"], accum=small[0:1, 0:1])
        ACT(lr[0:1, 192:256], lr[0:1, 128:192], AF.Identity, ["lr"], ["lr", "small"], accum=small[0:1, 1:2])
        ACT(small[0:1, 0:2], small[0:1, 0:2], AF.Exp, ["small"], ["small"])
        TTo("dve", small[0:1, 2:3], small[0:1, 1:2], small[0:1, 0:1], ALU.subtract, ["small"], ["small"])
        TS("dve", small[0:1, 3:4], small[0:1, 2:3], -lam_init, None, ALU.add, None, ["small"], ["small"])
        MM(PS[7][:, 0:1], ones_f[0:1, :], small[0:1, 3:4], True, True, ["ones_f", "small"], ["ps7"])
        CP("dve", neglam[:], PS[7][:, 0:1], ["ps7"], ["neglam"])
        P.dma("sp", sg_bc[:], subln_d[e], [], ["sg_bc"])
        TS("dve", sg_bc[:], sg_bc[:], 1.0 - lam_init, None, ALU.mult, None, ["sg_bc"], ["sg_bc"])

        ntile_out = 18 if with_ctx else 16

        def out_proj(oT, nch, Wo, wokey):
            WoL = sb("WoL", [128, nch, D], BF16)
            WoC = sb("WoC", [128, nch, D], BF16)
            for ci in range(nch):
                TTo("pool", WoL[:, ci, :], Wo[:, ci, :], G1[:], ALU.mult, [wokey, "G1"], ["WoL"])
                TTo("pool", WoC[:, ci, :], Wo[:, ci, :], G1c[:], ALU.mult, [wokey, "G1c"], ["WoC"])
            for tile in range(ntile_out):
                W_, wk_ = (WoL, "WoL") if tile < 16 else (WoC, "WoC")
                for half in range(2):
                    pb = PS[5 + half]
                    pk = f"ps{5 + half}"
                    for ci in range(nch):
                        MM(pb[:, 0:512], oT[:, ci, tile * 128:(tile + 1) * 128], W_[:, ci, half * 512:(half + 1) * 512],
                           ci == 0, ci == nch - 1, [("oT", tile), wk_], [pk])
                    residual_add_direct(tile, half, pb, pk)

        for g in range(DBG['nagroups'] if DBG['na'] else 0):
            P.barrier()
            mem["p"] = grp_base
            Wq = sb("Wq", [128, 8, 256], BF16)
            Wk = sb("Wk", [128, 8, 256], BF16)
            Wv = sb("Wv", [128, 8, 256], BF16)
            Wo = sb("Wo", [128, 2, D], BF16)
            Ball = sb("Ball", [128, 4, 16, 64], BF16)
            qaT = sb("qaT", [128, 2, TT], BF16)
            kaT = sb("kaT", [128, 2, TT], BF16)
            va = sb("va", [128, 18, 4, 65], BF16)
            oT = sb("oT", [128, 2, TT], BF16)
            Eb = [sb("E0", [128, 512], BF16), sb("E1", [128, 512], BF16)]
            rc = sb("rc", [128, 4])
            otile = sb("otile", [128, 256], BF16)
            P.dma("pool", Wq[:], wview(attw_d[e, :, g * 256:(g + 1) * 256]), [], ["Wq"])
            P.dma("pool", Wk[:], wview(attw_d[e, :, 1024 + g * 256:1024 + (g + 1) * 256]), [], ["Wk"])
            P.dma("pool", Wv[:], wview(attw_d[e, :, 1536 + g * 256:1536 + (g + 1) * 256]), [], ["Wv"])
            P.dma("pool", Wo[:], attwo_d[e, g * 256:(g + 1) * 256, :].rearrange("(c p) n -> p c n", p=128), [], ["Wo"])
            P.dma("pool", Ball[:], nab_d[e, :, 4 * g:4 * g + 4, :, :], [], ["Ball"])
            ACT(Ball[:], Ball[:], AF.Copy, ["Ball"], ["Ball"], scale=8.0)
            MS("pool", va[:, :, :, 64:65], 1.0, ["va1"])
            for ci in range(2):
                def ev_q(tb, c0, n, pb, pk, ci=ci):
                    CP("act", qaT[:, ci, c0:c0 + n], pb[:, 0:n], [pk], [("qaT", ci, tb)])

                def ev_k(tb, c0, n, pb, pk, ci=ci):
                    CP("dve", kaT[:, ci, c0:c0 + n], pb[:, 0:n], [pk], [("kaT", ci, tb)])
                proj_fm(None, Wq, "Wq", slice(ci * 128, (ci + 1) * 128), hT, ev_q)
                proj_fm(None, Wk, "Wk", slice(ci * 128, (ci + 1) * 128), hT, ev_k)
            for tile in range(18):
                pb = PS[tile % 2]
                pk = f"ps{tile % 2}"
                for k in range(8):
                    MM(pb[:, 0:256], hT[:, k, tile * 128:(tile + 1) * 128], Wv[:, k, :], k == 0, k == 7,
                       ["Wv", ("hT", tile)], [pk])
                CP("act" if tile % 2 else "dve", va[:, tile, :, 0:64], pb[:, 0:256].rearrange("p (h d) -> p h d", d=64),
                   [pk], [("va", tile)])
            if g == 0 and DBG.get('dump'):
                dump(9, modT[:, l, s, :], ["modT"], 48)
                dump(10, am[:, :, :, :].rearrange("p a b c -> p (a b c)"), ["am"], 32)
                dump(11, bmT[:, l, :], ["bmT"], 48)
                dump(0, hT[:, 0, 0:512], hkeys(0, 4), 512)
                dump(1, G1[:, 0:512], ["G1"], 512)
                dump(2, qaT[:, 0, 0:512], [("qaT", 0, 0)], 512)
                dump(3, kaT[:, 0, 0:512], [("kaT", 0, 0)], 512)
                dump(4, va[:, 0, :, :].rearrange("p h d -> p (h d)"), [("va", 0), "va1"], 260)
                dump(5, Ball[:, 0, 3, :], ["Ball"], 64)
            qkeys = lambda ci: [("qaT", ci, tb) for tb in range(5)]
            kkeys = lambda ci: [("kaT", ci, tb) for tb in range(5)]
            na_steps = []
            for qr in range(DBG['nqr']):
                rs = min(max(qr - 4, 0), 24)
                t0 = rs // 2
                tiles = list(range(t0, t0 + (4 if rs % 2 == 0 else 5)))
                for hh in range(4):
                    na_steps.append((qr, hh, rs, tiles))

            def na_S(n):
                qr, hh, rs, tiles = na_steps[n]
                ci, hf = hh // 2, hh % 2
                pr = slice(hf * 64, (hf + 1) * 64)
                sbk = PS[n % 2]
                sk = f"ps{n % 2}"
                q_ap = qaT[pr, ci, qr * 64:(qr + 1) * 64]
                slots = []
                for si, tl in enumerate(tiles):
                    idx = []
                    for kr in (2 * tl, 2 * tl + 1):
                        idx.append(kr - qr + 7 if rs <= kr < rs + 8 else 15)
                    so = sbk[:, si * 64:(si + 1) * 64]
                    MM(so, kaT[pr, ci, tl * 128:(tl + 1) * 128], q_ap, True, False, qkeys(ci) + kkeys(ci), [sk])
                    MM(so, ident_lo[:, :], Ball[:, hh, idx[0], :], False, False, ["ident_b", "Ball"], [sk])
                    MM(so, ident_hi[:, :], Ball[:, hh, idx[1], :], False, True, ["ident_b", "Ball"], [sk])
                    slots.append(tl)
                for cj in range(2):
                    si = len(tiles) + cj
                    MM(sbk[:, si * 64:(si + 1) * 64], kaT[pr, ci, T + cj * 128:T + (cj + 1) * 128], q_ap, True, True,
                       qkeys(ci) + kkeys(ci), [sk])
                    slots.append(16 + cj)
                ns = len(slots)
                ACT(Eb[n % 2][:, 0:ns * 64], sbk[:, 0:ns * 64], AF.Exp, [sk], [f"E{n % 2}"], scale=0.125)
                return slots

            def na_PV(n, slots):
                qr, hh, rs, tiles = na_steps[n]
                ob = PS[2 + qr % 2]
                ok = f"ps{2 + qr % 2}"
                E_ = Eb[n % 2]
                ek = f"E{n % 2}"
                ns = len(slots)
                for si, tl in enumerate(slots):
                    MM(ob[0:64, hh * 65:(hh + 1) * 65], E_[:, si * 64:(si + 1) * 64], va[:, tl, hh, :], si == 0, si == ns - 1,
                       [ek, ("va", tl), "va1"], [ok])
                if hh == 3:
                    ov = ob[0:64, 0:260].rearrange("p (h d) -> p h d", d=65)
                    RCP(rc[0:64, :].unsqueeze(2), ov[:, :, 64:65], [ok], ["rc"])
                    TTo("dve", otile[0:64, :].rearrange("p (h d) -> p h d", d=64), ov[:, :, 0:64],
                        rc[0:64, :].unsqueeze(2).to_broadcast([64, 4, 64]), ALU.mult, [ok, "rc"], ["otile"])
                    for ci in range(2):
                        TR(PSB[4][:, ci * 64:(ci + 1) * 64], otile[0:64, ci * 128:(ci + 1) * 128], ident_b[0:64, 0:64],
                           ["otile", "ident_b"], ["ps4"])
                    for ci in range(2):
                        CP("dve", oT[:, ci, qr * 64:(qr + 1) * 64], PSB[4][:, ci * 64:(ci + 1) * 64], ["ps4"], [("oT", qr // 2)])

            pend_slots = {}
            if na_steps:
                pend_slots[0] = na_S(0)
            for n in range(len(na_steps)):
                if n + 1 < len(na_steps):
                    pend_slots[n + 1] = na_S(n + 1)
                na_PV(n, pend_slots.pop(n))
            if with_ctx:
                for hh in range(4):
                    ci, hf = hh // 2, hh % 2
                    pr = slice(hf * 64, (hf + 1) * 64)
                    sbk = PS[hh % 2]
                    sk = f"ps{hh % 2}"
                    for cj in range(2):
                        MM(sbk[:, cj * 256:(cj + 1) * 256], kaT[pr, ci, T + cj * 128:T + (cj + 1) * 128], qaT[pr, ci, T:TT],
                           True, True, qkeys(ci) + kkeys(ci), [sk])
                    E_ = Eb[hh % 2]
                    ek = f"E{hh % 2}"
                    ACT(E_[:, 0:512], sbk[:, 0:512], AF.Exp, [sk], [ek], scale=0.125)
                    for qt in range(2):
                        for cj in range(2):
                            MM(PS[2 + qt][:, hh * 65:(hh + 1) * 65], E_[:, cj * 256 + qt * 128:cj * 256 + (qt + 1) * 128],
                               va[:, 16 + cj, hh, :], cj == 0, cj == 1, [ek, ("va", 16 + cj), "va1"], [f"ps{2 + qt}"])
                for qt in range(2):
                    ob = PS[2 + qt]
                    ok = f"ps{2 + qt}"
                    ov = ob[:, 0:260].rearrange("p (h d) -> p h d", d=65)
                    RCP(rc[:, :].unsqueeze(2), ov[:, :, 64:65], [ok], ["rc"])
                    TTo("dve", otile[:, :].rearrange("p (h d) -> p h d", d=64), ov[:, :, 0:64],
                        rc[:, :].unsqueeze(2).to_broadcast([128, 4, 64]), ALU.mult, [ok, "rc"], ["otile"])
                    for ci in range(2):
                        TR(PSB[4][:, ci * 128:(ci + 1) * 128], otile[:, ci * 128:(ci + 1) * 128], ident_b[:],
                           ["otile", "ident_b"], ["ps4"])
                    CP("act", oT[:, :, T + qt * 128:T + (qt + 1) * 128], PSB[4][:, 0:256].rearrange("p (c t) -> p c t", t=128),
                       ["ps4"], [("oT", 16 + qt)])
            if g == 0 and DBG.get('dump'):
                dump(6, oT[:, 0, 0:512], [("oT", t) for t in range(4)], 512)
                dump(7, Eb[0][:, 0:512], ["E0"], 512)
                dump(8, otile[:, :], ["otile"], 256)
            out_proj(oT, 2, Wo, "Wo")

        for hb in range(4 if DBG['diff'] else 0):
            P.barrier()
            mem["p"] = grp_base
            W5 = sb("W5", [128, 8, 5, 128], BF16)
            Wo = sb("Wo1", [128, 1, D], BF16)
            cosT = sb("cosT", [128, T], BF16)
            sinT = sb("sinT", [128, T], BF16)
            qbT = sb("qbT", [128, TT], BF16)
            kbT = sb("kbT", [128, TT], BF16)
            vb = sb("vb", [128, 18, 129], BF16)
            oT = sb("oTb", [128, 1, TT], BF16)
            Eb = [sb(f"E{i}", [128, 512], BF16) for i in range(4)]
            SBK = [0, 1, 6, 7]
            t1 = sb("t1", [128, 512])
            t2 = sb("t2", [128, 512])
            dd = sb("dd", [128, 128])
            obt = sb("obt", [128, 128], BF16)
            r12 = sb("r12", [128, 4])
            cols = [512 + hb * 128, 3072 + hb * 128, 2048 + hb * 128, 3584 + hb * 128, 2560 + hb * 128]
            for i, c0 in enumerate(cols):
                P.dma("pool", W5[:, :, i, :], wview(attw_d[e, :, c0:c0 + 128]), [], [("W5", i)])
            P.dma("pool", Wo[:, 0, :], attwo_d[e, 512 + hb * 128:512 + (hb + 1) * 128, :], [], ["Wo1"])
            P.dma("pool", cosT[:], cos_d, [], ["cosT"])
            P.dma("pool", sinT[:], sin_d, [], ["sinT"])
            MS("pool", vb[:, :, 128:129], 1.0, ["vb1"])
            for (dstT, dname, i_raw, i_sw) in ((qbT, "qbT", 0, 1), (kbT, "kbT", 2, 3)):
                for tb in range(5):
                    n = 512 if tb < 4 else 256
                    c0 = tb * 512
                    hk = hkeys(c0 // 128, (c0 + n) // 128)
                    for k in range(8):
                        MM(PS[0][:, 0:n], W5[:, k, i_raw, :], hT[:, k, c0:c0 + n], k == 0, k == 7, [("W5", i_raw)] + hk, ["ps0"])
                    if tb < 4:
                        for k in range(8):
                            MM(PS[1][:, 0:n], W5[:, k, i_sw, :], hT[:, k, c0:c0 + n], k == 0, k == 7, [("W5", i_sw)] + hk, ["ps1"])
                        TTo("dve", t1[:], PS[0][:, 0:n], cosT[:, c0:c0 + n], ALU.mult, ["ps0", "cosT"], ["t1"])
                        TTo("dve", t2[:], PS[1][:, 0:n], sinT[:, c0:c0 + n], ALU.mult, ["ps1", "sinT"], ["t2"])
                        TTo("pool", dstT[:, c0:c0 + n], t1[:], t2[:], ALU.add, ["t1", "t2"], [(dname, tb)])
                    else:
                        CP("act", dstT[:, c0:c0 + n], PS[0][:, 0:n], ["ps0"], [(dname, tb)])
            for tile in range(18):
                pb = PS[tile % 2]
                pk = f"ps{tile % 2}"
                for k in range(8):
                    MM(pb[:, 0:128], hT[:, k, tile * 128:(tile + 1) * 128], W5[:, k, 4, :], k == 0, k == 7,
                       [("W5", 4), ("hT", tile)], [pk])
                CP("act" if tile % 2 else "dve", vb[:, tile, 0:128], pb[:, 0:128], [pk], [("vb", tile)])
            qk_all = [("qbT", tb) for tb in range(5)] + [("kbT", tb) for tb in range(5)]

            def diff_block(qc0, nq, kts):
                nsub = nq // 128
                acc = {}
                for sub in range(nsub):
                    for m in range(2):
                        a = sub * 2 + m
                        acc[(sub, m)] = (PS[2 + a // 3][:, (a % 3) * 129:(a % 3 + 1) * 129], f"ps{2 + a // 3}")
                for bnk in sorted(set(2 + (sub * 2 + m) // 3 for sub in range(nsub) for m in range(2))):
                    MS("dve", PS[bnk][:, :], 0.0, [f"ps{bnk}"])
                steps = [(ki, kt, m) for ki, kt in enumerate(kts) for m in range(2)]

                def emit_S(i):
                    ki, kt, m = steps[i]
                    pr = slice(m * 64, (m + 1) * 64)
                    sbk = PS[SBK[i % 4]]
                    sk = f"ps{SBK[i % 4]}"
                    E_ = Eb[i % 4]
                    ek = f"E{i % 4}"
                    MM(sbk[:, 0:nq], kbT[pr, kt * 128:(kt + 1) * 128], qbT[pr, qc0:qc0 + nq], True, True, qk_all, [sk])
                    ACT(E_[:, 0:nq], sbk[:, 0:nq], AF.Exp, [sk], [ek], scale=0.125)

                def emit_PV(i):
                    ki, kt, m = steps[i]
                    E_ = Eb[i % 4]
                    ek = f"E{i % 4}"
                    for sub in range(nsub):
                        ap, akey = acc[(sub, m)]
                        MM(ap, E_[:, sub * 128:(sub + 1) * 128], vb[:, kt, :], False, ki == len(kts) - 1,
                           [ek, ("vb", kt), "vb1"], [akey])

                LA = 2
                for i in range(len(steps) + LA):
                    if i < len(steps):
                        emit_S(i)
                    if i - LA >= 0:
                        emit_PV(i - LA)
                for sub in range(nsub):
                    (o1, k1), (o2, k2) = acc[(sub, 0)], acc[(sub, 1)]
                    RCP(r12[:, 0:1], o1[:, 128:129], [k1], ["r12"])
                    RCP(r12[:, 1:2], o2[:, 128:129], [k2], ["r12"])
                    TTo("dve", r12[:, 2:3], r12[:, 1:2], neglam[:, 0:1], ALU.mult, ["r12", "neglam"], ["r12"])
                    TS("dve", t1[:, 0:128], o1[:, 0:128], r12[:, 0:1], None, ALU.mult, None, [k1, "r12"], ["t1"])
                    STT("dve", dd[:], o2[:, 0:128], r12[:, 2:3], t1[:, 0:128], ALU.mult, ALU.add, [k2, "r12", "t1"], ["dd"])
                    rms_rstd(dd[:], "dd", t2[:, 0:128], 128)
                    STT("dve", obt[:], dd[:], rstd[:, 0:1], sg_bc[:], ALU.mult, ALU.mult, ["dd", "rstd", "sg_bc"], ["obt"])
                    TR(PSB[5][:, 0:128], obt[:], ident_b[:], ["obt", "ident_b"], ["ps5"])
                    tcol = qc0 + sub * 128
                    CP("act", oT[:, 0, tcol:tcol + 128], PSB[5][:, 0:128], ["ps5"], [("oT", tcol // 128)])

            for qb_ in range(4):
                diff_block(qb_ * 512, 512, list(range(18)))
            if with_ctx:
                diff_block(T, 256, [16, 17])
            out_proj(oT, 1, Wo, "Wo1")
        P.barrier()

    def odd_mixer(s, l, with_ctx):
        o = l // 2
        mem["p"] = phase_base
        hT = sb("hT", [128, 8, TT], BF16)
        G1 = sb("G1", [128, D])
        G1c = sb("G1c", [128, D])
        WoL = sb("WoL", [128, D], BF16)
        WoC = sb("WoC", [128, D], BF16)
        tmp_mark = mem["p"]
        xnb = sb("xnb", [128, D], BF16)
        junk = sb("junk", [128, D], BF16)
        gtmp = sb("gtmp", [128, 8, 128])
        build_G(G1, mslice(l, s, 2), gtmp, "G1")
        build_G(G1c, mslice(l, S, 2), gtmp, "G1c")
        compute_hT(s, l, hT, xnb, junk)
        P.barrier()
        mem["p"] = tmp_mark
        W5 = sb("W5", [128, 8, 5, 128], BF16)
        Wo = sb("Wo", [128, D], BF16)
        A = sb("A", [128, TT])
        B = sb("B", [128, TT])
        kk = sb("kk", [128, TT], BF16)
        sq = sb("sq", [128, TT], BF16)
        sgt = sb("sgt", [128, TT], BF16)
        qt_ = sb("qt", [128, TT], BF16)
        qh = sb("qh", [128, TT], BF16)
        kt_ = sb("kt", [128, TT], BF16)
        vv = sb("vv", [128, 18, 128], BF16)
        oTs = sb("oTs", [128, TT])
        tot = sb("tot", [128, 36])
        Ee = sb("Ee", [128, 36])
        Ep = sb("Ep", [128, 36])
        ATm2 = [sb("ATm0", [128, 128], BF16), sb("ATm1", [128, 128], BF16)]
        ktok2 = [sb("ktok0", [128, 128], BF16), sb("ktok1", [128, 128], BF16)]
        rmask = sb("rmask", [128, TT], BF16)
        MS("pool", rmask[:], 1.0, ["rmask"])
        MS("pool", rmask[:].rearrange("p (c k) -> p c k", k=64)[:, :, 0:1], 0.0, ["rmask"])
        R32 = [sb("R32a", [128, 128]), sb("R32b", [128, 128])]
        Rb = [sb("Rba", [128, 128], BF16), sb("Rbb", [128, 128], BF16)]
        ntile_out = 18 if with_ctx else 16
        NCH = 36
        for hd in range(8):
            P.barrier()
            cols = [hd * 128, 1024 + hd * 128, 2048 + hd * 128, 3072 + hd * 128, 4096 + hd * 128]
            for i, c0 in enumerate(cols):
                P.dma("pool", W5[:, :, i, :], wview(recw_d[o, :, c0:c0 + 128]), [], [("W5", i)])
            P.dma("pool", Wo[:], recwo_d[o, hd * 128:(hd + 1) * 128, :], [], ["Wo"])

            def ev_q(tb, c0, n, pb, pk):
                ACT(sq[:, c0:c0 + n], pb[:, 0:n], AF.Silu, [pk], ["sq"])

            def ev_g(tb, c0, n, pb, pk):
                ACT(sgt[:, c0:c0 + n], pb[:, 0:n], AF.Silu, [pk], ["sgt"])
            proj_fm(None, W5[:, :, 0, :], ("W5", 0), slice(0, 128), hT, ev_q)
            proj_fm(None, W5[:, :, 4, :], ("W5", 4), slice(0, 128), hT, ev_g)
            for tile in range(18):
                pb = PS[tile % 2]
                pk = f"ps{tile % 2}"
                for k in range(8):
                    MM(pb[:, 0:128], hT[:, k, tile * 128:(tile + 1) * 128], W5[:, k, 3, :], k == 0, k == 7,
                       [("W5", 3), ("hT", tile)], [pk])
                CP("dve", vv[:, tile, :], pb[:, 0:128], [pk], [("vv", tile)])
            vkeys = [("vv", t) for t in range(18)]
            for dr in range(2):
                lbc = lbT[:, dr, l, hd:hd + 1]
                omc = omlT[:, dr, l, hd:hd + 1]
                nomc = nomlT[:, dr, l, hd:hd + 1]

                def ev_f(tb, c0, n, pb, pk):
                    ACT(A[:, c0:c0 + n], pb[:, 0:n], AF.Sigmoid, [pk], ["A"])
                proj_fm(None, W5[:, :, 1 + dr, :], ("W5", 1 + dr), slice(0, 128), hT, ev_f)
                ACT(B[:], A[:], AF.Ln, ["A", "lbT", "omlT"], ["B"], scale=omc, bias=lbc)
                TS("dve", kk[:], A[:], nomc, omc, ALU.mult, ALU.add, ["A", "nomlT", "omlT"], ["kk"])
                P.op("dve", lambda E: E.tensor_tensor_scan(out=A[:], data0=rmask[:], data1=B[:], initial=0.0,
                                                            op0=ALU.mult, op1=ALU.add), ["B", "rmask", "kk"], ["A"])
                Av = A[:].rearrange("p (c k) -> p c k", k=64)
                Bv = B[:].rearrange("p (c k) -> p c k", k=64)
                CP("dve", tot[:].unsqueeze(2), Av[:, :, 63:64], ["A"], ["tot"])
                if dr == 1:
                    TTo("dve", A[:], B[:], A[:], ALU.subtract, ["A", "B"], ["A"])
                    TTo("dve", Av, Av, tot[:].unsqueeze(2).to_broadcast([128, NCH, 64]), ALU.add, ["A", "tot"], ["A"])
                ACT(Ee[:], tot[:], AF.Exp, ["tot"], ["Ee"])
                MS("dve", Ep[:], 0.0, ["Ep"])
                if dr == 0:
                    CP("dve", Ep[:, 1:32], Ee[:, 0:31], ["Ee"], ["Ep"])
                    CP("dve", Ep[:, 0:1], Ee[:, 35:36], ["Ee"], ["Ep"])
                    CP("dve", Ep[:, 33:36], Ee[:, 32:35], ["Ee"], ["Ep"])
                    order = [32, 33, 34, 35] + list(range(32))
                    msk = maskf
                else:
                    CP("dve", Ep[:, 0:31], Ee[:, 1:32], ["Ee"], ["Ep"])
                    CP("dve", Ep[:, 31:32], Ee[:, 32:33], ["Ee"], ["Ep"])
                    CP("dve", Ep[:, 32:35], Ee[:, 33:36], ["Ee"], ["Ep"])
                    order = [35, 34, 33, 32] + list(range(31, -1, -1))
                    msk = maskb
                ACT(qt_[:], A[:], AF.Exp, ["A"], ["qt"])
                ACT(kt_[:], A[:], AF.Exp, ["A"], ["kt"], scale=-1.0)
                TTo("pool", qt_[:], qt_[:], sq[:], ALU.mult, ["qt", "sq"], ["qt"])
                TTo("dve", kt_[:], kt_[:], kk[:], ALU.mult, ["kt", "kk"], ["kt"])
                TTo("pool", qh[:].rearrange("p (c k) -> p c k", k=64), qt_[:].rearrange("p (c k) -> p c k", k=64),
                    Ep[:].unsqueeze(2).to_broadcast([128, NCH, 64]), ALU.mult, ["qt", "Ep"], ["qh"])
                if hd == 0 and dr == 0 and DBG.get('dump'):
                    dump(0, B[:, 0:512], ["B"], 512)
                    dump(1, A[:, 0:512], ["A"], 512)
                    dump(2, kk[:, 0:512], ["kk"], 512)
                    dump(3, qt_[:, 0:512], ["qt"], 512)
                    dump(4, kt_[:, 0:512], ["kt"], 512)
                    dump(5, Ee[:, :], ["Ee"], 36)
                    dump(6, Ep[:, :], ["Ep"], 36)
                    dump(7, tot[:, :], ["tot"], 36)
                    dump(8, lbT[:].rearrange("p a b c -> p (a b c)"), ["lbT"], 64)
                    dump(9, vv[:, 0:4, :].rearrange("p a b -> p (a b)"), vkeys, 512)
                st_ = {"pp": 0, "first": True, "ucnt": 0}
                ABK = [2, 0]
                TBK = [3, 1]
                UBK = [6, 7]

                def stageA(ti):
                    tl = order[2 * ti] // 2
                    tc = slice(tl * 128, (tl + 1) * 128)
                    i2 = ti % 2
                    ab, tb_ = ABK[i2], TBK[i2]
                    MM(PS[ab][:, 0:128], kt_[:, tc], qt_[:, tc], True, True, ["kt", "qt"], [f"ps{ab}"])
                    TTo("dve", ATm2[i2][:], PS[ab][:, 0:128], msk[:], ALU.mult, [f"ps{ab}", "maskf", "maskb"], [("ATm", i2)])
                    TR(PSB[tb_][:, 0:128], kt_[:, tc], ident_b[:], ["kt", "ident_b"], [f"ps{tb_}"])
                    CP("act", ktok2[i2][:], PSB[tb_][:, 0:128], [f"ps{tb_}"], [("ktok", i2)])

                def stageB(ti):
                    ch_a, ch_b = order[2 * ti], order[2 * ti + 1]
                    tl = ch_a // 2
                    assert ch_b // 2 == tl
                    tc = slice(tl * 128, (tl + 1) * 128)
                    i2 = ti % 2
                    ob = PS[4 + ti % 2]
                    ok = f"ps{4 + ti % 2}"
                    chs = [ch_a, ch_b]
                    n_inter = sum(1 for ch in chs if not (st_["first"] and ch == chs[0]))
                    MM(ob[:, 0:128], vv[:, tl, :], ATm2[i2][:], True, n_inter == 0, [("ATm", i2)] + vkeys, [ok])
                    done = 0
                    for ch in chs:
                        hf = ch % 2
                        pp = st_["pp"]
                        ub = UBK[st_["ucnt"] % 2]
                        st_["ucnt"] += 1
                        uk = f"ps{ub}"
                        MM(PS[ub][:, 0:128], ktok2[i2][hf * 64:(hf + 1) * 64, :], vv[hf * 64:(hf + 1) * 64, tl, :], True, True,
                           [("ktok", i2)] + vkeys, [uk])
                        if not st_["first"]:
                            done += 1
                            MM(ob[:, hf * 64:(hf + 1) * 64], Rb[pp][:], qh[:, ch * 64:(ch + 1) * 64], False, done == n_inter,
                               [("Rb", pp), "qh"], [ok])
                        if st_["first"]:
                            CP("dve", Rb[pp][:], PS[ub][:, 0:128], [uk], [("Rb", pp)])
                            CP("dve", R32[pp][:], PS[ub][:, 0:128], [uk], [("R32", pp)])
                            st_["first"] = False
                        else:
                            STT("dve", Rb[1 - pp][:], R32[pp][:], Ep[:, ch:ch + 1], PS[ub][:, 0:128], ALU.mult, ALU.add,
                                [("R32", pp), "Ep", uk], [("Rb", 1 - pp)])
                            STT("dve", R32[1 - pp][:], R32[pp][:], Ep[:, ch:ch + 1], PS[ub][:, 0:128], ALU.mult, ALU.add,
                                [("R32", pp), "Ep", uk], [("R32", 1 - pp)])
                            st_["pp"] = 1 - pp
                    if dr == 0:
                        CP("act", oTs[:, tc], ob[:, 0:128], [ok], [("oTs", tl)])
                    else:
                        TTo("dve", oTs[:, tc], oTs[:, tc], ob[:, 0:128], ALU.add, [ok, ("oTs", tl)], [("oTs", tl)])

                stageA(0)
                for ti in range(18):
                    if ti + 1 < 18:
                        stageA(ti + 1)
                    stageB(ti)
            if hd == 0 and DBG.get('dump'):
                dump(10, oTs[:, 0:512], [("oTs", t) for t in range(4)], 512)
                dump(11, oTs[:, T:TT], [("oTs", t) for t in (16, 17)], 256)
                dump(12, R32[0][:], [("R32", 0)], 128)
            oTh = kk
            for tb in range(5):
                n = 512 if tb < 4 else 256
                c0 = tb * 512
                ok_ = [("oTs", t) for t in range(c0 // 128, (c0 + n) // 128)]
                ACT(A[:, c0:c0 + n], oTs[:, c0:c0 + n], AF.Square, ok_, ["A"])
                MM(PS[7][:, 0:n], ones_f[:], A[:, c0:c0 + n], True, True, ["ones_f", "A"], ["ps7"])
                ACT(B[:, c0:c0 + n], PS[7][:, 0:n], AF.Sqrt, ["ps7"], ["B"], scale=1.0 / 128, bias=EPS)
                RCP(B[:, c0:c0 + n], B[:, c0:c0 + n], ["B"], ["B"])
                TTo("dve", A[:, c0:c0 + n], oTs[:, c0:c0 + n], B[:, c0:c0 + n], ALU.mult, ok_ + ["B", "A"], ["A"])
                STT("dve", oTh[:, c0:c0 + n], A[:, c0:c0 + n], gnT[:, o:o + 1], sgt[:, c0:c0 + n], ALU.mult, ALU.mult,
                    ["A", "gnT", "sgt"], ["kk"])
            TTo("pool", WoL[:], Wo[:], G1[:], ALU.mult, ["Wo", "G1"], ["WoL"])
            TTo("pool", WoC[:], Wo[:], G1c[:], ALU.mult, ["Wo", "G1c"], ["WoC"])
            for tile in range(ntile_out):
                W_, wk_ = (WoL, "WoL") if tile < 16 else (WoC, "WoC")
                for half in range(2):
                    pb = PS[half]
                    pk = f"ps{half}"
                    MM(pb[:, 0:512], oTh[:, tile * 128:(tile + 1) * 128], W_[:, half * 512:(half + 1) * 512], True, True,
                       ["kk", wk_], [pk])
                    residual_add_direct(tile, half, pb, pk)
        P.barrier()

    def moe(s, l, with_ctx):
        mem["p"] = phase_base
        ntl = 18 if with_ctx else 16
        NW = 288 if with_ctx else 256
        njt = 3 if with_ctx else 2
        hn = sb("hn", [128, 18, D], BF16)
        G2 = sb("G2", [128, D])
        G2c = sb("G2c", [128, D])
        posm_b = sb("posm_b", [16, TT], BF16)
        w_b = sb("w_b", [16, TT], BF16)
        pos_tok = sb("pos_tok", [128, 18, NE])
        wr = sb("wr", [128, 8, NE])
        selT = sb("selT", [16, 128], BF16)
        loop_base = mem["p"]
        gtmp = sb("gtmp", [128, 8, 128])
        xn32 = sb("xn32", [128, D])
        junk = sb("junk", [128, D], BF16)
        hT32 = sb("hT32", [128, 8, 128])
        afft = sb("afft", [128, NE])
        affT = sb("affT", [16, TT])
        work = sb("work", [16, TT])
        msk = sb("msk", [16, TT])
        pos = sb("pos", [16, TT])
        mx = sb("mx", [16, 8])
        build_G(G2, mslice(l, s, 5), gtmp, "G2")
        build_G(G2c, mslice(l, S, 5), gtmp, "G2c")
        P.dma("sp", wr[:], wview(router_d[LI[l]]), [], ["wr"])
        for vi, v in enumerate((s, S)):
            STT("dve", am[:, 1, vi, :], mslice(l, v, 4), 1.0, ngT[:, l, 1, :], ALU.add, ALU.mult, ["modT", "ngT"], ["am"])
        for tile in range(ntl):
            vi = 0 if tile < 16 else 1
            v = s if tile < 16 else S
            src, sk = xsrc(tile)
            rms_rstd(src, sk, junk[:], D)
            TS("dve", xn32[:], src, rstd[:, 0:1], None, ALU.mult, None, [sk, "rstd"], ["xn32"])
            CP("act" if DBG.get("nopool") else "pool", hn[:, tile, :], xn32[:], ["xn32"], [("hn", tile)])
            if DBG.get('sub', 9) < 1:
                continue
            pbk = f"ps{tile % 2}"
            for c in range(8):
                TR(PSB[tile % 2][:, c * 128:(c + 1) * 128], hn[:, tile, c * 128:(c + 1) * 128], ident_b[:],
                   [("hn", tile), "ident_b"], [pbk])
            for c in range(8):
                if DBG.get('noevac'):
                    continue
                src_p = PSB[tile % 2][:, c * 128:(c + 1) * 128]
                if c % 2 == 0:
                    TS("dve", hT32[:, c, :], src_p, am[:, 1, vi, c:c + 1], mslice(l, v, 3)[:, c:c + 1], ALU.mult, ALU.add,
                       [pbk, "am", "modT"], [("hT32", c)])
                    continue
                if c % 2 == 0:
                    ACT(hT32[:, c, :], src_p, AF.Identity, [pbk, "am", "modT"], [("hT32", c)],
                        scale=am[:, 1, vi, c:c + 1], bias=mslice(l, v, 3)[:, c:c + 1])
                else:
                    TS("dve", hT32[:, c, :], src_p, am[:, 1, vi, c:c + 1], mslice(l, v, 3)[:, c:c + 1], ALU.mult, ALU.add,
                       [pbk, "am", "modT"], [("hT32", c)])
            if DBG.get('sub', 9) < 2:
                continue
            for c in range(8):
                MM(PS[2][:, 0:NE], hT32[:, c, :], wr[:, c, :], c == 0, c == 7, [("hT32", c), "wr"], ["ps2"])
            if DBG.get('sub', 9) < 3:
                continue
            ACT(afft[:], PS[2][:, 0:NE], AF.Exp, ["ps2"], ["afft", "ssq"], accum=ssq[:])
            RCP(rt[:], ssq[:], ["ssq"], ["rt"])
            TS("dve", afft[:], afft[:], rt[:, 0:1], None, ALU.mult, None, ["afft", "rt"], ["afft"])
            if not DBG.get("notr"):
                TR(PS[3][0:16, 0:128], afft[:], ident_f[:], ["afft", "ident_f"], ["ps3"])
                CP("act", affT[0:16, tile * 128:(tile + 1) * 128], PS[3][0:16, 0:128], ["ps3"], ["affT"])

        if DBG.get('stage', 9) < 1:
            P.barrier()
            return

        def route(c0, n, cap):
            CP("dve", work[:, c0:c0 + n], affT[:, c0:c0 + n], ["affT"], ["work"])
            nit = cap // 8
            for it_ in range(nit):
                P.op("dve", (lambda a, b: (lambda E: E.max(out=a, in_=b)))(mx[:], work[:, c0:c0 + n]), ["work"], ["mx"])
                if it_ < nit - 1:
                    P.op("dve", (lambda a, b, c_: (lambda E: E.match_replace(out=a, in_to_replace=b, in_values=c_, imm_value=-1.0)))(
                        work[:, c0:c0 + n], mx[:], work[:, c0:c0 + n]), ["work", "mx"], ["work"])
            TS("dve", msk[:, c0:c0 + n], affT[:, c0:c0 + n], mx[:, 7:8], None, ALU.is_ge, None, ["affT", "mx"], ["msk"])
            MS("dve", work[:, c0:c0 + n], 1.0, ["work"])
            P.op("dve", (lambda a, b, c_: (lambda E: E.tensor_tensor_scan(out=a, data0=b, data1=c_, initial=0.0, op0=ALU.mult, op1=ALU.add)))(
                pos[:, c0:c0 + n], work[:, c0:c0 + n], msk[:, c0:c0 + n]), ["work", "msk"], ["pos"])
            TTo("dve", pos[:, c0:c0 + n], pos[:, c0:c0 + n], msk[:, c0:c0 + n], ALU.mult, ["pos", "msk"], ["pos"])
            TS("dve", pos[:, c0:c0 + n], pos[:, c0:c0 + n], -1.0, None, ALU.add, None, ["pos"], ["pos"])
            CP("dve", posm_b[:, c0:c0 + n], pos[:, c0:c0 + n], ["pos"], ["posm_b"])
            TTo("dve", w_b[:, c0:c0 + n], affT[:, c0:c0 + n], msk[:, c0:c0 + n], ALU.mult, ["affT", "msk"], ["w_b"])

        route(0, T, 256)
        if with_ctx:
            route(T, L, 32)
        if DBG.get('stage', 9) < 2:
            P.barrier()
            return
        for tile in range(ntl):
            TR(PS[3][:, 0:NE], pos[0:16, tile * 128:(tile + 1) * 128], ident_f[0:16, 0:16], ["pos", "ident_f"], ["ps3"])
            CP("act", pos_tok[:, tile, :], PS[3][:, 0:NE], ["ps3"], ["pos_tok"])
        P.barrier()
        mem["p"] = loop_base
        Pe = sb("Pe", [128, 16, 256], BF16)
        Pce = sb("Pce", [128, 2, 32], BF16)
        PT = sb("PT", [128, 2, T], BF16)
        PTc = sb("PTc", [32, L], BF16)
        xsel = sb("xsel", [128, 8, 288], BF16)
        hid = [sb("hid0", [128, 288], BF16), sb("hid1", [128, 288], BF16)]
        sgl = sb("sgl", [128, 288])
        yy = sb("yy", [128, 3, D], BF16)
        wsb = sb("wsb", [128, 512])
        Wg = [sb("Wg0", [128, 8, 256], BF16), sb("Wg1", [128, 8, 256], BF16)]
        Wu = [sb("Wu0", [128, 8, 256], BF16), sb("Wu1", [128, 8, 256], BF16)]
        Wd = [sb("Wd0", [128, 2, D], BF16), sb("Wd1", [128, 2, D], BF16)]
        hnk = [("hn", t) for t in range(ntl)]
        wcnt = 0
        for e in range(DBG['experts']):
            TS("dve", selT[:], ones_f[0:16, :], ident_f[0:16, e:e + 1], None, ALU.mult, None, ["ones_f", "ident_f"], ["selT"])
            TTo("dve", Pe[:], iota_j[:].unsqueeze(1).to_broadcast([128, 16, 256]),
                pos_tok[:, 0:16, e:e + 1].to_broadcast([128, 16, 256]), ALU.is_equal, ["iota_j", "pos_tok"], ["Pe"])
            if with_ctx:
                TTo("dve", Pce[:], iota_j[:, 0:32].unsqueeze(1).to_broadcast([128, 2, 32]),
                    pos_tok[:, 16:18, e:e + 1].to_broadcast([128, 2, 32]), ALU.is_equal, ["iota_j", "pos_tok"], ["Pce"])
            for blk in range(4):
                bc = slice(blk * 512, (blk + 1) * 512)
                MM(PS[6][:, 0:512], selT[0:16, :], posm_b[0:16, bc], True, True, ["selT", "posm_b"], ["ps6"])
                MM(PS[7][:, 0:512], selT[0:16, :], w_b[0:16, bc], True, True, ["selT", "w_b"], ["ps7"])
                CP("act", wsb[:], PS[7][:, 0:512], ["ps7"], ["wsb"])
                for jt in range(2):
                    STT("dve", PT[:, jt, bc], PS[6][:, 0:512], iota_p[:, jt:jt + 1], wsb[:], ALU.is_equal, ALU.mult,
                        ["ps6", "iota_p", "wsb"], ["PT"])
            if with_ctx:
                MM(PS[6][0:32, 0:L], selT[0:16, 0:32], posm_b[0:16, T:TT], True, True, ["selT", "posm_b"], ["ps6"])
                MM(PS[7][0:32, 0:L], selT[0:16, 0:32], w_b[0:16, T:TT], True, True, ["selT", "w_b"], ["ps7"])
                CP("act", wsb[0:32, 0:L], PS[7][0:32, 0:L], ["ps7"], ["wsb"])
                STT("dve", PTc[:], PS[6][0:32, 0:L], iota_p[0:32, 0:1], wsb[0:32, 0:L], ALU.is_equal, ALU.mult,
                    ["ps6", "iota_p", "wsb"], ["PTc"])
            for c in range(8):
                pb = PS[6 + c % 2]
                pk = f"ps{6 + c % 2}"
                for tile in range(16):
                    MM(pb[:, 0:256], hn[:, tile, c * 128:(c + 1) * 128], Pe[:, tile, :], tile == 0, tile == 15, ["Pe"] + hnk, [pk])
                if with_ctx:
                    for ct in range(2):
                        MM(pb[:, 256:288], hn[:, 16 + ct, c * 128:(c + 1) * 128], Pce[:, ct, :], ct == 0, ct == 1, ["Pce"] + hnk, [pk])
                TS("dve", xsel[:, c, 0:256], pb[:, 0:256], am[:, 1, 0, c:c + 1], mslice(l, s, 3)[:, c:c + 1], ALU.mult, ALU.add,
                   [pk, "am", "modT"], [("xsel", c)])
                if with_ctx:
                    TS("dve", xsel[:, c, 256:288], pb[:, 256:288], am[:, 1, 1, c:c + 1], mslice(l, S, 3)[:, c:c + 1], ALU.mult, ALU.add,
                       [pk, "am", "modT"], [("xsel", c)])
            xk = [("xsel", c) for c in range(8)]
            pend = None

            def down(fc, wi, f2):
                for jt in range(njt):
                    rows = 128 if jt < 2 else 32
                    for half in range(2):
                        b = jt * 2 + half
                        MM(PS[b][0:rows, 0:512], hid[fc % 2][:, jt * 128:jt * 128 + rows], Wd[wi][:, f2, half * 512:(half + 1) * 512],
                           fc == 0, fc == 15, [f"hid{fc % 2}", f"Wd{wi}"], [f"ps{b}"])

            for fb in range(8):
                wi = wcnt % 2
                wcnt += 1
                P.dma("pool", Wg[wi][:], wview(wg_d[LI[l], e, :, fb * 256:(fb + 1) * 256]), [], [f"Wg{wi}"])
                P.dma("pool", Wu[wi][:], wview(wu_d[LI[l], e, :, fb * 256:(fb + 1) * 256]), [], [f"Wu{wi}"])
                P.dma("pool", Wd[wi][:], wd_d[LI[l], e, fb * 256:(fb + 1) * 256, :].rearrange("(c p) n -> p c n", p=128), [], [f"Wd{wi}"])
                for f2 in range(2):
                    fc = fb * 2 + f2
                    for c in range(8):
                        MM(PS[6][:, 0:NW], Wg[wi][:, c, f2 * 128:(f2 + 1) * 128], xsel[:, c, 0:NW], c == 0, c == 7, [f"Wg{wi}"] + xk, ["ps6"])
                    for c in range(8):
                        MM(PS[7][:, 0:NW], Wu[wi][:, c, f2 * 128:(f2 + 1) * 128], xsel[:, c, 0:NW], c == 0, c == 7, [f"Wu{wi}"] + xk, ["ps7"])
                    ACT(sgl[:, 0:NW], PS[6][:, 0:NW], AF.Silu, ["ps6"], ["sgl"])
                    TTo("dve", hid[fc % 2][:, 0:NW], sgl[:, 0:NW], PS[7][:, 0:NW], ALU.mult, ["sgl", "ps7"], [f"hid{fc % 2}"])
                    if pend is not None:
                        down(*pend)
                    pend = (fc, wi, f2)
            down(*pend)
            for jt in range(njt):
                rows = 128 if jt < 2 else 32
                G = G2 if jt < 2 else G2c
                gk = "G2" if jt < 2 else "G2c"
                for half in range(2):
                    b = jt * 2 + half
                    TTo("dve", yy[0:rows, jt, half * 512:(half + 1) * 512], PS[b][0:rows, 0:512], G[0:rows, half * 512:(half + 1) * 512],
                        ALU.mult, [f"ps{b}", gk], [("yy", jt)])
            for tile in range(16):
                for half in range(2):
                    b = (tile % 3) * 2 + half
                    for jt in range(2):
                        MM(PS[b][:, 0:512], PT[:, jt, tile * 128:(tile + 1) * 128], yy[:, jt, half * 512:(half + 1) * 512], jt == 0, jt == 1,
                           ["PT", ("yy", jt)], [f"ps{b}"])
                    TTo("dve", X[:, tile, half * 512:(half + 1) * 512], X[:, tile, half * 512:(half + 1) * 512], PS[b][:, 0:512], ALU.add,
                        [f"ps{b}", ("X", tile)], [("X", tile)])
            if with_ctx:
                for ct in range(2):
                    for half in range(2):
                        b = ct * 2 + half
                        MM(PS[b][:, 0:512], PTc[0:32, ct * 128:(ct + 1) * 128], yy[0:32, 2, half * 512:(half + 1) * 512], True, True,
                           ["PTc", ("yy", 2)], [f"ps{b}"])
                        TTo("dve", XC[:, ct, half * 512:(half + 1) * 512], XC[:, ct, half * 512:(half + 1) * 512], PS[b][:, 0:512], ALU.add,
                            [f"ps{b}", ("XC", ct)], [("XC", ct)])
        P.barrier()

    for s in range(S):
        for tile in range(16):
            P.dma("sp", X[:, tile, :], x_d[s, tile * 128:(tile + 1) * 128, :], [], [("X", tile)])
        for ct in range(2):
            P.dma("sp", XC[:, ct, :], ctx_d[s, ct * 128:(ct + 1) * 128, :], [], [("XC", ct)])
        for l in layers:
            last = (l == 3)
            if DBG['mix']:
                if l % 2 == 0:
                    even_mixer(s, l, not last)
                else:
                    odd_mixer(s, l, not last)
            if DBG['moe']:
                moe(s, l, not last)
        P.barrier()
        mem["p"] = phase_base
        fg = sb("fg", [128, D])
        junk = sb("junk", [128, D], BF16)
        ot = [sb("ot0", [128, D]), sb("ot1", [128, D])]
        if final:
            P.dma("sp", fg[:], fg_d, [], ["fg"])
        for tile in range(16):
            src, sk = xsrc(tile)
            o_ = ot[tile % 2]
            okey = f"ot{tile % 2}"
            if final:
                rms_rstd(src, sk, junk[:], D)
                STT("dve", o_[:], src, rstd[:, 0:1], fg[:], ALU.mult, ALU.mult, [sk, "rstd", "fg"], [okey])
            else:
                CP("dve", o_[:], src, [sk], [okey])
            P.dma("sp", out_d[s, tile * 128:(tile + 1) * 128, :], o_[:], [okey], [])
        if debug_ctx:
            for ct in range(2):
                P.dma("sp", outc_d[s, ct * 128:(ct + 1) * 128, :], XC[:, ct, :], [("XC", ct)], [])
        P.barrier()
    P.wait_dmas("sp")
    P.run()
    return nc, P


def _consts():
    c = {}
    c["c_ident"] = np.eye(128, dtype=np.float32)
    lo = np.eye(128, dtype=np.float32); lo[64:, :] = 0
    hi = np.eye(128, dtype=np.float32); hi[:64, :] = 0
    c["c_ident_lo"] = lo
    c["c_ident_hi"] = hi
    c["c_iota_j"] = np.broadcast_to(np.arange(256, dtype=np.float32)[None, :], (128, 256)).copy()
    p = np.arange(128, dtype=np.float32)
    c["c_iota_p"] = np.stack([p, p + 128, p, p], axis=1).copy()
    s_ = np.arange(128)[:, None]
    t_ = np.arange(128)[None, :]
    same = (s_ // 64) == (t_ // 64)
    c["c_maskf"] = (same & (s_ <= t_)).astype(np.float32)
    c["c_maskb"] = (same & (s_ >= t_)).astype(np.float32)
    t = np.arange(T)
    rows = (t // 64).astype(np.float32)
    colsf = (t % 64).astype(np.float32)
    inv = (10000.0 ** (-np.arange(16, dtype=np.float32) / 16)).astype(np.float32)
    ang = np.concatenate([rows[:, None] * inv, colsf[:, None] * inv], axis=-1).astype(np.float32)
    cos = np.cos(ang).astype(np.float32).T
    sin = np.sin(ang).astype(np.float32).T
    cos64 = np.concatenate([cos, cos], axis=0)
    sin64 = np.concatenate([-sin, sin], axis=0)
    c["rope_cos"] = np.concatenate([cos64, cos64], axis=0).astype(np.float32)
    c["rope_sin"] = np.concatenate([sin64, sin64], axis=0).astype(np.float32)
    return c


def _swap_perm(base):
    idx = []
    for m in range(8):
        o = base + m * 64
        idx += list(range(o + 32, o + 64)) + list(range(o, o + 32))
    return np.array(idx)


def _na_bias_table(rpb):
    kc = np.arange(64)[:, None]
    qc = np.arange(64)[None, :]
    cstart = np.clip(qc - 8, 0, 48)
    valid = (kc >= cstart) & (kc < cstart + 16)
    dcol = np.clip(kc - qc + 15, 0, 30)
    tab = np.full((64, 8, 16, 64), -3750.0, dtype=np.float32)
    g = rpb[:, :, dcol]
    g = np.transpose(g, (2, 0, 1, 3))
    vm = np.broadcast_to(valid[:, None, None, :], g.shape)
    tab[:, :, 0:15, :] = np.where(vm, g, np.float32(-3750.0))
    return np.concatenate([tab, tab], axis=0)


def prep_shared(inp, layers=(0, 1, 2, 3)):
    layers = list(layers) if len(layers) else [0]
    f = lambda a: np.ascontiguousarray(np.asarray(a, dtype=np.float32))
    d = dict(_consts())
    d["w_mod"] = f(np.asarray(inp["w_mod"])[layers])
    d["b_modT"] = f(np.transpose(np.asarray(inp["b_mod"]).reshape(4, 48, 128), (2, 0, 1)))
    d["norm_gT"] = f(np.transpose(np.asarray(inp["norm_g"]).reshape(4, 2, 8, 128), (3, 0, 1, 2)))
    w_in = np.asarray(inp["att_w_in"])
    d["att_w"] = f(np.concatenate([w_in, w_in[:, :, _swap_perm(512)], w_in[:, :, _swap_perm(2048)]], axis=2))
    d["att_wo"] = f(inp["att_w_out"])
    d["na_bias"] = f(np.stack([_na_bias_table(np.asarray(inp["na_rpb"])[e]) for e in range(2)], axis=0))
    d["lam_row"] = f(np.asarray(inp["diff_lambda"]).reshape(2, 1, 256))
    d["subln_bc"] = f(np.broadcast_to(np.asarray(inp["diff_subln_g"])[:, None, :], (2, 128, 128)))
    d["rec_w_in"] = f(inp["rec_w_in"])
    d["rec_wo"] = f(inp["rec_w_out"])
    d["rec_lbT"] = f(np.transpose(np.asarray(inp["rec_lb_logits"]).reshape(2, 4, 8, 128), (3, 0, 1, 2)))
    d["rec_gn"] = f(np.asarray(inp["rec_gnorm_g"]).T)
    d["router"] = f(np.asarray(inp["moe_router"])[layers])
    nex = max(1, DBG["experts"])
    d["wg"] = f(np.asarray(inp["moe_w_gate"])[layers][:, :nex])
    d["wu"] = f(np.asarray(inp["moe_w_up"])[layers][:, :nex])
    d["wd"] = f(np.asarray(inp["moe_w_down"])[layers][:, :nex])
    d["finalg_bc"] = f(np.broadcast_to(np.asarray(inp["final_g"])[None, :], (128, D)))
    return d


def prep_core(inp, samples):
    f = lambda a: np.ascontiguousarray(np.asarray(a, dtype=np.float32))
    x = np.asarray(inp["x"])
    ctx = np.asarray(inp["ctx"])
    c = np.asarray(inp["c"])
    cv = np.concatenate([c[samples], np.asarray(inp["c_ctx"])[None, :]], axis=0)
    V = cv.shape[0]
    return {
        "x": f(x[samples]),
        "ctx": f(ctx[samples]),
        "cvT": f(np.transpose(cv.reshape(V, 8, 128), (2, 1, 0))),
    }


_CACHE = {}


def kernel(**inputs):
    n_cores = 8
    S = 2
    if "nc" not in _CACHE:
        _CACHE["nc"] = build(S, [0, 1, 2, 3], final=True)[0]
    nc = _CACHE["nc"]
    shared = prep_shared(inputs)
    in_maps = []
    for i in range(n_cores):
        m = dict(shared)
        m.update(prep_core(inputs, list(range(i * S, (i + 1) * S))))
        in_maps.append(m)
    res = run_bass_kernel_spmd(nc, in_maps, core_ids=list(range(n_cores)))
    return np.concatenate([r["out"] for r in res.results], axis=0).astype(np.float32)
```

```python
import math
import numpy as np
import concourse.bass as bass
import concourse.mybir as mybir
from concourse.bass_utils import run_bass_kernel_spmd

F32 = mybir.dt.float32
BF16 = mybir.dt.bfloat16
AF = mybir.ActivationFunctionType
ALU = mybir.AluOpType

EPOCH = 28800


class Stream:
    def __init__(self, prog, name):
        self.prog = prog
        self.name = name
        self.sems = []
        self.n = 0

    def sem_for(self, e):
        while len(self.sems) <= e:
            self.sems.append(self.prog.nc.alloc_semaphore(f"{self.name}_{len(self.sems)}"))
        return self.sems[e]

    def bump(self, inc):
        e = self.n // EPOCH
        assert (self.n + inc - 1) // EPOCH == e
        self.n += inc
        return self.sem_for(e), (self, self.n)

    def loc(self, n):
        e = (n - 1) // EPOCH
        return self.sem_for(e), n - e * EPOCH


class Prog:
    ENGS = ("pe", "act", "dve", "pool", "sp")

    def __init__(self, nc, n_dma_sems=4):
        self.nc = nc
        self.q = {e: [] for e in self.ENGS}
        self.stream = {e: Stream(self, "s_" + e) for e in self.ENGS}
        self.seen = {e: {} for e in self.ENGS}
        self.last_w = {}
        self.readers = {}
        self.dma_streams = {}
        self.dma_rr = {}
        self.n_dma_sems = n_dma_sems
        self.ninst = 0

    def _deps(self, reads, writes):
        deps = []
        for k in reads:
            t = self.last_w.get(k)
            if t is not None:
                deps.append(t)
        for k in writes:
            t = self.last_w.get(k)
            if t is not None:
                deps.append(t)
            deps.extend(self.readers.get(k, ()))
        return deps

    def _commit(self, tok, reads, writes):
        for k in writes:
            self.last_w[k] = tok
            self.readers[k] = []
        for k in reads:
            if k in writes:
                continue
            self.readers.setdefault(k, []).append(tok)

    def _waits(self, eng, deps, skip_self=False):
        seen = self.seen[eng]
        best = {}
        for (st, n) in deps:
            if skip_self and st is self.stream[eng]:
                continue
            if seen.get(st, 0) >= n:
                continue
            if best.get(st, 0) < n:
                best[st] = n
        waits = []
        for st, n in best.items():
            seen[st] = n
            waits.append(st.loc(n))
        return waits

    def op(self, eng, fn, reads=(), writes=()):
        reads = tuple(reads)
        writes = tuple(writes) + tuple(k for k in reads if isinstance(k, str) and k.startswith("ps") and k[2:].isdigit())
        deps = self._deps(reads, writes)
        waits = self._waits(eng, deps, skip_self=(eng == "pe"))
        sem, tok = self.stream[eng].bump(1)

        def emit(E, waits=waits, fn=fn, sem=sem):
            for (s, v) in waits:
                E.wait_ge(s, v)
            fn(E).then_inc(sem, 1)

        self.q[eng].append(emit)
        self._commit(tok, reads, writes)
        self.ninst += 1
        return tok

    def dma(self, queue, out, in_, reads=(), writes=()):
        reads = tuple(reads)
        writes = tuple(writes)
        deps = self._deps(reads, writes)
        if queue not in self.dma_streams:
            self.dma_streams[queue] = [Stream(self, f"d_{queue}{i}") for i in range(self.n_dma_sems)]
            self.dma_rr[queue] = 0
        i = self.dma_rr[queue]
        self.dma_rr[queue] = (i + 1) % self.n_dma_sems
        st = self.dma_streams[queue][i]
        if st.n > 0:
            deps.append((st, st.n))
        waits = self._waits(queue, deps)
        sem, tok = st.bump(16)

        def emit(E, waits=waits, sem=sem, out=out, in_=in_):
            for (s, v) in waits:
                E.wait_ge(s, v)
            E.dma_start(out=out, in_=in_).then_inc(sem, 16)

        self.q[queue].append(emit)
        self._commit(tok, reads, writes)
        self.ninst += 1
        return tok

    def all_tokens(self):
        toks = []
        for e in self.ENGS:
            if self.stream[e].n:
                toks.append((self.stream[e], self.stream[e].n))
        for q, sts in self.dma_streams.items():
            for st in sts:
                if st.n:
                    toks.append((st, st.n))
        return toks

    def barrier(self):
        toks = self.all_tokens()
        for eng in self.ENGS:
            waits = self._waits(eng, toks)

            def emit(E, waits=waits):
                for (s, v) in waits:
                    E.wait_ge(s, v)

            self.q[eng].append(emit)

    def wait_dmas(self, eng):
        toks = [(st, st.n) for q, sts in self.dma_streams.items() for st in sts if st.n]
        waits = self._waits(eng, toks)

        def emit(E, waits=waits):
            for (s, v) in waits:
                E.wait_ge(s, v)

        self.q[eng].append(emit)

    def run(self):
        nc = self.nc
        with nc.Block() as block:
            @block.tensor
            def _(E):
                for f in self.q["pe"]:
                    f(E)

            @block.scalar
            def _(E):
                for f in self.q["act"]:
                    f(E)

            @block.vector
            def _(E):
                for f in self.q["dve"]:
                    f(E)

            @block.gpsimd
            def _(E):
                for f in self.q["pool"]:
                    f(E)

            @block.sync
            def _(E):
                for f in self.q["sp"]:
                    f(E)


DBG = {'na': 1, 'diff': 1, 'mix': 1, 'moe': 1, 'experts': 16, 'nagroups': 2, 'nqr': 32}
D = 1024
T = 2048
L = 256
TT = T + L
EPS = 1e-6
NE = 16
FF = 2048


def build(S, layers, final=True, debug_ctx=False):
    nc = bass.Bass("TRN2", target_bir_lowering=False)
    P = Prog(nc)
    V = S + 1

    def din(name, shape):
        return nc.dram_tensor(name, list(shape), F32, kind="ExternalInput").ap()

    x_d = din("x", [S, T, D])
    ctx_d = din("ctx", [S, L, D])
    cvT_d = din("cvT", [128, 8, V])
    NL = max(1, len(layers))
    LI = {l: i for i, l in enumerate(layers)}
    wmod_d = din("w_mod", [NL, D, 6 * D])
    bmT_d = din("b_modT", [128, 4, 48])
    ngT_d = din("norm_gT", [128, 4, 2, 8])
    attw_d = din("att_w", [2, D, 4096])
    attwo_d = din("att_wo", [2, D, D])
    nab_d = din("na_bias", [2, 128, 8, 16, 64])
    lam_d = din("lam_row", [2, 1, 256])
    subln_d = din("subln_bc", [2, 128, 128])
    cos_d = din("rope_cos", [128, T])
    sin_d = din("rope_sin", [128, T])
    recw_d = din("rec_w_in", [2, D, 5 * D])
    recwo_d = din("rec_wo", [2, D, D])
    lbT_d = din("rec_lbT", [128, 2, 4, 8])
    gn_d = din("rec_gn", [128, 2])
    router_d = din("router", [NL, D, NE])
    NEX = max(1, DBG["experts"])
    wg_d = din("wg", [NL, NEX, D, FF])
    wu_d = din("wu", [NL, NEX, D, FF])
    wd_d = din("wd", [NL, NEX, FF, D])
    fg_d = din("finalg_bc", [128, D])
    cid_d = din("c_ident", [128, 128])
    cij_d = din("c_iota_j", [128, 256])
    cip_d = din("c_iota_p", [128, 4])
    cmf_d = din("c_maskf", [128, 128])
    cmb_d = din("c_maskb", [128, 128])
    cil_d = din("c_ident_lo", [128, 128])
    cih_d = din("c_ident_hi", [128, 128])
    out_d = nc.dram_tensor("out", [S, T, D], F32, kind="ExternalOutput").ap()
    outc_d = nc.dram_tensor("out_ctx", [S, L, D], F32, kind="ExternalOutput").ap() if debug_ctx else None
    dbg_d = nc.dram_tensor("dbg", [16, 128, 512], F32, kind="ExternalOutput").ap() if debug_ctx else None

    base = (nc.sbuf_base + 63) // 64 * 64
    lim = nc.sbuf_top
    mem = {"p": base, "n": 0}

    def sb(name, shape, dt=F32):
        nb = int(np.prod(shape[1:])) * (2 if dt == BF16 else 4)
        nb = (nb + 63) // 64 * 64
        off = mem["p"]
        assert off + nb <= lim, f"SBUF overflow at {name}: {off + nb} > {lim}"
        mem["p"] = off + nb
        mem["n"] += 1
        return nc.alloc_sbuf_tensor_at(f"{name}_{mem['n']}", list(shape), dt, offset=off)

    PS = [nc.alloc_psum_tensor(f"ps{i}", [128, 512], F32) for i in range(8)]
    PSB = [p[:].bitcast(BF16) for p in PS]

    def MM(out, lhsT, rhs, st, sp, r, w):
        P.op("pe", lambda E: E.matmul(out, lhsT=lhsT, rhs=rhs, start=st, stop=sp), r, w)

    def TR(out, in_, idn, r, w):
        P.op("pe", lambda E: E.transpose(out=out, in_=in_, identity=idn), r, w)

    def ACT(out, in_, func, r, w, scale=None, bias=None, accum=None):
        kw = {}
        if scale is not None:
            kw["scale"] = scale
        if bias is not None:
            kw["bias"] = bias
        if accum is not None:
            kw["accum_out"] = accum
        P.op("act", lambda E: E.activation(out=out, in_=in_, func=func, **kw), r, w)

    def TS(eng, out, in0, s1, s2, op0, op1, r, w):
        if s2 is None:
            P.op(eng, lambda E: E.tensor_scalar(out=out, in0=in0, scalar1=s1, scalar2=None, op0=op0), r, w)
        else:
            P.op(eng, lambda E: E.tensor_scalar(out=out, in0=in0, scalar1=s1, scalar2=s2, op0=op0, op1=op1), r, w)

    def TTo(eng, out, in0, in1, op, r, w):
        P.op(eng, lambda E: E.tensor_tensor(out=out, in0=in0, in1=in1, op=op), r, w)

    def STT(eng, out, in0, sc, in1, op0, op1, r, w):
        P.op(eng, lambda E: E.scalar_tensor_tensor(out=out, in0=in0, scalar=sc, in1=in1, op0=op0, op1=op1), r, w)

    def CP(eng, out, in_, r, w):
        if eng == "act":
            P.op("act", lambda E: E.copy(out=out, in_=in_), r, w)
        else:
            P.op(eng, lambda E: E.tensor_copy(out=out, in_=in_), r, w)

    def MS(eng, ap, v, w):
        P.op(eng, lambda E: E.memset(ap, v), [], w)

    dbgbuf = {}

    def dump(i, ap, keys, n):
        if dbg_d is None:
            return
        if "t" not in dbgbuf:
            dbgbuf["t"] = nc.alloc_sbuf_tensor_at("dbgt", [128, 512], F32, offset=(lim - 4096) // 64 * 64)
        t = dbgbuf["t"]
        rows = ap.shape[0]
        P.op("dve", lambda E: E.memset(t[:], 0.0), [], ["dbgt"])
        P.op("dve", lambda E: E.tensor_copy(out=t[0:rows, 0:n], in_=ap), keys, ["dbgt"])
        P.dma("sp", dbg_d[i], t[:], ["dbgt"], [])

    def RCP(out, in_, r, w):
        P.op("dve", lambda E: E.reciprocal(out=out, in_=in_), r, w)

    def wview(src2d):
        return src2d.rearrange("(k p) n -> p k n", p=128)

    X = sb("X", [128, 16, D])
    XC = sb("XC", [128, 2, D])
    ident_f = sb("ident_f", [128, 128])
    ident_b = sb("ident_b", [128, 128], BF16)
    ones_f = sb("ones_f", [128, 128])
    ident_lo = sb("ident_lo", [128, 128], BF16)
    ident_hi = sb("ident_hi", [128, 128], BF16)
    iota_j = sb("iota_j", [128, 256])
    iota_p = sb("iota_p", [128, 4])
    maskf = sb("maskf", [128, 128])
    maskb = sb("maskb", [128, 128])
    modT = sb("modT", [128, 4, V, 48])
    bmT = sb("bmT", [128, 4, 48])
    ngT = sb("ngT", [128, 4, 2, 8])
    scT = sb("scT", [128, 8, V])
    lbT = sb("lbT", [128, 2, 4, 8])
    omlT = sb("omlT", [128, 2, 4, 8])
    nomlT = sb("nomlT", [128, 2, 4, 8])
    gnT = sb("gnT", [128, 2])
    am = sb("am", [128, 2, 2, 8])
    ssq = sb("ssq", [128, 1])
    rt = sb("rt", [128, 1])
    rstd = sb("rstd", [128, 1])
    neglam = sb("neglam", [128, 1])
    small = sb("small", [128, 16])
    lsum = sb("lsum", [128, 2, 8])
    phase_base = mem["p"]

    P.dma("sp", ident_f[:], cid_d, [], ["ident_f"])
    P.dma("pool", ident_b[:], cid_d, [], ["ident_b"])
    P.dma("pool", ident_lo[:], cil_d, [], ["ident_b"])
    P.dma("pool", ident_hi[:], cih_d, [], ["ident_b"])
    P.dma("sp", iota_j[:], cij_d, [], ["iota_j"])
    P.dma("sp", iota_p[:], cip_d, [], ["iota_p"])
    P.dma("sp", maskf[:], cmf_d, [], ["maskf"])
    P.dma("sp", maskb[:], cmb_d, [], ["maskb"])
    P.dma("sp", bmT[:], bmT_d, [], ["bmT"])
    P.dma("sp", ngT[:], ngT_d, [], ["ngT"])
    P.dma("sp", scT[:], cvT_d, [], ["scT"])
    P.dma("sp", lbT[:], lbT_d, [], ["lbT"])
    P.dma("sp", gnT[:], gn_d, [], ["gnT"])
    MS("pool", ones_f[:], 1.0, ["ones_f"])
    ACT(scT[:], scT[:], AF.Silu, ["scT"], ["scT"])

    ACT(lbT[:], lbT[:], AF.Exp, ["lbT"], ["lbT"])
    TTo("dve", lsum[:], lbT[:, :, 0, :], lbT[:, :, 1, :], ALU.add, ["lbT"], ["lsum"])
    TTo("dve", lsum[:], lsum[:], lbT[:, :, 2, :], ALU.add, ["lbT", "lsum"], ["lsum"])
    TTo("dve", lsum[:], lsum[:], lbT[:, :, 3, :], ALU.add, ["lbT", "lsum"], ["lsum"])
    RCP(lsum[:], lsum[:], ["lsum"], ["lsum"])
    for j in range(4):
        TTo("dve", lbT[:, :, j, :], lbT[:, :, j, :], lsum[:], ALU.mult, ["lbT", "lsum"], ["lbT"])
    TTo("dve", lbT[:, :, 2, :], lbT[:, :, 2, :], lbT[:, :, 1, :], ALU.add, ["lbT"], ["lbT"])
    TTo("dve", lbT[:, :, 3, :], lbT[:, :, 3, :], lbT[:, :, 2, :], ALU.add, ["lbT"], ["lbT"])
    MS("dve", lbT[:, :, 0, :], 0.0, ["lbT"])
    TS("dve", omlT[:], lbT[:], -1.0, 1.0, ALU.mult, ALU.add, ["lbT"], ["omlT"])
    TS("dve", nomlT[:], omlT[:], -1.0, None, ALU.mult, None, ["omlT"], ["nomlT"])

    mem["p"] = phase_base
    wm = [sb("wm0", [128, 8, 512]), sb("wm1", [128, 8, 512])]
    it = 0
    for l in layers:
        for jb in list(range(12)) + [0]:
            w_ = wm[it % 2]
            wk = f"wm{it % 2}"
            P.dma("sp", w_[:], wview(wmod_d[LI[l], :, jb * 512:(jb + 1) * 512]), [], [wk])
            pb = PS[it % 2]
            pk = f"ps{it % 2}"
            for j in range(4):
                for k in range(8):
                    MM(pb[:, j * V:(j + 1) * V], w_[:, k, j * 128:(j + 1) * 128], scT[:, k, :], k == 0, k == 7,
                       [wk, "scT"], [pk])
            TTo("dve", modT[:, l, :, jb * 4:(jb + 1) * 4],
                pb[:, 0:4 * V].rearrange("p (j v) -> p v j", v=V),
                bmT[:, l, jb * 4:(jb + 1) * 4].unsqueeze(1).to_broadcast([128, V, 4]),
                ALU.add, [pk, "bmT"], ["modT"])
            it += 1
    P.barrier()

    def mslice(l, v, i):
        return modT[:, l, v, i * 8:(i + 1) * 8]

    def rms_rstd(src, srckey, junk, d):
        ACT(junk, src, AF.Square, [srckey], ["junk", "ssq"], accum=ssq[:])
        ACT(rt[:], ssq[:], AF.Sqrt, ["ssq"], ["rt"], scale=1.0 / d, bias=EPS)
        RCP(rstd[:], rt[:], ["rt"], ["rstd"])

    def xsrc(tile):
        if tile < 16:
            return X[:, tile, :], ("X", tile)
        return XC[:, tile - 16, :], ("XC", tile - 16)

    def build_G(dst, gcol, gtmp, dkey):
        for c in range(8):
            TS("dve", gtmp[:, c, :], ones_f[:], gcol[:, c:c + 1], None, ALU.mult, None, ["ones_f", "modT"], [("gtmp", c)])
            MM(PS[6 + c // 4][:, (c % 4) * 128:(c % 4 + 1) * 128], gtmp[:, c, :], ident_f[:], True, True,
               [("gtmp", c), "ident_f"], [f"ps{6 + c // 4}"])
        CP("act", dst[:, 0:512], PS[6][:, :], ["ps6"], [dkey])
        CP("act", dst[:, 512:1024], PS[7][:, :], ["ps7"], [dkey])

    def residual_add(tile, half, pbank, pkey, G, gkey, tmp, tmpkey):
        dst, dk = xsrc(tile)
        TTo("dve", tmp[:], pbank[:, 0:512], G[:, half * 512:(half + 1) * 512], ALU.mult, [pkey, gkey], [tmpkey])
        TTo("pool", dst[:, half * 512:(half + 1) * 512], dst[:, half * 512:(half + 1) * 512], tmp[:], ALU.add,
            [tmpkey, dk], [dk])

    def residual_add_direct(tile, half, pbank, pkey):
        dst, dk = xsrc(tile)
        TTo("dve", dst[:, half * 512:(half + 1) * 512], dst[:, half * 512:(half + 1) * 512], pbank[:, 0:512], ALU.add,
            [pkey, dk], [dk])

    def compute_hT(s, l, hT, xnb, junk):
        for vi, v in enumerate((s, S)):
            STT("dve", am[:, 0, vi, :], mslice(l, v, 1), 1.0, ngT[:, l, 0, :], ALU.add, ALU.mult, ["modT", "ngT"], ["am"])
        for tile in range(18):
            vi = 0 if tile < 16 else 1
            v = s if tile < 16 else S
            src, sk = xsrc(tile)
            rms_rstd(src, sk, junk[:], D)
            TS("dve", xnb[:], src, rstd[:, 0:1], None, ALU.mult, None, [sk, "rstd"], ["xnb"])
            pb = PSB[tile % 2]
            pk = f"ps{tile % 2}"
            for c in range(8):
                TR(pb[:, c * 128:(c + 1) * 128], xnb[:, c * 128:(c + 1) * 128], ident_b[:], ["xnb", "ident_b"], [pk])
            for c in range(8):
                dst = hT[:, c, tile * 128:(tile + 1) * 128]
                if False:
                    pass
                else:
                    TS("dve", dst, pb[:, c * 128:(c + 1) * 128], am[:, 0, vi, c:c + 1], mslice(l, v, 0)[:, c:c + 1],
                       ALU.mult, ALU.add, [pk, "am", "modT"], [("hT", tile)])

    def hkeys(t0, t1):
        return [("hT", t) for t in range(t0, t1)]

    def proj_fm(dst_fn, W, wkey, wcols, hT, evac):
        for tb in range(5):
            n = 512 if tb < 4 else 256
            c0 = tb * 512
            pb = PS[tb % 2]
            pk = f"ps{tb % 2}"
            for k in range(8):
                MM(pb[:, 0:n], W[:, k, wcols], hT[:, k, c0:c0 + n], k == 0, k == 7,
                   [wkey] + hkeys(c0 // 128, (c0 + n) // 128), [pk])
            evac(tb, c0, n, pb, pk)

    def even_mixer(s, l, with_ctx):
        e = l // 2
        lam_init = 0.8 - 0.6 * math.exp(-0.3 * l)
        mem["p"] = phase_base
        hT = sb("hT", [128, 8, TT], BF16)
        xnb = sb("xnb", [128, D], BF16)
        junk = sb("junk", [128, D], BF16)
        G1 = sb("G1", [128, D])
        G1c = sb("G1c", [128, D])
        gtmp = sb("gtmp", [128, 8, 128])
        tmpy = sb("tmpy", [128, 512])
        sg_bc = sb("sg_bc", [128, 128])
        lr = sb("lr", [1, 256])
        grp_base = mem["p"]
        build_G(G1, mslice(l, s, 2), gtmp, "G1")
        build_G(G1c, mslice(l, S, 2), gtmp, "G1c")
        compute_hT(s, l, hT, xnb, junk)
        P.dma("sp", lr[:], lam_d[e], [], ["lr"])
        TTo("dve", lr[0:1, 0:64], lr[0:1, 0:64], lr[0:1, 64:128], ALU.mult, ["lr"], ["lr"])
        TTo("dve", lr[0:1, 128:192], lr[0:1, 128:192], lr[0:1, 192:256], ALU.mult, ["lr"], ["lr"])
        ACT(lr[0:1, 64:128], lr[0:1, 0:64], AF.Identity, ["lr"], ["lr", "small"], accum=small[0:1, 0:1])
        ACT(lr[0:1, 192:256], lr[0:1, 128:192], AF.Identity, ["lr"], ["lr", "small"], accum=small[0:1, 1:2])
        ACT(small[0:1, 0:2], small[0:1, 0:2], AF.Exp, ["small"], ["small"])
        TTo("dve", small[0:1, 2:3], small[0:1, 1:2], small[0:1, 0:1], ALU.subtract, ["small"], ["small"])
        TS("dve", small[0:1, 3:4], small[0:1, 2:3], -lam_init, None, ALU.add, None, ["small"], ["small"])
        MM(PS[7][:, 0:1], ones_f[0:1, :], small[0:1, 3:4], True, True, ["ones_f", "small"], ["ps7"])
        CP("dve", neglam[:], PS[7][:, 0:1], ["ps7"], ["neglam"])
        P.dma("sp", sg_bc[:], subln_d[e], [], ["sg_bc"])
        TS("dve", sg_bc[:], sg_bc[:], 1.0 - lam_init, None, ALU.mult, None, ["sg_bc"], ["sg_bc"])

        ntile_out = 18 if with_ctx else 16

        def out_proj(oT, nch, Wo, wokey):
            WoL = sb("WoL", [128, nch, D], BF16)
            WoC = sb("WoC", [128, nch, D], BF16)
            for ci in range(nch):
                TTo("pool", WoL[:, ci, :], Wo[:, ci, :], G1[:], ALU.mult, [wokey, "G1"], ["WoL"])
                TTo("pool", WoC[:, ci, :], Wo[:, ci, :], G1c[:], ALU.mult, [wokey, "G1c"], ["WoC"])
            for tile in range(ntile_out):
                W_, wk_ = (WoL, "WoL") if tile < 16 else (WoC, "WoC")
                for half in range(2):
                    pb = PS[5 + half]
                    pk = f"ps{5 + half}"
                    for ci in range(nch):
                        MM(pb[:, 0:512], oT[:, ci, tile * 128:(tile + 1) * 128], W_[:, ci, half * 512:(half + 1) * 512],
                           ci == 0, ci == nch - 1, [("oT", tile), wk_], [pk])
                    residual_add_direct(tile, half, pb, pk)

        for g in range(DBG['nagroups'] if DBG['na'] else 0):
            P.barrier()
            mem["p"] = grp_base
            Wq = sb("Wq", [128, 8, 256], BF16)
            Wk = sb("Wk", [128, 8, 256], BF16)
            Wv = sb("Wv", [128, 8, 256], BF16)
            Wo = sb("Wo", [128, 2, D], BF16)
            Ball = sb("Ball", [128, 4, 16, 64], BF16)
            qaT = sb("qaT", [128, 2, TT], BF16)
            kaT = sb("kaT", [128, 2, TT], BF16)
            va = sb("va", [128, 18, 4, 65], BF16)
            oT = sb("oT", [128, 2, TT], BF16)
            Eb = [sb("E0", [128, 512], BF16), sb("E1", [128, 512], BF16)]
            rc = sb("rc", [128, 4])
            otile = sb("otile", [128, 256], BF16)
            P.dma("pool", Wq[:], wview(attw_d[e, :, g * 256:(g + 1) * 256]), [], ["Wq"])
            P.dma("pool", Wk[:], wview(attw_d[e, :, 1024 + g * 256:1024 + (g + 1) * 256]), [], ["Wk"])
            P.dma("pool", Wv[:], wview(attw_d[e, :, 1536 + g * 256:1536 + (g + 1) * 256]), [], ["Wv"])
            P.dma("pool", Wo[:], attwo_d[e, g * 256:(g + 1) * 256, :].rearrange("(c p) n -> p c n", p=128), [], ["Wo"])
            P.dma("pool", Ball[:], nab_d[e, :, 4 * g:4 * g + 4, :, :], [], ["Ball"])
            ACT(Ball[:], Ball[:], AF.Copy, ["Ball"], ["Ball"], scale=8.0)
            MS("pool", va[:, :, :, 64:65], 1.0, ["va1"])
            for ci in range(2):
                def ev_q(tb, c0, n, pb, pk, ci=ci):
                    CP("act", qaT[:, ci, c0:c0 + n], pb[:, 0:n], [pk], [("qaT", ci, tb)])

                def ev_k(tb, c0, n, pb, pk, ci=ci):
                    CP("dve", kaT[:, ci, c0:c0 + n], pb[:, 0:n], [pk], [("kaT", ci, tb)])
                proj_fm(None, Wq, "Wq", slice(ci * 128, (ci + 1) * 128), hT, ev_q)
                proj_fm(None, Wk, "Wk", slice(ci * 128, (ci + 1) * 128), hT, ev_k)
            for tile in range(18):
                pb = PS[tile % 2]
                pk = f"ps{tile % 2}"
                for k in range(8):
                    MM(pb[:, 0:256], hT[:, k, tile * 128:(tile + 1) * 128], Wv[:, k, :], k == 0, k == 7,
                       ["Wv", ("hT", tile)], [pk])
                CP("act" if tile % 2 else "dve", va[:, tile, :, 0:64], pb[:, 0:256].rearrange("p (h d) -> p h d", d=64),
                   [pk], [("va", tile)])
            if g == 0 and DBG.get('dump'):
                dump(9, modT[:, l, s, :], ["modT"], 48)
                dump(10, am[:, :, :, :].rearrange("p a b c -> p (a b c)"), ["am"], 32)
                dump(11, bmT[:, l, :], ["bmT"], 48)
                dump(0, hT[:, 0, 0:512], hkeys(0, 4), 512)
                dump(1, G1[:, 0:512], ["G1"], 512)
                dump(2, qaT[:, 0, 0:512], [("qaT", 0, 0)], 512)
                dump(3, kaT[:, 0, 0:512], [("kaT", 0, 0)], 512)
                dump(4, va[:, 0, :, :].rearrange("p h d -> p (h d)"), [("va", 0), "va1"], 260)
                dump(5, Ball[:, 0, 3, :], ["Ball"], 64)
            qkeys = lambda ci: [("qaT", ci, tb) for tb in range(5)]
            kkeys = lambda ci: [("kaT", ci, tb) for tb in range(5)]
            na_steps = []
            for qr in range(DBG['nqr']):
                rs = min(max(qr - 4, 0), 24)
                t0 = rs // 2
                tiles = list(range(t0, t0 + (4 if rs % 2 == 0 else 5)))
                for hh in range(4):
                    na_steps.append((qr, hh, rs, tiles))

            def na_S(n):
                qr, hh, rs, tiles = na_steps[n]
                ci, hf = hh // 2, hh % 2
                pr = slice(hf * 64, (hf + 1) * 64)
                sbk = PS[n % 2]
                sk = f"ps{n % 2}"
                q_ap = qaT[pr, ci, qr * 64:(qr + 1) * 64]
                slots = []
                for si, tl in enumerate(tiles):
                    idx = []
                    for kr in (2 * tl, 2 * tl + 1):
                        idx.append(kr - qr + 7 if rs <= kr < rs + 8 else 15)
                    so = sbk[:, si * 64:(si + 1) * 64]
                    MM(so, kaT[pr, ci, tl * 128:(tl + 1) * 128], q_ap, True, False, qkeys(ci) + kkeys(ci), [sk])
                    MM(so, ident_lo[:, :], Ball[:, hh, idx[0], :], False, False, ["ident_b", "Ball"], [sk])
                    MM(so, ident_hi[:, :], Ball[:, hh, idx[1], :], False, True, ["ident_b", "Ball"], [sk])
                    slots.append(tl)
                for cj in range(2):
                    si = len(tiles) + cj
                    MM(sbk[:, si * 64:(si + 1) * 64], kaT[pr, ci, T + cj * 128:T + (cj + 1) * 128], q_ap, True, True,
                       qkeys(ci) + kkeys(ci), [sk])
                    slots.append(16 + cj)
                ns = len(slots)
                ACT(Eb[n % 2][:, 0:ns * 64], sbk[:, 0:ns * 64], AF.Exp, [sk], [f"E{n % 2}"], scale=0.125)
                return slots

            def na_PV(n, slots):
                qr, hh, rs, tiles = na_steps[n]
                ob = PS[2 + qr % 2]
                ok = f"ps{2 + qr % 2}"
                E_ = Eb[n % 2]
                ek = f"E{n % 2}"
                ns = len(slots)
                for si, tl in enumerate(slots):
                    MM(ob[0:64, hh * 65:(hh + 1) * 65], E_[:, si * 64:(si + 1) * 64], va[:, tl, hh, :], si == 0, si == ns - 1,
                       [ek, ("va", tl), "va1"], [ok])
                if hh == 3:
                    ov = ob[0:64, 0:260].rearrange("p (h d) -> p h d", d=65)
                    RCP(rc[0:64, :].unsqueeze(2), ov[:, :, 64:65], [ok], ["rc"])
                    TTo("dve", otile[0:64, :].rearrange("p (h d) -> p h d", d=64), ov[:, :, 0:64],
                        rc[0:64, :].unsqueeze(2).to_broadcast([64, 4, 64]), ALU.mult, [ok, "rc"], ["otile"])
                    for ci in range(2):
                        TR(PSB[4][:, ci * 64:(ci + 1) * 64], otile[0:64, ci * 128:(ci + 1) * 128], ident_b[0:64, 0:64],
                           ["otile", "ident_b"], ["ps4"])
                    for ci in range(2):
                        CP("dve", oT[:, ci, qr * 64:(qr + 1) * 64], PSB[4][:, ci * 64:(ci + 1) * 64], ["ps4"], [("oT", qr // 2)])

            pend_slots = {}
            if na_steps:
                pend_slots[0] = na_S(0)
            for n in range(len(na_steps)):
                if n + 1 < len(na_steps):
                    pend_slots[n + 1] = na_S(n + 1)
                na_PV(n, pend_slots.pop(n))
            if with_ctx:
                for hh in range(4):
                    ci, hf = hh // 2, hh % 2
                    pr = slice(hf * 64, (hf + 1) * 64)
                    sbk = PS[hh % 2]
                    sk = f"ps{hh % 2}"
                    for cj in range(2):
                        MM(sbk[:, cj * 256:(cj + 1) * 256], kaT[pr, ci, T + cj * 128:T + (cj + 1) * 128], qaT[pr, ci, T:TT],
                           True, True, qkeys(ci) + kkeys(ci), [sk])
                    E_ = Eb[hh % 2]
                    ek = f"E{hh % 2}"
                    ACT(E_[:, 0:512], sbk[:, 0:512], AF.Exp, [sk], [ek], scale=0.125)
                    for qt in range(2):
                        for cj in range(2):
                            MM(PS[2 + qt][:, hh * 65:(hh + 1) * 65], E_[:, cj * 256 + qt * 128:cj * 256 + (qt + 1) * 128],
                               va[:, 16 + cj, hh, :], cj == 0, cj == 1, [ek, ("va", 16 + cj), "va1"], [f"ps{2 + qt}"])
                for qt in range(2):
                    ob = PS[2 + qt]
                    ok = f"ps{2 + qt}"
                    ov = ob[:, 0:260].rearrange("p (h d) -> p h d", d=65)
                    RCP(rc[:, :].unsqueeze(2), ov[:, :, 64:65], [ok], ["rc"])
                    TTo("dve", otile[:, :].rearrange("p (h d) -> p h d", d=64), ov[:, :, 0:64],
                        rc[:, :].unsqueeze(2).to_broadcast([128, 4, 64]), ALU.mult, [ok, "rc"], ["otile"])
                    for ci in range(2):
                        TR(PSB[4][:, ci * 128:(ci + 1) * 128], otile[:, ci * 128:(ci + 1) * 128], ident_b[:],
                           ["otile", "ident_b"], ["ps4"])
                    CP("act", oT[:, :, T + qt * 128:T + (qt + 1) * 128], PSB[4][:, 0:256].rearrange("p (c t) -> p c t", t=128),
                       ["ps4"], [("oT", 16 + qt)])
            if g == 0 and DBG.get('dump'):
                dump(6, oT[:, 0, 0:512], [("oT", t) for t in range(4)], 512)
                dump(7, Eb[0][:, 0:512], ["E0"], 512)
                dump(8, otile[:, :], ["otile"], 256)
            out_proj(oT, 2, Wo, "Wo")

        for hb in range(4 if DBG['diff'] else 0):
            P.barrier()
            mem["p"] = grp_base
            W5 = sb("W5", [128, 8, 5, 128], BF16)
            Wo = sb("Wo1", [128, 1, D], BF16)
            cosT = sb("cosT", [128, T], BF16)
            sinT = sb("sinT", [128, T], BF16)
            qbT = sb("qbT", [128, TT], BF16)
            kbT = sb("kbT", [128, TT], BF16)
            vb = sb("vb", [128, 18, 129], BF16)
            oT = sb("oTb", [128, 1, TT], BF16)
            Eb = [sb(f"E{i}", [128, 512], BF16) for i in range(4)]
            SBK = [0, 1, 6, 7]
            t1 = sb("t1", [128, 512])
            t2 = sb("t2", [128, 512])
            dd = sb("dd", [128, 128])
            obt = sb("obt", [128, 128], BF16)
            r12 = sb("r12", [128, 4])
            cols = [512 + hb * 128, 3072 + hb * 128, 2048 + hb * 128, 3584 + hb * 128, 2560 + hb * 128]
            for i, c0 in enumerate(cols):
                P.dma("pool", W5[:, :, i, :], wview(attw_d[e, :, c0:c0 + 128]), [], [("W5", i)])
            P.dma("pool", Wo[:, 0, :], attwo_d[e, 512 + hb * 128:512 + (hb + 1) * 128, :], [], ["Wo1"])
            P.dma("pool", cosT[:], cos_d, [], ["cosT"])
            P.dma("pool", sinT[:], sin_d, [], ["sinT"])
            MS("pool", vb[:, :, 128:129], 1.0, ["vb1"])
            for (dstT, dname, i_raw, i_sw) in ((qbT, "qbT", 0, 1), (kbT, "kbT", 2, 3)):
                for tb in range(5):
                    n = 512 if tb < 4 else 256
                    c0 = tb * 512
                    hk = hkeys(c0 // 128, (c0 + n) // 128)
                    for k in range(8):
                        MM(PS[0][:, 0:n], W5[:, k, i_raw, :], hT[:, k, c0:c0 + n], k == 0, k == 7, [("W5", i_raw)] + hk, ["ps0"])
                    if tb < 4:
                        for k in range(8):
                            MM(PS[1][:, 0:n], W5[:, k, i_sw, :], hT[:, k, c0:c0 + n], k == 0, k == 7, [("W5", i_sw)] + hk, ["ps1"])
                        TTo("dve", t1[:], PS[0][:, 0:n], cosT[:, c0:c0 + n], ALU.mult, ["ps0", "cosT"], ["t1"])
                        TTo("dve", t2[:], PS[1][:, 0:n], sinT[:, c0:c0 + n], ALU.mult, ["ps1", "sinT"], ["t2"])
                        TTo("pool", dstT[:, c0:c0 + n], t1[:], t2[:], ALU.add, ["t1", "t2"], [(dname, tb)])
                    else:
                        CP("act", dstT[:, c0:c0 + n], PS[0][:, 0:n], ["ps0"], [(dname, tb)])
            for tile in range(18):
                pb = PS[tile % 2]
                pk = f"ps{tile % 2}"
                for k in range(8):
                    MM(pb[:, 0:128], hT[:, k, tile * 128:(tile + 1) * 128], W5[:, k, 4, :], k == 0, k == 7,
                       [("W5", 4), ("hT", tile)], [pk])
                CP("act" if tile % 2 else "dve", vb[:, tile, 0:128], pb[:, 0:128], [pk], [("vb", tile)])
            qk_all = [("qbT", tb) for tb in range(5)] + [("kbT", tb) for tb in range(5)]

            def diff_block(qc0, nq, kts):
                nsub = nq // 128
                acc = {}
                for sub in range(nsub):
                    for m in range(2):
                        a = sub * 2 + m
                        acc[(sub, m)] = (PS[2 + a // 3][:, (a % 3) * 129:(a % 3 + 1) * 129], f"ps{2 + a // 3}")
                for bnk in sorted(set(2 + (sub * 2 + m) // 3 for sub in range(nsub) for m in range(2))):
                    MS("dve", PS[bnk][:, :], 0.0, [f"ps{bnk}"])
                steps = [(ki, kt, m) for ki, kt in enumerate(kts) for m in range(2)]

                def emit_S(i):
                    ki, kt, m = steps[i]
                    pr = slice(m * 64, (m + 1) * 64)
                    sbk = PS[SBK[i % 4]]
                    sk = f"ps{SBK[i % 4]}"
                    E_ = Eb[i % 4]
                    ek = f"E{i % 4}"
                    MM(sbk[:, 0:nq], kbT[pr, kt * 128:(kt + 1) * 128], qbT[pr, qc0:qc0 + nq], True, True, qk_all, [sk])
                    ACT(E_[:, 0:nq], sbk[:, 0:nq], AF.Exp, [sk], [ek], scale=0.125)

                def emit_PV(i):
                    ki, kt, m = steps[i]
                    E_ = Eb[i % 4]
                    ek = f"E{i % 4}"
                    for sub in range(nsub):
                        ap, akey = acc[(sub, m)]
                        MM(ap, E_[:, sub * 128:(sub + 1) * 128], vb[:, kt, :], False, ki == len(kts) - 1,
                           [ek, ("vb", kt), "vb1"], [akey])

                LA = 2
                for i in range(len(steps) + LA):
                    if i < len(steps):
                        emit_S(i)
                    if i - LA >= 0:
                        emit_PV(i - LA)
                for sub in range(nsub):
                    (o1, k1), (o2, k2) = acc[(sub, 0)], acc[(sub, 1)]
                    RCP(r12[:, 0:1], o1[:, 128:129], [k1], ["r12"])
                    RCP(r12[:, 1:2], o2[:, 128:129], [k2], ["r12"])
                    TTo("dve", r12[:, 2:3], r12[:, 1:2], neglam[:, 0:1], ALU.mult, ["r12", "neglam"], ["r12"])
                    TS("dve", t1[:, 0:128], o1[:, 0:128], r12[:, 0:1], None, ALU.mult, None, [k1, "r12"], ["t1"])
                    STT("dve", dd[:], o2[:, 0:128], r12[:, 2:3], t1[:, 0:128], ALU.mult, ALU.add, [k2, "r12", "t1"], ["dd"])
                    rms_rstd(dd[:], "dd", t2[:, 0:128], 128)
                    STT("dve", obt[:], dd[:], rstd[:, 0:1], sg_bc[:], ALU.mult, ALU.mult, ["dd", "rstd", "sg_bc"], ["obt"])
                    TR(PSB[5][:, 0:128], obt[:], ident_b[:], ["obt", "ident_b"], ["ps5"])
                    tcol = qc0 + sub * 128
                    CP("act", oT[:, 0, tcol:tcol + 128], PSB[5][:, 0:128], ["ps5"], [("oT", tcol // 128)])

            for qb_ in range(4):
                diff_block(qb_ * 512, 512, list(range(18)))
            if with_ctx:
                diff_block(T, 256, [16, 17])
            out_proj(oT, 1, Wo, "Wo1")
        P.barrier()

    def odd_mixer(s, l, with_ctx):
        o = l // 2
        mem["p"] = phase_base
        hT = sb("hT", [128, 8, TT], BF16)
        G1 = sb("G1", [128, D])
        G1c = sb("G1c", [128, D])
        WoL = sb("WoL", [128, D], BF16)
        WoC = sb("WoC", [128, D], BF16)
        tmp_mark = mem["p"]
        xnb = sb("xnb", [128, D], BF16)
        junk = sb("junk", [128, D], BF16)
        gtmp = sb("gtmp", [128, 8, 128])
        build_G(G1, mslice(l, s, 2), gtmp, "G1")
        build_G(G1c, mslice(l, S, 2), gtmp, "G1c")
        compute_hT(s, l, hT, xnb, junk)
        P.barrier()
        mem["p"] = tmp_mark
        W5 = sb("W5", [128, 8, 5, 128], BF16)
        Wo = sb("Wo", [128, D], BF16)
        A = sb("A", [128, TT])
        B = sb("B", [128, TT])
        kk = sb("kk", [128, TT], BF16)
        sq = sb("sq", [128, TT], BF16)
        sgt = sb("sgt", [128, TT], BF16)
        qt_ = sb("qt", [128, TT], BF16)
        qh = sb("qh", [128, TT], BF16)
        kt_ = sb("kt", [128, TT], BF16)
        vv = sb("vv", [128, 18, 128], BF16)
        oTs = sb("oTs", [128, TT])
        tot = sb("tot", [128, 36])
        Ee = sb("Ee", [128, 36])
        Ep = sb("Ep", [128, 36])
        ATm2 = [sb("ATm0", [128, 128], BF16), sb("ATm1", [128, 128], BF16)]
        ktok2 = [sb("ktok0", [128, 128], BF16), sb("ktok1", [128, 128], BF16)]
        rmask = sb("rmask", [128, TT], BF16)
        MS("pool", rmask[:], 1.0, ["rmask"])
        MS("pool", rmask[:].rearrange("p (c k) -> p c k", k=64)[:, :, 0:1], 0.0, ["rmask"])
        R32 = [sb("R32a", [128, 128]), sb("R32b", [128, 128])]
        Rb = [sb("Rba", [128, 128], BF16), sb("Rbb", [128, 128], BF16)]
        ntile_out = 18 if with_ctx else 16
        NCH = 36
        for hd in range(8):
            cols = [hd * 128, 1024 + hd * 128, 2048 + hd * 128, 3072 + hd * 128, 4096 + hd * 128]
            for i, c0 in enumerate(cols):
                P.dma("pool", W5[:, :, i, :], wview(recw_d[o, :, c0:c0 + 128]), [], [("W5", i)])
            P.dma("pool", Wo[:], recwo_d[o, hd * 128:(hd + 1) * 128, :], [], ["Wo"])

            def ev_q(tb, c0, n, pb, pk):
                ACT(sq[:, c0:c0 + n], pb[:, 0:n], AF.Silu, [pk], ["sq"])

            def ev_g(tb, c0, n, pb, pk):
                ACT(sgt[:, c0:c0 + n], pb[:, 0:n], AF.Silu, [pk], ["sgt"])
            proj_fm(None, W5[:, :, 0, :], ("W5", 0), slice(0, 128), hT, ev_q)
            proj_fm(None, W5[:, :, 4, :], ("W5", 4), slice(0, 128), hT, ev_g)
            for tile in range(18):
                pb = PS[tile % 2]
                pk = f"ps{tile % 2}"
                for k in range(8):
                    MM(pb[:, 0:128], hT[:, k, tile * 128:(tile + 1) * 128], W5[:, k, 3, :], k == 0, k == 7,
                       [("W5", 3), ("hT", tile)], [pk])
                CP("dve", vv[:, tile, :], pb[:, 0:128], [pk], [("vv", tile)])
            vkeys = [("vv", t) for t in range(18)]
            for dr in range(2):
                lbc = lbT[:, dr, l, hd:hd + 1]
                omc = omlT[:, dr, l, hd:hd + 1]
                nomc = nomlT[:, dr, l, hd:hd + 1]

                def ev_f(tb, c0, n, pb, pk):
                    ACT(A[:, c0:c0 + n], pb[:, 0:n], AF.Sigmoid, [pk], ["A"])
                proj_fm(None, W5[:, :, 1 + dr, :], ("W5", 1 + dr), slice(0, 128), hT, ev_f)
                ACT(B[:], A[:], AF.Ln, ["A", "lbT", "omlT"], ["B"], scale=omc, bias=lbc)
                TS("dve", kk[:], A[:], nomc, omc, ALU.mult, ALU.add, ["A", "nomlT", "omlT"], ["kk"])
                P.op("dve", lambda E: E.tensor_tensor_scan(out=A[:], data0=rmask[:], data1=B[:], initial=0.0,
                                                            op0=ALU.mult, op1=ALU.add), ["B", "rmask", "kk"], ["A"])
                Av = A[:].rearrange("p (c k) -> p c k", k=64)
                Bv = B[:].rearrange("p (c k) -> p c k", k=64)
                CP("dve", tot[:].unsqueeze(2), Av[:, :, 63:64], ["A"], ["tot"])
                if dr == 1:
                    TTo("dve", A[:], B[:], A[:], ALU.subtract, ["A", "B"], ["A"])
                    TTo("dve", Av, Av, tot[:].unsqueeze(2).to_broadcast([128, NCH, 64]), ALU.add, ["A", "tot"], ["A"])
                ACT(Ee[:], tot[:], AF.Exp, ["tot"], ["Ee"])
                MS("dve", Ep[:], 0.0, ["Ep"])
                if dr == 0:
                    CP("dve", Ep[:, 1:32], Ee[:, 0:31], ["Ee"], ["Ep"])
                    CP("dve", Ep[:, 0:1], Ee[:, 35:36], ["Ee"], ["Ep"])
                    CP("dve", Ep[:, 33:36], Ee[:, 32:35], ["Ee"], ["Ep"])
                    order = [32, 33, 34, 35] + list(range(32))
                    msk = maskf
                else:
                    CP("dve", Ep[:, 0:31], Ee[:, 1:32], ["Ee"], ["Ep"])
                    CP("dve", Ep[:, 31:32], Ee[:, 32:33], ["Ee"], ["Ep"])
                    CP("dve", Ep[:, 32:35], Ee[:, 33:36], ["Ee"], ["Ep"])
                    order = [35, 34, 33, 32] + list(range(31, -1, -1))
                    msk = maskb
                ACT(qt_[:], A[:], AF.Exp, ["A"], ["qt"])
                ACT(kt_[:], A[:], AF.Exp, ["A"], ["kt"], scale=-1.0)
                TTo("pool", qt_[:], qt_[:], sq[:], ALU.mult, ["qt", "sq"], ["qt"])
                TTo("dve", kt_[:], kt_[:], kk[:], ALU.mult, ["kt", "kk"], ["kt"])
                TTo("pool", qh[:].rearrange("p (c k) -> p c k", k=64), qt_[:].rearrange("p (c k) -> p c k", k=64),
                    Ep[:].unsqueeze(2).to_broadcast([128, NCH, 64]), ALU.mult, ["qt", "Ep"], ["qh"])
                if hd == 0 and dr == 0 and DBG.get('dump'):
                    dump(0, B[:, 0:512], ["B"], 512)
                    dump(1, A[:, 0:512], ["A"], 512)
                    dump(2, kk[:, 0:512], ["kk"], 512)
                    dump(3, qt_[:, 0:512], ["qt"], 512)
                    dump(4, kt_[:, 0:512], ["kt"], 512)
                    dump(5, Ee[:, :], ["Ee"], 36)
                    dump(6, Ep[:, :], ["Ep"], 36)
                    dump(7, tot[:, :], ["tot"], 36)
                    dump(8, lbT[:].rearrange("p a b c -> p (a b c)"), ["lbT"], 64)
                    dump(9, vv[:, 0:4, :].rearrange("p a b -> p (a b)"), vkeys, 512)
                st_ = {"pp": 0, "first": True, "ucnt": 0}
                ABK = [2, 0]
                TBK = [3, 1]
                UBK = [6, 7]

                def stageA(ti):
                    tl = order[2 * ti] // 2
                    tc = slice(tl * 128, (tl + 1) * 128)
                    i2 = ti % 2
                    ab, tb_ = ABK[i2], TBK[i2]
                    MM(PS[ab][:, 0:128], kt_[:, tc], qt_[:, tc], True, True, ["kt", "qt"], [f"ps{ab}"])
                    TTo("dve", ATm2[i2][:], PS[ab][:, 0:128], msk[:], ALU.mult, [f"ps{ab}", "maskf", "maskb"], [("ATm", i2)])
                    TR(PSB[tb_][:, 0:128], kt_[:, tc], ident_b[:], ["kt", "ident_b"], [f"ps{tb_}"])
                    CP("act", ktok2[i2][:], PSB[tb_][:, 0:128], [f"ps{tb_}"], [("ktok", i2)])

                def stageB(ti):
                    ch_a, ch_b = order[2 * ti], order[2 * ti + 1]
                    tl = ch_a // 2
                    assert ch_b // 2 == tl
                    tc = slice(tl * 128, (tl + 1) * 128)
                    i2 = ti % 2
                    ob = PS[4 + ti % 2]
                    ok = f"ps{4 + ti % 2}"
                    chs = [ch_a, ch_b]
                    n_inter = sum(1 for ch in chs if not (st_["first"] and ch == chs[0]))
                    MM(ob[:, 0:128], vv[:, tl, :], ATm2[i2][:], True, n_inter == 0, [("ATm", i2)] + vkeys, [ok])
                    done = 0
                    for ch in chs:
                        hf = ch % 2
                        pp = st_["pp"]
                        ub = UBK[st_["ucnt"] % 2]
                        st_["ucnt"] += 1
                        uk = f"ps{ub}"
                        MM(PS[ub][:, 0:128], ktok2[i2][hf * 64:(hf + 1) * 64, :], vv[hf * 64:(hf + 1) * 64, tl, :], True, True,
                           [("ktok", i2)] + vkeys, [uk])
                        if not st_["first"]:
                            done += 1
                            MM(ob[:, hf * 64:(hf + 1) * 64], Rb[pp][:], qh[:, ch * 64:(ch + 1) * 64], False, done == n_inter,
                               [("Rb", pp), "qh"], [ok])
                        if st_["first"]:
                            CP("dve", Rb[pp][:], PS[ub][:, 0:128], [uk], [("Rb", pp)])
                            CP("dve", R32[pp][:], PS[ub][:, 0:128], [uk], [("R32", pp)])
                            st_["first"] = False
                        else:
                            STT("dve", Rb[1 - pp][:], R32[pp][:], Ep[:, ch:ch + 1], PS[ub][:, 0:128], ALU.mult, ALU.add,
                                [("R32", pp), "Ep", uk], [("Rb", 1 - pp)])
                            STT("dve", R32[1 - pp][:], R32[pp][:], Ep[:, ch:ch + 1], PS[ub][:, 0:128], ALU.mult, ALU.add,
                                [("R32", pp), "Ep", uk], [("R32", 1 - pp)])
                            st_["pp"] = 1 - pp
                    if dr == 0:
                        CP("act", oTs[:, tc], ob[:, 0:128], [ok], [("oTs", tl)])
                    else:
                        TTo("dve", oTs[:, tc], oTs[:, tc], ob[:, 0:128], ALU.add, [ok, ("oTs", tl)], [("oTs", tl)])

                stageA(0)
                for ti in range(18):
                    if ti + 1 < 18:
                        stageA(ti + 1)
                    stageB(ti)
            if hd == 0 and DBG.get('dump'):
                dump(10, oTs[:, 0:512], [("oTs", t) for t in range(4)], 512)
                dump(11, oTs[:, T:TT], [("oTs", t) for t in (16, 17)], 256)
                dump(12, R32[0][:], [("R32", 0)], 128)
            oTh = kk
            for tb in range(5):
                n = 512 if tb < 4 else 256
                c0 = tb * 512
                ok_ = [("oTs", t) for t in range(c0 // 128, (c0 + n) // 128)]
                ACT(A[:, c0:c0 + n], oTs[:, c0:c0 + n], AF.Square, ok_, ["A"])
                MM(PS[7][:, 0:n], ones_f[:], A[:, c0:c0 + n], True, True, ["ones_f", "A"], ["ps7"])
                ACT(B[:, c0:c0 + n], PS[7][:, 0:n], AF.Sqrt, ["ps7"], ["B"], scale=1.0 / 128, bias=EPS)
                RCP(B[:, c0:c0 + n], B[:, c0:c0 + n], ["B"], ["B"])
                TTo("dve", A[:, c0:c0 + n], oTs[:, c0:c0 + n], B[:, c0:c0 + n], ALU.mult, ok_ + ["B", "A"], ["A"])
                STT("dve", oTh[:, c0:c0 + n], A[:, c0:c0 + n], gnT[:, o:o + 1], sgt[:, c0:c0 + n], ALU.mult, ALU.mult,
                    ["A", "gnT", "sgt"], ["kk"])
            TTo("pool", WoL[:], Wo[:], G1[:], ALU.mult, ["Wo", "G1"], ["WoL"])
            TTo("pool", WoC[:], Wo[:], G1c[:], ALU.mult, ["Wo", "G1c"], ["WoC"])
            for tile in range(ntile_out):
                W_, wk_ = (WoL, "WoL") if tile < 16 else (WoC, "WoC")
                for half in range(2):
                    pb = PS[half]
                    pk = f"ps{half}"
                    MM(pb[:, 0:512], oTh[:, tile * 128:(tile + 1) * 128], W_[:, half * 512:(half + 1) * 512], True, True,
                       ["kk", wk_], [pk])
                    residual_add_direct(tile, half, pb, pk)
        P.barrier()

    def moe(s, l, with_ctx):
        mem["p"] = phase_base
        ntl = 18 if with_ctx else 16
        NW = 288 if with_ctx else 256
        njt = 3 if with_ctx else 2
        hn = sb("hn", [128, 18, D], BF16)
        G2 = sb("G2", [128, D])
        G2c = sb("G2c", [128, D])
        posm_b = sb("posm_b", [16, TT], BF16)
        w_b = sb("w_b", [16, TT], BF16)
        pos_tok = sb("pos_tok", [128, 18, NE])
        wr = sb("wr", [128, 8, NE])
        selT = sb("selT", [16, 128], BF16)
        loop_base = mem["p"]
        gtmp = sb("gtmp", [128, 8, 128])
        xn32 = sb("xn32", [128, D])
        junk = sb("junk", [128, D], BF16)
        hT32 = sb("hT32", [128, 8, 128])
        afft = sb("afft", [128, NE])
        affT = sb("affT", [16, TT])
        work = sb("work", [16, TT])
        msk = sb("msk", [16, TT])
        pos = sb("pos", [16, TT])
        mx = sb("mx", [16, 8])
        build_G(G2, mslice(l, s, 5), gtmp, "G2")
        build_G(G2c, mslice(l, S, 5), gtmp, "G2c")
        P.dma("sp", wr[:], wview(router_d[LI[l]]), [], ["wr"])
        for vi, v in enumerate((s, S)):
            STT("dve", am[:, 1, vi, :], mslice(l, v, 4), 1.0, ngT[:, l, 1, :], ALU.add, ALU.mult, ["modT", "ngT"], ["am"])
        for tile in range(ntl):
            vi = 0 if tile < 16 else 1
            v = s if tile < 16 else S
            src, sk = xsrc(tile)
            rms_rstd(src, sk, junk[:], D)
            TS("dve", xn32[:], src, rstd[:, 0:1], None, ALU.mult, None, [sk, "rstd"], ["xn32"])
            CP("act" if DBG.get("nopool") else "pool", hn[:, tile, :], xn32[:], ["xn32"], [("hn", tile)])
            if DBG.get('sub', 9) < 1:
                continue
            pbk = f"ps{tile % 2}"
            for c in range(8):
                TR(PSB[tile % 2][:, c * 128:(c + 1) * 128], hn[:, tile, c * 128:(c + 1) * 128], ident_b[:],
                   [("hn", tile), "ident_b"], [pbk])
            for c in range(8):
                if DBG.get('noevac'):
                    continue
                src_p = PSB[tile % 2][:, c * 128:(c + 1) * 128]
                if c % 2 == 0:
                    TS("dve", hT32[:, c, :], src_p, am[:, 1, vi, c:c + 1], mslice(l, v, 3)[:, c:c + 1], ALU.mult, ALU.add,
                       [pbk, "am", "modT"], [("hT32", c)])
                    continue
                if c % 2 == 0:
                    ACT(hT32[:, c, :], src_p, AF.Identity, [pbk, "am", "modT"], [("hT32", c)],
                        scale=am[:, 1, vi, c:c + 1], bias=mslice(l, v, 3)[:, c:c + 1])
                else:
                    TS("dve", hT32[:, c, :], src_p, am[:, 1, vi, c:c + 1], mslice(l, v, 3)[:, c:c + 1], ALU.mult, ALU.add,
                       [pbk, "am", "modT"], [("hT32", c)])
            if DBG.get('sub', 9) < 2:
                continue
            for c in range(8):
                MM(PS[2][:, 0:NE], hT32[:, c, :], wr[:, c, :], c == 0, c == 7, [("hT32", c), "wr"], ["ps2"])
            if DBG.get('sub', 9) < 3:
                continue
            ACT(afft[:], PS[2][:, 0:NE], AF.Exp, ["ps2"], ["afft", "ssq"], accum=ssq[:])
            RCP(rt[:], ssq[:], ["ssq"], ["rt"])
            TS("dve", afft[:], afft[:], rt[:, 0:1], None, ALU.mult, None, ["afft", "rt"], ["afft"])
            if not DBG.get("notr"):
                TR(PS[3][0:16, 0:128], afft[:], ident_f[:], ["afft", "ident_f"], ["ps3"])
                CP("act", affT[0:16, tile * 128:(tile + 1) * 128], PS[3][0:16, 0:128], ["ps3"], ["affT"])

        if DBG.get('stage', 9) < 1:
            P.barrier()
            return

        def route(c0, n, cap):
            CP("dve", work[:, c0:c0 + n], affT[:, c0:c0 + n], ["affT"], ["work"])
            nit = cap // 8
            for it_ in range(nit):
                P.op("dve", (lambda a, b: (lambda E: E.max(out=a, in_=b)))(mx[:], work[:, c0:c0 + n]), ["work"], ["mx"])
                if it_ < nit - 1:
                    P.op("dve", (lambda a, b, c_: (lambda E: E.match_replace(out=a, in_to_replace=b, in_values=c_, imm_value=-1.0)))(
                        work[:, c0:c0 + n], mx[:], work[:, c0:c0 + n]), ["work", "mx"], ["work"])
            TS("dve", msk[:, c0:c0 + n], affT[:, c0:c0 + n], mx[:, 7:8], None, ALU.is_ge, None, ["affT", "mx"], ["msk"])
            MS("dve", work[:, c0:c0 + n], 1.0, ["work"])
            P.op("dve", (lambda a, b, c_: (lambda E: E.tensor_tensor_scan(out=a, data0=b, data1=c_, initial=0.0, op0=ALU.mult, op1=ALU.add)))(
                pos[:, c0:c0 + n], work[:, c0:c0 + n], msk[:, c0:c0 + n]), ["work", "msk"], ["pos"])
            TTo("dve", pos[:, c0:c0 + n], pos[:, c0:c0 + n], msk[:, c0:c0 + n], ALU.mult, ["pos", "msk"], ["pos"])
            TS("dve", pos[:, c0:c0 + n], pos[:, c0:c0 + n], -1.0, None, ALU.add, None, ["pos"], ["pos"])
            CP("dve", posm_b[:, c0:c0 + n], pos[:, c0:c0 + n], ["pos"], ["posm_b"])
            TTo("dve", w_b[:, c0:c0 + n], affT[:, c0:c0 + n], msk[:, c0:c0 + n], ALU.mult, ["affT", "msk"], ["w_b"])

        route(0, T, 256)
        if with_ctx:
            route(T, L, 32)
        if DBG.get('stage', 9) < 2:
            P.barrier()
            return
        for tile in range(ntl):
            TR(PS[3][:, 0:NE], pos[0:16, tile * 128:(tile + 1) * 128], ident_f[0:16, 0:16], ["pos", "ident_f"], ["ps3"])
            CP("act", pos_tok[:, tile, :], PS[3][:, 0:NE], ["ps3"], ["pos_tok"])
        P.barrier()
        mem["p"] = loop_base
        Pe = sb("Pe", [128, 16, 256], BF16)
        Pce = sb("Pce", [128, 2, 32], BF16)
        PT = sb("PT", [128, 2, T], BF16)
        PTc = sb("PTc", [32, L], BF16)
        xsel = sb("xsel", [128, 8, 288], BF16)
        hid = [sb("hid0", [128, 288], BF16), sb("hid1", [128, 288], BF16)]
        sgl = sb("sgl", [128, 288])
        yy = sb("yy", [128, 3, D], BF16)
        wsb = sb("wsb", [128, 512])
        Wg = [sb("Wg0", [128, 8, 256], BF16), sb("Wg1", [128, 8, 256], BF16)]
        Wu = [sb("Wu0", [128, 8, 256], BF16), sb("Wu1", [128, 8, 256], BF16)]
        Wd = [sb("Wd0", [128, 2, D], BF16), sb("Wd1", [128, 2, D], BF16)]
        hnk = [("hn", t) for t in range(ntl)]
        wcnt = 0
        for e in range(DBG['experts']):
            TS("dve", selT[:], ones_f[0:16, :], ident_f[0:16, e:e + 1], None, ALU.mult, None, ["ones_f", "ident_f"], ["selT"])
            TTo("dve", Pe[:], iota_j[:].unsqueeze(1).to_broadcast([128, 16, 256]),
                pos_tok[:, 0:16, e:e + 1].to_broadcast([128, 16, 256]), ALU.is_equal, ["iota_j", "pos_tok"], ["Pe"])
            if with_ctx:
                TTo("dve", Pce[:], iota_j[:, 0:32].unsqueeze(1).to_broadcast([128, 2, 32]),
                    pos_tok[:, 16:18, e:e + 1].to_broadcast([128, 2, 32]), ALU.is_equal, ["iota_j", "pos_tok"], ["Pce"])
            for blk in range(4):
                bc = slice(blk * 512, (blk + 1) * 512)
                MM(PS[6][:, 0:512], selT[0:16, :], posm_b[0:16, bc], True, True, ["selT", "posm_b"], ["ps6"])
                MM(PS[7][:, 0:512], selT[0:16, :], w_b[0:16, bc], True, True, ["selT", "w_b"], ["ps7"])
                CP("act", wsb[:], PS[7][:, 0:512], ["ps7"], ["wsb"])
                for jt in range(2):
                    STT("dve", PT[:, jt, bc], PS[6][:, 0:512], iota_p[:, jt:jt + 1], wsb[:], ALU.is_equal, ALU.mult,
                        ["ps6", "iota_p", "wsb"], ["PT"])
            if with_ctx:
                MM(PS[6][0:32, 0:L], selT[0:16, 0:32], posm_b[0:16, T:TT], True, True, ["selT", "posm_b"], ["ps6"])
                MM(PS[7][0:32, 0:L], selT[0:16, 0:32], w_b[0:16, T:TT], True, True, ["selT", "w_b"], ["ps7"])
                CP("act", wsb[0:32, 0:L], PS[7][0:32, 0:L], ["ps7"], ["wsb"])
                STT("dve", PTc[:], PS[6][0:32, 0:L], iota_p[0:32, 0:1], wsb[0:32, 0:L], ALU.is_equal, ALU.mult,
                    ["ps6", "iota_p", "wsb"], ["PTc"])
            for c in range(8):
                pb = PS[6 + c % 2]
                pk = f"ps{6 + c % 2}"
                for tile in range(16):
                    MM(pb[:, 0:256], hn[:, tile, c * 128:(c + 1) * 128], Pe[:, tile, :], tile == 0, tile == 15, ["Pe"] + hnk, [pk])
                if with_ctx:
                    for ct in range(2):
                        MM(pb[:, 256:288], hn[:, 16 + ct, c * 128:(c + 1) * 128], Pce[:, ct, :], ct == 0, ct == 1, ["Pce"] + hnk, [pk])
                TS("dve", xsel[:, c, 0:256], pb[:, 0:256], am[:, 1, 0, c:c + 1], mslice(l, s, 3)[:, c:c + 1], ALU.mult, ALU.add,
                   [pk, "am", "modT"], [("xsel", c)])
                if with_ctx:
                    TS("dve", xsel[:, c, 256:288], pb[:, 256:288], am[:, 1, 1, c:c + 1], mslice(l, S, 3)[:, c:c + 1], ALU.mult, ALU.add,
                       [pk, "am", "modT"], [("xsel", c)])
            xk = [("xsel", c) for c in range(8)]
            pend = None

            def down(fc, wi, f2):
                for jt in range(njt):
                    rows = 128 if jt < 2 else 32
                    for half in range(2):
                        b = jt * 2 + half
                        MM(PS[b][0:rows, 0:512], hid[fc % 2][:, jt * 128:jt * 128 + rows], Wd[wi][:, f2, half * 512:(half + 1) * 512],
                           fc == 0, fc == 15, [f"hid{fc % 2}", f"Wd{wi}"], [f"ps{b}"])

            for fb in range(8):
                wi = wcnt % 2
                wcnt += 1
                P.dma("pool", Wg[wi][:], wview(wg_d[LI[l], e, :, fb * 256:(fb + 1) * 256]), [], [f"Wg{wi}"])
                P.dma("pool", Wu[wi][:], wview(wu_d[LI[l], e, :, fb * 256:(fb + 1) * 256]), [], [f"Wu{wi}"])
                P.dma("pool", Wd[wi][:], wd_d[LI[l], e, fb * 256:(fb + 1) * 256, :].rearrange("(c p) n -> p c n", p=128), [], [f"Wd{wi}"])
                for f2 in range(2):
                    fc = fb * 2 + f2
                    for c in range(8):
                        MM(PS[6][:, 0:NW], Wg[wi][:, c, f2 * 128:(f2 + 1) * 128], xsel[:, c, 0:NW], c == 0, c == 7, [f"Wg{wi}"] + xk, ["ps6"])
                    for c in range(8):
                        MM(PS[7][:, 0:NW], Wu[wi][:, c, f2 * 128:(f2 + 1) * 128], xsel[:, c, 0:NW], c == 0, c == 7, [f"Wu{wi}"] + xk, ["ps7"])
                    ACT(sgl[:, 0:NW], PS[6][:, 0:NW], AF.Silu, ["ps6"], ["sgl"])
                    TTo("dve", hid[fc % 2][:, 0:NW], sgl[:, 0:NW], PS[7][:, 0:NW], ALU.mult, ["sgl", "ps7"], [f"hid{fc % 2}"])
                    if pend is not None:
                        down(*pend)
                    pend = (fc, wi, f2)
            down(*pend)
            for jt in range(njt):
                rows = 128 if jt < 2 else 32
                G = G2 if jt < 2 else G2c
                gk = "G2" if jt < 2 else "G2c"
                for half in range(2):
                    b = jt * 2 + half
                    TTo("dve", yy[0:rows, jt, half * 512:(half + 1) * 512], PS[b][0:rows, 0:512], G[0:rows, half * 512:(half + 1) * 512],
                        ALU.mult, [f"ps{b}", gk], [("yy", jt)])
            for tile in range(16):
                for half in range(2):
                    b = (tile % 3) * 2 + half
                    for jt in range(2):
                        MM(PS[b][:, 0:512], PT[:, jt, tile * 128:(tile + 1) * 128], yy[:, jt, half * 512:(half + 1) * 512], jt == 0, jt == 1,
                           ["PT", ("yy", jt)], [f"ps{b}"])
                    TTo("dve", X[:, tile, half * 512:(half + 1) * 512], X[:, tile, half * 512:(half + 1) * 512], PS[b][:, 0:512], ALU.add,
                        [f"ps{b}", ("X", tile)], [("X", tile)])
            if with_ctx:
                for ct in range(2):
                    for half in range(2):
                        b = ct * 2 + half
                        MM(PS[b][:, 0:512], PTc[0:32, ct * 128:(ct + 1) * 128], yy[0:32, 2, half * 512:(half + 1) * 512], True, True,
                           ["PTc", ("yy", 2)], [f"ps{b}"])
                        TTo("dve", XC[:, ct, half * 512:(half + 1) * 512], XC[:, ct, half * 512:(half + 1) * 512], PS[b][:, 0:512], ALU.add,
                            [f"ps{b}", ("XC", ct)], [("XC", ct)])
        P.barrier()

    for s in range(S):
        for tile in range(16):
            P.dma("sp", X[:, tile, :], x_d[s, tile * 128:(tile + 1) * 128, :], [], [("X", tile)])
        for ct in range(2):
            P.dma("sp", XC[:, ct, :], ctx_d[s, ct * 128:(ct + 1) * 128, :], [], [("XC", ct)])
        for l in layers:
            last = (l == 3)
            if DBG['mix']:
                if l % 2 == 0:
                    even_mixer(s, l, not last)
                else:
                    odd_mixer(s, l, not last)
            if DBG['moe']:
                moe(s, l, not last)
        P.barrier()
        mem["p"] = phase_base
        fg = sb("fg", [128, D])
        junk = sb("junk", [128, D], BF16)
        ot = [sb("ot0", [128, D]), sb("ot1", [128, D])]
        if final:
            P.dma("sp", fg[:], fg_d, [], ["fg"])
        for tile in range(16):
            src, sk = xsrc(tile)
            o_ = ot[tile % 2]
            okey = f"ot{tile % 2}"
            if final:
                rms_rstd(src, sk, junk[:], D)
                STT("dve", o_[:], src, rstd[:, 0:1], fg[:], ALU.mult, ALU.mult, [sk, "rstd", "fg"], [okey])
            else:
                CP("dve", o_[:], src, [sk], [okey])
            P.dma("sp", out_d[s, tile * 128:(tile + 1) * 128, :], o_[:], [okey], [])
        if debug_ctx:
            for ct in range(2):
                P.dma("sp", outc_d[s, ct * 128:(ct + 1) * 128, :], XC[:, ct, :], [("XC", ct)], [])
        P.barrier()
    P.wait_dmas("sp")
    P.run()
    return nc, P


def _consts():
    c = {}
    c["c_ident"] = np.eye(128, dtype=np.float32)
    lo = np.eye(128, dtype=np.float32); lo[64:, :] = 0
    hi = np.eye(128, dtype=np.float32); hi[:64, :] = 0
    c["c_ident_lo"] = lo
    c["c_ident_hi"] = hi
    c["c_iota_j"] = np.broadcast_to(np.arange(256, dtype=np.float32)[None, :], (128, 256)).copy()
    p = np.arange(128, dtype=np.float32)
    c["c_iota_p"] = np.stack([p, p + 128, p, p], axis=1).copy()
    s_ = np.arange(128)[:, None]
    t_ = np.arange(128)[None, :]
    same = (s_ // 64) == (t_ // 64)
    c["c_maskf"] = (same & (s_ <= t_)).astype(np.float32)
    c["c_maskb"] = (same & (s_ >= t_)).astype(np.float32)
    t = np.arange(T)
    rows = (t // 64).astype(np.float32)
    colsf = (t % 64).astype(np.float32)
    inv = (10000.0 ** (-np.arange(16, dtype=np.float32) / 16)).astype(np.float32)
    ang = np.concatenate([rows[:, None] * inv, colsf[:, None] * inv], axis=-1).astype(np.float32)
    cos = np.cos(ang).astype(np.float32).T
    sin = np.sin(ang).astype(np.float32).T
    cos64 = np.concatenate([cos, cos], axis=0)
    sin64 = np.concatenate([-sin, sin], axis=0)
    c["rope_cos"] = np.concatenate([cos64, cos64], axis=0).astype(np.float32)
    c["rope_sin"] = np.concatenate([sin64, sin64], axis=0).astype(np.float32)
    return c


def _swap_perm(base):
    idx = []
    for m in range(8):
        o = base + m * 64
        idx += list(range(o + 32, o + 64)) + list(range(o, o + 32))
    return np.array(idx)


def _na_bias_table(rpb):
    kc = np.arange(64)[:, None]
    qc = np.arange(64)[None, :]
    cstart = np.clip(qc - 8, 0, 48)
    valid = (kc >= cstart) & (kc < cstart + 16)
    dcol = np.clip(kc - qc + 15, 0, 30)
    tab = np.full((64, 8, 16, 64), -3750.0, dtype=np.float32)
    g = rpb[:, :, dcol]
    g = np.transpose(g, (2, 0, 1, 3))
    vm = np.broadcast_to(valid[:, None, None, :], g.shape)
    tab[:, :, 0:15, :] = np.where(vm, g, np.float32(-3750.0))
    return np.concatenate([tab, tab], axis=0)


def prep_shared(inp, layers=(0, 1, 2, 3)):
    layers = list(layers) if len(layers) else [0]
    f = lambda a: np.ascontiguousarray(np.asarray(a, dtype=np.float32))
    d = dict(_consts())
    d["w_mod"] = f(np.asarray(inp["w_mod"])[layers])
    d["b_modT"] = f(np.transpose(np.asarray(inp["b_mod"]).reshape(4, 48, 128), (2, 0, 1)))
    d["norm_gT"] = f(np.transpose(np.asarray(inp["norm_g"]).reshape(4, 2, 8, 128), (3, 0, 1, 2)))
    w_in = np.asarray(inp["att_w_in"])
    d["att_w"] = f(np.concatenate([w_in, w_in[:, :, _swap_perm(512)], w_in[:, :, _swap_perm(2048)]], axis=2))
    d["att_wo"] = f(inp["att_w_out"])
    d["na_bias"] = f(np.stack([_na_bias_table(np.asarray(inp["na_rpb"])[e]) for e in range(2)], axis=0))
    d["lam_row"] = f(np.asarray(inp["diff_lambda"]).reshape(2, 1, 256))
    d["subln_bc"] = f(np.broadcast_to(np.asarray(inp["diff_subln_g"])[:, None, :], (2, 128, 128)))
    d["rec_w_in"] = f(inp["rec_w_in"])
    d["rec_wo"] = f(inp["rec_w_out"])
    d["rec_lbT"] = f(np.transpose(np.asarray(inp["rec_lb_logits"]).reshape(2, 4, 8, 128), (3, 0, 1, 2)))
    d["rec_gn"] = f(np.asarray(inp["rec_gnorm_g"]).T)
    d["router"] = f(np.asarray(inp["moe_router"])[layers])
    nex = max(1, DBG["experts"])
    d["wg"] = f(np.asarray(inp["moe_w_gate"])[layers][:, :nex])
    d["wu"] = f(np.asarray(inp["moe_w_up"])[layers][:, :nex])
    d["wd"] = f(np.asarray(inp["moe_w_down"])[layers][:, :nex])
    d["finalg_bc"] = f(np.broadcast_to(np.asarray(inp["final_g"])[None, :], (128, D)))
    return d


def prep_core(inp, samples):
    f = lambda a: np.ascontiguousarray(np.asarray(a, dtype=np.float32))
    x = np.asarray(inp["x"])
    ctx = np.asarray(inp["ctx"])
    c = np.asarray(inp["c"])
    cv = np.concatenate([c[samples], np.asarray(inp["c_ctx"])[None, :]], axis=0)
    V = cv.shape[0]
    return {
        "x": f(x[samples]),
        "ctx": f(ctx[samples]),
        "cvT": f(np.transpose(cv.reshape(V, 8, 128), (2, 1, 0))),
    }


_CACHE = {}


def kernel(**inputs):
    n_cores = 8
    S = 2
    if "nc" not in _CACHE:
        _CACHE["nc"] = build(S, [0, 1, 2, 3], final=True)[0]
    nc = _CACHE["nc"]
    shared = prep_shared(inputs)
    in_maps = []
    for i in range(n_cores):
        m = dict(shared)
        m.update(prep_core(inputs, list(range(i * S, (i + 1) * S))))
        in_maps.append(m)
    res = run_bass_kernel_spmd(nc, in_maps, core_ids=list(range(n_cores)))
    return np.concatenate([r["out"] for r in res.results], axis=0).astype(np.float32)
```
